# Optimizing a Trainium2 kernel written in Bass

```python
import math, functools
import jax, jax.numpy as jnp
from jax import lax
import numpy as np

D_MODEL = 1024
BATCH = 8
SEQ = 8192
DEPTH = 2

GRID_W = 64
CTX_LEN = 256
ROPE_BASE = 10000.0
Q_BLOCK = 128
NORM_EPS = 1e-6

MLA_HEADS = 8
MLA_NOPE = 64
MLA_ROPE = 32
MLA_V = 64
MLA_Q_RANK = 384
MLA_KV_RANK = 256

GDN_HEADS = 8
GDN_DK = 64
GDN_DV = 64
GDN_CONV = 5
GDN_CHUNK = 64
GDN_QKV = GDN_HEADS * (2 * GDN_DK + GDN_DV)

DIFF_HEADS = 4
DIFF_DH = 64

GQA_HEADS = 8
GQA_KV_HEADS = 2
GQA_DH = 64
GQA_GROUP = GQA_HEADS // GQA_KV_HEADS

MIX_WIDTH = 1024
EVEN_SPLITS = (MLA_Q_RANK, MLA_KV_RANK, MLA_ROPE, GDN_QKV, GDN_HEADS * GDN_DV, 2 * GDN_HEADS, 2 * GDN_HEADS)
ODD_SPLITS = (DIFF_HEADS * 2 * DIFF_DH, DIFF_HEADS * 2 * DIFF_DH, DIFF_HEADS * 2 * DIFF_DH,
              GQA_HEADS * GQA_DH, GQA_KV_HEADS * GQA_DH, GQA_KV_HEADS * GQA_DH)
EVEN_IN = sum(EVEN_SPLITS)
ODD_IN = sum(ODD_SPLITS)

FFN_DIM = 2816
N_EXPERTS = 8
TOP_K = 2
EXPERT_DIM = 3584
MOE_BLOCK = 512

kernel_name = 'hybrid_mla_gdn_diff_gqa_moe_dit'


def rms_norm(x, g):
    xf = x.astype(jnp.float32)
    y = xf * lax.rsqrt(jnp.mean(xf * xf, axis=-1, keepdims=True) + NORM_EPS)
    return (y * g.astype(jnp.float32)).astype(x.dtype)


def l2_normalize(x):
    xf = x.astype(jnp.float32)
    return xf * lax.rsqrt(jnp.sum(xf * xf, axis=-1, keepdims=True) + NORM_EPS)


def split_cols(z, sizes):
    cuts, acc = [], 0
    for s in sizes[:-1]:
        acc += s
        cuts.append(acc)
    return jnp.split(z, cuts, axis=-1)


def modulation(cvec, w, b):
    m = jax.nn.silu(cvec) @ w + b
    m = m.reshape(cvec.shape[:-1] + (6, cvec.shape[-1]))
    return tuple(jnp.expand_dims(m[..., k, :], -2) for k in range(6))


def axial_rope_tables(n_rows, rot_dim):
    n_freq = rot_dim // 4
    inv = 1.0 / (ROPE_BASE ** (jnp.arange(n_freq, dtype=jnp.float32) / n_freq))
    rows = jnp.repeat(jnp.arange(n_rows, dtype=jnp.float32), GRID_W)
    cols = jnp.tile(jnp.arange(GRID_W, dtype=jnp.float32), n_rows)
    ang_r = rows[:, None] * inv
    ang_c = cols[:, None] * inv
    return (jnp.cos(ang_r), jnp.sin(ang_r), jnp.cos(ang_c), jnp.sin(ang_c))


def _rotate_half(x, cos, sin):
    shape = (cos.shape[0],) + (1,) * (x.ndim - 3) + (cos.shape[1],)
    cs = cos.reshape(shape).astype(x.dtype)
    sn = sin.reshape(shape).astype(x.dtype)
    x1, x2 = jnp.split(x, 2, axis=-1)
    return jnp.concatenate([x1 * cs - x2 * sn, x2 * cs + x1 * sn], axis=-1)


def apply_axial_rope(x, tables):
    cos_r, sin_r, cos_c, sin_c = tables
    x_row, x_col = jnp.split(x, 2, axis=-1)
    return jnp.concatenate([_rotate_half(x_row, cos_r, sin_r), _rotate_half(x_col, cos_c, sin_c)], axis=-1)


def depthwise_conv_centred(x, w):
    width, ch = w.shape
    return lax.conv_general_dilated(
        x, w[:, None, :].astype(x.dtype), window_strides=(1,),
        padding=[(width // 2, width // 2)],
        dimension_numbers=('NWC', 'WIO', 'NWC'), feature_group_count=ch)


def sweep_query_blocks(fn, q):
    bsz, tq = q.shape[:2]
    nb = tq // Q_BLOCK
    qb = jnp.swapaxes(q.reshape((bsz, nb, Q_BLOCK) + q.shape[2:]), 0, 1)
    o = lax.map(fn, qb)
    return jnp.swapaxes(o, 0, 1).reshape((bsz, tq) + o.shape[3:])


def softmax_attention(q, k, v):
    scale = q.shape[-1] ** -0.5

    def block(qb):
        s = jnp.einsum('bqhgd,bkhd->bhgqk', qb, k, preferred_element_type=jnp.float32) * scale
        p = jax.nn.softmax(s, axis=-1).astype(v.dtype)
        return jnp.einsum('bhgqk,bkhe->bqhge', p, v)

    return sweep_query_blocks(block, q)


def differential_attention(q, k, v, lam):
    scale = q.shape[-1] ** -0.5

    def block(qb):
        s = jnp.einsum('bqhmd,bkhmd->bhmqk', qb, k, preferred_element_type=jnp.float32) * scale
        p = jax.nn.softmax(s, axis=-1)
        p = (p[:, :, 0] - lam * p[:, :, 1]).astype(v.dtype)
        return jnp.einsum('bhqk,bkhe->bqhe', p, v)

    return sweep_query_blocks(block, q)


def gated_delta_chunked(q, k, v, g, beta, s0):
    bsz, t, h, dk = k.shape
    dv = v.shape[-1]
    n = t // GDN_CHUNK

    def chunks(a):
        a = a.reshape((bsz, n, GDN_CHUNK, h) + a.shape[3:])
        return jnp.moveaxis(a, (1, 3), (0, 2))

    qc, kc, vc, bc = chunks(q), chunks(k), chunks(v), chunks(beta)
    gc = jnp.cumsum(chunks(g), axis=-1)
    pos = jnp.arange(GDN_CHUNK)
    incl = pos[:, None] >= pos[None, :]
    decay = jnp.exp(jnp.where(incl, gc[..., :, None] - gc[..., None, :], -jnp.inf))
    kbk = jnp.einsum('nbhcd,nbhsd->nbhcs', kc * bc[..., None], kc) * decay
    lower = jnp.where(pos[:, None] > pos[None, :], kbk, 0.0) + jnp.eye(GDN_CHUNK, dtype=kbk.dtype)
    rhs = jnp.concatenate([vc * bc[..., None], kc * (bc * jnp.exp(gc))[..., None]], axis=-1)
    sol = lax.linalg.triangular_solve(lower, rhs, left_side=True, lower=True, unit_diagonal=True)
    u, w = sol[..., :dv], sol[..., dv:]
    qk = jnp.einsum('nbhcd,nbhsd->nbhcs', qc, kc) * decay
    q_dec = qc * jnp.exp(gc)[..., None]
    k_dec = kc * jnp.exp(gc[..., -1:] - gc)[..., None]
    chunk_decay = jnp.exp(gc[..., -1])

    def step(state, inp):
        u_i, w_i, qk_i, qd_i, kd_i, cd_i = inp
        v_new = u_i - jnp.einsum('bhcd,bhde->bhce', w_i, state)
        o_i = jnp.einsum('bhcd,bhde->bhce', qd_i, state) + jnp.einsum('bhcs,bhse->bhce', qk_i, v_new)
        state = state * cd_i[..., None, None] + jnp.einsum('bhcd,bhce->bhde', kd_i, v_new)
        return state, o_i

    state, o = lax.scan(step, s0, (u, w, qk, q_dec, k_dec, chunk_decay))
    o = jnp.moveaxis(o, (0, 2), (1, 3)).reshape(bsz, t, h, dv)
    return o, state


def swiglu(h, w_gu, w_down):
    gate, up = jnp.split(h @ w_gu, 2, axis=-1)
    return (jax.nn.silu(gate) * up) @ w_down


def moe_swiglu(h, router_w, w_gu, w_down):
    shape = h.shape
    xt = h.reshape(-1, shape[-1])
    n_tok = xt.shape[0]
    nk = n_tok * TOP_K
    logits = jnp.einsum('nd,de->ne', xt, router_w, preferred_element_type=jnp.float32)
    top_logit, top_e = lax.top_k(logits, TOP_K)
    top_w = jax.nn.softmax(top_logit, axis=-1)
    flat_e = top_e.reshape(-1)
    flat_tok = jnp.repeat(jnp.arange(n_tok, dtype=jnp.int32), TOP_K)
    flat_w = top_w.reshape(-1)
    order = jnp.argsort(flat_e)
    e_sorted = flat_e[order]
    counts = jnp.bincount(flat_e, length=N_EXPERTS)
    padded = (counts + MOE_BLOCK - 1) // MOE_BLOCK * MOE_BLOCK
    pad_end = jnp.cumsum(padded)
    pad_start = pad_end - padded
    start = jnp.cumsum(counts) - counts
    dest = pad_start[e_sorted] + jnp.arange(nk, dtype=jnp.int32) - start[e_sorted]
    n_blocks = -(-nk // MOE_BLOCK) + N_EXPERTS
    rows = n_blocks * MOE_BLOCK
    buf_tok = jnp.full((rows,), n_tok, jnp.int32).at[dest].set(flat_tok[order])
    buf_w = jnp.zeros((rows,), jnp.float32).at[dest].set(flat_w[order])
    block_e = jnp.minimum(
        jnp.searchsorted(pad_end, jnp.arange(n_blocks) * MOE_BLOCK, side='right'), N_EXPERTS - 1)

    def run_block(args):
        tok, e = args
        xb = jnp.take(xt, tok, axis=0, mode='clip')
        return swiglu(xb, w_gu[e], w_down[e])

    y = lax.map(run_block, (buf_tok.reshape(n_blocks, MOE_BLOCK), block_e))
    y = y.reshape(rows, -1) * buf_w[:, None].astype(y.dtype)
    out = jnp.zeros_like(xt).at[buf_tok].add(y, mode='drop')
    return out.reshape(shape)


def mla_gdn_mixer(h_ctx, h_lat, rope_lat, need_ctx, w_in, q_norm, kv_norm, w_uq, w_ukv,
                  conv_w, a_log, dt_bias, out_norm, w_out):
    pc = split_cols(h_ctx @ w_in, EVEN_SPLITS)
    pl = split_cols(h_lat @ w_in, EVEN_SPLITS)

    def mla_qkv(cq, ckv, k_pe, rope):
        bsz, t = cq.shape[:2]
        q = (rms_norm(cq, q_norm) @ w_uq).reshape(bsz, t, MLA_HEADS, MLA_NOPE + MLA_ROPE)
        kv = (rms_norm(ckv, kv_norm) @ w_ukv).reshape(bsz, t, MLA_HEADS, MLA_NOPE + MLA_V)
        q_nope, q_pe = jnp.split(q, [MLA_NOPE], axis=-1)
        k_nope, v = jnp.split(kv, [MLA_NOPE], axis=-1)
        k_pe = k_pe[:, :, None, :]
        if rope is not None:
            q_pe = apply_axial_rope(q_pe, rope)
            k_pe = apply_axial_rope(k_pe, rope)
        q = jnp.concatenate([q_nope, q_pe], axis=-1)[:, :, :, None, :]
        k = jnp.concatenate([k_nope, jnp.broadcast_to(k_pe, (bsz, t, MLA_HEADS, MLA_ROPE))], axis=-1)
        return q, k, v

    qc, kc, vc = mla_qkv(pc[0], pc[1], pc[2], None)
    ql, kl, vl = mla_qkv(pl[0], pl[1], pl[2], rope_lat)
    bsz, t_lat = h_lat.shape[:2]
    mla_lat = softmax_attention(ql, jnp.concatenate([kc, kl], axis=1),
                                jnp.concatenate([vc, vl], axis=1)).reshape(bsz, t_lat, MLA_HEADS * MLA_V)

    def gdn_prepare(qkv, a, b):
        t = qkv.shape[1]
        qkv = jax.nn.silu(depthwise_conv_centred(qkv, conv_w))
        q, k, v = split_cols(qkv, (GDN_HEADS * GDN_DK, GDN_HEADS * GDN_DK, GDN_HEADS * GDN_DV))
        q = l2_normalize(q.reshape(bsz, t, GDN_HEADS, GDN_DK)) * GDN_DK ** -0.5
        k = l2_normalize(k.reshape(bsz, t, GDN_HEADS, GDN_DK))
        v = v.reshape(bsz, t, GDN_HEADS, GDN_DV).astype(jnp.float32)
        g = -jnp.exp(a_log.astype(jnp.float32)) * jax.nn.softplus(
            a.reshape(bsz, t, 2, GDN_HEADS).astype(jnp.float32) + dt_bias.astype(jnp.float32))
        beta = jax.nn.sigmoid(b.reshape(bsz, t, 2, GDN_HEADS).astype(jnp.float32))
        return q, k, v, g, beta

    def gdn_bidirectional(inputs, s_fwd, s_bwd):
        q, k, v, g, beta = inputs
        rev = lambda a: jnp.flip(a, axis=1)
        o_f, s_fwd = gated_delta_chunked(q, k, v, g[:, :, 0], beta[:, :, 0], s_fwd)
        o_b, s_bwd = gated_delta_chunked(rev(q), rev(k), rev(v), rev(g[:, :, 1]), rev(beta[:, :, 1]), s_bwd)
        return o_f + rev(o_b), s_fwd, s_bwd

    def gdn_output(o, z):
        t = z.shape[1]
        o = rms_norm(o, out_norm) * jax.nn.silu(z.reshape(bsz, t, GDN_HEADS, GDN_DV).astype(jnp.float32))
        return o.reshape(bsz, t, GDN_HEADS * GDN_DV).astype(z.dtype)

    s0 = jnp.zeros((bsz, GDN_HEADS, GDN_DK, GDN_DV), jnp.float32)
    o_c, s_f, s_b = gdn_bidirectional(gdn_prepare(pc[3], pc[5], pc[6]), s0, s0)
    o_l, _, _ = gdn_bidirectional(gdn_prepare(pl[3], pl[5], pl[6]), s_f, s_b)

    out_lat = jnp.concatenate([mla_lat, gdn_output(o_l, pl[4])], axis=-1) @ w_out
    if not need_ctx:
        return None, out_lat
    mla_ctx = softmax_attention(qc, kc, vc).reshape(bsz, h_ctx.shape[1], MLA_HEADS * MLA_V)
    out_ctx = jnp.concatenate([mla_ctx, gdn_output(o_c, pc[4])], axis=-1) @ w_out
    return out_ctx, out_lat


def diff_gqa_mixer(h_ctx, h_lat, rope_diff, rope_gqa, need_ctx, lambda_init, w_in, diff_lambda,
                   diff_norm, q_norm, k_norm, w_out):
    def heads(h, use_rope):
        bsz, t = h.shape[:2]
        dq, dk, dv, gq, gk, gv = split_cols(h @ w_in, ODD_SPLITS)
        dq = dq.reshape(bsz, t, DIFF_HEADS, 2, DIFF_DH)
        dk = dk.reshape(bsz, t, DIFF_HEADS, 2, DIFF_DH)
        dv = dv.reshape(bsz, t, DIFF_HEADS, 2 * DIFF_DH)
        gq = rms_norm(gq.reshape(bsz, t, GQA_HEADS, GQA_DH), q_norm)
        gk = rms_norm(gk.reshape(bsz, t, GQA_KV_HEADS, GQA_DH), k_norm)
        gv = gv.reshape(bsz, t, GQA_KV_HEADS, GQA_DH)
        if use_rope:
            dq, dk = apply_axial_rope(dq, rope_diff), apply_axial_rope(dk, rope_diff)
            gq, gk = apply_axial_rope(gq, rope_gqa), apply_axial_rope(gk, rope_gqa)
        return dq, dk, dv, gq.reshape(bsz, t, GQA_KV_HEADS, GQA_GROUP, GQA_DH), gk, gv

    lam_p = diff_lambda.astype(jnp.float32)
    lam = jnp.exp(jnp.sum(lam_p[0] * lam_p[1])) - jnp.exp(jnp.sum(lam_p[2] * lam_p[3])) + lambda_init

    def mix(dq, gq, dk, dv, gk, gv):
        bsz, t = dq.shape[:2]
        d = differential_attention(dq, dk, dv, lam)
        d = (rms_norm(d, diff_norm) * (1.0 - lambda_init)).reshape(bsz, t, DIFF_HEADS * 2 * DIFF_DH)
        a = softmax_attention(gq, gk, gv).reshape(bsz, t, GQA_HEADS * GQA_DH)
        return jnp.concatenate([d, a], axis=-1) @ w_out

    cdq, cdk, cdv, cgq, cgk, cgv = heads(h_ctx, False)
    ldq, ldk, ldv, lgq, lgk, lgv = heads(h_lat, True)
    cat = lambda a, b: jnp.concatenate([a, b], axis=1)
    out_lat = mix(ldq, lgq, cat(cdk, ldk), cat(cdv, ldv), cat(cgk, lgk), cat(cgv, lgv))
    out_ctx = mix(cdq, cgq, cdk, cdv, cgk, cgv) if need_ctx else None
    return out_ctx, out_lat


def setup_inputs(seed: int = 0) -> dict:
    key = jax.random.key(seed)
    keys = jax.random.split(key, 32)
    counter = iter(range(32))
    f32 = jnp.float32

    def normal(shape, scale):
        return jax.random.normal(keys[next(counter)], shape, f32) * scale

    def gain(shape):
        return 1.0 + 0.02 * jax.random.normal(keys[next(counter)], shape, f32)

    n_ev, n_od = (DEPTH + 1) // 2, DEPTH // 2
    d = D_MODEL
    a_log = jnp.log(jax.random.uniform(keys[next(counter)], (n_ev, 2, GDN_HEADS), f32, 1.0, 16.0))
    dt = jnp.exp(jax.random.uniform(keys[next(counter)], (n_ev, 2, GDN_HEADS), f32,
                                    math.log(1e-3), math.log(1e-1)))
    dt_bias = dt + jnp.log(-jnp.expm1(-dt))
    return {
        'x': normal((BATCH, SEQ, d), 1.0),
        'c': normal((BATCH, d), 1.0),
        'ctx': normal((BATCH, CTX_LEN, d), 1.0),
        'c_ctx': normal((d,), 1.0),
        'mod_w': normal((DEPTH, d, 6 * d), d ** -0.5),
        'mod_b': normal((DEPTH, 6 * d), 0.02),
        'norm_g': gain((DEPTH, 2, d)),
        'ev_w_in': normal((n_ev, d, EVEN_IN), d ** -0.5),
        'ev_mla_q_norm': gain((n_ev, MLA_Q_RANK)),
        'ev_mla_kv_norm': gain((n_ev, MLA_KV_RANK)),
        'ev_mla_w_uq': normal((n_ev, MLA_Q_RANK, MLA_HEADS * (MLA_NOPE + MLA_ROPE)), MLA_Q_RANK ** -0.5),
        'ev_mla_w_ukv': normal((n_ev, MLA_KV_RANK, MLA_HEADS * (MLA_NOPE + MLA_V)), MLA_KV_RANK ** -0.5),
        'ev_gdn_conv': normal((n_ev, GDN_CONV, GDN_QKV), GDN_CONV ** -0.5),
        'ev_gdn_a_log': a_log,
        'ev_gdn_dt_bias': dt_bias,
        'ev_gdn_out_norm': gain((n_ev, GDN_DV)),
        'ev_w_out': normal((n_ev, MIX_WIDTH, d), MIX_WIDTH ** -0.5),
        'ev_ffn_w_gu': normal((n_ev, d, 2 * FFN_DIM), d ** -0.5),
        'ev_ffn_w_down': normal((n_ev, FFN_DIM, d), FFN_DIM ** -0.5),
        'od_w_in': normal((n_od, d, ODD_IN), d ** -0.5),
        'od_diff_lambda': normal((n_od, 4, DIFF_DH), 0.1),
        'od_diff_norm': gain((n_od, 2 * DIFF_DH)),
        'od_gqa_q_norm': gain((n_od, GQA_DH)),
        'od_gqa_k_norm': gain((n_od, GQA_DH)),
        'od_w_out': normal((n_od, MIX_WIDTH, d), MIX_WIDTH ** -0.5),
        'od_router_w': normal((n_od, d, N_EXPERTS), d ** -0.5),
        'od_moe_w_gu': normal((n_od, N_EXPERTS, d, 2 * EXPERT_DIM), d ** -0.5),
        'od_moe_w_down': normal((n_od, N_EXPERTS, EXPERT_DIM, d), EXPERT_DIM ** -0.5),
        'final_norm': gain((d,)),
    }


def reference(x, c, ctx, c_ctx, mod_w, mod_b, norm_g, ev_w_in, ev_mla_q_norm, ev_mla_kv_norm,
              ev_mla_w_uq, ev_mla_w_ukv, ev_gdn_conv, ev_gdn_a_log, ev_gdn_dt_bias, ev_gdn_out_norm,
              ev_w_out, ev_ffn_w_gu, ev_ffn_w_down, od_w_in, od_diff_lambda, od_diff_norm,
              od_gqa_q_norm, od_gqa_k_norm, od_w_out, od_router_w, od_moe_w_gu, od_moe_w_down,
              final_norm):
    n_rows = x.shape[1] // GRID_W
    rope_mla = axial_rope_tables(n_rows, MLA_ROPE)
    rope_diff = axial_rope_tables(n_rows, DIFF_DH)
    rope_gqa = axial_rope_tables(n_rows, GQA_DH)
    for i in range(DEPTH):
        last = i == DEPTH - 1
        j = i // 2
        sh1, sc1, gt1, sh2, sc2, gt2 = modulation(c, mod_w[i], mod_b[i])
        csh1, csc1, cgt1, csh2, csc2, cgt2 = modulation(c_ctx, mod_w[i], mod_b[i])
        h_lat = rms_norm(x, norm_g[i, 0]) * (1.0 + sc1) + sh1
        h_ctx = rms_norm(ctx, norm_g[i, 0]) * (1.0 + csc1) + csh1
        if i % 2 == 0:
            mix_ctx, mix_lat = mla_gdn_mixer(
                h_ctx, h_lat, rope_mla, not last, ev_w_in[j], ev_mla_q_norm[j], ev_mla_kv_norm[j],
                ev_mla_w_uq[j], ev_mla_w_ukv[j], ev_gdn_conv[j], ev_gdn_a_log[j], ev_gdn_dt_bias[j],
                ev_gdn_out_norm[j], ev_w_out[j])
            ffn = functools.partial(swiglu, w_gu=ev_ffn_w_gu[j], w_down=ev_ffn_w_down[j])
        else:
            lambda_init = 0.8 - 0.6 * math.exp(-0.3 * i)
            mix_ctx, mix_lat = diff_gqa_mixer(
                h_ctx, h_lat, rope_diff, rope_gqa, not last, lambda_init, od_w_in[j],
                od_diff_lambda[j], od_diff_norm[j], od_gqa_q_norm[j], od_gqa_k_norm[j], od_w_out[j])
            ffn = functools.partial(moe_swiglu, router_w=od_router_w[j], w_gu=od_moe_w_gu[j],
                                    w_down=od_moe_w_down[j])
        x = x + gt1 * mix_lat
        x = x + gt2 * ffn(rms_norm(x, norm_g[i, 1]) * (1.0 + sc2) + sh2)
        if not last:
            ctx = ctx + cgt1 * mix_ctx
            ctx = ctx + cgt2 * ffn(rms_norm(ctx, norm_g[i, 1]) * (1.0 + csc2) + csh2)
    return rms_norm(x, final_norm)
```

```python
import numpy as np
from contextlib import ExitStack
import concourse.bass as bass
import concourse.mybir as mybir
from concourse.bass_utils import run_bass_kernel_spmd

F32 = mybir.dt.float32
BF16 = mybir.dt.bfloat16
AF = mybir.ActivationFunctionType
ALU = mybir.AluOpType
AX = mybir.AxisListType


class Buf:
    ALL = []

    def __init__(self, name, t):
        self.name = name
        self.t = t
        self.st = {}
        Buf.ALL.append(self)

    def __getitem__(self, idx):
        return self.t[idx]


class Sched:
    CE = ("pe", "dve", "act", "pool")

    def __init__(self, nc, stack, ndma=8):
        self.nc = nc
        self.stack = stack
        self.sem = {e: stack.enter_context(nc.semaphore("s_" + e)) for e in self.CE}
        self.cnt = {e: 0 for e in self.CE}
        self.q = {e: [] for e in ("pe", "dve", "act", "pool", "sp")}
        self.seen = {e: {} for e in self.q}
        self.dsem = {}
        self.dcnt = {}
        self.di = {}
        for qn in ("sp", "pool", "act"):
            self.dsem[qn] = [stack.enter_context(nc.semaphore("d_%s%d" % (qn, i))) for i in range(ndma)]
            self.dcnt[qn] = [0] * ndma
            self.di[qn] = 0
        self.out_tokens = []
        self.nbuf = 0

    def sb(self, name, shape, dt, st=None, side=None):
        t = (st or self.stack).enter_context(self.nc.sbuf_tensor(name, list(shape), dt, side=side))
        return Buf(name, t)

    def ps(self, name, shape, dt=F32, st=None):
        t = (st or self.stack).enter_context(self.nc.psum_tensor(name, list(shape), dt))
        b = Buf(name, t)
        b.whole = True
        return b

    def dram(self, name, shape, dt, kind="Internal"):
        t = self.nc.dram_tensor(name, list(shape), dt, kind=kind)
        return Buf(name, t)

    @staticmethod
    def _norm(x):
        if isinstance(x, Buf):
            return x, None
        if getattr(x[0], "whole", False):
            return x[0], None
        return x

    def _deps(self, reads, writes):
        toks = []
        for x in reads:
            b, k = self._norm(x)
            keys = [k] if k is not None else list(b.st.keys())
            if k is not None and None in b.st:
                keys.append(None)
            if k is None and None not in keys:
                keys.append(None)
            for kk in keys:
                s = b.st.get(kk)
                if s and s[0] is not None:
                    toks.append(("raw", s[0]))
        for x in writes:
            b, k = self._norm(x)
            keys = [k] if k is not None else list(b.st.keys())
            if k is not None and None in b.st:
                keys.append(None)
            if k is None and None not in keys:
                keys.append(None)
            for kk in keys:
                s = b.st.get(kk)
                if s:
                    if s[0] is not None:
                        toks.append(("waw", s[0]))
                    for r in s[1].values():
                        toks.append(("war", r))
        return toks

    def _update(self, reads, writes, tok):
        for x in reads:
            b, k = self._norm(x)
            s = b.st.setdefault(k, [None, {}])
            if tok[0] not in s[1] or s[1][tok[0]][2] < tok[2]:
                s[1][tok[0]] = tok
        for x in writes:
            b, k = self._norm(x)
            if k is None:
                b.st = {None: [tok, {}]}
            else:
                b.st[k] = [tok, {}]

    def _emit_waits(self, eng, toks):
        need = {}
        for kind, t in toks:
            semkey, semh, val, src = t
            if src == eng and eng == "pe":
                continue
            if self.seen[eng].get(semkey, 0) >= val:
                continue
            if semkey not in need or need[semkey][1] < val:
                need[semkey] = (semh, val)
        for semkey, (semh, val) in need.items():
            self.seen[eng][semkey] = val
            self.q[eng].append(("wait", semh, val))

    def op(self, eng, fn, reads=(), writes=()):
        toks = self._deps(reads, writes)
        self._emit_waits(eng, toks)
        self.cnt[eng] += 1
        tok = ("c_" + eng, self.sem[eng], self.cnt[eng], eng)
        self.q[eng].append(("op", fn, self.sem[eng], 1))
        self._update(reads, writes, tok)
        return tok

    def dma(self, qn, out, in_, reads=(), writes=(), **kw):
        toks = self._deps(reads, writes)
        i = self.di[qn]
        n = len(self.dsem[qn])
        r = i % n
        self.di[qn] = i + 1
        semh = self.dsem[qn][r]
        semkey = "d_%s%d" % (qn, r)
        if self.dcnt[qn][r] > 0:
            toks.append(("waw", (semkey, semh, self.dcnt[qn][r], "dma")))
        self._emit_waits(qn, toks)
        self.dcnt[qn][r] += 16
        tok = (semkey, semh, self.dcnt[qn][r], "dma")

        def fn(e, out=out, in_=in_, kw=kw):
            return e.dma_start(out=out, in_=in_, **kw)

        self.q[qn].append(("op", fn, semh, 16))
        self._update(reads, writes, tok)
        return tok

    def barrier(self):
        toks = []
        for e in self.CE:
            if self.cnt[e] > 0:
                toks.append(("c_" + e, self.sem[e], self.cnt[e], "x"))
        for qn in self.dsem:
            for r, semh in enumerate(self.dsem[qn]):
                if self.dcnt[qn][r] > 0:
                    toks.append(("d_%s%d" % (qn, r), semh, self.dcnt[qn][r], "dma"))
        for e in self.q:
            self._emit_waits(e, [("raw", t) for t in toks])
        for b in Buf.ALL:
            b.st = {}

    def wait_all(self, eng, toks):
        self._emit_waits(eng, [("raw", t) for t in toks])

    def emit(self):
        nc = self.nc
        emap = {"pe": "tensor", "dve": "vector", "act": "scalar", "pool": "gpsimd", "sp": "sync"}
        with nc.Block() as block:
            for en, bn in emap.items():
                items = self.q[en]

                def body(e, items=items):
                    for it in items:
                        if it[0] == "wait":
                            e.wait_ge(it[1], it[2])
                        else:
                            ins = it[1](e)
                            ins.then_inc(it[2], it[3])

                getattr(block, bn)(body)
        for en in self.q:
            self.q[en] = []


def _mm(S, out, lhsT, rhs, start, stop, R, W):
    return S.op("pe", lambda e: e.matmul(out, lhsT=lhsT, rhs=rhs, start=start, stop=stop), reads=R, writes=W)


def _tr(S, out, in_, ident, R, W):
    return S.op("pe", lambda e: e.transpose(out, in_, ident), reads=R, writes=W)


def _act(S, out, in_, func, R, W, scale=1.0, bias=0.0, eng="act"):
    return S.op(eng, lambda e: e.activation(out=out, in_=in_, func=func, bias=bias, scale=scale), reads=R, writes=W)


def _tt(S, eng, out, in0, in1, op, R, W):
    return S.op(eng, lambda e: e.tensor_tensor(out=out, in0=in0, in1=in1, op=op), reads=R, writes=W)


def _ts(S, eng, out, in0, s1, s2, op0, op1, R, W):
    if s2 is None:
        return S.op(eng, lambda e: e.tensor_scalar(out=out, in0=in0, scalar1=s1, scalar2=None, op0=op0), reads=R, writes=W)
    return S.op(eng, lambda e: e.tensor_scalar(out=out, in0=in0, scalar1=s1, scalar2=s2, op0=op0, op1=op1), reads=R, writes=W)


def _stt(S, eng, out, in0, scalar, in1, op0, op1, R, W):
    return S.op(eng, lambda e: e.scalar_tensor_tensor(out=out, in0=in0, scalar=scalar, in1=in1, op0=op0, op1=op1), reads=R, writes=W)


def _cp(S, eng, out, in_, R, W):
    if eng == "act":
        return S.op(eng, lambda e: e.copy(out=out, in_=in_), reads=R, writes=W)
    return S.op(eng, lambda e: e.tensor_copy(out=out, in_=in_), reads=R, writes=W)


def _ms(S, eng, ap, val, W):
    return S.op(eng, lambda e: e.memset(ap, val), reads=[], writes=W)


D = 1024
EPS = 1e-6


class K:
    pass


def build(TL, TC, stages=("l0", "l1"), dbg=()):
    Buf.ALL.clear()
    nc = bass.Bass("TRN2", target_bir_lowering=False)
    TT = TC + TL
    tiles = [(0, TC)] + [(TC + 512 * i, 512) for i in range(TL // 512)]
    g = K()
    g.nc, g.TL, g.TC, g.TT, g.tiles = nc, TL, TC, TT, tiles
    g.dbgset = set(dbg)
    g.stg_i = 0
    g.has_gdn = "gdn" in stages

    def din(name, shape, dt=F32):
        return Buf(name, nc.dram_tensor(name, list(shape), dt, kind="ExternalInput"))

    g.x_in = din("x", [TL, D])
    g.ctx_in = din("ctx", [TC, D])
    g.ccol = din("ccol", [128, 8, 2])
    g.modw = din("mod_w", [2, D, 6 * D])
    g.modb = din("modb", [128, 2, 48])
    g.normg = din("normg", [128, 2, 2, 8])
    g.fnorm = din("fnorm", [128, 8])
    g.out = Buf("out", nc.dram_tensor("out", [TL, D], F32, kind="ExternalOutput"))
    g.cosq96 = din("cosq96", [96, TT]); g.sinq96 = din("sinq96", [96, TT])
    g.cos32 = din("cos32", [32, TT]); g.sin32 = din("sin32", [32, TT])
    g.ev_w_in = din("ev_w_in", [1024, 2752]); g.ev_w_uq = din("ev_w_uq", [384, 768]); g.ev_w_ukv = din("ev_w_ukv", [256, 1024])
    g.qn_col = din("qn_col", [128, 3]); g.kvn_col = din("kvn_col", [128, 2])
    g.ev_w_out = din("ev_w_out", [1024, 1024]); g.ev_wgu = din("ev_wgu", [1024, 5632]); g.ev_wdn = din("ev_wdn", [2816, 1024])

    g.od_w_in = din("od_w_in", [1024, 2304]); g.od_w_out = din("od_w_out", [1024, 1024])
    g.gcols = din("gcols", [128, 4]); g.dlam = din("dlam", [128, 4, 64]); g.dncol = din("dncol", [128, 1])
    g.cosq64 = din("cosq64", [128, TT]); g.sinq64 = din("sinq64", [128, TT])
    g.cos64 = din("cos64", [128, TT]); g.sin64 = din("sin64", [128, TT])
    g.router = din("router", [1024, 8]); g.moe_gu = din("moe_gu", [8, 1024, 7168]); g.moe_dn = din("moe_dn", [8, 3584, 1024])
    g.conv_col = din("conv_col", [128, 12, 5]); g.alog_rep = din("alog_rep", [128, 16]); g.dtb_rep = din("dtb_rep", [128, 16])
    g.onrm_rep = din("onrm_rep", [128, 8, 64])
    import math
    g.lambda_init = 0.8 - 0.6 * math.exp(-0.3 * 1)

    with ExitStack() as st:
        S = Sched(nc, st)
        g.S = S
        g.xT_s = S.dram("xT_s", [8, 128, TT], F32)
        phase_const(g)
        phase_mod(g)
        phase_x0(g)
        S.barrier()
        if "l0" in stages or "p1" in stages:
            phase_l0_p1(g)
            S.barrier()
        if "l0" in stages or "mla" in stages:
            phase_l0_mla(g)
            S.barrier()
        if "gdn" in stages:
            phase_gdn_prep(g)
            S.barrier()
            phase_gdn(g)
            S.barrier()
        if "l0" in stages or "wout" in stages:
            phase_wout(g, 0, g.ev_w_out)
            S.barrier()
        if "l0" in stages or "ffn" in stages:
            phase_ffn0(g)
            S.barrier()
        if "l1" in stages or "l1a" in stages:
            phase_l1_p1(g)
            S.barrier()
            phase_l1_attn(g)
            S.barrier()
            phase_wout(g, 1, g.od_w_out)
            S.barrier()
        if "l1" in stages or "moe" in stages:
            phase_moe_a(g)
            S.barrier()
            phase_moe_b(g)
            S.barrier()
        phase_final(g)
        S.wait_all("sp", S.out_tokens)
        S.emit()
    return nc


def dbg_out(g, name, buf, ap, shape, dt=F32):
    S = g.S
    o = Buf(name, g.nc.dram_tensor(name, list(shape), dt, kind="ExternalOutput"))
    idx = tuple(slice(None) for _ in shape)
    tok = S.dma("pool", o.t[idx], ap, reads=[buf], writes=[o])
    S.out_tokens.append(tok)


def phase_const(g):
    S = g.S
    g.identF = S.sb("identF", [128, 128], F32)
    g.identB = S.sb("identB", [128, 128], BF16)
    g.onesF = S.sb("onesF", [128, 128], F32)
    g.onesB = S.sb("onesB", [128, 128], BF16)
    _ms(S, "pool", g.onesF[:, :], 1.0, [g.onesF])
    _ms(S, "pool", g.identF[:, :], 1.0, [g.identF])
    S.op("pool", lambda e: e.affine_select(out=g.identF[:, :], in_=g.identF[:, :], pattern=[[-1, 128]],
                                           compare_op=ALU.is_equal, fill=0.0, base=0, channel_multiplier=1),
         reads=[g.identF], writes=[g.identF])
    _cp(S, "dve", g.identB[:, :], g.identF[:, :], [g.identF], [g.identB])
    _cp(S, "dve", g.onesB[:, :], g.onesF[:, :], [g.onesF], [g.onesB])


def phase_mod(g):
    S = g.S
    nc = g.nc
    g.modT = S.sb("modT", [128, 2, 48, 2], F32)
    g.modA = S.sb("modA", [128, 2, 2, 8, 2], F32)
    g.fn = S.sb("fn", [128, 8], F32)
    with ExitStack() as ph:
        cc = S.sb("cc", [128, 8, 2], F32, ph)
        sc = S.sb("sc", [128, 8, 2], F32, ph)
        mb = S.sb("mb", [128, 2, 48], F32, ph)
        ng = S.sb("ng", [128, 2, 2, 8], F32, ph)
        wb = [S.sb("wb%d" % i, [128, 8, 768], F32, ph) for i in range(2)]
        psm = S.ps("psm", [128, 2, 48, 2], F32, ph)
        S.dma("sp", cc[:, :, :], g.ccol[:, :, :], reads=[g.ccol], writes=[cc])
        S.dma("sp", mb[:, :, :], g.modb[:, :, :], reads=[g.modb], writes=[mb])
        S.dma("sp", ng[:, :, :, :], g.normg[:, :, :, :], reads=[g.normg], writes=[ng])
        S.dma("sp", g.fn[:, :], g.fnorm[:, :], reads=[g.fnorm], writes=[g.fn])
        _act(S, sc[:, :, :], cc[:, :, :], AF.Silu, [cc], [sc])
        i = 0
        for l in range(2):
            wv = g.modw[l].rearrange("(kc p) n -> p kc n", p=128)
            for blk in range(8):
                w = wb[i % 2]
                i += 1
                S.dma("sp", w[:, :, :], wv[:, :, blk * 768:(blk + 1) * 768], reads=[g.modw], writes=[w])
                for fs in range(6):
                    s = blk * 6 + fs
                    for kc in range(8):
                        _mm(S, psm[:, l, s, :], w[:, kc, fs * 128:(fs + 1) * 128], sc[:, kc, :], kc == 0, kc == 7,
                            [w, sc], [(psm, (l, s))])
            for j in range(2):
                _tt(S, "dve", g.modT[:, l, :, j], psm[:, l, :, j], mb[:, l, :], ALU.add, [psm, mb], [(g.modT, (l, j))])
            for u in range(2):
                for j in range(2):
                    _stt(S, "dve", g.modA[:, l, u, :, j], g.modT[:, l, (3 * u + 1) * 8:(3 * u + 2) * 8, j], 1.0,
                         ng[:, l, u, :], ALU.add, ALU.mult, [g.modT, ng], [(g.modA, (l, u, j))])
        if "mod" in g.dbgset:
            dbg_out(g, "dbg_modT", g.modT, g.modT[:, :, :, :], [128, 2, 48, 2])
            dbg_out(g, "dbg_modA", g.modA, g.modA[:, :, :, :, :], [128, 2, 2, 8, 2])
        S.emit()


def tile_src(g, t0, nt):
    if t0 < g.TC:
        src, r0 = g.ctx_in, t0
    else:
        src, r0 = g.x_in, t0 - g.TC
    return src, src[r0:r0 + nt, :].rearrange("(s p) d -> p s d", p=128)


def phase_x0(g):
    S = g.S
    with ExitStack() as ph:
        xin = [S.sb("xin%d" % i, [128, 4, D], F32, ph) for i in range(2)]
        xt = [S.sb("xt%d" % i, [128, 8, 512], F32, ph) for i in range(2)]
        pst = [S.ps("pst%d" % i, [128, 512], F32, ph) for i in range(4)]
        k = 0
        for ti, (t0, nt) in enumerate(g.tiles):
            ns = nt // 128
            a = xin[ti % 2]
            b = xt[ti % 2]
            srcb, sap = tile_src(g, t0, nt)
            S.dma("sp", a[:, 0:ns, :], sap, reads=[srcb], writes=[a])
            for c in range(8):
                p = pst[k % 4]
                k += 1
                for s in range(ns):
                    _tr(S, p[:, s * 128:(s + 1) * 128], a[:, s, c * 128:(c + 1) * 128], g.identF[:, :],
                        [a, g.identF], [p])
                _cp(S, "dve" if c % 2 == 0 else "act", b[:, c, 0:nt], p[:, 0:nt], [p], [(b, c)])
            S.dma("pool", g.xT_s.t[:, :, t0:t0 + nt].rearrange("c p t -> p c t"), b[:, :, 0:nt],
                  reads=[b], writes=[(g.xT_s, ti)])
        S.emit()


def rstd_from_ss(S, eng, out_ap, ps_ap, n, R, W):
    _ts(S, eng, out_ap, ps_ap, 1.0 / n, EPS, ALU.mult, ALU.add, R, W)
    _act(S, out_ap, out_ap, AF.Sqrt, W, W)
    S.op("dve", lambda e: e.reciprocal(out=out_ap, in_=out_ap), reads=W, writes=W)


def phase_final(g):
    S = g.S
    with ExitStack() as ph:
        xt = [S.sb("fxt%d" % i, [128, 8, 512], F32, ph) for i in range(2)]
        sq = S.sb("fsq", [128, 8, 512], F32, ph)
        rs = S.sb("frs", [128, 512], F32, ph)
        yo = [S.sb("fyo%d" % i, [128, 4, D], F32, ph) for i in range(2)]
        pss = S.ps("fpss", [128, 512], F32, ph)
        pst = [S.ps("fpst%d" % i, [128, 512], F32, ph) for i in range(4)]
        k = 0
        for ti, (t0, nt) in enumerate(g.tiles):
            if t0 < g.TC:
                continue
            ns = nt // 128
            a = xt[ti % 2]
            y = yo[ti % 2]
            S.dma("sp", a[:, :, 0:nt], g.xT_s.t[:, :, t0:t0 + nt].rearrange("c p t -> p c t"),
                  reads=[(g.xT_s, ti)], writes=[a])
            for c in range(8):
                _tt(S, "pool" if c % 2 else "dve", sq[:, c, 0:nt], a[:, c, 0:nt], a[:, c, 0:nt], ALU.mult, [a], [(sq, c)])
            for c in range(8):
                _mm(S, pss[:, 0:nt], g.onesF[:, :], sq[:, c, 0:nt], c == 0, c == 7, [g.onesF, (sq, c)], [pss])
            rstd_from_ss(S, "dve", rs[:, 0:nt], pss[:, 0:nt], D, [pss], [rs])
            for c in range(8):
                _stt(S, "dve", sq[:, c, 0:nt], a[:, c, 0:nt], g.fn[:, c:c + 1], rs[:, 0:nt],
                     ALU.mult, ALU.mult, [a, g.fn, rs, (sq, c)], [(sq, c)])
            for s in range(ns):
                for h in range(2):
                    p = pst[k % 4]
                    k += 1
                    for cc in range(4):
                        c = h * 4 + cc
                        _tr(S, p[:, cc * 128:(cc + 1) * 128], sq[:, c, s * 128:(s + 1) * 128], g.identF[:, :],
                            [(sq, c), g.identF], [p])
                    _cp(S, "act" if h else "dve", y[:, s, h * 512:(h + 1) * 512], p[:, :], [p], [(y, (s, h))])
            r0 = t0 - g.TC
            tok = S.dma("pool", g.out.t[r0:r0 + nt, :].rearrange("(s p) d -> p s d", p=128), y[:, 0:ns, :],
                        reads=[y], writes=[(g.out, ti)])
            S.out_tokens.append(tok)
        S.emit()


def load_w(g, ph, name, src_buf, src_ap, K, N, stg):
    S = g.S
    kc = K // 128
    w = S.sb(name, [128, kc, N], BF16, ph)
    v = src_ap.rearrange("(kc p) n -> p kc n", p=128)
    i = 0
    for k0 in range(0, kc, 8):
        k1 = min(kc, k0 + 8)
        for n0 in range(0, N, 512):
            n1 = min(N, n0 + 512)
            s = stg[g.stg_i % len(stg)]
            g.stg_i += 1
            S.dma("sp", s[:, 0:k1 - k0, 0:n1 - n0], v[:, k0:k1, n0:n1], reads=[src_buf], writes=[s])
            eng = ("dve", "pool", "act")[g.stg_i % 3]
            _cp(S, eng, w[:, k0:k1, n0:n1], s[:, 0:k1 - k0, 0:n1 - n0], [s], [(w, (k0, n0))])
    return w


def make_rot(g, rot, w, view, off, n):
    S = g.S
    q = n // 4
    rv, wv = view(rot), view(w)
    _ms(S, "pool", rot[:, :, :], 0.0, [rot])
    for (d0, s0, sign) in ((0, q, -1.0), (q, 0, 1.0), (2 * q, 3 * q, -1.0), (3 * q, 2 * q, 1.0)):
        _ts(S, "dve", rv[:, :, :, off + d0:off + d0 + q], wv[:, :, :, off + s0:off + s0 + q], sign, None, ALU.mult, None,
            [w, rot], [rot])


def norm_mod(g, a, sq, rs, hT, pss, nt, A_ap, B_ap, n=D, nchunk=8, xr=()):
    S = g.S
    for c in range(nchunk):
        _tt(S, "pool" if c % 2 else "dve", sq[:, c, 0:nt], a[:, c, 0:nt], a[:, c, 0:nt], ALU.mult, [(a, c)], [(sq, c)])
    for c in range(nchunk):
        _mm(S, pss[:, 0:nt], g.onesF[:, :], sq[:, c, 0:nt], c == 0, c == nchunk - 1, [g.onesF, (sq, c)], [pss])
    rstd_from_ss(S, "dve", rs[:, 0:nt], pss[:, 0:nt], n, [pss], [rs])
    for c in range(nchunk):
        _stt(S, "dve", sq[:, c, 0:nt], a[:, c, 0:nt], A_ap(c), rs[:, 0:nt], ALU.mult, ALU.mult,
             [(a, c), rs, (sq, c)] + list(xr), [(sq, c)])
        if B_ap is None:
            _cp(S, "act", hT[:, c, 0:nt], sq[:, c, 0:nt], [(sq, c)], [(hT, c)])
        else:
            _act(S, hT[:, c, 0:nt], sq[:, c, 0:nt], AF.Identity, [(sq, c)] + list(xr), [(hT, c)], bias=B_ap(c))


def xT_view(g, t0, nt):
    return g.xT_s.t[:, :, t0:t0 + nt].rearrange("c p t -> p c t")


def phase_l0_p1(g):
    S = g.S
    nc = g.nc
    TT = g.TT
    g.qT_s = S.dram("qT_s", [8, 96, TT], BF16)
    g.kT_s = S.dram("kT_s", [8, 96, TT], BF16)
    g.v_s = S.dram("v_s", [TT, 8, 65], BF16)
    g.mixT_s = S.dram("mixT_s", [1024, TT], BF16)
    g.gq_s = S.dram("gq_s", [12, 128, TT], F32)
    g.ab_s = S.dram("ab_s", [32, TT], F32)
    g.z_s = S.dram("z_s", [TT, 512], F32)
    with ExitStack() as ph:
        g.stg_i = 0
        with ExitStack() as ph2:
            stg = [S.sb("stg%d" % i, [128, 8, 512], F32, ph2, side="right") for i in range(2)]
            w_in = load_w(g, ph, "w_in0", g.ev_w_in, g.ev_w_in.t[:, :], 1024, 2752, stg)
            w_uq = load_w(g, ph, "w_uq", g.ev_w_uq, g.ev_w_uq.t[:, :], 384, 768, stg)
            w_ukv = load_w(g, ph, "w_ukv", g.ev_w_ukv, g.ev_w_ukv.t[:, :], 256, 1024, stg)
            S.barrier()
            S.emit()
        w_uq_r = S.sb("w_uq_r", [128, 3, 768], BF16, ph)
        w_kpe_r = S.sb("w_kpe_r", [128, 8, 32], BF16, ph)
        make_rot(g, w_uq_r, w_uq, lambda b: b[:, :, :].rearrange("p k (h e) -> p k h e", e=96), 64, 32)
        _ms(S, "pool", w_kpe_r[:, :, :], 0.0, [w_kpe_r])
        for (d0, s0, sign) in ((0, 8, -1.0), (8, 0, 1.0), (16, 24, -1.0), (24, 16, 1.0)):
            _ts(S, "dve", w_kpe_r[:, :, d0:d0 + 8], w_in[:, :, 640 + s0:640 + s0 + 8], sign, None, ALU.mult, None,
                [w_in, w_kpe_r], [w_kpe_r])
        qn = S.sb("qn", [128, 3], F32, ph)
        kvn = S.sb("kvn", [128, 2], F32, ph)
        S.dma("sp", qn[:, :], g.qn_col[:, :], reads=[g.qn_col], writes=[qn])
        S.dma("sp", kvn[:, :], g.kvn_col[:, :], reads=[g.kvn_col], writes=[kvn])

        xa = [S.sb("p1xa%d" % i, [128, 8, 512], F32, ph) for i in range(2)]
        sq = S.sb("p1sq", [128, 8, 512], F32, ph)
        rs = S.sb("p1rs", [128, 512], F32, ph)
        hT = S.sb("p1hT", [128, 8, 512], BF16, ph)
        cq = S.sb("p1cq", [128, 3, 512], F32, ph)
        cqn = S.sb("p1cqn", [128, 3, 512], BF16, ph)
        ckv = S.sb("p1ckv", [128, 2, 512], F32, ph)
        ckvn = S.sb("p1ckvn", [128, 2, 512], BF16, ph)
        tq = [S.sb("p1tq%d" % i, [96, 512], F32, ph) for i in range(4)]
        qo = [S.sb("p1qo%d" % i, [96, 512], BF16, ph) for i in range(2)]
        kn = S.sb("p1kn", [64, 8, 512], BF16, ph)
        kp = S.sb("p1kp", [32, 512], BF16, ph)
        va = S.sb("p1va", [128, 4, 8, 65], BF16, ph)
        gqo = [S.sb("p1gq%d" % i, [128, 512], F32, ph) for i in range(3)]
        zo = [S.sb("p1zo%d" % i, [128, 512], F32, ph) for i in range(2)]
        cs = S.sb("p1cs", [96, 512], F32, ph)
        sn = S.sb("p1sn", [96, 512], F32, ph)
        ck = S.sb("p1ck", [32, 512], F32, ph)
        sk = S.sb("p1sk", [32, 512], F32, ph)
        pss = S.ps("p1pss", [128, 512], F32, ph)
        pp = [S.ps("p1pp%d" % i, [128, 512], F32, ph) for i in range(6)]
        _ms(S, "pool", va[:, :, :, :], 1.0, [va])
        pi = [0]

        def nextp():
            p = pp[pi[0] % 6]
            pi[0] += 1
            return p

        ev = [0]

        def evac(out_ap, in_ap, R, W):
            ev[0] += 1
            _cp(S, "act" if ev[0] % 2 else "dve", out_ap, in_ap, R, W)

        for ti, (t0, nt) in enumerate(g.tiles):
            j = 1 if t0 < g.TC else 0
            ns = nt // 128
            a = xa[ti % 2]
            S.dma("sp", a[:, :, 0:nt], xT_view(g, t0, nt), reads=[(g.xT_s, ti)], writes=[a])
            S.dma("sp", cs[:, 0:nt], g.cosq96[:, t0:t0 + nt], reads=[g.cosq96], writes=[cs])
            S.dma("sp", sn[:, 0:nt], g.sinq96[:, t0:t0 + nt], reads=[g.sinq96], writes=[sn])
            S.dma("sp", ck[:, 0:nt], g.cos32[:, t0:t0 + nt], reads=[g.cos32], writes=[ck])
            S.dma("sp", sk[:, 0:nt], g.sin32[:, t0:t0 + nt], reads=[g.sin32], writes=[sk])
            norm_mod(g, a, sq, rs, hT, pss, nt, lambda c: g.modA[:, 0, 0, c, j:j + 1], lambda c: g.modT[:, 0, c, j:j + 1])

            def proj(col0, m, lhs_w=w_in, rhs=hT, nk=8):
                p = nextp()
                for kc in range(nk):
                    _mm(S, p[0:m, 0:nt], lhs_w[:, kc, col0:col0 + m], rhs[:, kc, 0:nt], kc == 0, kc == nk - 1,
                        [lhs_w, rhs], [p])
                return p

            for c in range(3):
                p = proj(c * 128, 128)
                evac(cq[:, c, 0:nt], p[:, 0:nt], [p], [(cq, c)])
            for c in range(2):
                p = proj(384 + c * 128, 128)
                evac(ckv[:, c, 0:nt], p[:, 0:nt], [p], [(ckv, c)])
            norm_mod(g, cq, sq, rs, cqn, pss, nt, lambda c: qn[:, c:c + 1], None, n=384, nchunk=3, xr=[qn])
            norm_mod(g, ckv, sq, rs, ckvn, pss, nt, lambda c: kvn[:, c:c + 1], None, n=256, nchunk=2, xr=[kvn])
            for h in range(8):
                p1 = proj(h * 96, 96, w_uq, cqn, 3)
                p2 = proj(h * 96, 96, w_uq_r, cqn, 3)
                t = tq[h % 2]
                t2 = tq[2 + h % 2]
                o = qo[h % 2]
                _tt(S, "dve", t[:, 0:nt], p1[0:96, 0:nt], cs[:, 0:nt], ALU.mult, [p1, cs], [t])
                _tt(S, "dve", t2[:, 0:nt], p2[0:96, 0:nt], sn[:, 0:nt], ALU.mult, [p2, sn], [t2])
                _tt(S, "pool", o[:, 0:nt], t2[:, 0:nt], t[:, 0:nt], ALU.add, [t2, t], [o])
                S.dma("pool", g.qT_s.t[h, :, t0:t0 + nt], o[:, 0:nt], reads=[o], writes=[(g.qT_s, (h, ti))])
            for h in range(8):
                p = proj(h * 128, 64, w_ukv, ckvn, 2)
                evac(kn[:, h, 0:nt], p[0:64, 0:nt], [p], [(kn, h)])
            S.dma("pool", g.kT_s.t[:, 0:64, t0:t0 + nt].rearrange("h p t -> p h t"), kn[:, :, 0:nt],
                  reads=[kn], writes=[(g.kT_s, ("n", ti))])
            p1 = proj(640, 32)
            p2 = proj(0, 32, w_kpe_r, hT, 8)
            t = tq[0]
            t2 = tq[2]
            _tt(S, "dve", t[0:32, 0:nt], p1[0:32, 0:nt], ck[:, 0:nt], ALU.mult, [p1, ck], [t])
            _tt(S, "dve", t2[0:32, 0:nt], p2[0:32, 0:nt], sk[:, 0:nt], ALU.mult, [p2, sk], [t2])
            _tt(S, "pool", kp[:, 0:nt], t2[0:32, 0:nt], t[0:32, 0:nt], ALU.add, [t2, t], [kp])
            for h in range(8):
                S.dma("pool", g.kT_s.t[h, 64:96, t0:t0 + nt], kp[:, 0:nt], reads=[kp], writes=[(g.kT_s, ("p", h, ti))])
            vw = w_ukv[:, :, :].rearrange("p k (h e) -> p k h e", e=128)
            for s in range(ns):
                p = nextp()
                for kc in range(2):
                    _mm(S, p[:, :].rearrange("p (h e) -> p h e", e=64), ckvn[:, kc, s * 128:(s + 1) * 128],
                        vw[:, kc, :, 64:128], kc == 0, kc == 1, [ckvn, w_ukv], [p])
                evac(va[:, s, :, 0:64], p[:, :].rearrange("p (h e) -> p h e", e=64), [p], [(va, s)])
            S.dma("pool", g.v_s.t[t0:t0 + nt, :, :].rearrange("(s p) h e -> p s h e", p=128), va[:, 0:ns, :, :],
                  reads=[va], writes=[(g.v_s, ti)])
            for c in range(12):
                p = proj(672 + c * 128, 128)
                o = gqo[c % 3]
                evac(o[:, 0:nt], p[:, 0:nt], [p], [o])
                S.dma("pool", g.gq_s.t[c, :, t0:t0 + nt], o[:, 0:nt], reads=[o], writes=[(g.gq_s, (c, ti))])
            p = proj(2720, 32)
            o = gqo[0]
            evac(o[0:32, 0:nt], p[0:32, 0:nt], [p], [o])
            S.dma("pool", g.ab_s.t[:, t0:t0 + nt], o[0:32, 0:nt], reads=[o], writes=[(g.ab_s, ti)])
            for s in range(ns):
                p = nextp()
                for kc in range(8):
                    _mm(S, p[:, :], hT[:, kc, s * 128:(s + 1) * 128], w_in[:, kc, 2208:2720], kc == 0, kc == 7,
                        [hT, w_in], [p])
                o = zo[s % 2]
                _act(S, o[:, :], p[:, :], AF.Silu, [p], [o])
                S.dma("pool", g.z_s.t[t0 + s * 128:t0 + (s + 1) * 128, :], o[:, :], reads=[o], writes=[(g.z_s, (ti, s))])
        S.emit()


def attn_core(g, KT, V, QT, d, dva, kbs, acc, psc, pT, nq, cnt):
    S = g.S
    nkb = len(kbs)
    LA = len(psc) - 1
    slots = []

    def score(i):
        kb = kbs[i]
        sc = psc[cnt[0] % len(psc)]
        p = pT[cnt[0] % len(pT)]
        cnt[0] += 1
        _mm(S, sc[:, 0:nq], KT[0:d, kb * 128:(kb + 1) * 128], QT[0:d, 0:nq], True, True, [KT, QT], [sc])
        _act(S, p[:, 0:nq], sc[:, 0:nq], AF.Exp, [sc], [p])
        slots.append(p)

    for i in range(min(LA, nkb)):
        score(i)
    for i, kb in enumerate(kbs):
        if i + LA < nkb:
            score(i + LA)
        p = slots[i]
        for qs in range(nq // 128):
            _mm(S, acc[:, qs, 0:dva], p[:, qs * 128:(qs + 1) * 128], V[:, kb, 0:dva], i == 0 and qs == 0,
                i == nkb - 1 and qs == nq // 128 - 1, [p, V], [(acc, qs)])


def attn_core3(g, KT, V, QT, d, dv, kbs, accO, Pacc, psc, pT, nq, cnt):
    S = g.S
    nkb = len(kbs)
    LA = len(psc) - 1
    slots = []

    def score(i):
        kb = kbs[i]
        sc = psc[cnt[0] % len(psc)]
        p = pT[cnt[0] % len(pT)]
        cnt[0] += 1
        _mm(S, sc[:, 0:nq], KT[0:d, kb * 128:(kb + 1) * 128], QT[0:d, 0:nq], True, True, [KT, QT], [sc])
        _act(S, p[:, 0:nq], sc[:, 0:nq], AF.Exp, [sc], [p])
        slots.append(p)

    for i in range(min(LA, nkb)):
        score(i)
    for i, kb in enumerate(kbs):
        if i + LA < nkb:
            score(i + LA)
        p = slots[i]
        _mm(S, accO[0:dv, 0:nq], V[:, kb, 0:dv], p[:, 0:nq], i == 0, i == nkb - 1, [p, V], [accO])
        if i == 0:
            _cp(S, "pool", Pacc[:, 0:nq], p[:, 0:nq], [p], [Pacc])
        else:
            _tt(S, "pool", Pacc[:, 0:nq], Pacc[:, 0:nq], p[:, 0:nq], ALU.add, [Pacc, p], [Pacc])


def attn_simple_post(g, accO, Pacc, psm, rec, o, nq, dst_ap, dst_dep):
    S = g.S
    _mm(S, psm[0:64, 0:nq], g.onesF[:, 0:64], Pacc[:, 0:nq], True, True, [g.onesF, Pacc], [psm])
    S.op("dve", lambda e: e.reciprocal(out=rec[0:64, 0:nq], in_=psm[0:64, 0:nq]), reads=[psm], writes=[rec])
    _tt(S, "dve", o[0:64, 0:nq], accO[0:64, 0:nq], rec[0:64, 0:nq], ALU.mult, [accO, rec], [o])
    S.dma("pool", dst_ap, o[0:64, 0:nq], reads=[o], writes=[dst_dep])


def phase_l0_mla(g):
    S = g.S
    TT = g.TT
    NKB = TT // 128
    with ExitStack() as ph:
        KT = [S.sb("mKT%d" % i, [96, TT], BF16, ph) for i in range(2)]
        V = [S.sb("mV%d" % i, [128, NKB, 65], BF16, ph) for i in range(2)]
        QT = [S.sb("mQT%d" % i, [96, 512], BF16, ph) for i in range(2)]
        pT = [S.sb("mpT%d" % i, [128, 512], BF16, ph) for i in range(4)]
        Pacc = [S.sb("mPacc%d" % i, [128, 512], F32, ph) for i in range(2)]
        rec = S.sb("mrec", [64, 512], F32, ph)
        o = [S.sb("mo%d" % i, [64, 512], BF16, ph) for i in range(2)]
        psc = [S.ps("mpsc%d" % i, [128, 512], F32, ph) for i in range(4)]
        acc = [S.ps("macc%d" % i, [128, 512], F32, ph) for i in range(2)]
        psm = S.ps("mpsm", [128, 512], F32, ph)
        cnt = [0]
        qi = 0
        if not g.has_gdn:
            zt = S.sb("mzt", [128, 4, 512], BF16, ph)
            _ms(S, "pool", zt[:, :, :], 0.0, [zt])
            for ti, (t0, nt) in enumerate(g.tiles):
                S.dma("pool", g.mixT_s.t[512:1024, t0:t0 + nt].rearrange("(c p) t -> p c t", p=128), zt[:, :, 0:nt],
                      reads=[zt], writes=[(g.mixT_s, ("z", ti))])
        for h in range(8):
            kt, v = KT[h % 2], V[h % 2]
            S.dma("sp", kt[:, :], g.kT_s.t[h, :, :], reads=[g.kT_s], writes=[kt])
            S.dma("sp", v[:, :, :], g.v_s.t[:, h, :].rearrange("(n p) e -> p n e", p=128), reads=[g.v_s], writes=[v])
            for ti, (t0, nt) in enumerate(g.tiles):
                q = QT[qi % 2]
                ac = acc[qi % 2]
                pa = Pacc[qi % 2]
                oo = o[qi % 2]
                qi += 1
                S.dma("sp", q[:, 0:nt], g.qT_s.t[h, :, t0:t0 + nt], reads=[g.qT_s], writes=[q])
                kbs = list(range(g.TC // 128)) if t0 < g.TC else list(range(NKB))
                attn_core3(g, kt, v, q, 96, 64, kbs, ac, pa, psc, pT, nt, cnt)
                attn_simple_post(g, ac, pa, psm, rec, oo, nt, g.mixT_s.t[h * 64:(h + 1) * 64, t0:t0 + nt], (g.mixT_s, (h, ti)))
        S.emit()


def phase_wout(g, layer, w_src):
    S = g.S
    with ExitStack() as ph:
        with ExitStack() as ph2:
            stg = [S.sb("wo%d_stg%d" % (layer, i), [128, 8, 512], F32, ph2, side="right") for i in range(2)]
            w = load_w(g, ph, "w_out%d" % layer, w_src, w_src.t[:, :], 1024, 1024, stg)
            S.barrier()
            S.emit()
        xa = [S.sb("wo%dxa%d" % (layer, i), [128, 8, 512], F32, ph) for i in range(2)]
        m = [S.sb("wo%dm%d" % (layer, i), [128, 8, 512], BF16, ph) for i in range(2)]
        pp = [S.ps("wo%dpp%d" % (layer, i), [128, 512], F32, ph) for i in range(4)]
        k = 0
        for ti, (t0, nt) in enumerate(g.tiles):
            j = 1 if t0 < g.TC else 0
            if j == 1 and layer == 1:
                continue
            a, mm_ = xa[ti % 2], m[ti % 2]
            S.dma("sp", a[:, :, 0:nt], xT_view(g, t0, nt), reads=[(g.xT_s, ti)], writes=[a])
            S.dma("sp", mm_[:, :, 0:nt], g.mixT_s.t[:, t0:t0 + nt].rearrange("(c p) t -> p c t", p=128),
                  reads=[g.mixT_s], writes=[mm_])
            for dsl in range(8):
                p = pp[k % 4]
                k += 1
                for kc in range(8):
                    _mm(S, p[:, 0:nt], w[:, kc, dsl * 128:(dsl + 1) * 128], mm_[:, kc, 0:nt], kc == 0, kc == 7, [w, mm_], [p])
                _stt(S, "dve", a[:, dsl, 0:nt], p[:, 0:nt], g.modT[:, layer, 16 + dsl, j:j + 1], a[:, dsl, 0:nt],
                     ALU.mult, ALU.add, [p, (a, dsl)], [(a, dsl)])
            S.dma("pool", xT_view(g, t0, nt), a[:, :, 0:nt], reads=[a], writes=[(g.xT_s, ti)])
        S.emit()


def phase_ffn0(g):
    S = g.S
    NT = 256
    with ExitStack() as ph:
        with ExitStack() as ph2:
            stg = [S.sb("ff_stg%d" % i, [128, 8, 512], F32, ph2, side="right") for i in range(2)]
            wgu = load_w(g, ph, "ff_wgu", g.ev_wgu, g.ev_wgu.t[:, :], 1024, 5632, stg)
            wdn = load_w(g, ph, "ff_wdn", g.ev_wdn, g.ev_wdn.t[:, :], 2816, 1024, stg)
            S.barrier()
            S.emit()
        xa = [S.sb("ffxa%d" % i, [128, 8, NT], F32, ph) for i in range(2)]
        sq = S.sb("ffsq", [128, 8, NT], F32, ph)
        rs = S.sb("ffrs", [128, NT], F32, ph)
        hT = S.sb("ffhT", [128, 8, NT], BF16, ph)
        sg = [S.sb("ffsg%d" % i, [128, NT], F32, ph) for i in range(3)]
        act = [S.sb("ffact%d" % i, [128, NT], BF16, ph) for i in range(4)]
        pgu = [S.ps("ffpgu%d" % i, [128, 512], F32, ph) for i in range(4)]
        pss = pgu[0]
        yac = [S.ps("ffy%d" % i, [128, 2, NT], F32, ph) for i in range(4)]
        ti2 = 0
        for ti, (t0, nt) in enumerate(g.tiles):
            j = 1 if t0 < g.TC else 0
            for u0 in range(0, nt, NT):
                a = xa[ti2 % 2]
                ti2 += 1
                S.dma("sp", a[:, :, :], xT_view(g, t0 + u0, NT), reads=[(g.xT_s, ti)], writes=[a])
                norm_mod(g, a, sq, rs, hT, pss, NT, lambda c: g.modA[:, 0, 1, c, j:j + 1], lambda c: g.modT[:, 0, 24 + c, j:j + 1])
                def gu(f):
                    pg = pgu[(f % 2) * 2]
                    pu = pgu[(f % 2) * 2 + 1]
                    for kc in range(8):
                        _mm(S, pg[:, 0:NT], wgu[:, kc, f * 128:(f + 1) * 128], hT[:, kc, :], kc == 0, kc == 7, [wgu, hT], [pg])
                    for kc in range(8):
                        _mm(S, pu[:, 0:NT], wgu[:, kc, 2816 + f * 128:2816 + (f + 1) * 128], hT[:, kc, :], kc == 0, kc == 7,
                            [wgu, hT], [pu])
                    s_ = sg[f % 3]
                    ac = act[f % 4]
                    _act(S, s_[:, :], pg[:, 0:NT], AF.Silu, [pg], [s_])
                    _tt(S, "dve", ac[:, :], s_[:, :], pu[:, 0:NT], ALU.mult, [s_, pu], [ac])
                    return ac

                acs = {0: gu(0), 1: gu(1)}
                for f in range(22):
                    if f + 2 < 22:
                        acs[f + 2] = gu(f + 2)
                    ac = acs.pop(f)
                    for dsl in range(8):
                        _mm(S, yac[dsl // 2][:, dsl % 2, :], wdn[:, f, dsl * 128:(dsl + 1) * 128], ac[:, :],
                            f == 0 and dsl % 2 == 0, f == 21 and dsl % 2 == 1, [wdn, ac], [(yac[dsl // 2], dsl % 2)])
                for dsl in range(8):
                    _stt(S, "dve", a[:, dsl, :], yac[dsl // 2][:, dsl % 2, :], g.modT[:, 0, 40 + dsl, j:j + 1], a[:, dsl, :],
                         ALU.mult, ALU.add, [(yac[dsl // 2], dsl % 2), (a, dsl)], [(a, dsl)])
                S.dma("pool", xT_view(g, t0 + u0, NT), a[:, :, :], reads=[a], writes=[(g.xT_s, ti)])
        S.emit()


def phase_l1_p1(g):
    S = g.S
    TT = g.TT
    g.dqT_s = S.dram("dqT_s", [512, TT], BF16)
    g.dkT_s = S.dram("dkT_s", [512, TT], BF16)
    g.gqT_s = S.dram("gqT_s", [512, TT], BF16)
    g.gkT_s = S.dram("gkT_s", [128, TT], BF16)
    g.dv_s = S.dram("dv_s", [TT, 4, 129], BF16)
    g.gv_s = S.dram("gv_s", [TT, 2, 65], BF16)
    with ExitStack() as ph:
        with ExitStack() as ph2:
            stg = [S.sb("l1stg%d" % i, [128, 8, 512], F32, ph2, side="right") for i in range(2)]
            w = load_w(g, ph, "w_in1", g.od_w_in, g.od_w_in.t[:, :], 1024, 2304, stg)
            S.barrier()
            S.emit()
        wr = S.sb("w_in1r", [128, 8, 2304], BF16, ph)
        view = lambda b: b[:, :, :].rearrange("p k (h e) -> p k h e", e=64)
        make_rot(g, wr, w, view, 0, 64)
        gc = S.sb("l1gc", [128, 4], F32, ph)
        S.dma("sp", gc[:, :], g.gcols[:, :], reads=[g.gcols], writes=[gc])
        bd = S.sb("l1bd", [128, 128], F32, ph)
        _ms(S, "pool", bd[:, :], 0.0, [bd])
        _ms(S, "pool", bd[0:64, 0:64], 1.0, [bd])
        _ms(S, "pool", bd[64:128, 64:128], 1.0, [bd])
        xa = [S.sb("l1xa%d" % i, [128, 8, 512], F32, ph) for i in range(2)]
        sq = S.sb("l1sq", [128, 8, 512], F32, ph)
        rs = S.sb("l1rs", [128, 512], F32, ph)
        hT = S.sb("l1hT", [128, 8, 512], BF16, ph)
        cq = S.sb("l1cq", [128, 512], F32, ph)
        sq_ = S.sb("l1sq_", [128, 512], F32, ph)
        ck = S.sb("l1ck", [128, 512], F32, ph)
        sk = S.sb("l1sk", [128, 512], F32, ph)
        t1 = [S.sb("l1t1%d" % i, [128, 512], F32, ph) for i in range(2)]
        t2 = [S.sb("l1t2%d" % i, [128, 512], F32, ph) for i in range(2)]
        t3 = S.sb("l1t3", [128, 512], F32, ph)
        ob = [S.sb("l1ob%d" % i, [128, 512], BF16, ph) for i in range(3)]
        dva = S.sb("l1dva", [128, 4, 4, 129], BF16, ph)
        gva = S.sb("l1gva", [128, 4, 2, 65], BF16, ph)
        _ms(S, "pool", dva[:, :, :, :], 1.0, [dva])
        _ms(S, "pool", gva[:, :, :, :], 1.0, [gva])
        pss = S.ps("l1pss", [128, 512], F32, ph)
        pp = [S.ps("l1pp%d" % i, [128, 512], F32, ph) for i in range(6)]
        pi = [0]
        oi = [0]

        def nextp():
            p = pp[pi[0] % 6]
            pi[0] += 1
            return p

        for ti, (t0, nt) in enumerate(g.tiles):
            j = 1 if t0 < g.TC else 0
            ns = nt // 128
            a = xa[ti % 2]
            S.dma("sp", a[:, :, 0:nt], xT_view(g, t0, nt), reads=[(g.xT_s, ti)], writes=[a])
            S.dma("sp", cq[:, 0:nt], g.cosq64[:, t0:t0 + nt], reads=[g.cosq64], writes=[cq])
            S.dma("sp", sq_[:, 0:nt], g.sinq64[:, t0:t0 + nt], reads=[g.sinq64], writes=[sq_])
            S.dma("sp", ck[:, 0:nt], g.cos64[:, t0:t0 + nt], reads=[g.cos64], writes=[ck])
            S.dma("sp", sk[:, 0:nt], g.sin64[:, t0:t0 + nt], reads=[g.sin64], writes=[sk])
            norm_mod(g, a, sq, rs, hT, pss, nt, lambda c: g.modA[:, 1, 0, c, j:j + 1], lambda c: g.modT[:, 1, c, j:j + 1])

            def proj(wt, col0):
                p = nextp()
                for kc in range(8):
                    _mm(S, p[:, 0:nt], wt[:, kc, col0:col0 + 128], hT[:, kc, 0:nt], kc == 0, kc == 7, [wt, hT], [p])
                return p

            def roped(col0, cos_t, sin_t, dst, row0, gcol=None, grcol=None):
                p1 = proj(w, col0)
                p2 = proj(wr, col0)
                k = oi[0]
                oi[0] += 1
                a1, a2, o = t1[k % 2], t2[k % 2], ob[k % 3]
                if gcol is None:
                    _tt(S, "dve", a1[:, 0:nt], p1[:, 0:nt], cos_t[:, 0:nt], ALU.mult, [p1, cos_t], [a1])
                    _tt(S, "dve", a2[:, 0:nt], p2[:, 0:nt], sin_t[:, 0:nt], ALU.mult, [p2, sin_t], [a2])
                    _tt(S, "pool", o[:, 0:nt], a1[:, 0:nt], a2[:, 0:nt], ALU.add, [a1, a2], [o])
                else:
                    _act(S, t3[:, 0:nt], p1[:, 0:nt], AF.Square, [p1], [t3])
                    _mm(S, pss[:, 0:nt], bd[:, :], t3[:, 0:nt], True, True, [bd, t3], [pss])
                    rstd_from_ss(S, "dve", t3[:, 0:nt], pss[:, 0:nt], 64, [pss], [t3])
                    _stt(S, "dve", a1[:, 0:nt], p1[:, 0:nt], gcol, cos_t[:, 0:nt], ALU.mult, ALU.mult, [p1, cos_t, gc], [a1])
                    _stt(S, "dve", a2[:, 0:nt], p2[:, 0:nt], grcol, sin_t[:, 0:nt], ALU.mult, ALU.mult, [p2, sin_t, gc], [a2])
                    _tt(S, "pool", a1[:, 0:nt], a1[:, 0:nt], a2[:, 0:nt], ALU.add, [a1, a2], [a1])
                    _tt(S, "dve", o[:, 0:nt], a1[:, 0:nt], t3[:, 0:nt], ALU.mult, [a1, t3], [o])
                S.dma("pool", dst.t[row0:row0 + 128, t0:t0 + nt], o[:, 0:nt], reads=[o], writes=[(dst, (row0, ti))])

            for m in range(4):
                if j == 0:
                    roped(m * 128, cq, sq_, g.dqT_s, m * 128)
                roped(512 + m * 128, ck, sk, g.dkT_s, m * 128)
            if j == 0:
                for m in range(4):
                    roped(1536 + m * 128, cq, sq_, g.gqT_s, m * 128, gc[:, 0:1], gc[:, 1:2])
            roped(2048, ck, sk, g.gkT_s, 0, gc[:, 2:3], gc[:, 3:4])
            for s in range(ns):
                p = nextp()
                for kc in range(8):
                    _mm(S, p[:, :], hT[:, kc, s * 128:(s + 1) * 128], w[:, kc, 1024:1536], kc == 0, kc == 7, [hT, w], [p])
                _cp(S, "act", dva[:, s, :, 0:128], p[:, :].rearrange("p (h e) -> p h e", e=128), [p], [(dva, s)])
                p = nextp()
                for kc in range(8):
                    _mm(S, p[:, 0:128], hT[:, kc, s * 128:(s + 1) * 128], w[:, kc, 2176:2304], kc == 0, kc == 7, [hT, w], [p])
                _cp(S, "dve", gva[:, s, :, 0:64], p[:, 0:128].rearrange("p (h e) -> p h e", e=64), [p], [(gva, s)])
            S.dma("pool", g.dv_s.t[t0:t0 + nt, :, :].rearrange("(s p) h e -> p s h e", p=128), dva[:, 0:ns, :, :],
                  reads=[dva], writes=[(g.dv_s, ti)])
            S.dma("pool", g.gv_s.t[t0:t0 + nt, :, :].rearrange("(s p) h e -> p s h e", p=128), gva[:, 0:ns, :, :],
                  reads=[gva], writes=[(g.gv_s, ti)])
        S.emit()


def attn_core2(g, KT, V, QT, d, dva, kbs, accf, psc, pT, nq, cnt):
    S = g.S
    nkb = len(kbs)
    LA = len(psc) - 1
    slots = []

    def score(i):
        kb = kbs[i]
        sc = psc[cnt[0] % len(psc)]
        p = pT[cnt[0] % len(pT)]
        cnt[0] += 1
        _mm(S, sc[:, 0:nq], KT[0:d, kb * 128:(kb + 1) * 128], QT[0:d, 0:nq], True, True, [KT, QT], [sc])
        _act(S, p[:, 0:nq], sc[:, 0:nq], AF.Exp, [sc], [p])
        slots.append(p)

    for i in range(min(LA, nkb)):
        score(i)
    for i, kb in enumerate(kbs):
        if i + LA < nkb:
            score(i + LA)
        p = slots[i]
        for qs in range(nq // 128):
            b, ap, first, last = accf(qs)
            _mm(S, ap, p[:, qs * 128:(qs + 1) * 128], V[:, kb, 0:dva], i == 0 and first, i == nkb - 1 and last, [p, V], [b])


def phase_l1_attn(g):
    S = g.S
    TT = g.TT
    NKB = TT // 128
    li = g.lambda_init
    with ExitStack() as ph:
        KT = [S.sb("aKT%d" % i, [64, TT], BF16, ph) for i in range(2)]
        V = S.sb("aV", [128, NKB, 129], BF16, ph)
        QT = [S.sb("aQT%d" % i, [64, 512], BF16, ph) for i in range(2)]
        pT = [S.sb("apT%d" % i, [128, 512], BF16, ph) for i in range(4)]
        Pacc = [S.sb("aPacc%d" % i, [128, 512], F32, ph) for i in range(4)]
        rec = [S.sb("arec%d" % i, [128, 512], F32, ph) for i in range(2)]
        t1 = S.sb("at1", [128, 512], F32, ph)
        t2 = S.sb("at2", [128, 512], F32, ph)
        od = S.sb("aod", [128, 512], F32, ph)
        o = [S.sb("ao%d" % i, [128, 512], BF16, ph) for i in range(2)]
        lam = S.sb("alam", [128, 4], F32, ph)
        dl = S.sb("adl", [128, 4, 64], F32, ph)
        dn = S.sb("adn", [128, 1], F32, ph)
        tmp = S.sb("atmp", [128, 64], F32, ph)
        S.dma("sp", dl[:, :, :], g.dlam[:, :, :], reads=[g.dlam], writes=[dl])
        S.dma("sp", dn[:, :], g.dncol[:, :], reads=[g.dncol], writes=[dn])
        _ts(S, "dve", dn[:, :], dn[:, :], 1.0 - li, None, ALU.mult, None, [dn], [dn])
        for kk in range(2):
            _tt(S, "dve", tmp[:, 0:64], dl[:, 2 * kk, :], dl[:, 2 * kk + 1, :], ALU.mult, [dl], [tmp])
            S.op("dve", lambda e, kk=kk: e.tensor_reduce(out=lam[:, kk:kk + 1], in_=tmp[:, 0:64], axis=AX.X, op=ALU.add), reads=[tmp], writes=[lam])
        _act(S, lam[:, 0:2], lam[:, 0:2], AF.Exp, [lam], [lam])
        _tt(S, "dve", lam[:, 2:3], lam[:, 0:1], lam[:, 1:2], ALU.subtract, [lam], [lam])
        _ts(S, "dve", lam[:, 3:4], lam[:, 2:3], li, None, ALU.add, None, [lam], [lam])
        cnt = [0]
        qi = 0
        lat_tiles = [(ti, t0, nt) for ti, (t0, nt) in enumerate(g.tiles) if t0 >= g.TC]
        phd = ExitStack()
        psc = [S.ps("apsc%d" % i, [128, 512], F32, phd) for i in range(3)]
        acc = [S.ps("aacc%d" % i, [128, 512], F32, phd) for i in range(4)]
        psm = S.ps("apsm", [128, 512], F32, phd)
        for h in range(4):
            S.dma("sp", V[:, :, :], g.dv_s.t[:, h, :].rearrange("(n p) e -> p n e", p=128), reads=[g.dv_s], writes=[V])
            for m in range(2):
                S.dma("sp", KT[m][:, :], g.dkT_s.t[(2 * h + m) * 64:(2 * h + m + 1) * 64, :], reads=[g.dkT_s], writes=[KT[m]])
            for (ti, t0, nt) in lat_tiles:
                par = qi % 2
                qi += 1
                for m in range(2):
                    q = QT[m]
                    S.dma("sp", q[:, 0:nt], g.dqT_s.t[(2 * h + m) * 64:(2 * h + m + 1) * 64, t0:t0 + nt], reads=[g.dqT_s], writes=[q])
                    attn_core3(g, KT[m], V, q, 64, 128, list(range(NKB)), acc[2 * par + m], Pacc[2 * par + m], psc, pT[0:3], nt, cnt)
                a1, a2 = acc[2 * par], acc[2 * par + 1]
                for m in range(2):
                    _mm(S, psm[:, 0:nt], g.onesF[:, :], Pacc[2 * par + m][:, 0:nt], True, True, [g.onesF, Pacc[2 * par + m]], [psm])
                    S.op("dve", lambda e, m=m, nt=nt: e.reciprocal(out=rec[m][:, 0:nt], in_=psm[:, 0:nt]), reads=[psm], writes=[rec[m]])
                _tt(S, "dve", t1[:, 0:nt], a1[:, 0:nt], rec[0][:, 0:nt], ALU.mult, [a1, rec[0]], [t1])
                _stt(S, "dve", t2[:, 0:nt], a2[:, 0:nt], lam[:, 3:4], rec[1][:, 0:nt], ALU.mult, ALU.mult, [a2, lam, rec[1]], [t2])
                _tt(S, "pool", od[:, 0:nt], t1[:, 0:nt], t2[:, 0:nt], ALU.subtract, [t1, t2], [od])
                _tt(S, "pool", t1[:, 0:nt], od[:, 0:nt], od[:, 0:nt], ALU.mult, [od], [t1])
                _mm(S, psm[:, 0:nt], g.onesF[:, :], t1[:, 0:nt], True, True, [g.onesF, t1], [psm])
                rstd_from_ss(S, "dve", t2[:, 0:nt], psm[:, 0:nt], 128, [psm], [t2])
                oo = o[par]
                _stt(S, "dve", oo[:, 0:nt], od[:, 0:nt], dn[:, 0:1], t2[:, 0:nt], ALU.mult, ALU.mult, [od, dn, t2], [oo])
                S.dma("pool", g.mixT_s.t[h * 128:(h + 1) * 128, t0:t0 + nt], oo[:, 0:nt], reads=[oo], writes=[(g.mixT_s, (h, ti))])
        S.barrier()
        S.emit()
        phd.close()
        psc = [S.ps("bpsc%d" % i, [128, 512], F32, ph) for i in range(4)]
        acc = [S.ps("bacc%d" % i, [128, 512], F32, ph) for i in range(2)]
        psm = S.ps("bpsm", [128, 512], F32, ph)
        for kvh in range(2):
            S.dma("sp", V[:, :, 0:65], g.gv_s.t[:, kvh, :].rearrange("(n p) e -> p n e", p=128), reads=[g.gv_s], writes=[V])
            S.dma("sp", KT[0][:, :], g.gkT_s.t[kvh * 64:(kvh + 1) * 64, :], reads=[g.gkT_s], writes=[KT[0]])
            for grp in range(4):
                hd = kvh * 4 + grp
                for (ti, t0, nt) in lat_tiles:
                    par = qi % 2
                    qi += 1
                    q = QT[par]
                    S.dma("sp", q[:, 0:nt], g.gqT_s.t[hd * 64:(hd + 1) * 64, t0:t0 + nt], reads=[g.gqT_s], writes=[q])
                    attn_core3(g, KT[0], V, q, 64, 64, list(range(NKB)), acc[par], Pacc[par], psc, pT, nt, cnt)
                    attn_simple_post(g, acc[par], Pacc[par], psm, rec[0], o[par], nt,
                                     g.mixT_s.t[512 + hd * 64:512 + (hd + 1) * 64, t0:t0 + nt], (g.mixT_s, (8 + hd, ti)))
        S.emit()


def phase_moe_a(g):
    S = g.S
    TT = g.TT
    g.h2T_s = S.dram("h2T_s", [1024, TT], BF16)
    g.gT_s = S.dram("gT_s", [8, TT], F32)
    with ExitStack() as ph:
        rw = S.sb("mrw", [128, 8, 8], F32, ph)
        S.dma("sp", rw[:, :, :], g.router.t[:, :].rearrange("(kc p) e -> p kc e", p=128), reads=[g.router], writes=[rw])
        xa = [S.sb("maxa%d" % i, [128, 8, 512], F32, ph) for i in range(2)]
        sq = S.sb("masq", [128, 8, 512], F32, ph)
        rs = S.sb("mars", [128, 512], F32, ph)
        hT = [S.sb("mahT%d" % i, [128, 8, 512], BF16, ph) for i in range(2)]
        hF = S.sb("mahF", [128, 8, 512], F32, ph)
        lg = S.sb("malg", [128, 8], F32, ph)
        m8 = S.sb("mam8", [128, 8], F32, ph)
        sm = S.sb("masm", [128, 4], F32, ph)
        mask = S.sb("mamask", [128, 8], F32, ph)
        ex = S.sb("maex", [128, 8], F32, ph)
        gt = S.sb("magt", [128, 8], F32, ph)
        gT = [S.sb("magT%d" % i, [8, 512], F32, ph) for i in range(2)]
        pss = S.ps("mapss", [128, 512], F32, ph)
        pl = [S.ps("mapl%d" % i, [128, 512], F32, ph) for i in range(2)]
        pt = [S.ps("mapt%d" % i, [128, 512], F32, ph) for i in range(2)]
        k = 0
        for ti, (t0, nt) in enumerate(g.tiles):
            if t0 < g.TC:
                continue
            ns = nt // 128
            a = xa[ti % 2]
            h = hT[ti % 2]
            gTt = gT[ti % 2]
            S.dma("sp", a[:, :, 0:nt], xT_view(g, t0, nt), reads=[(g.xT_s, ti)], writes=[a])
            norm_mod(g, a, sq, rs, h, pss, nt, lambda c: g.modA[:, 1, 1, c, 0:1], lambda c: g.modT[:, 1, 24 + c, 0:1])
            S.dma("pool", g.h2T_s.t[:, t0:t0 + nt].rearrange("(c p) t -> p c t", p=128), h[:, :, 0:nt], reads=[h],
                  writes=[(g.h2T_s, ti)])
            for c in range(8):
                _ts(S, "dve", hF[:, c, 0:nt], sq[:, c, 0:nt], g.modT[:, 1, 24 + c, 0:1], None, ALU.add, None, [(sq, c)], [(hF, c)])
            for s in range(ns):
                p = pl[k % 2]
                p2 = pt[k % 2]
                k += 1
                for kc in range(8):
                    _mm(S, p[:, 0:8], hF[:, kc, s * 128:(s + 1) * 128], rw[:, kc, :], kc == 0, kc == 7, [hF, rw], [p])
                _cp(S, "dve", lg[:, :], p[:, 0:8], [p], [lg])
                S.op("dve", lambda e: e.max(out=m8[:, :], in_=lg[:, :]), reads=[lg], writes=[m8])
                _ts(S, "dve", sm[:, 0:1], m8[:, 0:1], -1.0, None, ALU.mult, None, [m8], [sm])
                _ts(S, "dve", mask[:, :], lg[:, :], m8[:, 1:2], None, ALU.is_ge, None, [lg, m8], [mask])
                _act(S, ex[:, :], lg[:, :], AF.Exp, [lg, sm], [ex], bias=sm[:, 0:1])
                _act(S, sm[:, 1:2], m8[:, 1:2], AF.Exp, [m8, sm], [sm], bias=sm[:, 0:1])
                _ts(S, "dve", sm[:, 2:3], sm[:, 1:2], 1.0, None, ALU.add, None, [sm], [sm])
                S.op("dve", lambda e: e.reciprocal(out=sm[:, 3:4], in_=sm[:, 2:3]), reads=[sm], writes=[sm])
                _stt(S, "dve", gt[:, :], ex[:, :], sm[:, 3:4], mask[:, :], ALU.mult, ALU.mult, [ex, sm, mask], [gt])
                _tr(S, p2[0:8, 0:128], gt[:, :], g.identF[:, :], [gt, g.identF], [p2])
                _cp(S, "dve", gTt[:, s * 128:(s + 1) * 128], p2[0:8, 0:128], [p2], [gTt])
            S.dma("pool", g.gT_s.t[:, t0:t0 + nt], gTt[:, 0:nt], reads=[gTt], writes=[(g.gT_s, ti)])
        S.emit()


def phase_moe_b(g):
    S = g.S
    NT = 256
    FH = 1792
    NF = FH // 128
    with ExitStack() as ph:
        stg = [S.sb("mbstg%d" % i, [128, 8, 512], F32, ph, side="right") for i in range(2)]
        sel = S.sb("mbsel", [8, 8, 128], F32, ph)
        for e in range(8):
            _ts(S, "dve", sel[:, e, :], g.onesF[0:8, :], g.identF[0:8, e:e + 1], None, ALU.mult, None, [g.onesF, g.identF], [(sel, e)])
        wg = S.sb("mbwg", [128, 8, FH], BF16, ph)
        wu = S.sb("mbwu", [128, 8, FH], BF16, ph)
        wd = S.sb("mbwd", [128, NF, 1024], BF16, ph)
        xa = [S.sb("mbxa%d" % i, [128, 8, NT], F32, ph) for i in range(2)]
        hT = [S.sb("mbhT%d" % i, [128, 8, NT], BF16, ph) for i in range(2)]
        gTt = [S.sb("mbgT%d" % i, [8, NT], F32, ph) for i in range(2)]
        gbs = S.sb("mbgbs", [128, NT], F32, ph)
        sg = [S.sb("mbsg%d" % i, [128, NT], F32, ph) for i in range(3)]
        tg = [S.sb("mbtg%d" % i, [128, NT], F32, ph) for i in range(3)]
        act = [S.sb("mbact%d" % i, [128, NT], BF16, ph) for i in range(4)]
        pgu = [S.ps("mbpgu%d" % i, [128, 512], F32, ph) for i in range(4)]
        yac = [S.ps("mby%d" % i, [128, 2, NT], F32, ph) for i in range(4)]
        lat = [(ti, t0, nt) for ti, (t0, nt) in enumerate(g.tiles) if t0 >= g.TC]
        it = 0
        for e in range(8):
            for fh in range(2):
                def ld(dst, src_ap, K, N):
                    kc = K // 128
                    v = src_ap.rearrange("(kc p) n -> p kc n", p=128)
                    for k0 in range(0, kc, 8):
                        k1 = min(kc, k0 + 8)
                        for n0 in range(0, N, 512):
                            n1 = min(N, n0 + 512)
                            s = stg[g.stg_i % 2]
                            g.stg_i += 1
                            S.dma("sp", s[:, 0:k1 - k0, 0:n1 - n0], v[:, k0:k1, n0:n1], reads=[g.moe_gu, g.moe_dn], writes=[s])
                            _cp(S, ("dve", "pool", "act")[g.stg_i % 3], dst[:, k0:k1, n0:n1], s[:, 0:k1 - k0, 0:n1 - n0], [s], [dst])
                ld(wg, g.moe_gu.t[e, :, fh * FH:(fh + 1) * FH], 1024, FH)
                ld(wu, g.moe_gu.t[e, :, 3584 + fh * FH:3584 + (fh + 1) * FH], 1024, FH)
                ld(wd, g.moe_dn.t[e, fh * FH:(fh + 1) * FH, :], FH, 1024)
                for (ti, t0, nt) in lat:
                    for u0 in range(0, nt, NT):
                        a, h, gt_ = xa[it % 2], hT[it % 2], gTt[it % 2]
                        it += 1
                        S.dma("sp", h[:, :, :], g.h2T_s.t[:, t0 + u0:t0 + u0 + NT].rearrange("(c p) t -> p c t", p=128),
                              reads=[g.h2T_s], writes=[h])
                        S.dma("sp", gt_[:, :], g.gT_s.t[:, t0 + u0:t0 + u0 + NT], reads=[g.gT_s], writes=[gt_])
                        S.dma("sp", a[:, :, :], xT_view(g, t0 + u0, NT), reads=[(g.xT_s, (ti, u0))], writes=[a])
                        _mm(S, pgu[0][:, 0:NT], sel[:, e, :], gt_[:, :], True, True, [(sel, e), gt_], [pgu[0]])
                        _cp(S, "dve", gbs[:, :], pgu[0][:, 0:NT], [pgu[0]], [gbs])
                        def gu(f, h=h):
                            pg = pgu[(f % 2) * 2]
                            pu = pgu[(f % 2) * 2 + 1]
                            for kc in range(8):
                                _mm(S, pg[:, 0:NT], wg[:, kc, f * 128:(f + 1) * 128], h[:, kc, :], kc == 0, kc == 7, [wg, h], [pg])
                            for kc in range(8):
                                _mm(S, pu[:, 0:NT], wu[:, kc, f * 128:(f + 1) * 128], h[:, kc, :], kc == 0, kc == 7, [wu, h], [pu])
                            s_, t_, ac = sg[f % 3], tg[f % 3], act[f % 4]
                            _act(S, s_[:, :], pg[:, 0:NT], AF.Silu, [pg], [s_])
                            _tt(S, "dve", t_[:, :], s_[:, :], pu[:, 0:NT], ALU.mult, [s_, pu], [t_])
                            _tt(S, "pool", ac[:, :], t_[:, :], gbs[:, :], ALU.mult, [t_, gbs], [ac])
                            return ac

                        acs = {0: gu(0), 1: gu(1)}
                        for f in range(NF):
                            if f + 2 < NF:
                                acs[f + 2] = gu(f + 2)
                            ac = acs.pop(f)
                            for dsl in range(8):
                                _mm(S, yac[dsl // 2][:, dsl % 2, :], wd[:, f, dsl * 128:(dsl + 1) * 128], ac[:, :],
                                    f == 0 and dsl % 2 == 0, f == NF - 1 and dsl % 2 == 1, [wd, ac], [yac[dsl // 2]])
                        for dsl in range(8):
                            _stt(S, "dve", a[:, dsl, :], yac[dsl // 2][:, dsl % 2, :], g.modT[:, 1, 40 + dsl, 0:1], a[:, dsl, :],
                                 ALU.mult, ALU.add, [yac[dsl // 2], (a, dsl)], [(a, dsl)])
                        S.dma("pool", xT_view(g, t0 + u0, NT), a[:, :, :], reads=[a], writes=[(g.xT_s, (ti, u0))])
                S.barrier()
        S.emit()


def phase_gdn_prep(g):
    S = g.S
    TT = g.TT
    g.qkvn_s = S.dram("qkvn_s", [12, 128, TT], F32)
    with ExitStack() as ph:
        cw = S.sb("gpcw", [128, 12, 5], F32, ph)
        S.dma("sp", cw[:, :, :], g.conv_col[:, :, :], reads=[g.conv_col], writes=[cw])
        bd = S.sb("gpbd", [128, 128], F32, ph)
        _ms(S, "pool", bd[:, :], 0.0, [bd])
        _ms(S, "pool", bd[0:64, 0:64], 1.0, [bd])
        _ms(S, "pool", bd[64:128, 64:128], 1.0, [bd])
        xh = [S.sb("gpxh%d" % i, [128, 12, 516], F32, ph) for i in range(2)]
        acc = [S.sb("gpacc%d" % i, [128, 512], F32, ph) for i in range(3)]
        y = [S.sb("gpy%d" % i, [128, 512], F32, ph) for i in range(3)]
        sq = [S.sb("gpsq%d" % i, [128, 512], F32, ph) for i in range(2)]
        rn = [S.sb("gprn%d" % i, [128, 512], F32, ph) for i in range(2)]
        pss = [S.ps("gppss%d" % i, [128, 512], F32, ph) for i in range(2)]
        k = 0
        for ti, (t0, nt) in enumerate(g.tiles):
            x = xh[ti % 2]
            lo = 0 if t0 < g.TC else g.TC
            hi = g.TC if t0 < g.TC else TT
            a0 = max(lo, t0 - 2)
            a1 = min(hi, t0 + nt + 2)
            if a0 > t0 - 2:
                _ms(S, "pool", x[:, :, 0:2], 0.0, [x])
            if a1 < t0 + nt + 2:
                _ms(S, "pool", x[:, :, nt + 2:nt + 4], 0.0, [x])
            S.dma("sp", x[:, :, a0 - (t0 - 2):a1 - (t0 - 2)], g.gq_s.t[:, :, a0:a1].rearrange("c p t -> p c t"),
                  reads=[g.gq_s], writes=[x])
            for c in range(12):
                ac, yy = acc[k % 3], y[k % 3]
                s_, r_ = sq[k % 2], rn[k % 2]
                ps = pss[k % 2]
                k += 1
                _ts(S, "dve", ac[:, 0:nt], x[:, c, 0:nt], cw[:, c, 0:1], None, ALU.mult, None, [x, cw], [ac])
                for j in range(1, 5):
                    _stt(S, "dve", ac[:, 0:nt], x[:, c, j:j + nt], cw[:, c, j:j + 1], ac[:, 0:nt], ALU.mult, ALU.add, [x, cw, ac], [ac])
                _act(S, yy[:, 0:nt], ac[:, 0:nt], AF.Silu, [ac], [yy])
                if c < 8:
                    _tt(S, "pool", s_[:, 0:nt], yy[:, 0:nt], yy[:, 0:nt], ALU.mult, [yy], [s_])
                    _mm(S, ps[:, 0:nt], bd[:, :], s_[:, 0:nt], True, True, [bd, s_], [ps])
                    rstd_from_ss(S, "dve", r_[:, 0:nt], ps[:, 0:nt], 1, [ps], [r_])
                    if c < 4:
                        _stt(S, "dve", yy[:, 0:nt], yy[:, 0:nt], 0.125, r_[:, 0:nt], ALU.mult, ALU.mult, [yy, r_], [yy])
                    else:
                        _tt(S, "dve", yy[:, 0:nt], yy[:, 0:nt], r_[:, 0:nt], ALU.mult, [yy, r_], [yy])
                S.dma("pool", g.qkvn_s.t[c, :, t0:t0 + nt], yy[:, 0:nt], reads=[yy], writes=[(g.qkvn_s, (c, ti))])
        S.emit()


def phase_gdn(g):
    S = g.S
    TT = g.TT
    C = 64
    g.o_s = S.dram("gdn_o_s", [2, TT, 512], F32)
    with ExitStack() as ph:
        def sb(name, shape, dt=F32):
            return S.sb("gd_" + name, shape, dt, ph)
        alr = sb("alr", [64, 16])
        dtb = sb("dtb", [64, 16])
        S.dma("sp", alr[:, :], g.alog_rep[0:64, :], reads=[g.alog_rep], writes=[alr])
        S.dma("sp", dtb[:, :], g.dtb_rep[0:64, :], reads=[g.dtb_rep], writes=[dtb])
        nA = sb("nA", [64, 16])
        _act(S, nA[:, :], alr[:, :], AF.Exp, [alr], [nA])
        _ts(S, "dve", nA[:, :], nA[:, :], -1.0, None, ALU.mult, None, [nA], [nA])
        def mask(name, op, sgn=1):
            m = sb(name, [64, 8, 64])
            _ms(S, "pool", m[:, :, :], 1.0, [m])
            S.op("pool", lambda e: e.affine_select(out=m[:, :, :], in_=m[:, :, :], pattern=[[0, 8], [-sgn, 64]],
                                                   compare_op=op, fill=0.0, base=0, channel_multiplier=sgn), reads=[m], writes=[m])
            return m
        mI = [mask("mIf", ALU.is_ge), mask("mIb", ALU.is_ge, -1)]
        mS = [mask("mSf", ALU.is_gt), mask("mSb", ALU.is_gt, -1)]
        I8 = mask("I8", ALU.is_equal)
        Tri = [mI[1], mI[0]]
        St = [sb("S%d" % d, [64, 8, 64]) for d in range(2)]
        for d in range(2):
            _ms(S, "pool", St[d][:, :, :], 0.0, [St[d]])
        nb = 4
        QK = [sb("QK%d" % i, [64, 16, C]) for i in range(nb)]
        Vf = [sb("Vf%d" % i, [64, 8, C]) for i in range(nb)]
        abT = [sb("abT%d" % i, [32, C]) for i in range(nb)]
        names = ["Ktm", "Vtm", "abm", "t16", "gg", "be", "gc", "egc", "gtot", "cd", "kdsc", "bsc", "nbe"]
        TD = [{}, {}]
        big = ["NG", "Mm", "E", "D", "Ds", "P0", "DT", "qkmT", "PT0", "Pa", "PTa", "Pb", "PTb", "TTa", "TTb", "bV", "bK", "U", "WT",
               "vnew", "o2", "od", "Kd", "P0b"]
        for dd in range(2):
            for n in names:
                TD[dd][n] = sb("%s_%d" % (n, dd), [64, 512] if n in ("Ktm", "Vtm") else [64, 32])
            for n in big:
                TD[dd][n] = sb("%s_%d" % (n, dd), [64, 8, 64],
                               F32)
        pb = [S.ps("gd_p%d" % i, [128, 512], F32, ph) for i in range(8)]
        pi = [0]

        def P():
            p = pb[pi[0] % 8]
            pi[0] += 1
            return p

        def v3(p):
            return p[0:64, :].rearrange("p (h e) -> p h e", e=64)

        ev = [0]

        def evac(out_ap, in_ap, R, W):
            ev[0] += 1
            _cp(S, "act" if ev[0] % 2 else "dve", out_ap, in_ap, R, W)

        def chunk(d, c0, it):
            T = TD[d]
            qk, vf, ab = QK[it % nb], Vf[it % nb], abT[it % nb]
            S.dma("sp", qk[:, :, :], g.qkvn_s.t[0:8, :, c0:c0 + C].rearrange("c (hh d) t -> d (c hh) t", d=64),
                  reads=[g.qkvn_s], writes=[qk])
            S.dma("sp", vf[:, :, :], g.qkvn_s.t[8:12, :, c0:c0 + C].rearrange("c (hh d) t -> d (c hh) t", d=64),
                  reads=[g.qkvn_s], writes=[vf])
            S.dma("sp", ab[:, :], g.ab_s.t[:, c0:c0 + C], reads=[g.ab_s], writes=[ab])
            Ktm, Vtm, abm = T["Ktm"], T["Vtm"], T["abm"]
            p = P()
            for h in range(8):
                _tr(S, p[0:64, h * 64:(h + 1) * 64], qk[:, 8 + h, :], g.identF[0:64, 0:64], [qk, g.identF], [p])
            evac(Ktm[:, :], p[0:64, :], [p], [Ktm])
            p = P()
            for h in range(8):
                _tr(S, p[0:64, h * 64:(h + 1) * 64], vf[:, h, :], g.identF[0:64, 0:64], [vf, g.identF], [p])
            evac(Vtm[:, :], p[0:64, :], [p], [Vtm])
            p = P()
            _tr(S, p[0:64, 0:32], ab[:, :], g.identF[0:32, 0:32], [ab, g.identF], [p])
            evac(abm[:, :], p[0:64, 0:32], [p], [abm])
            t16, gg, be, gc, egc, gtot, cd, kdsc, bsc, nbe = (T[n] for n in ("t16", "gg", "be", "gc", "egc", "gtot", "cd", "kdsc", "bsc", "nbe"))
            r8 = slice(d * 8, d * 8 + 8)
            _tt(S, "dve", t16[:, 0:8], abm[:, r8], dtb[:, r8], ALU.add, [abm, dtb], [t16])
            _act(S, t16[:, 0:8], t16[:, 0:8], AF.Exp, [t16], [t16])
            _act(S, t16[:, 0:8], t16[:, 0:8], AF.Ln, [t16], [t16], bias=1.0)
            _tt(S, "dve", gg[:, 0:8], t16[:, 0:8], nA[:, r8], ALU.mult, [t16, nA], [gg])
            _act(S, be[:, 0:8], abm[:, 16 + d * 8:24 + d * 8], AF.Sigmoid, [abm], [be])
            _ts(S, "dve", nbe[:, 0:8], be[:, 0:8], -1.0, None, ALU.mult, None, [be], [nbe])
            p = P()
            _mm(S, p[0:64, 0:8], Tri[d][:, 0, :], gg[:, 0:8], True, True, [Tri[d], gg], [p])
            evac(gc[:, 0:8], p[0:64, 0:8], [p], [gc])
            p = P()
            _mm(S, p[0:64, 0:8], g.onesF[0:64, 0:64], gg[:, 0:8], True, True, [g.onesF, gg], [p])
            evac(gtot[:, 0:8], p[0:64, 0:8], [p], [gtot])
            _act(S, egc[:, 0:8], gc[:, 0:8], AF.Exp, [gc], [egc])
            _act(S, cd[:, 0:8], gtot[:, 0:8], AF.Exp, [gtot], [cd])
            _tt(S, "dve", kdsc[:, 0:8], gtot[:, 0:8], gc[:, 0:8], ALU.subtract, [gtot, gc], [kdsc])
            _act(S, kdsc[:, 0:8], kdsc[:, 0:8], AF.Exp, [kdsc], [kdsc])
            _tt(S, "dve", bsc[:, 0:8], be[:, 0:8], egc[:, 0:8], ALU.mult, [be, egc], [bsc])
            NG, Mm, E, Dm, Ds = T["NG"], T["Mm"], T["E"], T["D"], T["Ds"]
            for h in range(8):
                _ts(S, "dve", NG[:, h, :], g.onesF[0:64, 0:64], gg[:, h:h + 1], -1.0, ALU.mult, ALU.mult, [g.onesF, gg], [(NG, h)])
            p = P()
            for h in range(8):
                _mm(S, v3(p)[:, h, :], NG[:, h, :], Tri[d][:, 0, :], True, True, [(NG, h), Tri[d]], [p])
            for h in range(8):
                _ts(S, "dve", Mm[:, h, :], v3(p)[:, h, :], gc[:, h:h + 1], 0.0, ALU.add, ALU.min, [p, gc], [(Mm, h)])
            _act(S, E[:, :, :], Mm[:, :, :], AF.Exp, [Mm], [E])
            _tt(S, "pool", Dm[:, :, :], E[:, :, :], mI[d][:, :, :], ALU.mult, [E, mI[d]], [Dm])
            _tt(S, "pool", Ds[:, :, :], E[:, :, :], mS[d][:, :, :], ALU.mult, [E, mS[d]], [Ds])
            P0, DT, qkmT, PT0 = T["P0"], T["DT"], T["qkmT"], T["PT0"]
            p = P()
            for h in range(8):
                _mm(S, v3(p)[:, h, :], qk[:, 8 + h, :], qk[:, 8 + h, :], True, True, [qk], [p])
            for h in range(8):
                _stt(S, "dve", P0[:, h, :], v3(p)[:, h, :], nbe[:, h:h + 1], Ds[:, h, :], ALU.mult, ALU.mult, [p, nbe, Ds], [(P0, h)])
            p = P()
            for h in range(8):
                _tr(S, v3(p)[:, h, :], Dm[:, h, :], g.identF[0:64, 0:64], [Dm, g.identF], [p])
            evac(DT[:, :, :], v3(p), [p], [DT])
            p = P()
            for h in range(8):
                _mm(S, v3(p)[:, h, :], qk[:, 8 + h, :], qk[:, h, :], True, True, [qk], [p])
            _tt(S, "dve", qkmT[:, :, :], v3(p), DT[:, :, :], ALU.mult, [p, DT], [qkmT])
            p = P()
            for h in range(8):
                _tr(S, v3(p)[:, h, :], P0[:, h, :], g.identF[0:64, 0:64], [(P0, h), g.identF], [p])
            evac(PT0[:, :, :], v3(p), [p], [PT0])
            TTa, TTb = T["TTa"], T["TTb"]
            _tt(S, "pool", TTa[:, :, :], PT0[:, :, :], I8[:, :, :], ALU.add, [PT0, I8], [TTa])
            Pk, PTk = P0, PT0
            cur, nxt = TTa, TTb
            alt = [(T["Pa"], T["PTa"]), (T["Pb"], T["PTb"])]
            for lv in range(1, 6):
                Pn, PTn = alt[lv % 2]
                p = P()
                for h in range(8):
                    _mm(S, v3(p)[:, h, :], PTk[:, h, :], Pk[:, h, :], True, True, [Pk, PTk], [p])
                evac(Pn[:, :, :], v3(p), [p], [Pn])
                if lv < 5:
                    p = P()
                    for h in range(8):
                        _mm(S, v3(p)[:, h, :], Pk[:, h, :], PTk[:, h, :], True, True, [Pk, PTk], [p])
                    evac(PTn[:, :, :], v3(p), [p], [PTn])
                p = P()
                for h in range(8):
                    _mm(S, v3(p)[:, h, :], Pn[:, h, :], cur[:, h, :], True, True, [Pn, cur], [p])
                _tt(S, "dve", nxt[:, :, :], v3(p), cur[:, :, :], ALU.add, [p, cur], [nxt])
                cur, nxt = nxt, cur
                Pk, PTk = Pn, PTn
            TT_ = cur
            bV, bK, U, WT = T["bV"], T["bK"], T["U"], T["WT"]
            Kv = Ktm[:, :].rearrange("p (h e) -> p h e", e=64)
            Vv = Vtm[:, :].rearrange("p (h e) -> p h e", e=64)
            for h in range(8):
                _ts(S, "dve", bV[:, h, :], Vv[:, h, :], be[:, h:h + 1], None, ALU.mult, None, [Vtm, be], [(bV, h)])
                _ts(S, "pool", bK[:, h, :], Kv[:, h, :], bsc[:, h:h + 1], None, ALU.mult, None, [Ktm, bsc], [(bK, h)])
            p = P()
            for h in range(8):
                _mm(S, v3(p)[:, h, :], TT_[:, h, :], bV[:, h, :], True, True, [TT_, bV], [p])
            evac(U[:, :, :], v3(p), [p], [U])
            p = P()
            for h in range(8):
                _mm(S, v3(p)[:, h, :], bK[:, h, :], TT_[:, h, :], True, True, [TT_, bK], [p])
            evac(WT[:, :, :], v3(p), [p], [WT])
            Sd = St[d]
            vnew, o2, od, Kd = T["vnew"], T["o2"], T["od"], T["Kd"]
            for h in range(8):
                _ts(S, "pool", Kd[:, h, :], Kv[:, h, :], kdsc[:, h:h + 1], None, ALU.mult, None, [Ktm, kdsc], [(Kd, h)])
            pw = P()
            for h in range(8):
                _mm(S, v3(pw)[:, h, :], WT[:, h, :], Sd[:, h, :], True, True, [WT, Sd], [pw])
            pq = P()
            for h in range(8):
                _mm(S, v3(pq)[:, h, :], qk[:, h, :], Sd[:, h, :], True, True, [qk, Sd], [pq])
            _tt(S, "dve", vnew[:, :, :], U[:, :, :], v3(pw), ALU.subtract, [U, pw], [vnew])
            p2 = P()
            for h in range(8):
                _mm(S, v3(p2)[:, h, :], qkmT[:, h, :], vnew[:, h, :], True, True, [qkmT, vnew], [p2])
            evac(o2[:, :, :], v3(p2), [p2], [o2])
            for h in range(8):
                _stt(S, "dve", od[:, h, :], v3(pq)[:, h, :], egc[:, h:h + 1], o2[:, h, :], ALU.mult, ALU.add, [pq, egc, o2], [(od, h)])
            p3 = P()
            for h in range(8):
                _mm(S, v3(p3)[:, h, :], Kd[:, h, :], vnew[:, h, :], True, True, [Kd, vnew], [p3])
            for h in range(8):
                _stt(S, "dve", Sd[:, h, :], Sd[:, h, :], cd[:, h:h + 1], v3(p3)[:, h, :], ALU.mult, ALU.add, [Sd, cd, p3], [Sd])
            odf = od[:, :, :].rearrange("p h e -> p (h e)")
            S.dma("pool", g.o_s.t[d, c0:c0 + C, :], odf, reads=[od], writes=[(g.o_s, (d, c0))])

        nctx = g.TC // C
        nlat = g.TL // C
        it = 0
        orders = [list(range(nctx)) + [nctx + i for i in range(nlat)],
                  list(range(nctx - 1, -1, -1)) + [nctx + i for i in range(nlat - 1, -1, -1)]]
        for step in range(nctx + nlat):
            for d in range(2):
                chunk(d, orders[d][step] * C, it)
                it += 1
        S.barrier()
        S.emit()
    with ExitStack() as ph:
        onr = S.sb("gc_onr", [128, 8, 64], F32, ph)
        S.dma("sp", onr[:, :, :], g.onrm_rep[:, :, :], reads=[g.onrm_rep], writes=[onr])
        o0 = [S.sb("gc_o0%d" % i, [128, 8, 64], F32, ph) for i in range(2)]
        o1 = [S.sb("gc_o1%d" % i, [128, 8, 64], F32, ph) for i in range(2)]
        zt = [S.sb("gc_z%d" % i, [128, 8, 64], F32, ph) for i in range(2)]
        sqo = S.sb("gc_sq", [128, 8, 64], F32, ph)
        ss = S.sb("gc_ss", [128, 16], F32, ph)
        ob = [S.sb("gc_ob%d" % i, [128, 8, 64], BF16, ph) for i in range(2)]
        oT = [S.sb("gc_oT%d" % i, [128, 4, 128], BF16, ph) for i in range(2)]
        ptb = [S.ps("gc_ptb%d" % i, [128, 512], BF16, ph) for i in range(2)]
        for bi in range(TT // 128):
            r0 = bi * 128
            a, b, z, o_b, o_t, pt = o0[bi % 2], o1[bi % 2], zt[bi % 2], ob[bi % 2], oT[bi % 2], ptb[bi % 2]
            S.dma("sp", a[:, :, :], g.o_s.t[0, r0:r0 + 128, :].rearrange("p (h e) -> p h e", e=64), reads=[g.o_s], writes=[a])
            S.dma("sp", b[:, :, :], g.o_s.t[1, r0:r0 + 128, :].rearrange("p (h e) -> p h e", e=64), reads=[g.o_s], writes=[b])
            S.dma("sp", z[:, :, :], g.z_s.t[r0:r0 + 128, :].rearrange("p (h e) -> p h e", e=64), reads=[g.z_s], writes=[z])
            _tt(S, "pool", a[:, :, :], a[:, :, :], b[:, :, :], ALU.add, [a, b], [a])
            _tt(S, "pool", sqo[:, :, :], a[:, :, :], a[:, :, :], ALU.mult, [a], [sqo])
            S.op("dve", lambda e: e.tensor_reduce(out=ss[:, 0:8], in_=sqo[:, :, :], axis=AX.X, op=ALU.add), reads=[sqo], writes=[ss])
            rstd_from_ss(S, "dve", ss[:, 8:16], ss[:, 0:8], 64, [ss], [ss])
            _tt(S, "pool", z[:, :, :], z[:, :, :], onr[:, :, :], ALU.mult, [z, onr], [z])
            for h in range(8):
                _stt(S, "dve", o_b[:, h, :], a[:, h, :], ss[:, 8 + h:9 + h], z[:, h, :], ALU.mult, ALU.mult, [a, ss, z], [o_b])
            obf = o_b[:, :, :].rearrange("p h e -> p (h e)")
            for c in range(4):
                _tr(S, pt[:, c * 128:(c + 1) * 128], obf[:, c * 128:(c + 1) * 128], g.identB[:, :], [o_b, g.identB], [pt])
            _cp(S, "act", o_t[:, :, :], pt[:, :].rearrange("p (c t) -> p c t", t=128), [pt], [o_t])
            S.dma("pool", g.mixT_s.t[512:1024, r0:r0 + 128].rearrange("(c p) t -> p c t", p=128), o_t[:, :, :], reads=[o_t],
                  writes=[(g.mixT_s, ("g", bi))])
        S.emit()


def rope_tab(n, TL, TC):
    nf = n // 4
    inv = 1.0 / (10000.0 ** (np.arange(nf, dtype=np.float32) / nf))
    t = np.arange(TL)
    rows = (t // 64).astype(np.float32)
    cols = (t % 64).astype(np.float32)
    ang_r = rows[None, :] * inv[:, None]
    ang_c = cols[None, :] * inv[:, None]
    cos = np.concatenate([np.cos(ang_r), np.cos(ang_r), np.cos(ang_c), np.cos(ang_c)], 0)
    sin = np.concatenate([np.sin(ang_r), np.sin(ang_r), np.sin(ang_c), np.sin(ang_c)], 0)
    cos = np.concatenate([np.ones((n, TC), np.float32), cos.astype(np.float32)], 1)
    sin = np.concatenate([np.zeros((n, TC), np.float32), sin.astype(np.float32)], 1)
    return np.ascontiguousarray(cos), np.ascontiguousarray(sin)


def col(v, k):
    return np.ascontiguousarray(np.asarray(v, np.float32).reshape(k, 128).T)


def prep_core(inp, b, TL, TC):
    f = lambda a: np.ascontiguousarray(np.asarray(a, np.float32))
    TT = TL + TC
    d = {}
    d["x"] = f(inp["x"][b])
    d["ctx"] = f(inp["ctx"][b])
    d["ccol"] = np.ascontiguousarray(np.stack([col(inp["c"][b], 8), col(inp["c_ctx"], 8)], -1))
    d["mod_w"] = f(inp["mod_w"])
    d["modb"] = np.ascontiguousarray(f(inp["mod_b"]).reshape(2, 48, 128).transpose(2, 0, 1))
    d["normg"] = np.ascontiguousarray(f(inp["norm_g"]).reshape(2, 2, 8, 128).transpose(3, 0, 1, 2))
    d["fnorm"] = col(inp["final_norm"], 8)
    c32, s32 = rope_tab(32, TL, TC)
    sc = np.float32(96 ** -0.5)
    d["cosq96"] = np.ascontiguousarray(np.concatenate([np.full((64, TT), sc, np.float32), c32 * sc], 0))
    d["sinq96"] = np.ascontiguousarray(np.concatenate([np.zeros((64, TT), np.float32), s32 * sc], 0))
    d["cos32"], d["sin32"] = c32, s32
    d["ev_w_in"] = f(inp["ev_w_in"][0])
    d["ev_w_uq"] = f(inp["ev_mla_w_uq"][0])
    d["ev_w_ukv"] = f(inp["ev_mla_w_ukv"][0])
    d["qn_col"] = col(inp["ev_mla_q_norm"][0], 3)
    d["kvn_col"] = col(inp["ev_mla_kv_norm"][0], 2)
    d["ev_w_out"] = f(inp["ev_w_out"][0])
    d["ev_wgu"] = f(inp["ev_ffn_w_gu"][0])
    d["ev_wdn"] = f(inp["ev_ffn_w_down"][0])
    c64, s64 = rope_tab(64, TL, TC)
    c64 = np.ascontiguousarray(np.concatenate([c64, c64], 0)); s64 = np.ascontiguousarray(np.concatenate([s64, s64], 0))
    d["cos64"], d["sin64"] = c64, s64
    d["cosq64"], d["sinq64"] = c64 * np.float32(0.125), s64 * np.float32(0.125)
    d["od_w_in"] = f(inp["od_w_in"][0])
    d["od_w_out"] = f(inp["od_w_out"][0])
    d["router"] = f(inp["od_router_w"][0]); d["moe_gu"] = f(inp["od_moe_w_gu"][0]); d["moe_dn"] = f(inp["od_moe_w_down"][0])
    d["conv_col"] = np.ascontiguousarray(f(inp["ev_gdn_conv"][0]).reshape(5, 12, 128).transpose(2, 1, 0))
    d["alog_rep"] = np.ascontiguousarray(np.broadcast_to(f(inp["ev_gdn_a_log"][0]).reshape(1, 16), (128, 16)))
    d["dtb_rep"] = np.ascontiguousarray(np.broadcast_to(f(inp["ev_gdn_dt_bias"][0]).reshape(1, 16), (128, 16)))
    d["onrm_rep"] = np.ascontiguousarray(np.broadcast_to(f(inp["ev_gdn_out_norm"][0]).reshape(1, 1, 64), (128, 8, 64)))
    perm = np.concatenate([np.arange(16, 32), np.arange(0, 16), np.arange(48, 64), np.arange(32, 48)])
    gq = f(inp["od_gqa_q_norm"][0]); gk = f(inp["od_gqa_k_norm"][0])
    d["gcols"] = np.ascontiguousarray(np.stack([np.tile(gq, 2), np.tile(gq[perm], 2), np.tile(gk, 2), np.tile(gk[perm], 2)], -1))
    d["dlam"] = np.ascontiguousarray(np.broadcast_to(f(inp["od_diff_lambda"][0])[None], (128, 4, 64)))
    d["dncol"] = np.ascontiguousarray(f(inp["od_diff_norm"][0]).reshape(128, 1))
    return d


_NC_CACHE = {}


def kernel(**inputs):
    TL, TC = 8192, 256
    inp = {k: np.asarray(v) for k, v in inputs.items()}
    if "nc" not in _NC_CACHE:
        _NC_CACHE["nc"] = build(TL, TC, stages=("l0", "gdn", "l1"))
    nc = _NC_CACHE["nc"]
    shared = prep_core(inp, 0, TL, TC)
    in_maps = [shared]
    for b in range(1, 8):
        d = dict(shared)
        d["x"] = np.ascontiguousarray(inp["x"][b], dtype=np.float32)
        d["ctx"] = np.ascontiguousarray(inp["ctx"][b], dtype=np.float32)
        d["ccol"] = np.ascontiguousarray(np.stack([col(inp["c"][b], 8), col(inp["c_ctx"], 8)], -1))
        in_maps.append(d)
    res = run_bass_kernel_spmd(nc, in_maps, core_ids=list(range(8)))
    out = np.stack([np.asarray(r["out"], dtype=np.float32) for r in res.results], 0)
    return out
```

```python
import numpy as np
from contextlib import ExitStack
import concourse.bass as bass
import concourse.mybir as mybir
from concourse.bass_utils import run_bass_kernel_spmd

F32 = mybir.dt.float32
BF16 = mybir.dt.bfloat16
AF = mybir.ActivationFunctionType
ALU = mybir.AluOpType
AX = mybir.AxisListType


class Buf:
    ALL = []

    def __init__(self, name, t):
        self.name = name
        self.t = t
        self.st = {}
        Buf.ALL.append(self)

    def __getitem__(self, idx):
        return self.t[idx]


class Sched:
    CE = ("pe", "dve", "act", "pool")

    def __init__(self, nc, stack, ndma=8):
        self.nc = nc
        self.stack = stack
        self.sem = {e: stack.enter_context(nc.semaphore("s_" + e)) for e in self.CE}
        self.cnt = {e: 0 for e in self.CE}
        self.q = {e: [] for e in ("pe", "dve", "act", "pool", "sp")}
        self.seen = {e: {} for e in self.q}
        self.dsem = {}
        self.dcnt = {}
        self.di = {}
        for qn in ("sp", "pool", "act"):
            self.dsem[qn] = [stack.enter_context(nc.semaphore("d_%s%d" % (qn, i))) for i in range(ndma)]
            self.dcnt[qn] = [0] * ndma
            self.di[qn] = 0
        self.out_tokens = []
        self.nbuf = 0

    def sb(self, name, shape, dt, st=None, side=None):
        t = (st or self.stack).enter_context(self.nc.sbuf_tensor(name, list(shape), dt, side=side))
        return Buf(name, t)

    def ps(self, name, shape, dt=F32, st=None):
        t = (st or self.stack).enter_context(self.nc.psum_tensor(name, list(shape), dt))
        b = Buf(name, t)
        b.whole = True
        return b

    def dram(self, name, shape, dt, kind="Internal"):
        t = self.nc.dram_tensor(name, list(shape), dt, kind=kind)
        return Buf(name, t)

    @staticmethod
    def _norm(x):
        if isinstance(x, Buf):
            return x, None
        if getattr(x[0], "whole", False):
            return x[0], None
        return x

    def _deps(self, reads, writes):
        toks = []
        for x in reads:
            b, k = self._norm(x)
            keys = [k] if k is not None else list(b.st.keys())
            if k is not None and None in b.st:
                keys.append(None)
            if k is None and None not in keys:
                keys.append(None)
            for kk in keys:
                s = b.st.get(kk)
                if s and s[0] is not None:
                    toks.append(("raw", s[0]))
        for x in writes:
            b, k = self._norm(x)
            keys = [k] if k is not None else list(b.st.keys())
            if k is not None and None in b.st:
                keys.append(None)
            if k is None and None not in keys:
                keys.append(None)
            for kk in keys:
                s = b.st.get(kk)
                if s:
                    if s[0] is not None:
                        toks.append(("waw", s[0]))
                    for r in s[1].values():
                        toks.append(("war", r))
        return toks

    def _update(self, reads, writes, tok):
        for x in reads:
            b, k = self._norm(x)
            s = b.st.setdefault(k, [None, {}])
            if tok[0] not in s[1] or s[1][tok[0]][2] < tok[2]:
                s[1][tok[0]] = tok
        for x in writes:
            b, k = self._norm(x)
            if k is None:
                b.st = {None: [tok, {}]}
            else:
                b.st[k] = [tok, {}]

    def _emit_waits(self, eng, toks):
        need = {}
        for kind, t in toks:
            semkey, semh, val, src = t
            if src == eng and eng == "pe":
                continue
            if self.seen[eng].get(semkey, 0) >= val:
                continue
            if semkey not in need or need[semkey][1] < val:
                need[semkey] = (semh, val)
        for semkey, (semh, val) in need.items():
            self.seen[eng][semkey] = val
            self.q[eng].append(("wait", semh, val))

    def op(self, eng, fn, reads=(), writes=()):
        toks = self._deps(reads, writes)
        self._emit_waits(eng, toks)
        self.cnt[eng] += 1
        tok = ("c_" + eng, self.sem[eng], self.cnt[eng], eng)
        self.q[eng].append(("op", fn, self.sem[eng], 1))
        self._update(reads, writes, tok)
        return tok

    def dma(self, qn, out, in_, reads=(), writes=(), **kw):
        toks = self._deps(reads, writes)
        i = self.di[qn]
        n = len(self.dsem[qn])
        r = i % n
        self.di[qn] = i + 1
        semh = self.dsem[qn][r]
        semkey = "d_%s%d" % (qn, r)
        if self.dcnt[qn][r] > 0:
            toks.append(("waw", (semkey, semh, self.dcnt[qn][r], "dma")))
        self._emit_waits(qn, toks)
        self.dcnt[qn][r] += 16
        tok = (semkey, semh, self.dcnt[qn][r], "dma")

        def fn(e, out=out, in_=in_, kw=kw):
            return e.dma_start(out=out, in_=in_, **kw)

        self.q[qn].append(("op", fn, semh, 16))
        self._update(reads, writes, tok)
        return tok

    def barrier(self):
        toks = []
        for e in self.CE:
            if self.cnt[e] > 0:
                toks.append(("c_" + e, self.sem[e], self.cnt[e], "x"))
        for qn in self.dsem:
            for r, semh in enumerate(self.dsem[qn]):
                if self.dcnt[qn][r] > 0:
                    toks.append(("d_%s%d" % (qn, r), semh, self.dcnt[qn][r], "dma"))
        for e in self.q:
            self._emit_waits(e, [("raw", t) for t in toks])
        for b in Buf.ALL:
            b.st = {}

    def wait_all(self, eng, toks):
        self._emit_waits(eng, [("raw", t) for t in toks])

    def emit(self):
        nc = self.nc
        emap = {"pe": "tensor", "dve": "vector", "act": "scalar", "pool": "gpsimd", "sp": "sync"}
        with nc.Block() as block:
            for en, bn in emap.items():
                items = self.q[en]

                def body(e, items=items):
                    for it in items:
                        if it[0] == "wait":
                            e.wait_ge(it[1], it[2])
                        else:
                            ins = it[1](e)
                            ins.then_inc(it[2], it[3])

                getattr(block, bn)(body)
        for en in self.q:
            self.q[en] = []


def _mm(S, out, lhsT, rhs, start, stop, R, W):
    return S.op("pe", lambda e: e.matmul(out, lhsT=lhsT, rhs=rhs, start=start, stop=stop), reads=R, writes=W)


def _tr(S, out, in_, ident, R, W):
    return S.op("pe", lambda e: e.transpose(out, in_, ident), reads=R, writes=W)


def _act(S, out, in_, func, R, W, scale=1.0, bias=0.0, eng="act"):
    return S.op(eng, lambda e: e.activation(out=out, in_=in_, func=func, bias=bias, scale=scale), reads=R, writes=W)


def _tt(S, eng, out, in0, in1, op, R, W):
    return S.op(eng, lambda e: e.tensor_tensor(out=out, in0=in0, in1=in1, op=op), reads=R, writes=W)


def _ts(S, eng, out, in0, s1, s2, op0, op1, R, W):
    if s2 is None:
        return S.op(eng, lambda e: e.tensor_scalar(out=out, in0=in0, scalar1=s1, scalar2=None, op0=op0), reads=R, writes=W)
    return S.op(eng, lambda e: e.tensor_scalar(out=out, in0=in0, scalar1=s1, scalar2=s2, op0=op0, op1=op1), reads=R, writes=W)


def _stt(S, eng, out, in0, scalar, in1, op0, op1, R, W):
    return S.op(eng, lambda e: e.scalar_tensor_tensor(out=out, in0=in0, scalar=scalar, in1=in1, op0=op0, op1=op1), reads=R, writes=W)


def _cp(S, eng, out, in_, R, W):
    if eng == "act":
        return S.op(eng, lambda e: e.copy(out=out, in_=in_), reads=R, writes=W)
    return S.op(eng, lambda e: e.tensor_copy(out=out, in_=in_), reads=R, writes=W)


def _ms(S, eng, ap, val, W):
    return S.op(eng, lambda e: e.memset(ap, val), reads=[], writes=W)


D = 1024
EPS = 1e-6


class K:
    pass


def build(TL, TC, stages=("l0", "l1"), dbg=()):
    Buf.ALL.clear()
    nc = bass.Bass("TRN2", target_bir_lowering=False)
    TT = TC + TL
    tiles = [(0, TC)] + [(TC + 512 * i, 512) for i in range(TL // 512)]
    g = K()
    g.nc, g.TL, g.TC, g.TT, g.tiles = nc, TL, TC, TT, tiles
    g.dbgset = set(dbg)
    g.stg_i = 0
    g.has_gdn = "gdn" in stages

    def din(name, shape, dt=F32):
        return Buf(name, nc.dram_tensor(name, list(shape), dt, kind="ExternalInput"))

    g.x_in = din("x", [TL, D])
    g.ctx_in = din("ctx", [TC, D])
    g.ccol = din("ccol", [128, 8, 2])
    g.modw = din("mod_w", [2, D, 6 * D])
    g.modb = din("modb", [128, 2, 48])
    g.normg = din("normg", [128, 2, 2, 8])
    g.fnorm = din("fnorm", [128, 8])
    g.out = Buf("out", nc.dram_tensor("out", [TL, D], F32, kind="ExternalOutput"))
    g.cosq96 = din("cosq96", [96, TT]); g.sinq96 = din("sinq96", [96, TT])
    g.cos32 = din("cos32", [32, TT]); g.sin32 = din("sin32", [32, TT])
    g.ev_w_in = din("ev_w_in", [1024, 2752]); g.ev_w_uq = din("ev_w_uq", [384, 768]); g.ev_w_ukv = din("ev_w_ukv", [256, 1024])
    g.qn_col = din("qn_col", [128, 3]); g.kvn_col = din("kvn_col", [128, 2])
    g.ev_w_out = din("ev_w_out", [1024, 1024]); g.ev_wgu = din("ev_wgu", [1024, 5632]); g.ev_wdn = din("ev_wdn", [2816, 1024])

    g.od_w_in = din("od_w_in", [1024, 2304]); g.od_w_out = din("od_w_out", [1024, 1024])
    g.gcols = din("gcols", [128, 4]); g.dlam = din("dlam", [128, 4, 64]); g.dncol = din("dncol", [128, 1])
    g.cosq64 = din("cosq64", [128, TT]); g.sinq64 = din("sinq64", [128, TT])
    g.cos64 = din("cos64", [128, TT]); g.sin64 = din("sin64", [128, TT])
    g.router = din("router", [1024, 8]); g.moe_gu = din("moe_gu", [8, 1024, 7168]); g.moe_dn = din("moe_dn", [8, 3584, 1024])
    g.conv_col = din("conv_col", [128, 12, 5]); g.alog_rep = din("alog_rep", [128, 16]); g.dtb_rep = din("dtb_rep", [128, 16])
    g.onrm_rep = din("onrm_rep", [128, 8, 64])
    import math
    g.lambda_init = 0.8 - 0.6 * math.exp(-0.3 * 1)

    with ExitStack() as st:
        S = Sched(nc, st)
        g.S = S
        g.xT_s = S.dram("xT_s", [8, 128, TT], F32)
        phase_const(g)
        phase_mod(g)
        phase_x0(g)
        S.barrier()
        if "l0" in stages or "p1" in stages:
            phase_l0_p1(g)
            S.barrier()
        if "l0" in stages or "mla" in stages:
            phase_l0_mla(g)
            S.barrier()
        if "gdn" in stages:
            phase_gdn_prep(g)
            S.barrier()
            phase_gdn(g)
            S.barrier()
        if "l0" in stages or "wout" in stages:
            phase_wout(g, 0, g.ev_w_out)
            S.barrier()
        if "l0" in stages or "ffn" in stages:
            phase_ffn0(g)
            S.barrier()
        if "l1" in stages or "l1a" in stages:
            phase_l1_p1(g)
            S.barrier()
            phase_l1_attn(g)
            S.barrier()
            phase_wout(g, 1, g.od_w_out)
            S.barrier()
        if "l1" in stages or "moe" in stages:
            phase_moe_a(g)
            S.barrier()
            phase_moe_b(g)
            S.barrier()
        phase_final(g)
        S.wait_all("sp", S.out_tokens)
        S.emit()
    return nc


def dbg_out(g, name, buf, ap, shape, dt=F32):
    S = g.S
    o = Buf(name, g.nc.dram_tensor(name, list(shape), dt, kind="ExternalOutput"))
    idx = tuple(slice(None) for _ in shape)
    tok = S.dma("pool", o.t[idx], ap, reads=[buf], writes=[o])
    S.out_tokens.append(tok)


def phase_const(g):
    S = g.S
    g.identF = S.sb("identF", [128, 128], F32)
    g.identB = S.sb("identB", [128, 128], BF16)
    g.onesF = S.sb("onesF", [128, 128], F32)
    g.onesB = S.sb("onesB", [128, 128], BF16)
    _ms(S, "pool", g.onesF[:, :], 1.0, [g.onesF])
    _ms(S, "pool", g.identF[:, :], 1.0, [g.identF])
    S.op("pool", lambda e: e.affine_select(out=g.identF[:, :], in_=g.identF[:, :], pattern=[[-1, 128]],
                                           compare_op=ALU.is_equal, fill=0.0, base=0, channel_multiplier=1),
         reads=[g.identF], writes=[g.identF])
    g.sel65 = S.sb("sel65", [65, 64], F32)
    _ms(S, "pool", g.sel65[:, :], 0.0, [g.sel65])
    _ms(S, "pool", g.sel65[64:65, :], 1.0, [g.sel65])
    _cp(S, "dve", g.identB[:, :], g.identF[:, :], [g.identF], [g.identB])
    _cp(S, "dve", g.onesB[:, :], g.onesF[:, :], [g.onesF], [g.onesB])


def phase_mod(g):
    S = g.S
    nc = g.nc
    g.modT = S.sb("modT", [128, 2, 48, 2], F32)
    g.modA = S.sb("modA", [128, 2, 2, 8, 2], F32)
    g.fn = S.sb("fn", [128, 8], F32)
    with ExitStack() as ph:
        cc = S.sb("cc", [128, 8, 2], F32, ph)
        sc = S.sb("sc", [128, 8, 2], F32, ph)
        mb = S.sb("mb", [128, 2, 48], F32, ph)
        ng = S.sb("ng", [128, 2, 2, 8], F32, ph)
        wb = [S.sb("wb%d" % i, [128, 8, 768], F32, ph) for i in range(2)]
        psm = S.ps("psm", [128, 2, 48, 2], F32, ph)
        S.dma("sp", cc[:, :, :], g.ccol[:, :, :], reads=[g.ccol], writes=[cc])
        S.dma("sp", mb[:, :, :], g.modb[:, :, :], reads=[g.modb], writes=[mb])
        S.dma("sp", ng[:, :, :, :], g.normg[:, :, :, :], reads=[g.normg], writes=[ng])
        S.dma("sp", g.fn[:, :], g.fnorm[:, :], reads=[g.fnorm], writes=[g.fn])
        _act(S, sc[:, :, :], cc[:, :, :], AF.Silu, [cc], [sc])
        i = 0
        for l in range(2):
            wv = g.modw[l].rearrange("(kc p) n -> p kc n", p=128)
            for blk in range(8):
                w = wb[i % 2]
                i += 1
                S.dma("sp", w[:, :, :], wv[:, :, blk * 768:(blk + 1) * 768], reads=[g.modw], writes=[w])
                for fs in range(6):
                    s = blk * 6 + fs
                    for kc in range(8):
                        _mm(S, psm[:, l, s, :], w[:, kc, fs * 128:(fs + 1) * 128], sc[:, kc, :], kc == 0, kc == 7,
                            [w, sc], [(psm, (l, s))])
            for j in range(2):
                _tt(S, "dve", g.modT[:, l, :, j], psm[:, l, :, j], mb[:, l, :], ALU.add, [psm, mb], [(g.modT, (l, j))])
            for u in range(2):
                for j in range(2):
                    _stt(S, "dve", g.modA[:, l, u, :, j], g.modT[:, l, (3 * u + 1) * 8:(3 * u + 2) * 8, j], 1.0,
                         ng[:, l, u, :], ALU.add, ALU.mult, [g.modT, ng], [(g.modA, (l, u, j))])
        if "mod" in g.dbgset:
            dbg_out(g, "dbg_modT", g.modT, g.modT[:, :, :, :], [128, 2, 48, 2])
            dbg_out(g, "dbg_modA", g.modA, g.modA[:, :, :, :, :], [128, 2, 2, 8, 2])
        S.emit()


def tile_src(g, t0, nt):
    if t0 < g.TC:
        src, r0 = g.ctx_in, t0
    else:
        src, r0 = g.x_in, t0 - g.TC
    return src, src[r0:r0 + nt, :].rearrange("(s p) d -> p s d", p=128)


def phase_x0(g):
    S = g.S
    with ExitStack() as ph:
        xin = [S.sb("xin%d" % i, [128, 4, D], F32, ph) for i in range(2)]
        xt = [S.sb("xt%d" % i, [128, 8, 512], F32, ph) for i in range(2)]
        pst = [S.ps("pst%d" % i, [128, 512], F32, ph) for i in range(4)]
        k = 0
        for ti, (t0, nt) in enumerate(g.tiles):
            ns = nt // 128
            a = xin[ti % 2]
            b = xt[ti % 2]
            srcb, sap = tile_src(g, t0, nt)
            S.dma("sp", a[:, 0:ns, :], sap, reads=[srcb], writes=[a])
            for c in range(8):
                p = pst[k % 4]
                k += 1
                for s in range(ns):
                    _tr(S, p[:, s * 128:(s + 1) * 128], a[:, s, c * 128:(c + 1) * 128], g.identF[:, :],
                        [a, g.identF], [p])
                _cp(S, "dve" if c % 2 == 0 else "act", b[:, c, 0:nt], p[:, 0:nt], [p], [(b, c)])
            S.dma("pool", g.xT_s.t[:, :, t0:t0 + nt].rearrange("c p t -> p c t"), b[:, :, 0:nt],
                  reads=[b], writes=[(g.xT_s, ti)])
        S.emit()


def rstd_from_ss(S, eng, out_ap, ps_ap, n, R, W):
    _ts(S, eng, out_ap, ps_ap, 1.0 / n, EPS, ALU.mult, ALU.add, R, W)
    _act(S, out_ap, out_ap, AF.Sqrt, W, W)
    S.op("dve", lambda e: e.reciprocal(out=out_ap, in_=out_ap), reads=W, writes=W)


def phase_final(g):
    S = g.S
    with ExitStack() as ph:
        xt = [S.sb("fxt%d" % i, [128, 8, 512], F32, ph) for i in range(2)]
        sq = S.sb("fsq", [128, 8, 512], F32, ph)
        rs = S.sb("frs", [128, 512], F32, ph)
        yo = [S.sb("fyo%d" % i, [128, 4, D], F32, ph) for i in range(2)]
        pss = S.ps("fpss", [128, 512], F32, ph)
        pst = [S.ps("fpst%d" % i, [128, 512], F32, ph) for i in range(4)]
        k = 0
        for ti, (t0, nt) in enumerate(g.tiles):
            if t0 < g.TC:
                continue
            ns = nt // 128
            a = xt[ti % 2]
            y = yo[ti % 2]
            S.dma("sp", a[:, :, 0:nt], g.xT_s.t[:, :, t0:t0 + nt].rearrange("c p t -> p c t"),
                  reads=[(g.xT_s, ti)], writes=[a])
            for c in range(8):
                _tt(S, "pool" if c % 2 else "dve", sq[:, c, 0:nt], a[:, c, 0:nt], a[:, c, 0:nt], ALU.mult, [a], [(sq, c)])
            for c in range(8):
                _mm(S, pss[:, 0:nt], g.onesF[:, :], sq[:, c, 0:nt], c == 0, c == 7, [g.onesF, (sq, c)], [pss])
            rstd_from_ss(S, "dve", rs[:, 0:nt], pss[:, 0:nt], D, [pss], [rs])
            for c in range(8):
                _stt(S, "dve", sq[:, c, 0:nt], a[:, c, 0:nt], g.fn[:, c:c + 1], rs[:, 0:nt],
                     ALU.mult, ALU.mult, [a, g.fn, rs, (sq, c)], [(sq, c)])
            for s in range(ns):
                for h in range(2):
                    p = pst[k % 4]
                    k += 1
                    for cc in range(4):
                        c = h * 4 + cc
                        _tr(S, p[:, cc * 128:(cc + 1) * 128], sq[:, c, s * 128:(s + 1) * 128], g.identF[:, :],
                            [(sq, c), g.identF], [p])
                    _cp(S, "act" if h else "dve", y[:, s, h * 512:(h + 1) * 512], p[:, :], [p], [(y, (s, h))])
            r0 = t0 - g.TC
            tok = S.dma("pool", g.out.t[r0:r0 + nt, :].rearrange("(s p) d -> p s d", p=128), y[:, 0:ns, :],
                        reads=[y], writes=[(g.out, ti)])
            S.out_tokens.append(tok)
        S.emit()


def load_w(g, ph, name, src_buf, src_ap, K, N, stg):
    S = g.S
    kc = K // 128
    w = S.sb(name, [128, kc, N], BF16, ph)
    v = src_ap.rearrange("(kc p) n -> p kc n", p=128)
    i = 0
    for k0 in range(0, kc, 8):
        k1 = min(kc, k0 + 8)
        for n0 in range(0, N, 512):
            n1 = min(N, n0 + 512)
            s = stg[g.stg_i % len(stg)]
            g.stg_i += 1
            S.dma("sp", s[:, 0:k1 - k0, 0:n1 - n0], v[:, k0:k1, n0:n1], reads=[src_buf], writes=[s])
            eng = ("dve", "pool", "act")[g.stg_i % 3]
            _cp(S, eng, w[:, k0:k1, n0:n1], s[:, 0:k1 - k0, 0:n1 - n0], [s], [(w, (k0, n0))])
    return w


def make_rot(g, rot, w, view, off, n):
    S = g.S
    q = n // 4
    rv, wv = view(rot), view(w)
    _ms(S, "pool", rot[:, :, :], 0.0, [rot])
    for (d0, s0, sign) in ((0, q, -1.0), (q, 0, 1.0), (2 * q, 3 * q, -1.0), (3 * q, 2 * q, 1.0)):
        _ts(S, "dve", rv[:, :, :, off + d0:off + d0 + q], wv[:, :, :, off + s0:off + s0 + q], sign, None, ALU.mult, None,
            [w, rot], [rot])


def norm_mod(g, a, sq, rs, hT, pss, nt, A_ap, B_ap, n=D, nchunk=8, xr=()):
    S = g.S
    for c in range(nchunk):
        _tt(S, "pool" if c % 2 else "dve", sq[:, c, 0:nt], a[:, c, 0:nt], a[:, c, 0:nt], ALU.mult, [(a, c)], [(sq, c)])
    for c in range(nchunk):
        _mm(S, pss[:, 0:nt], g.onesF[:, :], sq[:, c, 0:nt], c == 0, c == nchunk - 1, [g.onesF, (sq, c)], [pss])
    rstd_from_ss(S, "dve", rs[:, 0:nt], pss[:, 0:nt], n, [pss], [rs])
    for c in range(nchunk):
        _stt(S, "dve", sq[:, c, 0:nt], a[:, c, 0:nt], A_ap(c), rs[:, 0:nt], ALU.mult, ALU.mult,
             [(a, c), rs, (sq, c)] + list(xr), [(sq, c)])
        if B_ap is None:
            _cp(S, "act", hT[:, c, 0:nt], sq[:, c, 0:nt], [(sq, c)], [(hT, c)])
        else:
            _act(S, hT[:, c, 0:nt], sq[:, c, 0:nt], AF.Identity, [(sq, c)] + list(xr), [(hT, c)], bias=B_ap(c))


def xT_view(g, t0, nt):
    return g.xT_s.t[:, :, t0:t0 + nt].rearrange("c p t -> p c t")


def phase_l0_p1(g):
    S = g.S
    nc = g.nc
    TT = g.TT
    g.qT_s = S.dram("qT_s", [8, 96, TT], BF16)
    g.kT_s = S.dram("kT_s", [8, 96, TT], BF16)
    g.v_s = S.dram("v_s", [TT, 8, 65], BF16)
    g.mixT_s = S.dram("mixT_s", [1024, TT], BF16)
    g.gq_s = S.dram("gq_s", [12, 128, TT], F32)
    g.ab_s = S.dram("ab_s", [32, TT], F32)
    g.z_s = S.dram("z_s", [TT, 512], F32)
    with ExitStack() as ph:
        g.stg_i = 0
        with ExitStack() as ph2:
            stg = [S.sb("stg%d" % i, [128, 8, 512], F32, ph2, side="right") for i in range(2)]
            w_in = load_w(g, ph, "w_in0", g.ev_w_in, g.ev_w_in.t[:, :], 1024, 2752, stg)
            w_uq = load_w(g, ph, "w_uq", g.ev_w_uq, g.ev_w_uq.t[:, :], 384, 768, stg)
            w_ukv = load_w(g, ph, "w_ukv", g.ev_w_ukv, g.ev_w_ukv.t[:, :], 256, 1024, stg)
            S.barrier()
            S.emit()
        w_uq_r = S.sb("w_uq_r", [128, 3, 768], BF16, ph)
        w_kpe_r = S.sb("w_kpe_r", [128, 8, 32], BF16, ph)
        make_rot(g, w_uq_r, w_uq, lambda b: b[:, :, :].rearrange("p k (h e) -> p k h e", e=96), 64, 32)
        _ms(S, "pool", w_kpe_r[:, :, :], 0.0, [w_kpe_r])
        for (d0, s0, sign) in ((0, 8, -1.0), (8, 0, 1.0), (16, 24, -1.0), (24, 16, 1.0)):
            _ts(S, "dve", w_kpe_r[:, :, d0:d0 + 8], w_in[:, :, 640 + s0:640 + s0 + 8], sign, None, ALU.mult, None,
                [w_in, w_kpe_r], [w_kpe_r])
        qn = S.sb("qn", [128, 3], F32, ph)
        kvn = S.sb("kvn", [128, 2], F32, ph)
        S.dma("sp", qn[:, :], g.qn_col[:, :], reads=[g.qn_col], writes=[qn])
        S.dma("sp", kvn[:, :], g.kvn_col[:, :], reads=[g.kvn_col], writes=[kvn])

        xa = [S.sb("p1xa%d" % i, [128, 8, 512], F32, ph) for i in range(2)]
        sq = S.sb("p1sq", [128, 8, 512], F32, ph)
        rs = S.sb("p1rs", [128, 512], F32, ph)
        hT = S.sb("p1hT", [128, 8, 512], BF16, ph)
        cq = S.sb("p1cq", [128, 3, 512], F32, ph)
        cqn = S.sb("p1cqn", [128, 3, 512], BF16, ph)
        ckv = S.sb("p1ckv", [128, 2, 512], F32, ph)
        ckvn = S.sb("p1ckvn", [128, 2, 512], BF16, ph)
        tq = [S.sb("p1tq%d" % i, [96, 512], F32, ph) for i in range(4)]
        qo = [S.sb("p1qo%d" % i, [96, 512], BF16, ph) for i in range(2)]
        kn = S.sb("p1kn", [64, 8, 512], BF16, ph)
        kp = S.sb("p1kp", [32, 512], BF16, ph)
        va = S.sb("p1va", [128, 4, 8, 65], BF16, ph)
        gqo = [S.sb("p1gq%d" % i, [128, 512], F32, ph) for i in range(3)]
        zo = [S.sb("p1zo%d" % i, [128, 512], F32, ph) for i in range(2)]
        cs = S.sb("p1cs", [96, 512], F32, ph)
        sn = S.sb("p1sn", [96, 512], F32, ph)
        ck = S.sb("p1ck", [32, 512], F32, ph)
        sk = S.sb("p1sk", [32, 512], F32, ph)
        pss = S.ps("p1pss", [128, 512], F32, ph)
        pp = [S.ps("p1pp%d" % i, [128, 512], F32, ph) for i in range(6)]
        _ms(S, "pool", va[:, :, :, :], 1.0, [va])
        pi = [0]

        def nextp():
            p = pp[pi[0] % 6]
            pi[0] += 1
            return p

        ev = [0]

        def evac(out_ap, in_ap, R, W):
            ev[0] += 1
            _cp(S, "act" if ev[0] % 2 else "dve", out_ap, in_ap, R, W)

        for ti, (t0, nt) in enumerate(g.tiles):
            j = 1 if t0 < g.TC else 0
            ns = nt // 128
            a = xa[ti % 2]
            S.dma("sp", a[:, :, 0:nt], xT_view(g, t0, nt), reads=[(g.xT_s, ti)], writes=[a])
            S.dma("sp", cs[:, 0:nt], g.cosq96[:, t0:t0 + nt], reads=[g.cosq96], writes=[cs])
            S.dma("sp", sn[:, 0:nt], g.sinq96[:, t0:t0 + nt], reads=[g.sinq96], writes=[sn])
            S.dma("sp", ck[:, 0:nt], g.cos32[:, t0:t0 + nt], reads=[g.cos32], writes=[ck])
            S.dma("sp", sk[:, 0:nt], g.sin32[:, t0:t0 + nt], reads=[g.sin32], writes=[sk])
            norm_mod(g, a, sq, rs, hT, pss, nt, lambda c: g.modA[:, 0, 0, c, j:j + 1], lambda c: g.modT[:, 0, c, j:j + 1])

            def proj(col0, m, lhs_w=w_in, rhs=hT, nk=8):
                p = nextp()
                for kc in range(nk):
                    _mm(S, p[0:m, 0:nt], lhs_w[:, kc, col0:col0 + m], rhs[:, kc, 0:nt], kc == 0, kc == nk - 1,
                        [lhs_w, rhs], [p])
                return p

            for c in range(3):
                p = proj(c * 128, 128)
                evac(cq[:, c, 0:nt], p[:, 0:nt], [p], [(cq, c)])
            for c in range(2):
                p = proj(384 + c * 128, 128)
                evac(ckv[:, c, 0:nt], p[:, 0:nt], [p], [(ckv, c)])
            norm_mod(g, cq, sq, rs, cqn, pss, nt, lambda c: qn[:, c:c + 1], None, n=384, nchunk=3, xr=[qn])
            norm_mod(g, ckv, sq, rs, ckvn, pss, nt, lambda c: kvn[:, c:c + 1], None, n=256, nchunk=2, xr=[kvn])
            for h in range(8):
                p1 = proj(h * 96, 96, w_uq, cqn, 3)
                p2 = proj(h * 96, 96, w_uq_r, cqn, 3)
                t = tq[h % 2]
                t2 = tq[2 + h % 2]
                o = qo[h % 2]
                _tt(S, "dve", t[:, 0:nt], p1[0:96, 0:nt], cs[:, 0:nt], ALU.mult, [p1, cs], [t])
                _tt(S, "dve", t2[:, 0:nt], p2[0:96, 0:nt], sn[:, 0:nt], ALU.mult, [p2, sn], [t2])
                _tt(S, "pool", o[:, 0:nt], t2[:, 0:nt], t[:, 0:nt], ALU.add, [t2, t], [o])
                S.dma("pool", g.qT_s.t[h, :, t0:t0 + nt], o[:, 0:nt], reads=[o], writes=[(g.qT_s, (h, ti))])
            for h in range(8):
                p = proj(h * 128, 64, w_ukv, ckvn, 2)
                evac(kn[:, h, 0:nt], p[0:64, 0:nt], [p], [(kn, h)])
            S.dma("pool", g.kT_s.t[:, 0:64, t0:t0 + nt].rearrange("h p t -> p h t"), kn[:, :, 0:nt],
                  reads=[kn], writes=[(g.kT_s, ("n", ti))])
            p1 = proj(640, 32)
            p2 = proj(0, 32, w_kpe_r, hT, 8)
            t = tq[0]
            t2 = tq[2]
            _tt(S, "dve", t[0:32, 0:nt], p1[0:32, 0:nt], ck[:, 0:nt], ALU.mult, [p1, ck], [t])
            _tt(S, "dve", t2[0:32, 0:nt], p2[0:32, 0:nt], sk[:, 0:nt], ALU.mult, [p2, sk], [t2])
            _tt(S, "pool", kp[:, 0:nt], t2[0:32, 0:nt], t[0:32, 0:nt], ALU.add, [t2, t], [kp])
            for h in range(8):
                S.dma("pool", g.kT_s.t[h, 64:96, t0:t0 + nt], kp[:, 0:nt], reads=[kp], writes=[(g.kT_s, ("p", h, ti))])
            vw = w_ukv[:, :, :].rearrange("p k (h e) -> p k h e", e=128)
            for s in range(ns):
                p = nextp()
                for kc in range(2):
                    _mm(S, p[:, :].rearrange("p (h e) -> p h e", e=64), ckvn[:, kc, s * 128:(s + 1) * 128],
                        vw[:, kc, :, 64:128], kc == 0, kc == 1, [ckvn, w_ukv], [p])
                evac(va[:, s, :, 0:64], p[:, :].rearrange("p (h e) -> p h e", e=64), [p], [(va, s)])
            S.dma("pool", g.v_s.t[t0:t0 + nt, :, :].rearrange("(s p) h e -> p s h e", p=128), va[:, 0:ns, :, :],
                  reads=[va], writes=[(g.v_s, ti)])
            for c in range(12):
                p = proj(672 + c * 128, 128)
                o = gqo[c % 3]
                evac(o[:, 0:nt], p[:, 0:nt], [p], [o])
                S.dma("pool", g.gq_s.t[c, :, t0:t0 + nt], o[:, 0:nt], reads=[o], writes=[(g.gq_s, (c, ti))])
            p = proj(2720, 32)
            o = gqo[0]
            evac(o[0:32, 0:nt], p[0:32, 0:nt], [p], [o])
            S.dma("pool", g.ab_s.t[:, t0:t0 + nt], o[0:32, 0:nt], reads=[o], writes=[(g.ab_s, ti)])
            for s in range(ns):
                p = nextp()
                for kc in range(8):
                    _mm(S, p[:, :], hT[:, kc, s * 128:(s + 1) * 128], w_in[:, kc, 2208:2720], kc == 0, kc == 7,
                        [hT, w_in], [p])
                o = zo[s % 2]
                _act(S, o[:, :], p[:, :], AF.Silu, [p], [o])
                S.dma("pool", g.z_s.t[t0 + s * 128:t0 + (s + 1) * 128, :], o[:, :], reads=[o], writes=[(g.z_s, (ti, s))])
        S.emit()


def attn_core(g, KT, V, QT, d, dva, kbs, acc, psc, pT, nq, cnt):
    S = g.S
    nkb = len(kbs)
    LA = len(psc) - 1
    slots = []

    def score(i):
        kb = kbs[i]
        sc = psc[cnt[0] % len(psc)]
        p = pT[cnt[0] % len(pT)]
        cnt[0] += 1
        _mm(S, sc[:, 0:nq], KT[0:d, kb * 128:(kb + 1) * 128], QT[0:d, 0:nq], True, True, [KT, QT], [sc])
        _act(S, p[:, 0:nq], sc[:, 0:nq], AF.Exp, [sc], [p])
        slots.append(p)

    for i in range(min(LA, nkb)):
        score(i)
    for i, kb in enumerate(kbs):
        if i + LA < nkb:
            score(i + LA)
        p = slots[i]
        for qs in range(nq // 128):
            _mm(S, acc[:, qs, 0:dva], p[:, qs * 128:(qs + 1) * 128], V[:, kb, 0:dva], i == 0 and qs == 0,
                i == nkb - 1 and qs == nq // 128 - 1, [p, V], [(acc, qs)])


def attn_core3(g, KT, V, QT, d, dv, kbs, accO, Pacc, psc, pT, nq, cnt):
    S = g.S
    nkb = len(kbs)
    LA = len(psc) - 1
    slots = []

    def score(i):
        kb = kbs[i]
        sc = psc[cnt[0] % len(psc)]
        p = pT[cnt[0] % len(pT)]
        cnt[0] += 1
        _mm(S, sc[:, 0:nq], KT[0:d, kb * 128:(kb + 1) * 128], QT[0:d, 0:nq], True, True, [KT, QT], [sc])
        _act(S, p[:, 0:nq], sc[:, 0:nq], AF.Exp, [sc], [p])
        slots.append(p)

    for i in range(min(LA, nkb)):
        score(i)
    seen = {"dve": False, "pool": False}
    for i, kb in enumerate(kbs):
        if i + LA < nkb:
            score(i + LA)
        p = slots[i]
        _mm(S, accO[0:dv, 0:nq], V[:, kb, 0:dv], p[:, 0:nq], i == 0, i == nkb - 1, [p, V], [accO])
        if Pacc is not None:
            eng = "pool" if i % 3 == 2 else "dve"
            pa = Pacc[1] if eng == "pool" else Pacc[0]
            if not seen[eng]:
                seen[eng] = True
                _cp(S, eng, pa[:, 0:nq], p[:, 0:nq], [p], [pa])
            else:
                _tt(S, eng, pa[:, 0:nq], pa[:, 0:nq], p[:, 0:nq], ALU.add, [pa, p], [pa])


def attn_simple_post(g, accO, psm, oS, rec, o, nq, dst_ap, dst_dep):
    S = g.S
    _cp(S, "dve", oS[0:65, 0:nq], accO[0:65, 0:nq], [accO], [oS])
    _mm(S, psm[0:64, 0:nq], g.sel65[0:65, :], oS[0:65, 0:nq], True, True, [g.sel65, oS], [psm])
    S.op("dve", lambda e: e.reciprocal(out=rec[0:64, 0:nq], in_=psm[0:64, 0:nq]), reads=[psm], writes=[rec])
    _tt(S, "dve", o[0:64, 0:nq], oS[0:64, 0:nq], rec[0:64, 0:nq], ALU.mult, [oS, rec], [o])
    S.dma("pool", dst_ap, o[0:64, 0:nq], reads=[o], writes=[dst_dep])


def phase_l0_mla(g):
    S = g.S
    TT = g.TT
    NKB = TT // 128
    with ExitStack() as ph:
        KT = [S.sb("mKT%d" % i, [96, TT], BF16, ph) for i in range(2)]
        V = [S.sb("mV%d" % i, [128, NKB, 65], BF16, ph) for i in range(2)]
        QT = [S.sb("mQT%d" % i, [96, 512], BF16, ph) for i in range(2)]
        pT = [S.sb("mpT%d" % i, [128, 512], BF16, ph) for i in range(4)]
        oS = [S.sb("moS%d" % i, [65, 512], F32, ph) for i in range(2)]
        rec = S.sb("mrec", [64, 512], F32, ph)
        o = [S.sb("mo%d" % i, [64, 512], BF16, ph) for i in range(2)]
        psc = [S.ps("mpsc%d" % i, [128, 512], F32, ph) for i in range(4)]
        acc = [S.ps("macc%d" % i, [128, 512], F32, ph) for i in range(2)]
        psm = S.ps("mpsm", [128, 512], F32, ph)
        cnt = [0]
        qi = 0
        if not g.has_gdn:
            zt = S.sb("mzt", [128, 4, 512], BF16, ph)
            _ms(S, "pool", zt[:, :, :], 0.0, [zt])
            for ti, (t0, nt) in enumerate(g.tiles):
                S.dma("pool", g.mixT_s.t[512:1024, t0:t0 + nt].rearrange("(c p) t -> p c t", p=128), zt[:, :, 0:nt],
                      reads=[zt], writes=[(g.mixT_s, ("z", ti))])
        for h in range(8):
            kt, v = KT[h % 2], V[h % 2]
            S.dma("sp", kt[:, :], g.kT_s.t[h, :, :], reads=[g.kT_s], writes=[kt])
            S.dma("sp", v[:, :, :], g.v_s.t[:, h, :].rearrange("(n p) e -> p n e", p=128), reads=[g.v_s], writes=[v])
            for ti, (t0, nt) in enumerate(g.tiles):
                q = QT[qi % 2]
                ac = acc[qi % 2]
                os_ = oS[qi % 2]
                oo = o[qi % 2]
                qi += 1
                S.dma("sp", q[:, 0:nt], g.qT_s.t[h, :, t0:t0 + nt], reads=[g.qT_s], writes=[q])
                kbs = list(range(g.TC // 128)) if t0 < g.TC else list(range(NKB))
                attn_core3(g, kt, v, q, 96, 65, kbs, ac, None, psc, pT, nt, cnt)
                attn_simple_post(g, ac, psm, os_, rec, oo, nt, g.mixT_s.t[h * 64:(h + 1) * 64, t0:t0 + nt], (g.mixT_s, (h, ti)))
        S.emit()


def phase_wout(g, layer, w_src):
    S = g.S
    with ExitStack() as ph:
        with ExitStack() as ph2:
            stg = [S.sb("wo%d_stg%d" % (layer, i), [128, 8, 512], F32, ph2, side="right") for i in range(2)]
            w = load_w(g, ph, "w_out%d" % layer, w_src, w_src.t[:, :], 1024, 1024, stg)
            S.barrier()
            S.emit()
        xa = [S.sb("wo%dxa%d" % (layer, i), [128, 8, 512], F32, ph) for i in range(2)]
        m = [S.sb("wo%dm%d" % (layer, i), [128, 8, 512], BF16, ph) for i in range(2)]
        pp = [S.ps("wo%dpp%d" % (layer, i), [128, 512], F32, ph) for i in range(4)]
        k = 0
        for ti, (t0, nt) in enumerate(g.tiles):
            j = 1 if t0 < g.TC else 0
            if j == 1 and layer == 1:
                continue
            a, mm_ = xa[ti % 2], m[ti % 2]
            S.dma("sp", a[:, :, 0:nt], xT_view(g, t0, nt), reads=[(g.xT_s, ti)], writes=[a])
            S.dma("sp", mm_[:, :, 0:nt], g.mixT_s.t[:, t0:t0 + nt].rearrange("(c p) t -> p c t", p=128),
                  reads=[g.mixT_s], writes=[mm_])
            for dsl in range(8):
                p = pp[k % 4]
                k += 1
                for kc in range(8):
                    _mm(S, p[:, 0:nt], w[:, kc, dsl * 128:(dsl + 1) * 128], mm_[:, kc, 0:nt], kc == 0, kc == 7, [w, mm_], [p])
                _stt(S, "dve", a[:, dsl, 0:nt], p[:, 0:nt], g.modT[:, layer, 16 + dsl, j:j + 1], a[:, dsl, 0:nt],
                     ALU.mult, ALU.add, [p, (a, dsl)], [(a, dsl)])
            S.dma("pool", xT_view(g, t0, nt), a[:, :, 0:nt], reads=[a], writes=[(g.xT_s, ti)])
        S.emit()


def phase_ffn0(g):
    S = g.S
    NT = 256
    with ExitStack() as ph:
        with ExitStack() as ph2:
            stg = [S.sb("ff_stg%d" % i, [128, 8, 512], F32, ph2, side="right") for i in range(2)]
            wgu = load_w(g, ph, "ff_wgu", g.ev_wgu, g.ev_wgu.t[:, :], 1024, 5632, stg)
            wdn = load_w(g, ph, "ff_wdn", g.ev_wdn, g.ev_wdn.t[:, :], 2816, 1024, stg)
            S.barrier()
            S.emit()
        xa = [S.sb("ffxa%d" % i, [128, 8, NT], F32, ph) for i in range(2)]
        sq = S.sb("ffsq", [128, 8, NT], F32, ph)
        rs = S.sb("ffrs", [128, NT], F32, ph)
        hT = S.sb("ffhT", [128, 8, NT], BF16, ph)
        sg = [S.sb("ffsg%d" % i, [128, NT], F32, ph) for i in range(3)]
        act = [S.sb("ffact%d" % i, [128, NT], BF16, ph) for i in range(4)]
        pgu = [S.ps("ffpgu%d" % i, [128, 512], F32, ph) for i in range(4)]
        pss = pgu[0]
        yac = [S.ps("ffy%d" % i, [128, 2, NT], F32, ph) for i in range(4)]
        ti2 = 0
        for ti, (t0, nt) in enumerate(g.tiles):
            j = 1 if t0 < g.TC else 0
            for u0 in range(0, nt, NT):
                a = xa[ti2 % 2]
                ti2 += 1
                S.dma("sp", a[:, :, :], xT_view(g, t0 + u0, NT), reads=[(g.xT_s, ti)], writes=[a])
                norm_mod(g, a, sq, rs, hT, pss, NT, lambda c: g.modA[:, 0, 1, c, j:j + 1], lambda c: g.modT[:, 0, 24 + c, j:j + 1])
                def gu(f):
                    pg = pgu[(f % 2) * 2]
                    pu = pgu[(f % 2) * 2 + 1]
                    for kc in range(8):
                        _mm(S, pg[:, 0:NT], wgu[:, kc, f * 128:(f + 1) * 128], hT[:, kc, :], kc == 0, kc == 7, [wgu, hT], [pg])
                    for kc in range(8):
                        _mm(S, pu[:, 0:NT], wgu[:, kc, 2816 + f * 128:2816 + (f + 1) * 128], hT[:, kc, :], kc == 0, kc == 7,
                            [wgu, hT], [pu])
                    s_ = sg[f % 3]
                    ac = act[f % 4]
                    _act(S, s_[:, :], pg[:, 0:NT], AF.Silu, [pg], [s_])
                    _tt(S, "dve", ac[:, :], s_[:, :], pu[:, 0:NT], ALU.mult, [s_, pu], [ac])
                    return ac

                acs = {0: gu(0), 1: gu(1)}
                for f in range(22):
                    if f + 2 < 22:
                        acs[f + 2] = gu(f + 2)
                    ac = acs.pop(f)
                    for dsl in range(8):
                        _mm(S, yac[dsl // 2][:, dsl % 2, :], wdn[:, f, dsl * 128:(dsl + 1) * 128], ac[:, :],
                            f == 0 and dsl % 2 == 0, f == 21 and dsl % 2 == 1, [wdn, ac], [(yac[dsl // 2], dsl % 2)])
                for dsl in range(8):
                    _stt(S, "dve", a[:, dsl, :], yac[dsl // 2][:, dsl % 2, :], g.modT[:, 0, 40 + dsl, j:j + 1], a[:, dsl, :],
                         ALU.mult, ALU.add, [(yac[dsl // 2], dsl % 2), (a, dsl)], [(a, dsl)])
                S.dma("pool", xT_view(g, t0 + u0, NT), a[:, :, :], reads=[a], writes=[(g.xT_s, ti)])
        S.emit()


def phase_l1_p1(g):
    S = g.S
    TT = g.TT
    g.dqT_s = S.dram("dqT_s", [512, TT], BF16)
    g.dkT_s = S.dram("dkT_s", [512, TT], BF16)
    g.gqT_s = S.dram("gqT_s", [512, TT], BF16)
    g.gkT_s = S.dram("gkT_s", [128, TT], BF16)
    g.dv_s = S.dram("dv_s", [TT, 4, 129], BF16)
    g.gv_s = S.dram("gv_s", [TT, 2, 65], BF16)
    with ExitStack() as ph:
        with ExitStack() as ph2:
            stg = [S.sb("l1stg%d" % i, [128, 8, 512], F32, ph2, side="right") for i in range(2)]
            w = load_w(g, ph, "w_in1", g.od_w_in, g.od_w_in.t[:, :], 1024, 2304, stg)
            S.barrier()
            S.emit()
        wr = S.sb("w_in1r", [128, 8, 2304], BF16, ph)
        view = lambda b: b[:, :, :].rearrange("p k (h e) -> p k h e", e=64)
        make_rot(g, wr, w, view, 0, 64)
        gc = S.sb("l1gc", [128, 4], F32, ph)
        S.dma("sp", gc[:, :], g.gcols[:, :], reads=[g.gcols], writes=[gc])
        bd = S.sb("l1bd", [128, 128], F32, ph)
        _ms(S, "pool", bd[:, :], 0.0, [bd])
        _ms(S, "pool", bd[0:64, 0:64], 1.0, [bd])
        _ms(S, "pool", bd[64:128, 64:128], 1.0, [bd])
        xa = [S.sb("l1xa%d" % i, [128, 8, 512], F32, ph) for i in range(2)]
        sq = S.sb("l1sq", [128, 8, 512], F32, ph)
        rs = S.sb("l1rs", [128, 512], F32, ph)
        hT = S.sb("l1hT", [128, 8, 512], BF16, ph)
        cq = S.sb("l1cq", [128, 512], F32, ph)
        sq_ = S.sb("l1sq_", [128, 512], F32, ph)
        ck = S.sb("l1ck", [128, 512], F32, ph)
        sk = S.sb("l1sk", [128, 512], F32, ph)
        t1 = [S.sb("l1t1%d" % i, [128, 512], F32, ph) for i in range(2)]
        t2 = [S.sb("l1t2%d" % i, [128, 512], F32, ph) for i in range(2)]
        t3 = S.sb("l1t3", [128, 512], F32, ph)
        ob = [S.sb("l1ob%d" % i, [128, 512], BF16, ph) for i in range(3)]
        dva = S.sb("l1dva", [128, 4, 4, 129], BF16, ph)
        gva = S.sb("l1gva", [128, 4, 2, 65], BF16, ph)
        _ms(S, "pool", dva[:, :, :, :], 1.0, [dva])
        _ms(S, "pool", gva[:, :, :, :], 1.0, [gva])
        pss = S.ps("l1pss", [128, 512], F32, ph)
        pp = [S.ps("l1pp%d" % i, [128, 512], F32, ph) for i in range(6)]
        pi = [0]
        oi = [0]

        def nextp():
            p = pp[pi[0] % 6]
            pi[0] += 1
            return p

        for ti, (t0, nt) in enumerate(g.tiles):
            j = 1 if t0 < g.TC else 0
            ns = nt // 128
            a = xa[ti % 2]
            S.dma("sp", a[:, :, 0:nt], xT_view(g, t0, nt), reads=[(g.xT_s, ti)], writes=[a])
            S.dma("sp", cq[:, 0:nt], g.cosq64[:, t0:t0 + nt], reads=[g.cosq64], writes=[cq])
            S.dma("sp", sq_[:, 0:nt], g.sinq64[:, t0:t0 + nt], reads=[g.sinq64], writes=[sq_])
            S.dma("sp", ck[:, 0:nt], g.cos64[:, t0:t0 + nt], reads=[g.cos64], writes=[ck])
            S.dma("sp", sk[:, 0:nt], g.sin64[:, t0:t0 + nt], reads=[g.sin64], writes=[sk])
            norm_mod(g, a, sq, rs, hT, pss, nt, lambda c: g.modA[:, 1, 0, c, j:j + 1], lambda c: g.modT[:, 1, c, j:j + 1])

            def proj(wt, col0):
                p = nextp()
                for kc in range(8):
                    _mm(S, p[:, 0:nt], wt[:, kc, col0:col0 + 128], hT[:, kc, 0:nt], kc == 0, kc == 7, [wt, hT], [p])
                return p

            def roped(col0, cos_t, sin_t, dst, row0, gcol=None, grcol=None):
                p1 = proj(w, col0)
                p2 = proj(wr, col0)
                k = oi[0]
                oi[0] += 1
                a1, a2, o = t1[k % 2], t2[k % 2], ob[k % 3]
                if gcol is None:
                    _tt(S, "dve", a1[:, 0:nt], p1[:, 0:nt], cos_t[:, 0:nt], ALU.mult, [p1, cos_t], [a1])
                    _tt(S, "dve", a2[:, 0:nt], p2[:, 0:nt], sin_t[:, 0:nt], ALU.mult, [p2, sin_t], [a2])
                    _tt(S, "pool", o[:, 0:nt], a1[:, 0:nt], a2[:, 0:nt], ALU.add, [a1, a2], [o])
                else:
                    _act(S, t3[:, 0:nt], p1[:, 0:nt], AF.Square, [p1], [t3])
                    _mm(S, pss[:, 0:nt], bd[:, :], t3[:, 0:nt], True, True, [bd, t3], [pss])
                    rstd_from_ss(S, "dve", t3[:, 0:nt], pss[:, 0:nt], 64, [pss], [t3])
                    _stt(S, "dve", a1[:, 0:nt], p1[:, 0:nt], gcol, cos_t[:, 0:nt], ALU.mult, ALU.mult, [p1, cos_t, gc], [a1])
                    _stt(S, "dve", a2[:, 0:nt], p2[:, 0:nt], grcol, sin_t[:, 0:nt], ALU.mult, ALU.mult, [p2, sin_t, gc], [a2])
                    _tt(S, "pool", a1[:, 0:nt], a1[:, 0:nt], a2[:, 0:nt], ALU.add, [a1, a2], [a1])
                    _tt(S, "dve", o[:, 0:nt], a1[:, 0:nt], t3[:, 0:nt], ALU.mult, [a1, t3], [o])
                S.dma("pool", dst.t[row0:row0 + 128, t0:t0 + nt], o[:, 0:nt], reads=[o], writes=[(dst, (row0, ti))])

            for m in range(4):
                if j == 0:
                    roped(m * 128, cq, sq_, g.dqT_s, m * 128)
                roped(512 + m * 128, ck, sk, g.dkT_s, m * 128)
            if j == 0:
                for m in range(4):
                    roped(1536 + m * 128, cq, sq_, g.gqT_s, m * 128, gc[:, 0:1], gc[:, 1:2])
            roped(2048, ck, sk, g.gkT_s, 0, gc[:, 2:3], gc[:, 3:4])
            for s in range(ns):
                p = nextp()
                for kc in range(8):
                    _mm(S, p[:, :], hT[:, kc, s * 128:(s + 1) * 128], w[:, kc, 1024:1536], kc == 0, kc == 7, [hT, w], [p])
                _cp(S, "act", dva[:, s, :, 0:128], p[:, :].rearrange("p (h e) -> p h e", e=128), [p], [(dva, s)])
                p = nextp()
                for kc in range(8):
                    _mm(S, p[:, 0:128], hT[:, kc, s * 128:(s + 1) * 128], w[:, kc, 2176:2304], kc == 0, kc == 7, [hT, w], [p])
                _cp(S, "dve", gva[:, s, :, 0:64], p[:, 0:128].rearrange("p (h e) -> p h e", e=64), [p], [(gva, s)])
            S.dma("pool", g.dv_s.t[t0:t0 + nt, :, :].rearrange("(s p) h e -> p s h e", p=128), dva[:, 0:ns, :, :],
                  reads=[dva], writes=[(g.dv_s, ti)])
            S.dma("pool", g.gv_s.t[t0:t0 + nt, :, :].rearrange("(s p) h e -> p s h e", p=128), gva[:, 0:ns, :, :],
                  reads=[gva], writes=[(g.gv_s, ti)])
        S.emit()


def attn_core2(g, KT, V, QT, d, dva, kbs, accf, psc, pT, nq, cnt):
    S = g.S
    nkb = len(kbs)
    LA = len(psc) - 1
    slots = []

    def score(i):
        kb = kbs[i]
        sc = psc[cnt[0] % len(psc)]
        p = pT[cnt[0] % len(pT)]
        cnt[0] += 1
        _mm(S, sc[:, 0:nq], KT[0:d, kb * 128:(kb + 1) * 128], QT[0:d, 0:nq], True, True, [KT, QT], [sc])
        _act(S, p[:, 0:nq], sc[:, 0:nq], AF.Exp, [sc], [p])
        slots.append(p)

    for i in range(min(LA, nkb)):
        score(i)
    for i, kb in enumerate(kbs):
        if i + LA < nkb:
            score(i + LA)
        p = slots[i]
        for qs in range(nq // 128):
            b, ap, first, last = accf(qs)
            _mm(S, ap, p[:, qs * 128:(qs + 1) * 128], V[:, kb, 0:dva], i == 0 and first, i == nkb - 1 and last, [p, V], [b])


def phase_l1_attn(g):
    S = g.S
    TT = g.TT
    NKB = TT // 128
    li = g.lambda_init
    with ExitStack() as ph:
        KT = [S.sb("aKT%d" % i, [64, TT], BF16, ph) for i in range(2)]
        V = S.sb("aV", [128, NKB, 129], BF16, ph)
        QT = [S.sb("aQT%d" % i, [64, 512], BF16, ph) for i in range(2)]
        pT = [S.sb("apT%d" % i, [128, 512], BF16, ph) for i in range(4)]
        Pacc = [(S.sb("aPaccD%d" % i, [128, 512], F32, ph), S.sb("aPaccP%d" % i, [128, 512], F32, ph)) for i in range(4)]
        oS = [S.sb("aoS%d" % i, [65, 512], F32, ph) for i in range(2)]
        rec = [S.sb("arec%d" % i, [128, 512], F32, ph) for i in range(2)]
        t1 = S.sb("at1", [128, 512], F32, ph)
        t2 = S.sb("at2", [128, 512], F32, ph)
        od = S.sb("aod", [128, 512], F32, ph)
        o = [S.sb("ao%d" % i, [128, 512], BF16, ph) for i in range(2)]
        lam = S.sb("alam", [128, 4], F32, ph)
        dl = S.sb("adl", [128, 4, 64], F32, ph)
        dn = S.sb("adn", [128, 1], F32, ph)
        tmp = S.sb("atmp", [128, 64], F32, ph)
        S.dma("sp", dl[:, :, :], g.dlam[:, :, :], reads=[g.dlam], writes=[dl])
        S.dma("sp", dn[:, :], g.dncol[:, :], reads=[g.dncol], writes=[dn])
        _ts(S, "dve", dn[:, :], dn[:, :], 1.0 - li, None, ALU.mult, None, [dn], [dn])
        for kk in range(2):
            _tt(S, "dve", tmp[:, 0:64], dl[:, 2 * kk, :], dl[:, 2 * kk + 1, :], ALU.mult, [dl], [tmp])
            S.op("dve", lambda e, kk=kk: e.tensor_reduce(out=lam[:, kk:kk + 1], in_=tmp[:, 0:64], axis=AX.X, op=ALU.add), reads=[tmp], writes=[lam])
        _act(S, lam[:, 0:2], lam[:, 0:2], AF.Exp, [lam], [lam])
        _tt(S, "dve", lam[:, 2:3], lam[:, 0:1], lam[:, 1:2], ALU.subtract, [lam], [lam])
        _ts(S, "dve", lam[:, 3:4], lam[:, 2:3], li, None, ALU.add, None, [lam], [lam])
        cnt = [0]
        qi = 0
        lat_tiles = [(ti, t0, nt) for ti, (t0, nt) in enumerate(g.tiles) if t0 >= g.TC]
        phd = ExitStack()
        psc = [S.ps("apsc%d" % i, [128, 512], F32, phd) for i in range(3)]
        acc = [S.ps("aacc%d" % i, [128, 512], F32, phd) for i in range(4)]
        psm = S.ps("apsm", [128, 512], F32, phd)
        for h in range(4):
            S.dma("sp", V[:, :, :], g.dv_s.t[:, h, :].rearrange("(n p) e -> p n e", p=128), reads=[g.dv_s], writes=[V])
            for m in range(2):
                S.dma("sp", KT[m][:, :], g.dkT_s.t[(2 * h + m) * 64:(2 * h + m + 1) * 64, :], reads=[g.dkT_s], writes=[KT[m]])
            for (ti, t0, nt) in lat_tiles:
                par = qi % 2
                qi += 1
                for m in range(2):
                    q = QT[m]
                    S.dma("sp", q[:, 0:nt], g.dqT_s.t[(2 * h + m) * 64:(2 * h + m + 1) * 64, t0:t0 + nt], reads=[g.dqT_s], writes=[q])
                    attn_core3(g, KT[m], V, q, 64, 128, list(range(NKB)), acc[2 * par + m], Pacc[2 * par + m], psc, pT[0:3], nt, cnt)
                a1, a2 = acc[2 * par], acc[2 * par + 1]
                for m in range(2):
                    pd, pp_ = Pacc[2 * par + m]
                    _mm(S, psm[:, 0:nt], g.onesF[:, :], pd[:, 0:nt], True, False, [g.onesF, pd], [psm])
                    _mm(S, psm[:, 0:nt], g.onesF[:, :], pp_[:, 0:nt], False, True, [g.onesF, pp_], [psm])
                    S.op("dve", lambda e, m=m, nt=nt: e.reciprocal(out=rec[m][:, 0:nt], in_=psm[:, 0:nt]), reads=[psm], writes=[rec[m]])
                _tt(S, "dve", t1[:, 0:nt], a1[:, 0:nt], rec[0][:, 0:nt], ALU.mult, [a1, rec[0]], [t1])
                _stt(S, "dve", t2[:, 0:nt], a2[:, 0:nt], lam[:, 3:4], rec[1][:, 0:nt], ALU.mult, ALU.mult, [a2, lam, rec[1]], [t2])
                _tt(S, "pool", od[:, 0:nt], t1[:, 0:nt], t2[:, 0:nt], ALU.subtract, [t1, t2], [od])
                _tt(S, "pool", t1[:, 0:nt], od[:, 0:nt], od[:, 0:nt], ALU.mult, [od], [t1])
                _mm(S, psm[:, 0:nt], g.onesF[:, :], t1[:, 0:nt], True, True, [g.onesF, t1], [psm])
                rstd_from_ss(S, "dve", t2[:, 0:nt], psm[:, 0:nt], 128, [psm], [t2])
                oo = o[par]
                _stt(S, "dve", oo[:, 0:nt], od[:, 0:nt], dn[:, 0:1], t2[:, 0:nt], ALU.mult, ALU.mult, [od, dn, t2], [oo])
                S.dma("pool", g.mixT_s.t[h * 128:(h + 1) * 128, t0:t0 + nt], oo[:, 0:nt], reads=[oo], writes=[(g.mixT_s, (h, ti))])
        S.barrier()
        S.emit()
        phd.close()
        psc = [S.ps("bpsc%d" % i, [128, 512], F32, ph) for i in range(4)]
        acc = [S.ps("bacc%d" % i, [128, 512], F32, ph) for i in range(2)]
        psm = S.ps("bpsm", [128, 512], F32, ph)
        for kvh in range(2):
            S.dma("sp", V[:, :, 0:65], g.gv_s.t[:, kvh, :].rearrange("(n p) e -> p n e", p=128), reads=[g.gv_s], writes=[V])
            S.dma("sp", KT[0][:, :], g.gkT_s.t[kvh * 64:(kvh + 1) * 64, :], reads=[g.gkT_s], writes=[KT[0]])
            for grp in range(4):
                hd = kvh * 4 + grp
                for (ti, t0, nt) in lat_tiles:
                    par = qi % 2
                    qi += 1
                    q = QT[par]
                    S.dma("sp", q[:, 0:nt], g.gqT_s.t[hd * 64:(hd + 1) * 64, t0:t0 + nt], reads=[g.gqT_s], writes=[q])
                    attn_core3(g, KT[0], V, q, 64, 65, list(range(NKB)), acc[par], None, psc, pT, nt, cnt)
                    attn_simple_post(g, acc[par], psm, oS[par], rec[0], o[par], nt,
                                     g.mixT_s.t[512 + hd * 64:512 + (hd + 1) * 64, t0:t0 + nt], (g.mixT_s, (8 + hd, ti)))
        S.emit()


def phase_moe_a(g):
    S = g.S
    TT = g.TT
    g.h2T_s = S.dram("h2T_s", [1024, TT], BF16)
    g.gT_s = S.dram("gT_s", [8, TT], F32)
    with ExitStack() as ph:
        rw = S.sb("mrw", [128, 8, 8], F32, ph)
        S.dma("sp", rw[:, :, :], g.router.t[:, :].rearrange("(kc p) e -> p kc e", p=128), reads=[g.router], writes=[rw])
        xa = [S.sb("maxa%d" % i, [128, 8, 512], F32, ph) for i in range(2)]
        sq = S.sb("masq", [128, 8, 512], F32, ph)
        rs = S.sb("mars", [128, 512], F32, ph)
        hT = [S.sb("mahT%d" % i, [128, 8, 512], BF16, ph) for i in range(2)]
        hF = S.sb("mahF", [128, 8, 512], F32, ph)
        lg = S.sb("malg", [128, 8], F32, ph)
        m8 = S.sb("mam8", [128, 8], F32, ph)
        sm = S.sb("masm", [128, 4], F32, ph)
        mask = S.sb("mamask", [128, 8], F32, ph)
        ex = S.sb("maex", [128, 8], F32, ph)
        gt = S.sb("magt", [128, 8], F32, ph)
        gT = [S.sb("magT%d" % i, [8, 512], F32, ph) for i in range(2)]
        pss = S.ps("mapss", [128, 512], F32, ph)
        pl = [S.ps("mapl%d" % i, [128, 512], F32, ph) for i in range(2)]
        pt = [S.ps("mapt%d" % i, [128, 512], F32, ph) for i in range(2)]
        k = 0
        for ti, (t0, nt) in enumerate(g.tiles):
            if t0 < g.TC:
                continue
            ns = nt // 128
            a = xa[ti % 2]
            h = hT[ti % 2]
            gTt = gT[ti % 2]
            S.dma("sp", a[:, :, 0:nt], xT_view(g, t0, nt), reads=[(g.xT_s, ti)], writes=[a])
            norm_mod(g, a, sq, rs, h, pss, nt, lambda c: g.modA[:, 1, 1, c, 0:1], lambda c: g.modT[:, 1, 24 + c, 0:1])
            S.dma("pool", g.h2T_s.t[:, t0:t0 + nt].rearrange("(c p) t -> p c t", p=128), h[:, :, 0:nt], reads=[h],
                  writes=[(g.h2T_s, ti)])
            for c in range(8):
                _ts(S, "dve", hF[:, c, 0:nt], sq[:, c, 0:nt], g.modT[:, 1, 24 + c, 0:1], None, ALU.add, None, [(sq, c)], [(hF, c)])
            for s in range(ns):
                p = pl[k % 2]
                p2 = pt[k % 2]
                k += 1
                for kc in range(8):
                    _mm(S, p[:, 0:8], hF[:, kc, s * 128:(s + 1) * 128], rw[:, kc, :], kc == 0, kc == 7, [hF, rw], [p])
                _cp(S, "dve", lg[:, :], p[:, 0:8], [p], [lg])
                S.op("dve", lambda e: e.max(out=m8[:, :], in_=lg[:, :]), reads=[lg], writes=[m8])
                _ts(S, "dve", sm[:, 0:1], m8[:, 0:1], -1.0, None, ALU.mult, None, [m8], [sm])
                _ts(S, "dve", mask[:, :], lg[:, :], m8[:, 1:2], None, ALU.is_ge, None, [lg, m8], [mask])
                _act(S, ex[:, :], lg[:, :], AF.Exp, [lg, sm], [ex], bias=sm[:, 0:1])
                _act(S, sm[:, 1:2], m8[:, 1:2], AF.Exp, [m8, sm], [sm], bias=sm[:, 0:1])
                _ts(S, "dve", sm[:, 2:3], sm[:, 1:2], 1.0, None, ALU.add, None, [sm], [sm])
                S.op("dve", lambda e: e.reciprocal(out=sm[:, 3:4], in_=sm[:, 2:3]), reads=[sm], writes=[sm])
                _stt(S, "dve", gt[:, :], ex[:, :], sm[:, 3:4], mask[:, :], ALU.mult, ALU.mult, [ex, sm, mask], [gt])
                _tr(S, p2[0:8, 0:128], gt[:, :], g.identF[:, :], [gt, g.identF], [p2])
                _cp(S, "dve", gTt[:, s * 128:(s + 1) * 128], p2[0:8, 0:128], [p2], [gTt])
            S.dma("pool", g.gT_s.t[:, t0:t0 + nt], gTt[:, 0:nt], reads=[gTt], writes=[(g.gT_s, ti)])
        S.emit()


def phase_moe_b(g):
    S = g.S
    NT = 256
    FH = 1792
    NF = FH // 128
    with ExitStack() as ph:
        stg = [S.sb("mbstg%d" % i, [128, 8, 512], F32, ph, side="right") for i in range(2)]
        sel = S.sb("mbsel", [8, 8, 128], F32, ph)
        for e in range(8):
            _ts(S, "dve", sel[:, e, :], g.onesF[0:8, :], g.identF[0:8, e:e + 1], None, ALU.mult, None, [g.onesF, g.identF], [(sel, e)])
        wg = S.sb("mbwg", [128, 8, FH], BF16, ph)
        wu = S.sb("mbwu", [128, 8, FH], BF16, ph)
        wd = S.sb("mbwd", [128, NF, 1024], BF16, ph)
        xa = [S.sb("mbxa%d" % i, [128, 8, NT], F32, ph) for i in range(2)]
        hT = [S.sb("mbhT%d" % i, [128, 8, NT], BF16, ph) for i in range(2)]
        gTt = [S.sb("mbgT%d" % i, [8, NT], F32, ph) for i in range(2)]
        gbs = S.sb("mbgbs", [128, NT], F32, ph)
        sg = [S.sb("mbsg%d" % i, [128, NT], F32, ph) for i in range(3)]
        tg = [S.sb("mbtg%d" % i, [128, NT], F32, ph) for i in range(3)]
        act = [S.sb("mbact%d" % i, [128, NT], BF16, ph) for i in range(4)]
        pgu = [S.ps("mbpgu%d" % i, [128, 512], F32, ph) for i in range(4)]
        yac = [S.ps("mby%d" % i, [128, 2, NT], F32, ph) for i in range(4)]
        lat = [(ti, t0, nt) for ti, (t0, nt) in enumerate(g.tiles) if t0 >= g.TC]
        it = 0
        for e in range(8):
            for fh in range(2):
                def ld(dst, src_ap, K, N):
                    kc = K // 128
                    v = src_ap.rearrange("(kc p) n -> p kc n", p=128)
                    for k0 in range(0, kc, 8):
                        k1 = min(kc, k0 + 8)
                        for n0 in range(0, N, 512):
                            n1 = min(N, n0 + 512)
                            s = stg[g.stg_i % 2]
                            g.stg_i += 1
                            S.dma("sp", s[:, 0:k1 - k0, 0:n1 - n0], v[:, k0:k1, n0:n1], reads=[g.moe_gu, g.moe_dn], writes=[s])
                            _cp(S, ("dve", "pool", "act")[g.stg_i % 3], dst[:, k0:k1, n0:n1], s[:, 0:k1 - k0, 0:n1 - n0], [s], [dst])
                ld(wg, g.moe_gu.t[e, :, fh * FH:(fh + 1) * FH], 1024, FH)
                ld(wu, g.moe_gu.t[e, :, 3584 + fh * FH:3584 + (fh + 1) * FH], 1024, FH)
                ld(wd, g.moe_dn.t[e, fh * FH:(fh + 1) * FH, :], FH, 1024)
                for (ti, t0, nt) in lat:
                    for u0 in range(0, nt, NT):
                        a, h, gt_ = xa[it % 2], hT[it % 2], gTt[it % 2]
                        it += 1
                        S.dma("sp", h[:, :, :], g.h2T_s.t[:, t0 + u0:t0 + u0 + NT].rearrange("(c p) t -> p c t", p=128),
                              reads=[g.h2T_s], writes=[h])
                        S.dma("sp", gt_[:, :], g.gT_s.t[:, t0 + u0:t0 + u0 + NT], reads=[g.gT_s], writes=[gt_])
                        S.dma("sp", a[:, :, :], xT_view(g, t0 + u0, NT), reads=[(g.xT_s, (ti, u0))], writes=[a])
                        _mm(S, pgu[0][:, 0:NT], sel[:, e, :], gt_[:, :], True, True, [(sel, e), gt_], [pgu[0]])
                        _cp(S, "dve", gbs[:, :], pgu[0][:, 0:NT], [pgu[0]], [gbs])
                        def gu(f, h=h):
                            pg = pgu[(f % 2) * 2]
                            pu = pgu[(f % 2) * 2 + 1]
                            for kc in range(8):
                                _mm(S, pg[:, 0:NT], wg[:, kc, f * 128:(f + 1) * 128], h[:, kc, :], kc == 0, kc == 7, [wg, h], [pg])
                            for kc in range(8):
                                _mm(S, pu[:, 0:NT], wu[:, kc, f * 128:(f + 1) * 128], h[:, kc, :], kc == 0, kc == 7, [wu, h], [pu])
                            s_, t_, ac = sg[f % 3], tg[f % 3], act[f % 4]
                            _act(S, s_[:, :], pg[:, 0:NT], AF.Silu, [pg], [s_])
                            _tt(S, "dve", t_[:, :], s_[:, :], pu[:, 0:NT], ALU.mult, [s_, pu], [t_])
                            _tt(S, "pool", ac[:, :], t_[:, :], gbs[:, :], ALU.mult, [t_, gbs], [ac])
                            return ac

                        acs = {0: gu(0), 1: gu(1)}
                        for f in range(NF):
                            if f + 2 < NF:
                                acs[f + 2] = gu(f + 2)
                            ac = acs.pop(f)
                            for dsl in range(8):
                                _mm(S, yac[dsl // 2][:, dsl % 2, :], wd[:, f, dsl * 128:(dsl + 1) * 128], ac[:, :],
                                    f == 0 and dsl % 2 == 0, f == NF - 1 and dsl % 2 == 1, [wd, ac], [yac[dsl // 2]])
                        for dsl in range(8):
                            _stt(S, "dve", a[:, dsl, :], yac[dsl // 2][:, dsl % 2, :], g.modT[:, 1, 40 + dsl, 0:1], a[:, dsl, :],
                                 ALU.mult, ALU.add, [yac[dsl // 2], (a, dsl)], [(a, dsl)])
                        S.dma("pool", xT_view(g, t0 + u0, NT), a[:, :, :], reads=[a], writes=[(g.xT_s, (ti, u0))])
                S.barrier()
        S.emit()


def phase_gdn_prep(g):
    S = g.S
    TT = g.TT
    g.qkvn_s = S.dram("qkvn_s", [12, 128, TT], F32)
    with ExitStack() as ph:
        cw = S.sb("gpcw", [128, 12, 5], F32, ph)
        S.dma("sp", cw[:, :, :], g.conv_col[:, :, :], reads=[g.conv_col], writes=[cw])
        bd = S.sb("gpbd", [128, 128], F32, ph)
        _ms(S, "pool", bd[:, :], 0.0, [bd])
        _ms(S, "pool", bd[0:64, 0:64], 1.0, [bd])
        _ms(S, "pool", bd[64:128, 64:128], 1.0, [bd])
        xh = [S.sb("gpxh%d" % i, [128, 12, 516], F32, ph) for i in range(2)]
        acc = [S.sb("gpacc%d" % i, [128, 512], F32, ph) for i in range(3)]
        y = [S.sb("gpy%d" % i, [128, 512], F32, ph) for i in range(3)]
        sq = [S.sb("gpsq%d" % i, [128, 512], F32, ph) for i in range(2)]
        rn = [S.sb("gprn%d" % i, [128, 512], F32, ph) for i in range(2)]
        pss = [S.ps("gppss%d" % i, [128, 512], F32, ph) for i in range(2)]
        k = 0
        for ti, (t0, nt) in enumerate(g.tiles):
            x = xh[ti % 2]
            lo = 0 if t0 < g.TC else g.TC
            hi = g.TC if t0 < g.TC else TT
            a0 = max(lo, t0 - 2)
            a1 = min(hi, t0 + nt + 2)
            if a0 > t0 - 2:
                _ms(S, "pool", x[:, :, 0:2], 0.0, [x])
            if a1 < t0 + nt + 2:
                _ms(S, "pool", x[:, :, nt + 2:nt + 4], 0.0, [x])
            S.dma("sp", x[:, :, a0 - (t0 - 2):a1 - (t0 - 2)], g.gq_s.t[:, :, a0:a1].rearrange("c p t -> p c t"),
                  reads=[g.gq_s], writes=[x])
            for c in range(12):
                ac, yy = acc[k % 3], y[k % 3]
                s_, r_ = sq[k % 2], rn[k % 2]
                ps = pss[k % 2]
                k += 1
                _ts(S, "dve", ac[:, 0:nt], x[:, c, 0:nt], cw[:, c, 0:1], None, ALU.mult, None, [x, cw], [ac])
                for j in range(1, 5):
                    _stt(S, "dve", ac[:, 0:nt], x[:, c, j:j + nt], cw[:, c, j:j + 1], ac[:, 0:nt], ALU.mult, ALU.add, [x, cw, ac], [ac])
                _act(S, yy[:, 0:nt], ac[:, 0:nt], AF.Silu, [ac], [yy])
                if c < 8:
                    _tt(S, "pool", s_[:, 0:nt], yy[:, 0:nt], yy[:, 0:nt], ALU.mult, [yy], [s_])
                    _mm(S, ps[:, 0:nt], bd[:, :], s_[:, 0:nt], True, True, [bd, s_], [ps])
                    rstd_from_ss(S, "dve", r_[:, 0:nt], ps[:, 0:nt], 1, [ps], [r_])
                    if c < 4:
                        _stt(S, "dve", yy[:, 0:nt], yy[:, 0:nt], 0.125, r_[:, 0:nt], ALU.mult, ALU.mult, [yy, r_], [yy])
                    else:
                        _tt(S, "dve", yy[:, 0:nt], yy[:, 0:nt], r_[:, 0:nt], ALU.mult, [yy, r_], [yy])
                S.dma("pool", g.qkvn_s.t[c, :, t0:t0 + nt], yy[:, 0:nt], reads=[yy], writes=[(g.qkvn_s, (c, ti))])
        S.emit()


def phase_gdn(g):
    S = g.S
    TT = g.TT
    C = 64
    g.o_s = S.dram("gdn_o_s", [2, TT, 512], F32)
    with ExitStack() as ph:
        def sb(name, shape, dt=F32):
            return S.sb("gd_" + name, shape, dt, ph)
        alr = sb("alr", [64, 16])
        dtb = sb("dtb", [64, 16])
        S.dma("sp", alr[:, :], g.alog_rep[0:64, :], reads=[g.alog_rep], writes=[alr])
        S.dma("sp", dtb[:, :], g.dtb_rep[0:64, :], reads=[g.dtb_rep], writes=[dtb])
        nA = sb("nA", [64, 16])
        _act(S, nA[:, :], alr[:, :], AF.Exp, [alr], [nA])
        _ts(S, "dve", nA[:, :], nA[:, :], -1.0, None, ALU.mult, None, [nA], [nA])
        def mask(name, op, sgn=1):
            m = sb(name, [64, 8, 64])
            _ms(S, "pool", m[:, :, :], 1.0, [m])
            S.op("pool", lambda e: e.affine_select(out=m[:, :, :], in_=m[:, :, :], pattern=[[0, 8], [-sgn, 64]],
                                                   compare_op=op, fill=0.0, base=0, channel_multiplier=sgn), reads=[m], writes=[m])
            return m
        mI = [mask("mIf", ALU.is_ge), mask("mIb", ALU.is_ge, -1)]
        mS = [mask("mSf", ALU.is_gt), mask("mSb", ALU.is_gt, -1)]
        I8 = mask("I8", ALU.is_equal)
        Tri = [mI[1], mI[0]]
        St = [sb("S%d" % d, [64, 8, 64]) for d in range(2)]
        for d in range(2):
            _ms(S, "pool", St[d][:, :, :], 0.0, [St[d]])
        nb = 4
        QK = [sb("QK%d" % i, [64, 16, C]) for i in range(nb)]
        Vf = [sb("Vf%d" % i, [64, 8, C]) for i in range(nb)]
        abT = [sb("abT%d" % i, [32, C]) for i in range(nb)]
        names = ["Ktm", "Vtm", "abm", "t16", "gg", "be", "gc", "egc", "gtot", "cd", "kdsc", "bsc", "nbe"]
        TD = [{}, {}]
        big = ["NG", "Mm", "E", "D", "Ds", "P0", "DT", "qkmT", "PT0", "Pa", "PTa", "Pb", "PTb", "TTa", "TTb", "bV", "bK", "U", "WT",
               "vnew", "o2", "od", "Kd", "P0b"]
        for dd in range(2):
            for n in names:
                TD[dd][n] = sb("%s_%d" % (n, dd), [64, 512] if n in ("Ktm", "Vtm") else [64, 32])
            for n in big:
                TD[dd][n] = sb("%s_%d" % (n, dd), [64, 8, 64],
                               F32)
        pb = [S.ps("gd_p%d" % i, [128, 512], F32, ph) for i in range(8)]
        pi = [0]

        def P():
            p = pb[pi[0] % 8]
            pi[0] += 1
            return p

        def v3(p):
            return p[0:64, :].rearrange("p (h e) -> p h e", e=64)

        ev = [0]

        def evac(out_ap, in_ap, R, W):
            ev[0] += 1
            _cp(S, "act" if ev[0] % 2 else "dve", out_ap, in_ap, R, W)

        def chunk(d, c0, it):
            T = TD[d]
            qk, vf, ab = QK[it % nb], Vf[it % nb], abT[it % nb]
            S.dma("sp", qk[:, :, :], g.qkvn_s.t[0:8, :, c0:c0 + C].rearrange("c (hh d) t -> d (c hh) t", d=64),
                  reads=[g.qkvn_s], writes=[qk])
            S.dma("sp", vf[:, :, :], g.qkvn_s.t[8:12, :, c0:c0 + C].rearrange("c (hh d) t -> d (c hh) t", d=64),
                  reads=[g.qkvn_s], writes=[vf])
            S.dma("sp", ab[:, :], g.ab_s.t[:, c0:c0 + C], reads=[g.ab_s], writes=[ab])
            Ktm, Vtm, abm = T["Ktm"], T["Vtm"], T["abm"]
            p = P()
            for h in range(8):
                _tr(S, p[0:64, h * 64:(h + 1) * 64], qk[:, 8 + h, :], g.identF[0:64, 0:64], [qk, g.identF], [p])
            evac(Ktm[:, :], p[0:64, :], [p], [Ktm])
            p = P()
            for h in range(8):
                _tr(S, p[0:64, h * 64:(h + 1) * 64], vf[:, h, :], g.identF[0:64, 0:64], [vf, g.identF], [p])
            evac(Vtm[:, :], p[0:64, :], [p], [Vtm])
            p = P()
            _tr(S, p[0:64, 0:32], ab[:, :], g.identF[0:32, 0:32], [ab, g.identF], [p])
            evac(abm[:, :], p[0:64, 0:32], [p], [abm])
            t16, gg, be, gc, egc, gtot, cd, kdsc, bsc, nbe = (T[n] for n in ("t16", "gg", "be", "gc", "egc", "gtot", "cd", "kdsc", "bsc", "nbe"))
            r8 = slice(d * 8, d * 8 + 8)
            _tt(S, "dve", t16[:, 0:8], abm[:, r8], dtb[:, r8], ALU.add, [abm, dtb], [t16])
            _act(S, t16[:, 0:8], t16[:, 0:8], AF.Exp, [t16], [t16])
            _act(S, t16[:, 0:8], t16[:, 0:8], AF.Ln, [t16], [t16], bias=1.0)
            _tt(S, "dve", gg[:, 0:8], t16[:, 0:8], nA[:, r8], ALU.mult, [t16, nA], [gg])
            _act(S, be[:, 0:8], abm[:, 16 + d * 8:24 + d * 8], AF.Sigmoid, [abm], [be])
            _ts(S, "dve", nbe[:, 0:8], be[:, 0:8], -1.0, None, ALU.mult, None, [be], [nbe])
            p = P()
            _mm(S, p[0:64, 0:8], Tri[d][:, 0, :], gg[:, 0:8], True, True, [Tri[d], gg], [p])
            evac(gc[:, 0:8], p[0:64, 0:8], [p], [gc])
            p = P()
            _mm(S, p[0:64, 0:8], g.onesF[0:64, 0:64], gg[:, 0:8], True, True, [g.onesF, gg], [p])
            evac(gtot[:, 0:8], p[0:64, 0:8], [p], [gtot])
            _act(S, egc[:, 0:8], gc[:, 0:8], AF.Exp, [gc], [egc])
            _act(S, cd[:, 0:8], gtot[:, 0:8], AF.Exp, [gtot], [cd])
            _tt(S, "dve", kdsc[:, 0:8], gtot[:, 0:8], gc[:, 0:8], ALU.subtract, [gtot, gc], [kdsc])
            _act(S, kdsc[:, 0:8], kdsc[:, 0:8], AF.Exp, [kdsc], [kdsc])
            _tt(S, "dve", bsc[:, 0:8], be[:, 0:8], egc[:, 0:8], ALU.mult, [be, egc], [bsc])
            NG, Mm, E, Dm, Ds = T["NG"], T["Mm"], T["E"], T["D"], T["Ds"]
            for h in range(8):
                _ts(S, "dve", NG[:, h, :], g.onesF[0:64, 0:64], gg[:, h:h + 1], -1.0, ALU.mult, ALU.mult, [g.onesF, gg], [(NG, h)])
            p = P()
            for h in range(8):
                _mm(S, v3(p)[:, h, :], NG[:, h, :], Tri[d][:, 0, :], True, True, [(NG, h), Tri[d]], [p])
            for h in range(8):
                _ts(S, "dve", Mm[:, h, :], v3(p)[:, h, :], gc[:, h:h + 1], 0.0, ALU.add, ALU.min, [p, gc], [(Mm, h)])
            _act(S, E[:, :, :], Mm[:, :, :], AF.Exp, [Mm], [E])
            _tt(S, "pool", Dm[:, :, :], E[:, :, :], mI[d][:, :, :], ALU.mult, [E, mI[d]], [Dm])
            _tt(S, "pool", Ds[:, :, :], E[:, :, :], mS[d][:, :, :], ALU.mult, [E, mS[d]], [Ds])
            P0, DT, qkmT, PT0 = T["P0"], T["DT"], T["qkmT"], T["PT0"]
            p = P()
            for h in range(8):
                _mm(S, v3(p)[:, h, :], qk[:, 8 + h, :], qk[:, 8 + h, :], True, True, [qk], [p])
            for h in range(8):
                _stt(S, "dve", P0[:, h, :], v3(p)[:, h, :], nbe[:, h:h + 1], Ds[:, h, :], ALU.mult, ALU.mult, [p, nbe, Ds], [(P0, h)])
            p = P()
            for h in range(8):
                _tr(S, v3(p)[:, h, :], Dm[:, h, :], g.identF[0:64, 0:64], [Dm, g.identF], [p])
            evac(DT[:, :, :], v3(p), [p], [DT])
            p = P()
            for h in range(8):
                _mm(S, v3(p)[:, h, :], qk[:, 8 + h, :], qk[:, h, :], True, True, [qk], [p])
            _tt(S, "dve", qkmT[:, :, :], v3(p), DT[:, :, :], ALU.mult, [p, DT], [qkmT])
            p = P()
            for h in range(8):
                _tr(S, v3(p)[:, h, :], P0[:, h, :], g.identF[0:64, 0:64], [(P0, h), g.identF], [p])
            evac(PT0[:, :, :], v3(p), [p], [PT0])
            TTa, TTb = T["TTa"], T["TTb"]
            _tt(S, "pool", TTa[:, :, :], PT0[:, :, :], I8[:, :, :], ALU.add, [PT0, I8], [TTa])
            Pk, PTk = P0, PT0
            cur, nxt = TTa, TTb
            alt = [(T["Pa"], T["PTa"]), (T["Pb"], T["PTb"])]
            for lv in range(1, 6):
                Pn, PTn = alt[lv % 2]
                p = P()
                for h in range(8):
                    _mm(S, v3(p)[:, h, :], PTk[:, h, :], Pk[:, h, :], True, True, [Pk, PTk], [p])
                evac(Pn[:, :, :], v3(p), [p], [Pn])
                if lv < 5:
                    p = P()
                    for h in range(8):
                        _mm(S, v3(p)[:, h, :], Pk[:, h, :], PTk[:, h, :], True, True, [Pk, PTk], [p])
                    evac(PTn[:, :, :], v3(p), [p], [PTn])
                p = P()
                for h in range(8):
                    _mm(S, v3(p)[:, h, :], Pn[:, h, :], cur[:, h, :], True, True, [Pn, cur], [p])
                _tt(S, "dve", nxt[:, :, :], v3(p), cur[:, :, :], ALU.add, [p, cur], [nxt])
                cur, nxt = nxt, cur
                Pk, PTk = Pn, PTn
            TT_ = cur
            bV, bK, U, WT = T["bV"], T["bK"], T["U"], T["WT"]
            Kv = Ktm[:, :].rearrange("p (h e) -> p h e", e=64)
            Vv = Vtm[:, :].rearrange("p (h e) -> p h e", e=64)
            for h in range(8):
                _ts(S, "dve", bV[:, h, :], Vv[:, h, :], be[:, h:h + 1], None, ALU.mult, None, [Vtm, be], [(bV, h)])
                _ts(S, "pool", bK[:, h, :], Kv[:, h, :], bsc[:, h:h + 1], None, ALU.mult, None, [Ktm, bsc], [(bK, h)])
            p = P()
            for h in range(8):
                _mm(S, v3(p)[:, h, :], TT_[:, h, :], bV[:, h, :], True, True, [TT_, bV], [p])
            evac(U[:, :, :], v3(p), [p], [U])
            p = P()
            for h in range(8):
                _mm(S, v3(p)[:, h, :], bK[:, h, :], TT_[:, h, :], True, True, [TT_, bK], [p])
            evac(WT[:, :, :], v3(p), [p], [WT])
            Sd = St[d]
            vnew, o2, od, Kd = T["vnew"], T["o2"], T["od"], T["Kd"]
            for h in range(8):
                _ts(S, "pool", Kd[:, h, :], Kv[:, h, :], kdsc[:, h:h + 1], None, ALU.mult, None, [Ktm, kdsc], [(Kd, h)])
            pw = P()
            for h in range(8):
                _mm(S, v3(pw)[:, h, :], WT[:, h, :], Sd[:, h, :], True, True, [WT, Sd], [pw])
            pq = P()
            for h in range(8):
                _mm(S, v3(pq)[:, h, :], qk[:, h, :], Sd[:, h, :], True, True, [qk, Sd], [pq])
            _tt(S, "dve", vnew[:, :, :], U[:, :, :], v3(pw), ALU.subtract, [U, pw], [vnew])
            p2 = P()
            for h in range(8):
                _mm(S, v3(p2)[:, h, :], qkmT[:, h, :], vnew[:, h, :], True, True, [qkmT, vnew], [p2])
            evac(o2[:, :, :], v3(p2), [p2], [o2])
            for h in range(8):
                _stt(S, "dve", od[:, h, :], v3(pq)[:, h, :], egc[:, h:h + 1], o2[:, h, :], ALU.mult, ALU.add, [pq, egc, o2], [(od, h)])
            p3 = P()
            for h in range(8):
                _mm(S, v3(p3)[:, h, :], Kd[:, h, :], vnew[:, h, :], True, True, [Kd, vnew], [p3])
            for h in range(8):
                _stt(S, "dve", Sd[:, h, :], Sd[:, h, :], cd[:, h:h + 1], v3(p3)[:, h, :], ALU.mult, ALU.add, [Sd, cd, p3], [Sd])
            odf = od[:, :, :].rearrange("p h e -> p (h e)")
            S.dma("pool", g.o_s.t[d, c0:c0 + C, :], odf, reads=[od], writes=[(g.o_s, (d, c0))])

        nctx = g.TC // C
        nlat = g.TL // C
        it = 0
        orders = [list(range(nctx)) + [nctx + i for i in range(nlat)],
                  list(range(nctx - 1, -1, -1)) + [nctx + i for i in range(nlat - 1, -1, -1)]]
        for step in range(nctx + nlat):
            for d in range(2):
                chunk(d, orders[d][step] * C, it)
                it += 1
        S.barrier()
        S.emit()
    with ExitStack() as ph:
        onr = S.sb("gc_onr", [128, 8, 64], F32, ph)
        S.dma("sp", onr[:, :, :], g.onrm_rep[:, :, :], reads=[g.onrm_rep], writes=[onr])
        o0 = [S.sb("gc_o0%d" % i, [128, 8, 64], F32, ph) for i in range(2)]
        o1 = [S.sb("gc_o1%d" % i, [128, 8, 64], F32, ph) for i in range(2)]
        zt = [S.sb("gc_z%d" % i, [128, 8, 64], F32, ph) for i in range(2)]
        sqo = S.sb("gc_sq", [128, 8, 64], F32, ph)
        ss = S.sb("gc_ss", [128, 16], F32, ph)
        ob = [S.sb("gc_ob%d" % i, [128, 8, 64], BF16, ph) for i in range(2)]
        oT = [S.sb("gc_oT%d" % i, [128, 4, 128], BF16, ph) for i in range(2)]
        ptb = [S.ps("gc_ptb%d" % i, [128, 512], BF16, ph) for i in range(2)]
        for bi in range(TT // 128):
            r0 = bi * 128
            a, b, z, o_b, o_t, pt = o0[bi % 2], o1[bi % 2], zt[bi % 2], ob[bi % 2], oT[bi % 2], ptb[bi % 2]
            S.dma("sp", a[:, :, :], g.o_s.t[0, r0:r0 + 128, :].rearrange("p (h e) -> p h e", e=64), reads=[g.o_s], writes=[a])
            S.dma("sp", b[:, :, :], g.o_s.t[1, r0:r0 + 128, :].rearrange("p (h e) -> p h e", e=64), reads=[g.o_s], writes=[b])
            S.dma("sp", z[:, :, :], g.z_s.t[r0:r0 + 128, :].rearrange("p (h e) -> p h e", e=64), reads=[g.z_s], writes=[z])
            _tt(S, "pool", a[:, :, :], a[:, :, :], b[:, :, :], ALU.add, [a, b], [a])
            _tt(S, "pool", sqo[:, :, :], a[:, :, :], a[:, :, :], ALU.mult, [a], [sqo])
            S.op("dve", lambda e: e.tensor_reduce(out=ss[:, 0:8], in_=sqo[:, :, :], axis=AX.X, op=ALU.add), reads=[sqo], writes=[ss])
            rstd_from_ss(S, "dve", ss[:, 8:16], ss[:, 0:8], 64, [ss], [ss])
            _tt(S, "pool", z[:, :, :], z[:, :, :], onr[:, :, :], ALU.mult, [z, onr], [z])
            for h in range(8):
                _stt(S, "dve", o_b[:, h, :], a[:, h, :], ss[:, 8 + h:9 + h], z[:, h, :], ALU.mult, ALU.mult, [a, ss, z], [o_b])
            obf = o_b[:, :, :].rearrange("p h e -> p (h e)")
            for c in range(4):
                _tr(S, pt[:, c * 128:(c + 1) * 128], obf[:, c * 128:(c + 1) * 128], g.identB[:, :], [o_b, g.identB], [pt])
            _cp(S, "act", o_t[:, :, :], pt[:, :].rearrange("p (c t) -> p c t", t=128), [pt], [o_t])
            S.dma("pool", g.mixT_s.t[512:1024, r0:r0 + 128].rearrange("(c p) t -> p c t", p=128), o_t[:, :, :], reads=[o_t],
                  writes=[(g.mixT_s, ("g", bi))])
        S.emit()


def rope_tab(n, TL, TC):
    nf = n // 4
    inv = 1.0 / (10000.0 ** (np.arange(nf, dtype=np.float32) / nf))
    t = np.arange(TL)
    rows = (t // 64).astype(np.float32)
    cols = (t % 64).astype(np.float32)
    ang_r = rows[None, :] * inv[:, None]
    ang_c = cols[None, :] * inv[:, None]
    cos = np.concatenate([np.cos(ang_r), np.cos(ang_r), np.cos(ang_c), np.cos(ang_c)], 0)
    sin = np.concatenate([np.sin(ang_r), np.sin(ang_r), np.sin(ang_c), np.sin(ang_c)], 0)
    cos = np.concatenate([np.ones((n, TC), np.float32), cos.astype(np.float32)], 1)
    sin = np.concatenate([np.zeros((n, TC), np.float32), sin.astype(np.float32)], 1)
    return np.ascontiguousarray(cos), np.ascontiguousarray(sin)


def col(v, k):
    return np.ascontiguousarray(np.asarray(v, np.float32).reshape(k, 128).T)


def prep_core(inp, b, TL, TC):
    f = lambda a: np.ascontiguousarray(np.asarray(a, np.float32))
    TT = TL + TC
    d = {}
    d["x"] = f(inp["x"][b])
    d["ctx"] = f(inp["ctx"][b])
    d["ccol"] = np.ascontiguousarray(np.stack([col(inp["c"][b], 8), col(inp["c_ctx"], 8)], -1))
    d["mod_w"] = f(inp["mod_w"])
    d["modb"] = np.ascontiguousarray(f(inp["mod_b"]).reshape(2, 48, 128).transpose(2, 0, 1))
    d["normg"] = np.ascontiguousarray(f(inp["norm_g"]).reshape(2, 2, 8, 128).transpose(3, 0, 1, 2))
    d["fnorm"] = col(inp["final_norm"], 8)
    c32, s32 = rope_tab(32, TL, TC)
    sc = np.float32(96 ** -0.5)
    d["cosq96"] = np.ascontiguousarray(np.concatenate([np.full((64, TT), sc, np.float32), c32 * sc], 0))
    d["sinq96"] = np.ascontiguousarray(np.concatenate([np.zeros((64, TT), np.float32), s32 * sc], 0))
    d["cos32"], d["sin32"] = c32, s32
    d["ev_w_in"] = f(inp["ev_w_in"][0])
    d["ev_w_uq"] = f(inp["ev_mla_w_uq"][0])
    d["ev_w_ukv"] = f(inp["ev_mla_w_ukv"][0])
    d["qn_col"] = col(inp["ev_mla_q_norm"][0], 3)
    d["kvn_col"] = col(inp["ev_mla_kv_norm"][0], 2)
    d["ev_w_out"] = f(inp["ev_w_out"][0])
    d["ev_wgu"] = f(inp["ev_ffn_w_gu"][0])
    d["ev_wdn"] = f(inp["ev_ffn_w_down"][0])
    c64, s64 = rope_tab(64, TL, TC)
    c64 = np.ascontiguousarray(np.concatenate([c64, c64], 0)); s64 = np.ascontiguousarray(np.concatenate([s64, s64], 0))
    d["cos64"], d["sin64"] = c64, s64
    d["cosq64"], d["sinq64"] = c64 * np.float32(0.125), s64 * np.float32(0.125)
    d["od_w_in"] = f(inp["od_w_in"][0])
    d["od_w_out"] = f(inp["od_w_out"][0])
    d["router"] = f(inp["od_router_w"][0]); d["moe_gu"] = f(inp["od_moe_w_gu"][0]); d["moe_dn"] = f(inp["od_moe_w_down"][0])
    d["conv_col"] = np.ascontiguousarray(f(inp["ev_gdn_conv"][0]).reshape(5, 12, 128).transpose(2, 1, 0))
    d["alog_rep"] = np.ascontiguousarray(np.broadcast_to(f(inp["ev_gdn_a_log"][0]).reshape(1, 16), (128, 16)))
    d["dtb_rep"] = np.ascontiguousarray(np.broadcast_to(f(inp["ev_gdn_dt_bias"][0]).reshape(1, 16), (128, 16)))
    d["onrm_rep"] = np.ascontiguousarray(np.broadcast_to(f(inp["ev_gdn_out_norm"][0]).reshape(1, 1, 64), (128, 8, 64)))
    perm = np.concatenate([np.arange(16, 32), np.arange(0, 16), np.arange(48, 64), np.arange(32, 48)])
    gq = f(inp["od_gqa_q_norm"][0]); gk = f(inp["od_gqa_k_norm"][0])
    d["gcols"] = np.ascontiguousarray(np.stack([np.tile(gq, 2), np.tile(gq[perm], 2), np.tile(gk, 2), np.tile(gk[perm], 2)], -1))
    d["dlam"] = np.ascontiguousarray(np.broadcast_to(f(inp["od_diff_lambda"][0])[None], (128, 4, 64)))
    d["dncol"] = np.ascontiguousarray(f(inp["od_diff_norm"][0]).reshape(128, 1))
    return d


_NC_CACHE = {}


def kernel(**inputs):
    TL, TC = 8192, 256
    inp = {k: np.asarray(v) for k, v in inputs.items()}
    if "nc" not in _NC_CACHE:
        _NC_CACHE["nc"] = build(TL, TC, stages=("l0", "gdn", "l1"))
    nc = _NC_CACHE["nc"]
    shared = prep_core(inp, 0, TL, TC)
    in_maps = [shared]
    for b in range(1, 8):
        d = dict(shared)
        d["x"] = np.ascontiguousarray(inp["x"][b], dtype=np.float32)
        d["ctx"] = np.ascontiguousarray(inp["ctx"][b], dtype=np.float32)
        d["ccol"] = np.ascontiguousarray(np.stack([col(inp["c"][b], 8), col(inp["c_ctx"], 8)], -1))
        in_maps.append(d)
    res = run_bass_kernel_spmd(nc, in_maps, core_ids=list(range(8)))
    out = np.stack([np.asarray(r["out"], dtype=np.float32) for r in res.results], 0)
    return out
```

```python
import numpy as np
from contextlib import ExitStack
import concourse.bass as bass
import concourse.mybir as mybir
from concourse.bass_utils import run_bass_kernel_spmd

F32 = mybir.dt.float32
BF16 = mybir.dt.bfloat16
AF = mybir.ActivationFunctionType
ALU = mybir.AluOpType
AX = mybir.AxisListType


class Buf:
    ALL = []

    def __init__(self, name, t):
        self.name = name
        self.t = t
        self.st = {}
        Buf.ALL.append(self)

    def __getitem__(self, idx):
        return self.t[idx]


class Sched:
    CE = ("pe", "dve", "act", "pool")

    def __init__(self, nc, stack, ndma=8):
        self.nc = nc
        self.stack = stack
        self.sem = {e: stack.enter_context(nc.semaphore("s_" + e)) for e in self.CE}
        self.cnt = {e: 0 for e in self.CE}
        self.q = {e: [] for e in ("pe", "dve", "act", "pool", "sp")}
        self.seen = {e: {} for e in self.q}
        self.dsem = {}
        self.dcnt = {}
        self.di = {}
        for qn in ("sp", "pool", "act"):
            self.dsem[qn] = [stack.enter_context(nc.semaphore("d_%s%d" % (qn, i))) for i in range(ndma)]
            self.dcnt[qn] = [0] * ndma
            self.di[qn] = 0
        self.out_tokens = []
        self.nbuf = 0

    def sb(self, name, shape, dt, st=None, side=None):
        t = (st or self.stack).enter_context(self.nc.sbuf_tensor(name, list(shape), dt, side=side))
        return Buf(name, t)

    def ps(self, name, shape, dt=F32, st=None):
        t = (st or self.stack).enter_context(self.nc.psum_tensor(name, list(shape), dt))
        b = Buf(name, t)
        b.whole = True
        return b

    def dram(self, name, shape, dt, kind="Internal"):
        t = self.nc.dram_tensor(name, list(shape), dt, kind=kind)
        return Buf(name, t)

    @staticmethod
    def _norm(x):
        if isinstance(x, Buf):
            return x, None
        if getattr(x[0], "whole", False):
            return x[0], None
        return x

    def _deps(self, reads, writes):
        toks = []
        for x in reads:
            b, k = self._norm(x)
            keys = [k] if k is not None else list(b.st.keys())
            if k is not None and None in b.st:
                keys.append(None)
            if k is None and None not in keys:
                keys.append(None)
            for kk in keys:
                s = b.st.get(kk)
                if s and s[0] is not None:
                    toks.append(("raw", s[0]))
        for x in writes:
            b, k = self._norm(x)
            keys = [k] if k is not None else list(b.st.keys())
            if k is not None and None in b.st:
                keys.append(None)
            if k is None and None not in keys:
                keys.append(None)
            for kk in keys:
                s = b.st.get(kk)
                if s:
                    if s[0] is not None:
                        toks.append(("waw", s[0]))
                    for r in s[1].values():
                        toks.append(("war", r))
        return toks

    def _update(self, reads, writes, tok):
        for x in reads:
            b, k = self._norm(x)
            s = b.st.setdefault(k, [None, {}])
            if tok[0] not in s[1] or s[1][tok[0]][2] < tok[2]:
                s[1][tok[0]] = tok
        for x in writes:
            b, k = self._norm(x)
            if k is None:
                b.st = {None: [tok, {}]}
            else:
                b.st[k] = [tok, {}]

    def _emit_waits(self, eng, toks):
        need = {}
        for kind, t in toks:
            semkey, semh, val, src = t
            if src == eng and eng == "pe":
                continue
            if self.seen[eng].get(semkey, 0) >= val:
                continue
            if semkey not in need or need[semkey][1] < val:
                need[semkey] = (semh, val)
        for semkey, (semh, val) in need.items():
            self.seen[eng][semkey] = val
            self.q[eng].append(("wait", semh, val))

    def op(self, eng, fn, reads=(), writes=()):
        toks = self._deps(reads, writes)
        self._emit_waits(eng, toks)
        self.cnt[eng] += 1
        tok = ("c_" + eng, self.sem[eng], self.cnt[eng], eng)
        self.q[eng].append(("op", fn, self.sem[eng], 1))
        self._update(reads, writes, tok)
        return tok

    def dma(self, qn, out, in_, reads=(), writes=(), **kw):
        toks = self._deps(reads, writes)
        i = self.di[qn]
        n = len(self.dsem[qn])
        r = i % n
        self.di[qn] = i + 1
        semh = self.dsem[qn][r]
        semkey = "d_%s%d" % (qn, r)
        if self.dcnt[qn][r] > 0:
            toks.append(("waw", (semkey, semh, self.dcnt[qn][r], "dma")))
        self._emit_waits(qn, toks)
        self.dcnt[qn][r] += 16
        tok = (semkey, semh, self.dcnt[qn][r], "dma")

        def fn(e, out=out, in_=in_, kw=kw):
            return e.dma_start(out=out, in_=in_, **kw)

        self.q[qn].append(("op", fn, semh, 16))
        self._update(reads, writes, tok)
        return tok

    def barrier(self):
        toks = []
        for e in self.CE:
            if self.cnt[e] > 0:
                toks.append(("c_" + e, self.sem[e], self.cnt[e], "x"))
        for qn in self.dsem:
            for r, semh in enumerate(self.dsem[qn]):
                if self.dcnt[qn][r] > 0:
                    toks.append(("d_%s%d" % (qn, r), semh, self.dcnt[qn][r], "dma"))
        for e in self.q:
            self._emit_waits(e, [("raw", t) for t in toks])
        for b in Buf.ALL:
            b.st = {}

    def wait_all(self, eng, toks):
        self._emit_waits(eng, [("raw", t) for t in toks])

    def emit(self):
        nc = self.nc
        emap = {"pe": "tensor", "dve": "vector", "act": "scalar", "pool": "gpsimd", "sp": "sync"}
        with nc.Block() as block:
            for en, bn in emap.items():
                items = self.q[en]

                def body(e, items=items):
                    for it in items:
                        if it[0] == "wait":
                            e.wait_ge(it[1], it[2])
                        else:
                            ins = it[1](e)
                            ins.then_inc(it[2], it[3])

                getattr(block, bn)(body)
        for en in self.q:
            self.q[en] = []


def _mm(S, out, lhsT, rhs, start, stop, R, W):
    return S.op("pe", lambda e: e.matmul(out, lhsT=lhsT, rhs=rhs, start=start, stop=stop), reads=R, writes=W)


def _tr(S, out, in_, ident, R, W):
    return S.op("pe", lambda e: e.transpose(out, in_, ident), reads=R, writes=W)


def _act(S, out, in_, func, R, W, scale=1.0, bias=0.0, eng="act"):
    return S.op(eng, lambda e: e.activation(out=out, in_=in_, func=func, bias=bias, scale=scale), reads=R, writes=W)


def _tt(S, eng, out, in0, in1, op, R, W):
    return S.op(eng, lambda e: e.tensor_tensor(out=out, in0=in0, in1=in1, op=op), reads=R, writes=W)


def _ts(S, eng, out, in0, s1, s2, op0, op1, R, W):
    if s2 is None:
        return S.op(eng, lambda e: e.tensor_scalar(out=out, in0=in0, scalar1=s1, scalar2=None, op0=op0), reads=R, writes=W)
    return S.op(eng, lambda e: e.tensor_scalar(out=out, in0=in0, scalar1=s1, scalar2=s2, op0=op0, op1=op1), reads=R, writes=W)


def _stt(S, eng, out, in0, scalar, in1, op0, op1, R, W):
    return S.op(eng, lambda e: e.scalar_tensor_tensor(out=out, in0=in0, scalar=scalar, in1=in1, op0=op0, op1=op1), reads=R, writes=W)


def _cp(S, eng, out, in_, R, W):
    if eng == "act":
        return S.op(eng, lambda e: e.copy(out=out, in_=in_), reads=R, writes=W)
    return S.op(eng, lambda e: e.tensor_copy(out=out, in_=in_), reads=R, writes=W)


def _ms(S, eng, ap, val, W):
    return S.op(eng, lambda e: e.memset(ap, val), reads=[], writes=W)


D = 1024
EPS = 1e-6


class K:
    pass


def build(TL, TC, stages=("l0", "l1"), dbg=()):
    Buf.ALL.clear()
    nc = bass.Bass("TRN2", target_bir_lowering=False)
    TT = TC + TL
    tiles = [(0, TC)] + [(TC + 512 * i, 512) for i in range(TL // 512)]
    g = K()
    g.nc, g.TL, g.TC, g.TT, g.tiles = nc, TL, TC, TT, tiles
    g.dbgset = set(dbg)
    g.stg_i = 0
    g.has_gdn = "gdn" in stages

    def din(name, shape, dt=F32):
        return Buf(name, nc.dram_tensor(name, list(shape), dt, kind="ExternalInput"))

    g.x_in = din("x", [TL, D])
    g.ctx_in = din("ctx", [TC, D])
    g.ccol = din("ccol", [128, 8, 2])
    g.modw = din("mod_w", [2, D, 6 * D])
    g.modb = din("modb", [128, 2, 48])
    g.normg = din("normg", [128, 2, 2, 8])
    g.fnorm = din("fnorm", [128, 8])
    g.out = Buf("out", nc.dram_tensor("out", [TL, D], F32, kind="ExternalOutput"))
    g.cosq96 = din("cosq96", [96, TT]); g.sinq96 = din("sinq96", [96, TT])
    g.cos32 = din("cos32", [32, TT]); g.sin32 = din("sin32", [32, TT])
    g.ev_w_in = din("ev_w_in", [1024, 2752]); g.ev_w_uq = din("ev_w_uq", [384, 768]); g.ev_w_ukv = din("ev_w_ukv", [256, 1024])
    g.qn_col = din("qn_col", [128, 3]); g.kvn_col = din("kvn_col", [128, 2])
    g.ev_w_out = din("ev_w_out", [1024, 1024]); g.ev_wgu = din("ev_wgu", [1024, 5632]); g.ev_wdn = din("ev_wdn", [2816, 1024])

    g.od_w_in = din("od_w_in", [1024, 2304]); g.od_w_out = din("od_w_out", [1024, 1024])
    g.gcols = din("gcols", [128, 4]); g.dlam = din("dlam", [128, 4, 64]); g.dncol = din("dncol", [128, 1])
    g.cosq64 = din("cosq64", [128, TT]); g.sinq64 = din("sinq64", [128, TT])
    g.cos64 = din("cos64", [128, TT]); g.sin64 = din("sin64", [128, TT])
    g.router = din("router", [1024, 8]); g.moe_gu = din("moe_gu", [8, 1024, 7168]); g.moe_dn = din("moe_dn", [8, 3584, 1024])
    g.conv_col = din("conv_col", [128, 12, 5]); g.alog_rep = din("alog_rep", [128, 16]); g.dtb_rep = din("dtb_rep", [128, 16])
    g.onrm_rep = din("onrm_rep", [128, 8, 64])
    import math
    g.lambda_init = 0.8 - 0.6 * math.exp(-0.3 * 1)

    with ExitStack() as st:
        S = Sched(nc, st)
        g.S = S
        g.xT_s = S.dram("xT_s", [8, 128, TT], F32)
        phase_const(g)
        phase_mod(g)
        phase_x0(g)
        S.barrier()
        if "l0" in stages or "p1" in stages:
            phase_l0_p1(g)
            S.barrier()
        if "l0" in stages or "mla" in stages:
            phase_l0_mla(g)
            S.barrier()
        if "gdn" in stages:
            phase_gdn_prep(g)
            S.barrier()
            phase_gdn(g)
            S.barrier()
        if "l0" in stages or "wout" in stages:
            phase_wout(g, 0, g.ev_w_out)
            S.barrier()
        if "l0" in stages or "ffn" in stages:
            phase_ffn0(g)
            S.barrier()
        if "l1" in stages or "l1a" in stages:
            phase_l1_p1(g)
            S.barrier()
            phase_l1_attn(g)
            S.barrier()
            phase_wout(g, 1, g.od_w_out)
            S.barrier()
        if "l1" in stages or "moe" in stages:
            phase_moe_a(g)
            S.barrier()
            phase_moe_b(g)
            S.barrier()
        phase_final(g)
        S.wait_all("sp", S.out_tokens)
        S.emit()
    return nc


def dbg_out(g, name, buf, ap, shape, dt=F32):
    S = g.S
    o = Buf(name, g.nc.dram_tensor(name, list(shape), dt, kind="ExternalOutput"))
    idx = tuple(slice(None) for _ in shape)
    tok = S.dma("pool", o.t[idx], ap, reads=[buf], writes=[o])
    S.out_tokens.append(tok)


def phase_const(g):
    S = g.S
    g.identF = S.sb("identF", [128, 128], F32)
    g.identB = S.sb("identB", [128, 128], BF16)
    g.onesF = S.sb("onesF", [128, 128], F32)
    g.onesB = S.sb("onesB", [128, 128], BF16)
    _ms(S, "pool", g.onesF[:, :], 1.0, [g.onesF])
    _ms(S, "pool", g.identF[:, :], 1.0, [g.identF])
    S.op("pool", lambda e: e.affine_select(out=g.identF[:, :], in_=g.identF[:, :], pattern=[[-1, 128]],
                                           compare_op=ALU.is_equal, fill=0.0, base=0, channel_multiplier=1),
         reads=[g.identF], writes=[g.identF])
    g.sel65 = S.sb("sel65", [65, 64], F32)
    _ms(S, "pool", g.sel65[:, :], 0.0, [g.sel65])
    _ms(S, "pool", g.sel65[64:65, :], 1.0, [g.sel65])
    _cp(S, "dve", g.identB[:, :], g.identF[:, :], [g.identF], [g.identB])
    _cp(S, "dve", g.onesB[:, :], g.onesF[:, :], [g.onesF], [g.onesB])


def phase_mod(g):
    S = g.S
    nc = g.nc
    g.modT = S.sb("modT", [128, 2, 48, 2], F32)
    g.modA = S.sb("modA", [128, 2, 2, 8, 2], F32)
    g.fn = S.sb("fn", [128, 8], F32)
    with ExitStack() as ph:
        cc = S.sb("cc", [128, 8, 2], F32, ph)
        sc = S.sb("sc", [128, 8, 2], F32, ph)
        mb = S.sb("mb", [128, 2, 48], F32, ph)
        ng = S.sb("ng", [128, 2, 2, 8], F32, ph)
        wb = [S.sb("wb%d" % i, [128, 8, 768], F32, ph) for i in range(2)]
        psm = S.ps("psm", [128, 2, 48, 2], F32, ph)
        S.dma("sp", cc[:, :, :], g.ccol[:, :, :], reads=[g.ccol], writes=[cc])
        S.dma("sp", mb[:, :, :], g.modb[:, :, :], reads=[g.modb], writes=[mb])
        S.dma("sp", ng[:, :, :, :], g.normg[:, :, :, :], reads=[g.normg], writes=[ng])
        S.dma("sp", g.fn[:, :], g.fnorm[:, :], reads=[g.fnorm], writes=[g.fn])
        _act(S, sc[:, :, :], cc[:, :, :], AF.Silu, [cc], [sc])
        i = 0
        for l in range(2):
            wv = g.modw[l].rearrange("(kc p) n -> p kc n", p=128)
            for blk in range(8):
                w = wb[i % 2]
                i += 1
                S.dma("sp", w[:, :, :], wv[:, :, blk * 768:(blk + 1) * 768], reads=[g.modw], writes=[w])
                for fs in range(6):
                    s = blk * 6 + fs
                    for kc in range(8):
                        _mm(S, psm[:, l, s, :], w[:, kc, fs * 128:(fs + 1) * 128], sc[:, kc, :], kc == 0, kc == 7,
                            [w, sc], [(psm, (l, s))])
            for j in range(2):
                _tt(S, "dve", g.modT[:, l, :, j], psm[:, l, :, j], mb[:, l, :], ALU.add, [psm, mb], [(g.modT, (l, j))])
            for u in range(2):
                for j in range(2):
                    _stt(S, "dve", g.modA[:, l, u, :, j], g.modT[:, l, (3 * u + 1) * 8:(3 * u + 2) * 8, j], 1.0,
                         ng[:, l, u, :], ALU.add, ALU.mult, [g.modT, ng], [(g.modA, (l, u, j))])
        if "mod" in g.dbgset:
            dbg_out(g, "dbg_modT", g.modT, g.modT[:, :, :, :], [128, 2, 48, 2])
            dbg_out(g, "dbg_modA", g.modA, g.modA[:, :, :, :, :], [128, 2, 2, 8, 2])
        S.emit()


def tile_src(g, t0, nt):
    if t0 < g.TC:
        src, r0 = g.ctx_in, t0
    else:
        src, r0 = g.x_in, t0 - g.TC
    return src, src[r0:r0 + nt, :].rearrange("(s p) d -> p s d", p=128)


def phase_x0(g):
    S = g.S
    with ExitStack() as ph:
        xin = [S.sb("xin%d" % i, [128, 4, D], F32, ph) for i in range(2)]
        xt = [S.sb("xt%d" % i, [128, 8, 512], F32, ph) for i in range(2)]
        pst = [S.ps("pst%d" % i, [128, 512], F32, ph) for i in range(4)]
        k = 0
        for ti, (t0, nt) in enumerate(g.tiles):
            ns = nt // 128
            a = xin[ti % 2]
            b = xt[ti % 2]
            srcb, sap = tile_src(g, t0, nt)
            S.dma("sp", a[:, 0:ns, :], sap, reads=[srcb], writes=[a])
            for c in range(8):
                p = pst[k % 4]
                k += 1
                for s in range(ns):
                    _tr(S, p[:, s * 128:(s + 1) * 128], a[:, s, c * 128:(c + 1) * 128], g.identF[:, :],
                        [a, g.identF], [p])
                _cp(S, "dve" if c % 2 == 0 else "act", b[:, c, 0:nt], p[:, 0:nt], [p], [(b, c)])
            S.dma("pool", g.xT_s.t[:, :, t0:t0 + nt].rearrange("c p t -> p c t"), b[:, :, 0:nt],
                  reads=[b], writes=[(g.xT_s, ti)])
        S.emit()


def rstd_from_ss(S, eng, out_ap, ps_ap, n, R, W):
    _ts(S, eng, out_ap, ps_ap, 1.0 / n, EPS, ALU.mult, ALU.add, R, W)
    _act(S, out_ap, out_ap, AF.Sqrt, W, W)
    S.op("dve", lambda e: e.reciprocal(out=out_ap, in_=out_ap), reads=W, writes=W)


def phase_final(g):
    S = g.S
    with ExitStack() as ph:
        xt = [S.sb("fxt%d" % i, [128, 8, 512], F32, ph) for i in range(2)]
        sq = S.sb("fsq", [128, 8, 512], F32, ph)
        rs = S.sb("frs", [128, 512], F32, ph)
        yo = [S.sb("fyo%d" % i, [128, 4, D], F32, ph) for i in range(2)]
        pss = S.ps("fpss", [128, 512], F32, ph)
        pst = [S.ps("fpst%d" % i, [128, 512], F32, ph) for i in range(4)]
        k = 0
        for ti, (t0, nt) in enumerate(g.tiles):
            if t0 < g.TC:
                continue
            ns = nt // 128
            a = xt[ti % 2]
            y = yo[ti % 2]
            S.dma("sp", a[:, :, 0:nt], g.xT_s.t[:, :, t0:t0 + nt].rearrange("c p t -> p c t"),
                  reads=[(g.xT_s, ti)], writes=[a])
            for c in range(8):
                _tt(S, "pool" if c % 2 else "dve", sq[:, c, 0:nt], a[:, c, 0:nt], a[:, c, 0:nt], ALU.mult, [a], [(sq, c)])
            for c in range(8):
                _mm(S, pss[:, 0:nt], g.onesF[:, :], sq[:, c, 0:nt], c == 0, c == 7, [g.onesF, (sq, c)], [pss])
            rstd_from_ss(S, "dve", rs[:, 0:nt], pss[:, 0:nt], D, [pss], [rs])
            for c in range(8):
                _stt(S, "dve", sq[:, c, 0:nt], a[:, c, 0:nt], g.fn[:, c:c + 1], rs[:, 0:nt],
                     ALU.mult, ALU.mult, [a, g.fn, rs, (sq, c)], [(sq, c)])
            for s in range(ns):
                for h in range(2):
                    p = pst[k % 4]
                    k += 1
                    for cc in range(4):
                        c = h * 4 + cc
                        _tr(S, p[:, cc * 128:(cc + 1) * 128], sq[:, c, s * 128:(s + 1) * 128], g.identF[:, :],
                            [(sq, c), g.identF], [p])
                    _cp(S, "act" if h else "dve", y[:, s, h * 512:(h + 1) * 512], p[:, :], [p], [(y, (s, h))])
            r0 = t0 - g.TC
            tok = S.dma("pool", g.out.t[r0:r0 + nt, :].rearrange("(s p) d -> p s d", p=128), y[:, 0:ns, :],
                        reads=[y], writes=[(g.out, ti)])
            S.out_tokens.append(tok)
        S.emit()


def load_w(g, ph, name, src_buf, src_ap, K, N, stg):
    S = g.S
    kc = K // 128
    w = S.sb(name, [128, kc, N], BF16, ph)
    v = src_ap.rearrange("(kc p) n -> p kc n", p=128)
    i = 0
    for k0 in range(0, kc, 8):
        k1 = min(kc, k0 + 8)
        for n0 in range(0, N, 512):
            n1 = min(N, n0 + 512)
            s = stg[g.stg_i % len(stg)]
            g.stg_i += 1
            S.dma("sp", s[:, 0:k1 - k0, 0:n1 - n0], v[:, k0:k1, n0:n1], reads=[src_buf], writes=[s])
            eng = ("dve", "pool", "act")[g.stg_i % 3]
            _cp(S, eng, w[:, k0:k1, n0:n1], s[:, 0:k1 - k0, 0:n1 - n0], [s], [(w, (k0, n0))])
    return w


def make_rot(g, rot, w, view, off, n):
    S = g.S
    q = n // 4
    rv, wv = view(rot), view(w)
    _ms(S, "pool", rot[:, :, :], 0.0, [rot])
    for (d0, s0, sign) in ((0, q, -1.0), (q, 0, 1.0), (2 * q, 3 * q, -1.0), (3 * q, 2 * q, 1.0)):
        _ts(S, "dve", rv[:, :, :, off + d0:off + d0 + q], wv[:, :, :, off + s0:off + s0 + q], sign, None, ALU.mult, None,
            [w, rot], [rot])


def norm_mod(g, a, sq, rs, hT, pss, nt, A_ap, B_ap, n=D, nchunk=8, xr=()):
    S = g.S
    for c in range(nchunk):
        _tt(S, "pool" if c % 2 else "dve", sq[:, c, 0:nt], a[:, c, 0:nt], a[:, c, 0:nt], ALU.mult, [(a, c)], [(sq, c)])
    for c in range(nchunk):
        _mm(S, pss[:, 0:nt], g.onesF[:, :], sq[:, c, 0:nt], c == 0, c == nchunk - 1, [g.onesF, (sq, c)], [pss])
    rstd_from_ss(S, "dve", rs[:, 0:nt], pss[:, 0:nt], n, [pss], [rs])
    for c in range(nchunk):
        _stt(S, "dve", sq[:, c, 0:nt], a[:, c, 0:nt], A_ap(c), rs[:, 0:nt], ALU.mult, ALU.mult,
             [(a, c), rs, (sq, c)] + list(xr), [(sq, c)])
        if B_ap is None:
            _cp(S, "act", hT[:, c, 0:nt], sq[:, c, 0:nt], [(sq, c)], [(hT, c)])
        else:
            _act(S, hT[:, c, 0:nt], sq[:, c, 0:nt], AF.Identity, [(sq, c)] + list(xr), [(hT, c)], bias=B_ap(c))


def xT_view(g, t0, nt):
    return g.xT_s.t[:, :, t0:t0 + nt].rearrange("c p t -> p c t")


def phase_l0_p1(g):
    S = g.S
    nc = g.nc
    TT = g.TT
    g.qT_s = S.dram("qT_s", [8, 96, TT], BF16)
    g.kT_s = S.dram("kT_s", [8, 96, TT], BF16)
    g.v_s = S.dram("v_s", [TT, 8, 65], BF16)
    g.mixT_s = S.dram("mixT_s", [1024, TT], BF16)
    g.gq_s = S.dram("gq_s", [12, 128, TT], F32)
    g.ab_s = S.dram("ab_s", [32, TT], F32)
    g.z_s = S.dram("z_s", [TT, 512], F32)
    with ExitStack() as ph:
        g.stg_i = 0
        with ExitStack() as ph2:
            stg = [S.sb("stg%d" % i, [128, 8, 512], F32, ph2, side="right") for i in range(2)]
            w_in = load_w(g, ph, "w_in0", g.ev_w_in, g.ev_w_in.t[:, :], 1024, 2752, stg)
            w_uq = load_w(g, ph, "w_uq", g.ev_w_uq, g.ev_w_uq.t[:, :], 384, 768, stg)
            w_ukv = load_w(g, ph, "w_ukv", g.ev_w_ukv, g.ev_w_ukv.t[:, :], 256, 1024, stg)
            S.barrier()
            S.emit()
        w_uq_r = S.sb("w_uq_r", [128, 3, 768], BF16, ph)
        w_kpe_r = S.sb("w_kpe_r", [128, 8, 32], BF16, ph)
        make_rot(g, w_uq_r, w_uq, lambda b: b[:, :, :].rearrange("p k (h e) -> p k h e", e=96), 64, 32)
        _ms(S, "pool", w_kpe_r[:, :, :], 0.0, [w_kpe_r])
        for (d0, s0, sign) in ((0, 8, -1.0), (8, 0, 1.0), (16, 24, -1.0), (24, 16, 1.0)):
            _ts(S, "dve", w_kpe_r[:, :, d0:d0 + 8], w_in[:, :, 640 + s0:640 + s0 + 8], sign, None, ALU.mult, None,
                [w_in, w_kpe_r], [w_kpe_r])
        qn = S.sb("qn", [128, 3], F32, ph)
        kvn = S.sb("kvn", [128, 2], F32, ph)
        S.dma("sp", qn[:, :], g.qn_col[:, :], reads=[g.qn_col], writes=[qn])
        S.dma("sp", kvn[:, :], g.kvn_col[:, :], reads=[g.kvn_col], writes=[kvn])

        xa = [S.sb("p1xa%d" % i, [128, 8, 512], F32, ph) for i in range(2)]
        sq = S.sb("p1sq", [128, 8, 512], F32, ph)
        rs = S.sb("p1rs", [128, 512], F32, ph)
        hT = S.sb("p1hT", [128, 8, 512], BF16, ph)
        cq = S.sb("p1cq", [128, 3, 512], F32, ph)
        cqn = S.sb("p1cqn", [128, 3, 512], BF16, ph)
        ckv = S.sb("p1ckv", [128, 2, 512], F32, ph)
        ckvn = S.sb("p1ckvn", [128, 2, 512], BF16, ph)
        tq = [S.sb("p1tq%d" % i, [96, 512], F32, ph) for i in range(4)]
        qo = [S.sb("p1qo%d" % i, [96, 512], BF16, ph) for i in range(2)]
        kn = S.sb("p1kn", [64, 8, 512], BF16, ph)
        kp = S.sb("p1kp", [32, 512], BF16, ph)
        va = S.sb("p1va", [128, 4, 8, 65], BF16, ph)
        gqo = [S.sb("p1gq%d" % i, [128, 512], F32, ph) for i in range(3)]
        zo = [S.sb("p1zo%d" % i, [128, 512], F32, ph) for i in range(2)]
        cs = S.sb("p1cs", [96, 512], F32, ph)
        sn = S.sb("p1sn", [96, 512], F32, ph)
        ck = S.sb("p1ck", [32, 512], F32, ph)
        sk = S.sb("p1sk", [32, 512], F32, ph)
        pss = S.ps("p1pss", [128, 512], F32, ph)
        pp = [S.ps("p1pp%d" % i, [128, 512], F32, ph) for i in range(6)]
        _ms(S, "pool", va[:, :, :, :], 1.0, [va])
        pi = [0]

        def nextp():
            p = pp[pi[0] % 6]
            pi[0] += 1
            return p

        ev = [0]

        def evac(out_ap, in_ap, R, W):
            ev[0] += 1
            _cp(S, "act" if ev[0] % 2 else "dve", out_ap, in_ap, R, W)

        for ti, (t0, nt) in enumerate(g.tiles):
            j = 1 if t0 < g.TC else 0
            ns = nt // 128
            a = xa[ti % 2]
            S.dma("sp", a[:, :, 0:nt], xT_view(g, t0, nt), reads=[(g.xT_s, ti)], writes=[a])
            S.dma("sp", cs[:, 0:nt], g.cosq96[:, t0:t0 + nt], reads=[g.cosq96], writes=[cs])
            S.dma("sp", sn[:, 0:nt], g.sinq96[:, t0:t0 + nt], reads=[g.sinq96], writes=[sn])
            S.dma("sp", ck[:, 0:nt], g.cos32[:, t0:t0 + nt], reads=[g.cos32], writes=[ck])
            S.dma("sp", sk[:, 0:nt], g.sin32[:, t0:t0 + nt], reads=[g.sin32], writes=[sk])
            norm_mod(g, a, sq, rs, hT, pss, nt, lambda c: g.modA[:, 0, 0, c, j:j + 1], lambda c: g.modT[:, 0, c, j:j + 1])

            def proj(col0, m, lhs_w=w_in, rhs=hT, nk=8):
                p = nextp()
                for kc in range(nk):
                    _mm(S, p[0:m, 0:nt], lhs_w[:, kc, col0:col0 + m], rhs[:, kc, 0:nt], kc == 0, kc == nk - 1,
                        [lhs_w, rhs], [p])
                return p

            for c in range(3):
                p = proj(c * 128, 128)
                evac(cq[:, c, 0:nt], p[:, 0:nt], [p], [(cq, c)])
            for c in range(2):
                p = proj(384 + c * 128, 128)
                evac(ckv[:, c, 0:nt], p[:, 0:nt], [p], [(ckv, c)])
            norm_mod(g, cq, sq, rs, cqn, pss, nt, lambda c: qn[:, c:c + 1], None, n=384, nchunk=3, xr=[qn])
            norm_mod(g, ckv, sq, rs, ckvn, pss, nt, lambda c: kvn[:, c:c + 1], None, n=256, nchunk=2, xr=[kvn])
            for h in range(8):
                p1 = proj(h * 96, 96, w_uq, cqn, 3)
                p2 = proj(h * 96, 96, w_uq_r, cqn, 3)
                t = tq[h % 2]
                t2 = tq[2 + h % 2]
                o = qo[h % 2]
                _tt(S, "dve", t[:, 0:nt], p1[0:96, 0:nt], cs[:, 0:nt], ALU.mult, [p1, cs], [t])
                _tt(S, "dve", t2[:, 0:nt], p2[0:96, 0:nt], sn[:, 0:nt], ALU.mult, [p2, sn], [t2])
                _tt(S, "pool", o[:, 0:nt], t2[:, 0:nt], t[:, 0:nt], ALU.add, [t2, t], [o])
                S.dma("pool", g.qT_s.t[h, :, t0:t0 + nt], o[:, 0:nt], reads=[o], writes=[(g.qT_s, (h, ti))])
            for h in range(8):
                p = proj(h * 128, 64, w_ukv, ckvn, 2)
                evac(kn[:, h, 0:nt], p[0:64, 0:nt], [p], [(kn, h)])
            S.dma("pool", g.kT_s.t[:, 0:64, t0:t0 + nt].rearrange("h p t -> p h t"), kn[:, :, 0:nt],
                  reads=[kn], writes=[(g.kT_s, ("n", ti))])
            p1 = proj(640, 32)
            p2 = proj(0, 32, w_kpe_r, hT, 8)
            t = tq[0]
            t2 = tq[2]
            _tt(S, "dve", t[0:32, 0:nt], p1[0:32, 0:nt], ck[:, 0:nt], ALU.mult, [p1, ck], [t])
            _tt(S, "dve", t2[0:32, 0:nt], p2[0:32, 0:nt], sk[:, 0:nt], ALU.mult, [p2, sk], [t2])
            _tt(S, "pool", kp[:, 0:nt], t2[0:32, 0:nt], t[0:32, 0:nt], ALU.add, [t2, t], [kp])
            for h in range(8):
                S.dma("pool", g.kT_s.t[h, 64:96, t0:t0 + nt], kp[:, 0:nt], reads=[kp], writes=[(g.kT_s, ("p", h, ti))])
            vw = w_ukv[:, :, :].rearrange("p k (h e) -> p k h e", e=128)
            for s in range(ns):
                p = nextp()
                for kc in range(2):
                    _mm(S, p[:, :].rearrange("p (h e) -> p h e", e=64), ckvn[:, kc, s * 128:(s + 1) * 128],
                        vw[:, kc, :, 64:128], kc == 0, kc == 1, [ckvn, w_ukv], [p])
                evac(va[:, s, :, 0:64], p[:, :].rearrange("p (h e) -> p h e", e=64), [p], [(va, s)])
            S.dma("pool", g.v_s.t[t0:t0 + nt, :, :].rearrange("(s p) h e -> p s h e", p=128), va[:, 0:ns, :, :],
                  reads=[va], writes=[(g.v_s, ti)])
            for c in range(12):
                p = proj(672 + c * 128, 128)
                o = gqo[c % 3]
                evac(o[:, 0:nt], p[:, 0:nt], [p], [o])
                S.dma("pool", g.gq_s.t[c, :, t0:t0 + nt], o[:, 0:nt], reads=[o], writes=[(g.gq_s, (c, ti))])
            p = proj(2720, 32)
            o = gqo[0]
            evac(o[0:32, 0:nt], p[0:32, 0:nt], [p], [o])
            S.dma("pool", g.ab_s.t[:, t0:t0 + nt], o[0:32, 0:nt], reads=[o], writes=[(g.ab_s, ti)])
            for s in range(ns):
                p = nextp()
                for kc in range(8):
                    _mm(S, p[:, :], hT[:, kc, s * 128:(s + 1) * 128], w_in[:, kc, 2208:2720], kc == 0, kc == 7,
                        [hT, w_in], [p])
                o = zo[s % 2]
                _act(S, o[:, :], p[:, :], AF.Silu, [p], [o])
                S.dma("pool", g.z_s.t[t0 + s * 128:t0 + (s + 1) * 128, :], o[:, :], reads=[o], writes=[(g.z_s, (ti, s))])
        S.emit()


def attn_core(g, KT, V, QT, d, dva, kbs, acc, psc, pT, nq, cnt):
    S = g.S
    nkb = len(kbs)
    LA = len(psc) - 1
    slots = []

    def score(i):
        kb = kbs[i]
        sc = psc[cnt[0] % len(psc)]
        p = pT[cnt[0] % len(pT)]
        cnt[0] += 1
        _mm(S, sc[:, 0:nq], KT[0:d, kb * 128:(kb + 1) * 128], QT[0:d, 0:nq], True, True, [KT, QT], [sc])
        _act(S, p[:, 0:nq], sc[:, 0:nq], AF.Exp, [sc], [p])
        slots.append(p)

    for i in range(min(LA, nkb)):
        score(i)
    for i, kb in enumerate(kbs):
        if i + LA < nkb:
            score(i + LA)
        p = slots[i]
        for qs in range(nq // 128):
            _mm(S, acc[:, qs, 0:dva], p[:, qs * 128:(qs + 1) * 128], V[:, kb, 0:dva], i == 0 and qs == 0,
                i == nkb - 1 and qs == nq // 128 - 1, [p, V], [(acc, qs)])


def attn_core3(g, KT, V, QT, d, dv, kbs, accO, Pacc, psc, pT, nq, cnt):
    S = g.S
    nkb = len(kbs)
    LA = len(psc) - 1
    slots = []

    def score(i):
        kb = kbs[i]
        sc = psc[cnt[0] % len(psc)]
        p = pT[cnt[0] % len(pT)]
        cnt[0] += 1
        _mm(S, sc[:, 0:nq], KT[0:d, kb * 128:(kb + 1) * 128], QT[0:d, 0:nq], True, True, [KT, QT], [sc])
        _act(S, p[:, 0:nq], sc[:, 0:nq], AF.Exp, [sc], [p])
        slots.append(p)

    for i in range(min(LA, nkb)):
        score(i)
    seen = {"dve": False, "pool": False}
    for i, kb in enumerate(kbs):
        if i + LA < nkb:
            score(i + LA)
        p = slots[i]
        _mm(S, accO[0:dv, 0:nq], V[:, kb, 0:dv], p[:, 0:nq], i == 0, i == nkb - 1, [p, V], [accO])
        if Pacc is not None:
            eng = "pool" if i % 3 == 2 else "dve"
            pa = Pacc[1] if eng == "pool" else Pacc[0]
            if not seen[eng]:
                seen[eng] = True
                _cp(S, eng, pa[:, 0:nq], p[:, 0:nq], [p], [pa])
            else:
                _tt(S, eng, pa[:, 0:nq], pa[:, 0:nq], p[:, 0:nq], ALU.add, [pa, p], [pa])


def attn_simple_post(g, accO, psm, oS, rec, o, nq, dst_ap, dst_dep):
    S = g.S
    _cp(S, "dve", oS[0:65, 0:nq], accO[0:65, 0:nq], [accO], [oS])
    _mm(S, psm[0:64, 0:nq], g.sel65[0:65, :], oS[0:65, 0:nq], True, True, [g.sel65, oS], [psm])
    S.op("dve", lambda e: e.reciprocal(out=rec[0:64, 0:nq], in_=psm[0:64, 0:nq]), reads=[psm], writes=[rec])
    _tt(S, "dve", o[0:64, 0:nq], oS[0:64, 0:nq], rec[0:64, 0:nq], ALU.mult, [oS, rec], [o])
    S.dma("pool", dst_ap, o[0:64, 0:nq], reads=[o], writes=[dst_dep])


def phase_l0_mla(g):
    S = g.S
    TT = g.TT
    NKB = TT // 128
    with ExitStack() as ph:
        KT = [S.sb("mKT%d" % i, [96, TT], BF16, ph) for i in range(2)]
        V = [S.sb("mV%d" % i, [128, NKB, 65], BF16, ph) for i in range(2)]
        QT = [S.sb("mQT%d" % i, [96, 512], BF16, ph) for i in range(2)]
        pT = [S.sb("mpT%d" % i, [128, 512], BF16, ph) for i in range(4)]
        oS = [S.sb("moS%d" % i, [65, 512], F32, ph) for i in range(2)]
        rec = S.sb("mrec", [64, 512], F32, ph)
        o = [S.sb("mo%d" % i, [64, 512], BF16, ph) for i in range(2)]
        psc = [S.ps("mpsc%d" % i, [128, 512], F32, ph) for i in range(4)]
        acc = [S.ps("macc%d" % i, [128, 512], F32, ph) for i in range(2)]
        psm = S.ps("mpsm", [128, 512], F32, ph)
        cnt = [0]
        qi = 0
        if not g.has_gdn:
            zt = S.sb("mzt", [128, 4, 512], BF16, ph)
            _ms(S, "pool", zt[:, :, :], 0.0, [zt])
            for ti, (t0, nt) in enumerate(g.tiles):
                S.dma("pool", g.mixT_s.t[512:1024, t0:t0 + nt].rearrange("(c p) t -> p c t", p=128), zt[:, :, 0:nt],
                      reads=[zt], writes=[(g.mixT_s, ("z", ti))])
        for h in range(8):
            kt, v = KT[h % 2], V[h % 2]
            S.dma("sp", kt[:, :], g.kT_s.t[h, :, :], reads=[g.kT_s], writes=[kt])
            S.dma("sp", v[:, :, :], g.v_s.t[:, h, :].rearrange("(n p) e -> p n e", p=128), reads=[g.v_s], writes=[v])
            for ti, (t0, nt) in enumerate(g.tiles):
                q = QT[qi % 2]
                ac = acc[qi % 2]
                os_ = oS[qi % 2]
                oo = o[qi % 2]
                qi += 1
                S.dma("sp", q[:, 0:nt], g.qT_s.t[h, :, t0:t0 + nt], reads=[g.qT_s], writes=[q])
                kbs = list(range(g.TC // 128)) if t0 < g.TC else list(range(NKB))
                attn_core3(g, kt, v, q, 96, 65, kbs, ac, None, psc, pT, nt, cnt)
                attn_simple_post(g, ac, psm, os_, rec, oo, nt, g.mixT_s.t[h * 64:(h + 1) * 64, t0:t0 + nt], (g.mixT_s, (h, ti)))
        S.emit()


def phase_wout(g, layer, w_src):
    S = g.S
    with ExitStack() as ph:
        with ExitStack() as ph2:
            stg = [S.sb("wo%d_stg%d" % (layer, i), [128, 8, 512], F32, ph2, side="right") for i in range(2)]
            w = load_w(g, ph, "w_out%d" % layer, w_src, w_src.t[:, :], 1024, 1024, stg)
            S.barrier()
            S.emit()
        xa = [S.sb("wo%dxa%d" % (layer, i), [128, 8, 512], F32, ph) for i in range(2)]
        m = [S.sb("wo%dm%d" % (layer, i), [128, 8, 512], BF16, ph) for i in range(2)]
        pp = [S.ps("wo%dpp%d" % (layer, i), [128, 512], F32, ph) for i in range(4)]
        k = 0
        for ti, (t0, nt) in enumerate(g.tiles):
            j = 1 if t0 < g.TC else 0
            if j == 1 and layer == 1:
                continue
            a, mm_ = xa[ti % 2], m[ti % 2]
            S.dma("sp", a[:, :, 0:nt], xT_view(g, t0, nt), reads=[(g.xT_s, ti)], writes=[a])
            S.dma("sp", mm_[:, :, 0:nt], g.mixT_s.t[:, t0:t0 + nt].rearrange("(c p) t -> p c t", p=128),
                  reads=[g.mixT_s], writes=[mm_])
            for dsl in range(8):
                p = pp[k % 4]
                k += 1
                for kc in range(8):
                    _mm(S, p[:, 0:nt], w[:, kc, dsl * 128:(dsl + 1) * 128], mm_[:, kc, 0:nt], kc == 0, kc == 7, [w, mm_], [p])
                _stt(S, "dve", a[:, dsl, 0:nt], p[:, 0:nt], g.modT[:, layer, 16 + dsl, j:j + 1], a[:, dsl, 0:nt],
                     ALU.mult, ALU.add, [p, (a, dsl)], [(a, dsl)])
            S.dma("pool", xT_view(g, t0, nt), a[:, :, 0:nt], reads=[a], writes=[(g.xT_s, ti)])
        S.emit()


def phase_ffn0(g):
    S = g.S
    NT = 256
    with ExitStack() as ph:
        with ExitStack() as ph2:
            stg = [S.sb("ff_stg%d" % i, [128, 8, 512], F32, ph2, side="right") for i in range(2)]
            wgu = load_w(g, ph, "ff_wgu", g.ev_wgu, g.ev_wgu.t[:, :], 1024, 5632, stg)
            wdn = load_w(g, ph, "ff_wdn", g.ev_wdn, g.ev_wdn.t[:, :], 2816, 1024, stg)
            S.barrier()
            S.emit()
        xa = [S.sb("ffxa%d" % i, [128, 8, NT], F32, ph) for i in range(2)]
        sq = S.sb("ffsq", [128, 8, NT], F32, ph)
        rs = S.sb("ffrs", [128, NT], F32, ph)
        hT = S.sb("ffhT", [128, 8, NT], BF16, ph)
        sg = [S.sb("ffsg%d" % i, [128, NT], F32, ph) for i in range(3)]
        act = [S.sb("ffact%d" % i, [128, NT], BF16, ph) for i in range(4)]
        pgu = [S.ps("ffpgu%d" % i, [128, 512], F32, ph) for i in range(4)]
        pss = pgu[0]
        yac = [S.ps("ffy%d" % i, [128, 2, NT], F32, ph) for i in range(4)]
        ti2 = 0
        for ti, (t0, nt) in enumerate(g.tiles):
            j = 1 if t0 < g.TC else 0
            for u0 in range(0, nt, NT):
                a = xa[ti2 % 2]
                ti2 += 1
                S.dma("sp", a[:, :, :], xT_view(g, t0 + u0, NT), reads=[(g.xT_s, ti)], writes=[a])
                norm_mod(g, a, sq, rs, hT, pss, NT, lambda c: g.modA[:, 0, 1, c, j:j + 1], lambda c: g.modT[:, 0, 24 + c, j:j + 1])
                def gu(f):
                    pg = pgu[(f % 2) * 2]
                    pu = pgu[(f % 2) * 2 + 1]
                    for kc in range(8):
                        _mm(S, pg[:, 0:NT], wgu[:, kc, f * 128:(f + 1) * 128], hT[:, kc, :], kc == 0, kc == 7, [wgu, hT], [pg])
                    for kc in range(8):
                        _mm(S, pu[:, 0:NT], wgu[:, kc, 2816 + f * 128:2816 + (f + 1) * 128], hT[:, kc, :], kc == 0, kc == 7,
                            [wgu, hT], [pu])
                    s_ = sg[f % 3]
                    ac = act[f % 4]
                    _act(S, s_[:, :], pg[:, 0:NT], AF.Silu, [pg], [s_])
                    _tt(S, "dve", ac[:, :], s_[:, :], pu[:, 0:NT], ALU.mult, [s_, pu], [ac])
                    return ac

                acs = {0: gu(0), 1: gu(1)}
                for f in range(22):
                    if f + 2 < 22:
                        acs[f + 2] = gu(f + 2)
                    ac = acs.pop(f)
                    for dsl in range(8):
                        _mm(S, yac[dsl // 2][:, dsl % 2, :], wdn[:, f, dsl * 128:(dsl + 1) * 128], ac[:, :],
                            f == 0 and dsl % 2 == 0, f == 21 and dsl % 2 == 1, [wdn, ac], [(yac[dsl // 2], dsl % 2)])
                for dsl in range(8):
                    _stt(S, "dve", a[:, dsl, :], yac[dsl // 2][:, dsl % 2, :], g.modT[:, 0, 40 + dsl, j:j + 1], a[:, dsl, :],
                         ALU.mult, ALU.add, [(yac[dsl // 2], dsl % 2), (a, dsl)], [(a, dsl)])
                S.dma("pool", xT_view(g, t0 + u0, NT), a[:, :, :], reads=[a], writes=[(g.xT_s, ti)])
        S.emit()


def phase_l1_p1(g):
    S = g.S
    TT = g.TT
    g.dqT_s = S.dram("dqT_s", [512, TT], BF16)
    g.dkT_s = S.dram("dkT_s", [512, TT], BF16)
    g.gqT_s = S.dram("gqT_s", [512, TT], BF16)
    g.gkT_s = S.dram("gkT_s", [128, TT], BF16)
    g.dv_s = S.dram("dv_s", [TT, 4, 129], BF16)
    g.gv_s = S.dram("gv_s", [TT, 2, 65], BF16)
    with ExitStack() as ph:
        with ExitStack() as ph2:
            stg = [S.sb("l1stg%d" % i, [128, 8, 512], F32, ph2, side="right") for i in range(2)]
            w = load_w(g, ph, "w_in1", g.od_w_in, g.od_w_in.t[:, :], 1024, 2304, stg)
            S.barrier()
            S.emit()
        wr = S.sb("w_in1r", [128, 8, 2304], BF16, ph)
        view = lambda b: b[:, :, :].rearrange("p k (h e) -> p k h e", e=64)
        make_rot(g, wr, w, view, 0, 64)
        gc = S.sb("l1gc", [128, 4], F32, ph)
        S.dma("sp", gc[:, :], g.gcols[:, :], reads=[g.gcols], writes=[gc])
        bd = S.sb("l1bd", [128, 128], F32, ph)
        _ms(S, "pool", bd[:, :], 0.0, [bd])
        _ms(S, "pool", bd[0:64, 0:64], 1.0, [bd])
        _ms(S, "pool", bd[64:128, 64:128], 1.0, [bd])
        xa = [S.sb("l1xa%d" % i, [128, 8, 512], F32, ph) for i in range(2)]
        sq = S.sb("l1sq", [128, 8, 512], F32, ph)
        rs = S.sb("l1rs", [128, 512], F32, ph)
        hT = S.sb("l1hT", [128, 8, 512], BF16, ph)
        cq = S.sb("l1cq", [128, 512], F32, ph)
        sq_ = S.sb("l1sq_", [128, 512], F32, ph)
        ck = S.sb("l1ck", [128, 512], F32, ph)
        sk = S.sb("l1sk", [128, 512], F32, ph)
        t1 = [S.sb("l1t1%d" % i, [128, 512], F32, ph) for i in range(2)]
        t2 = [S.sb("l1t2%d" % i, [128, 512], F32, ph) for i in range(2)]
        t3 = S.sb("l1t3", [128, 512], F32, ph)
        ob = [S.sb("l1ob%d" % i, [128, 512], BF16, ph) for i in range(3)]
        dva = S.sb("l1dva", [128, 4, 4, 129], BF16, ph)
        gva = S.sb("l1gva", [128, 4, 2, 65], BF16, ph)
        _ms(S, "pool", dva[:, :, :, :], 1.0, [dva])
        _ms(S, "pool", gva[:, :, :, :], 1.0, [gva])
        pss = S.ps("l1pss", [128, 512], F32, ph)
        pp = [S.ps("l1pp%d" % i, [128, 512], F32, ph) for i in range(6)]
        pi = [0]
        oi = [0]

        def nextp():
            p = pp[pi[0] % 6]
            pi[0] += 1
            return p

        for ti, (t0, nt) in enumerate(g.tiles):
            j = 1 if t0 < g.TC else 0
            ns = nt // 128
            a = xa[ti % 2]
            S.dma("sp", a[:, :, 0:nt], xT_view(g, t0, nt), reads=[(g.xT_s, ti)], writes=[a])
            S.dma("sp", cq[:, 0:nt], g.cosq64[:, t0:t0 + nt], reads=[g.cosq64], writes=[cq])
            S.dma("sp", sq_[:, 0:nt], g.sinq64[:, t0:t0 + nt], reads=[g.sinq64], writes=[sq_])
            S.dma("sp", ck[:, 0:nt], g.cos64[:, t0:t0 + nt], reads=[g.cos64], writes=[ck])
            S.dma("sp", sk[:, 0:nt], g.sin64[:, t0:t0 + nt], reads=[g.sin64], writes=[sk])
            norm_mod(g, a, sq, rs, hT, pss, nt, lambda c: g.modA[:, 1, 0, c, j:j + 1], lambda c: g.modT[:, 1, c, j:j + 1])

            def proj(wt, col0):
                p = nextp()
                for kc in range(8):
                    _mm(S, p[:, 0:nt], wt[:, kc, col0:col0 + 128], hT[:, kc, 0:nt], kc == 0, kc == 7, [wt, hT], [p])
                return p

            def roped(col0, cos_t, sin_t, dst, row0, gcol=None, grcol=None):
                p1 = proj(w, col0)
                p2 = proj(wr, col0)
                k = oi[0]
                oi[0] += 1
                a1, a2, o = t1[k % 2], t2[k % 2], ob[k % 3]
                if gcol is None:
                    _tt(S, "dve", a1[:, 0:nt], p1[:, 0:nt], cos_t[:, 0:nt], ALU.mult, [p1, cos_t], [a1])
                    _tt(S, "dve", a2[:, 0:nt], p2[:, 0:nt], sin_t[:, 0:nt], ALU.mult, [p2, sin_t], [a2])
                    _tt(S, "pool", o[:, 0:nt], a1[:, 0:nt], a2[:, 0:nt], ALU.add, [a1, a2], [o])
                else:
                    _act(S, t3[:, 0:nt], p1[:, 0:nt], AF.Square, [p1], [t3])
                    _mm(S, pss[:, 0:nt], bd[:, :], t3[:, 0:nt], True, True, [bd, t3], [pss])
                    rstd_from_ss(S, "dve", t3[:, 0:nt], pss[:, 0:nt], 64, [pss], [t3])
                    _stt(S, "dve", a1[:, 0:nt], p1[:, 0:nt], gcol, cos_t[:, 0:nt], ALU.mult, ALU.mult, [p1, cos_t, gc], [a1])
                    _stt(S, "dve", a2[:, 0:nt], p2[:, 0:nt], grcol, sin_t[:, 0:nt], ALU.mult, ALU.mult, [p2, sin_t, gc], [a2])
                    _tt(S, "pool", a1[:, 0:nt], a1[:, 0:nt], a2[:, 0:nt], ALU.add, [a1, a2], [a1])
                    _tt(S, "dve", o[:, 0:nt], a1[:, 0:nt], t3[:, 0:nt], ALU.mult, [a1, t3], [o])
                S.dma("pool", dst.t[row0:row0 + 128, t0:t0 + nt], o[:, 0:nt], reads=[o], writes=[(dst, (row0, ti))])

            for m in range(4):
                if j == 0:
                    roped(m * 128, cq, sq_, g.dqT_s, m * 128)
                roped(512 + m * 128, ck, sk, g.dkT_s, m * 128)
            if j == 0:
                for m in range(4):
                    roped(1536 + m * 128, cq, sq_, g.gqT_s, m * 128, gc[:, 0:1], gc[:, 1:2])
            roped(2048, ck, sk, g.gkT_s, 0, gc[:, 2:3], gc[:, 3:4])
            for s in range(ns):
                p = nextp()
                for kc in range(8):
                    _mm(S, p[:, :], hT[:, kc, s * 128:(s + 1) * 128], w[:, kc, 1024:1536], kc == 0, kc == 7, [hT, w], [p])
                _cp(S, "act", dva[:, s, :, 0:128], p[:, :].rearrange("p (h e) -> p h e", e=128), [p], [(dva, s)])
                p = nextp()
                for kc in range(8):
                    _mm(S, p[:, 0:128], hT[:, kc, s * 128:(s + 1) * 128], w[:, kc, 2176:2304], kc == 0, kc == 7, [hT, w], [p])
                _cp(S, "dve", gva[:, s, :, 0:64], p[:, 0:128].rearrange("p (h e) -> p h e", e=64), [p], [(gva, s)])
            S.dma("pool", g.dv_s.t[t0:t0 + nt, :, :].rearrange("(s p) h e -> p s h e", p=128), dva[:, 0:ns, :, :],
                  reads=[dva], writes=[(g.dv_s, ti)])
            S.dma("pool", g.gv_s.t[t0:t0 + nt, :, :].rearrange("(s p) h e -> p s h e", p=128), gva[:, 0:ns, :, :],
                  reads=[gva], writes=[(g.gv_s, ti)])
        S.emit()


def attn_core2(g, KT, V, QT, d, dva, kbs, accf, psc, pT, nq, cnt):
    S = g.S
    nkb = len(kbs)
    LA = len(psc) - 1
    slots = []

    def score(i):
        kb = kbs[i]
        sc = psc[cnt[0] % len(psc)]
        p = pT[cnt[0] % len(pT)]
        cnt[0] += 1
        _mm(S, sc[:, 0:nq], KT[0:d, kb * 128:(kb + 1) * 128], QT[0:d, 0:nq], True, True, [KT, QT], [sc])
        _act(S, p[:, 0:nq], sc[:, 0:nq], AF.Exp, [sc], [p])
        slots.append(p)

    for i in range(min(LA, nkb)):
        score(i)
    for i, kb in enumerate(kbs):
        if i + LA < nkb:
            score(i + LA)
        p = slots[i]
        for qs in range(nq // 128):
            b, ap, first, last = accf(qs)
            _mm(S, ap, p[:, qs * 128:(qs + 1) * 128], V[:, kb, 0:dva], i == 0 and first, i == nkb - 1 and last, [p, V], [b])


def phase_l1_attn(g):
    S = g.S
    TT = g.TT
    NKB = TT // 128
    li = g.lambda_init
    with ExitStack() as ph:
        KT = [S.sb("aKT%d" % i, [128, TT], BF16, ph) for i in range(2)]
        V = S.sb("aV", [128, NKB, 129], BF16, ph)
        QT = [S.sb("aQT%d" % i, [128, 512], BF16, ph) for i in range(2)]
        for b_ in KT + QT:
            _ms(S, "pool", b_[64:128, :], 0.0, [(b_, "pad")])
        pT = [S.sb("apT%d" % i, [128, 512], BF16, ph) for i in range(4)]
        Pacc = [(S.sb("aPaccD%d" % i, [128, 512], F32, ph), S.sb("aPaccP%d" % i, [128, 512], F32, ph)) for i in range(4)]
        oS = [S.sb("aoS%d" % i, [65, 512], F32, ph) for i in range(2)]
        rec = [S.sb("arec%d" % i, [128, 512], F32, ph) for i in range(2)]
        t1 = S.sb("at1", [128, 512], F32, ph)
        t2 = S.sb("at2", [128, 512], F32, ph)
        od = S.sb("aod", [128, 512], F32, ph)
        o = [S.sb("ao%d" % i, [128, 512], BF16, ph) for i in range(2)]
        lam = S.sb("alam", [128, 4], F32, ph)
        dl = S.sb("adl", [128, 4, 64], F32, ph)
        dn = S.sb("adn", [128, 1], F32, ph)
        tmp = S.sb("atmp", [128, 64], F32, ph)
        S.dma("sp", dl[:, :, :], g.dlam[:, :, :], reads=[g.dlam], writes=[dl])
        S.dma("sp", dn[:, :], g.dncol[:, :], reads=[g.dncol], writes=[dn])
        _ts(S, "dve", dn[:, :], dn[:, :], 1.0 - li, None, ALU.mult, None, [dn], [dn])
        for kk in range(2):
            _tt(S, "dve", tmp[:, 0:64], dl[:, 2 * kk, :], dl[:, 2 * kk + 1, :], ALU.mult, [dl], [tmp])
            S.op("dve", lambda e, kk=kk: e.tensor_reduce(out=lam[:, kk:kk + 1], in_=tmp[:, 0:64], axis=AX.X, op=ALU.add), reads=[tmp], writes=[lam])
        _act(S, lam[:, 0:2], lam[:, 0:2], AF.Exp, [lam], [lam])
        _tt(S, "dve", lam[:, 2:3], lam[:, 0:1], lam[:, 1:2], ALU.subtract, [lam], [lam])
        _ts(S, "dve", lam[:, 3:4], lam[:, 2:3], li, None, ALU.add, None, [lam], [lam])
        cnt = [0]
        qi = 0
        lat_tiles = [(ti, t0, nt) for ti, (t0, nt) in enumerate(g.tiles) if t0 >= g.TC]
        phd = ExitStack()
        psc = [S.ps("apsc%d" % i, [128, 512], F32, phd) for i in range(3)]
        acc = [S.ps("aacc%d" % i, [128, 512], F32, phd) for i in range(4)]
        psm = S.ps("apsm", [128, 512], F32, phd)
        for h in range(4):
            S.dma("sp", V[:, :, :], g.dv_s.t[:, h, :].rearrange("(n p) e -> p n e", p=128), reads=[g.dv_s], writes=[V])
            for m in range(2):
                S.dma("sp", KT[m][0:64, :], g.dkT_s.t[(2 * h + m) * 64:(2 * h + m + 1) * 64, :], reads=[g.dkT_s], writes=[(KT[m], "d")])
            for (ti, t0, nt) in lat_tiles:
                par = qi % 2
                qi += 1
                for m in range(2):
                    q = QT[m]
                    S.dma("sp", q[0:64, 0:nt], g.dqT_s.t[(2 * h + m) * 64:(2 * h + m + 1) * 64, t0:t0 + nt], reads=[g.dqT_s], writes=[(q, "d")])
                    attn_core3(g, KT[m], V, q, 128, 128, list(range(NKB)), acc[2 * par + m], Pacc[2 * par + m], psc, pT[0:3], nt, cnt)
                a1, a2 = acc[2 * par], acc[2 * par + 1]
                for m in range(2):
                    pd, pp_ = Pacc[2 * par + m]
                    _mm(S, psm[:, 0:nt], g.onesF[:, :], pd[:, 0:nt], True, False, [g.onesF, pd], [psm])
                    _mm(S, psm[:, 0:nt], g.onesF[:, :], pp_[:, 0:nt], False, True, [g.onesF, pp_], [psm])
                    S.op("dve", lambda e, m=m, nt=nt: e.reciprocal(out=rec[m][:, 0:nt], in_=psm[:, 0:nt]), reads=[psm], writes=[rec[m]])
                _tt(S, "dve", t1[:, 0:nt], a1[:, 0:nt], rec[0][:, 0:nt], ALU.mult, [a1, rec[0]], [t1])
                _stt(S, "dve", t2[:, 0:nt], a2[:, 0:nt], lam[:, 3:4], rec[1][:, 0:nt], ALU.mult, ALU.mult, [a2, lam, rec[1]], [t2])
                _tt(S, "pool", od[:, 0:nt], t1[:, 0:nt], t2[:, 0:nt], ALU.subtract, [t1, t2], [od])
                _tt(S, "pool", t1[:, 0:nt], od[:, 0:nt], od[:, 0:nt], ALU.mult, [od], [t1])
                _mm(S, psm[:, 0:nt], g.onesF[:, :], t1[:, 0:nt], True, True, [g.onesF, t1], [psm])
                rstd_from_ss(S, "dve", t2[:, 0:nt], psm[:, 0:nt], 128, [psm], [t2])
                oo = o[par]
                _stt(S, "dve", oo[:, 0:nt], od[:, 0:nt], dn[:, 0:1], t2[:, 0:nt], ALU.mult, ALU.mult, [od, dn, t2], [oo])
                S.dma("pool", g.mixT_s.t[h * 128:(h + 1) * 128, t0:t0 + nt], oo[:, 0:nt], reads=[oo], writes=[(g.mixT_s, (h, ti))])
        S.barrier()
        S.emit()
        phd.close()
        psc = [S.ps("bpsc%d" % i, [128, 512], F32, ph) for i in range(4)]
        acc = [S.ps("bacc%d" % i, [128, 512], F32, ph) for i in range(2)]
        psm = S.ps("bpsm", [128, 512], F32, ph)
        for kvh in range(2):
            S.dma("sp", V[:, :, 0:65], g.gv_s.t[:, kvh, :].rearrange("(n p) e -> p n e", p=128), reads=[g.gv_s], writes=[V])
            S.dma("sp", KT[0][0:64, :], g.gkT_s.t[kvh * 64:(kvh + 1) * 64, :], reads=[g.gkT_s], writes=[(KT[0], "d")])
            for grp in range(4):
                hd = kvh * 4 + grp
                for (ti, t0, nt) in lat_tiles:
                    par = qi % 2
                    qi += 1
                    q = QT[par]
                    S.dma("sp", q[0:64, 0:nt], g.gqT_s.t[hd * 64:(hd + 1) * 64, t0:t0 + nt], reads=[g.gqT_s], writes=[(q, "d")])
                    attn_core3(g, KT[0], V, q, 128, 65, list(range(NKB)), acc[par], None, psc, pT, nt, cnt)
                    attn_simple_post(g, acc[par], psm, oS[par], rec[0], o[par], nt,
                                     g.mixT_s.t[512 + hd * 64:512 + (hd + 1) * 64, t0:t0 + nt], (g.mixT_s, (8 + hd, ti)))
        S.emit()


def phase_moe_a(g):
    S = g.S
    TT = g.TT
    g.h2T_s = S.dram("h2T_s", [1024, TT], BF16)
    g.gT_s = S.dram("gT_s", [8, TT], F32)
    with ExitStack() as ph:
        rw = S.sb("mrw", [128, 8, 8], F32, ph)
        S.dma("sp", rw[:, :, :], g.router.t[:, :].rearrange("(kc p) e -> p kc e", p=128), reads=[g.router], writes=[rw])
        xa = [S.sb("maxa%d" % i, [128, 8, 512], F32, ph) for i in range(2)]
        sq = S.sb("masq", [128, 8, 512], F32, ph)
        rs = S.sb("mars", [128, 512], F32, ph)
        hT = [S.sb("mahT%d" % i, [128, 8, 512], BF16, ph) for i in range(2)]
        hF = S.sb("mahF", [128, 8, 512], F32, ph)
        lg = S.sb("malg", [128, 8], F32, ph)
        m8 = S.sb("mam8", [128, 8], F32, ph)
        sm = S.sb("masm", [128, 4], F32, ph)
        mask = S.sb("mamask", [128, 8], F32, ph)
        ex = S.sb("maex", [128, 8], F32, ph)
        gt = S.sb("magt", [128, 8], F32, ph)
        gT = [S.sb("magT%d" % i, [8, 512], F32, ph) for i in range(2)]
        pss = S.ps("mapss", [128, 512], F32, ph)
        pl = [S.ps("mapl%d" % i, [128, 512], F32, ph) for i in range(2)]
        pt = [S.ps("mapt%d" % i, [128, 512], F32, ph) for i in range(2)]
        k = 0
        for ti, (t0, nt) in enumerate(g.tiles):
            if t0 < g.TC:
                continue
            ns = nt // 128
            a = xa[ti % 2]
            h = hT[ti % 2]
            gTt = gT[ti % 2]
            S.dma("sp", a[:, :, 0:nt], xT_view(g, t0, nt), reads=[(g.xT_s, ti)], writes=[a])
            norm_mod(g, a, sq, rs, h, pss, nt, lambda c: g.modA[:, 1, 1, c, 0:1], lambda c: g.modT[:, 1, 24 + c, 0:1])
            S.dma("pool", g.h2T_s.t[:, t0:t0 + nt].rearrange("(c p) t -> p c t", p=128), h[:, :, 0:nt], reads=[h],
                  writes=[(g.h2T_s, ti)])
            for c in range(8):
                _ts(S, "dve", hF[:, c, 0:nt], sq[:, c, 0:nt], g.modT[:, 1, 24 + c, 0:1], None, ALU.add, None, [(sq, c)], [(hF, c)])
            for s in range(ns):
                p = pl[k % 2]
                p2 = pt[k % 2]
                k += 1
                for kc in range(8):
                    _mm(S, p[:, 0:8], hF[:, kc, s * 128:(s + 1) * 128], rw[:, kc, :], kc == 0, kc == 7, [hF, rw], [p])
                _cp(S, "dve", lg[:, :], p[:, 0:8], [p], [lg])
                S.op("dve", lambda e: e.max(out=m8[:, :], in_=lg[:, :]), reads=[lg], writes=[m8])
                _ts(S, "dve", sm[:, 0:1], m8[:, 0:1], -1.0, None, ALU.mult, None, [m8], [sm])
                _ts(S, "dve", mask[:, :], lg[:, :], m8[:, 1:2], None, ALU.is_ge, None, [lg, m8], [mask])
                _act(S, ex[:, :], lg[:, :], AF.Exp, [lg, sm], [ex], bias=sm[:, 0:1])
                _act(S, sm[:, 1:2], m8[:, 1:2], AF.Exp, [m8, sm], [sm], bias=sm[:, 0:1])
                _ts(S, "dve", sm[:, 2:3], sm[:, 1:2], 1.0, None, ALU.add, None, [sm], [sm])
                S.op("dve", lambda e: e.reciprocal(out=sm[:, 3:4], in_=sm[:, 2:3]), reads=[sm], writes=[sm])
                _stt(S, "dve", gt[:, :], ex[:, :], sm[:, 3:4], mask[:, :], ALU.mult, ALU.mult, [ex, sm, mask], [gt])
                _tr(S, p2[0:8, 0:128], gt[:, :], g.identF[:, :], [gt, g.identF], [p2])
                _cp(S, "dve", gTt[:, s * 128:(s + 1) * 128], p2[0:8, 0:128], [p2], [gTt])
            S.dma("pool", g.gT_s.t[:, t0:t0 + nt], gTt[:, 0:nt], reads=[gTt], writes=[(g.gT_s, ti)])
        S.emit()


def phase_moe_b(g):
    S = g.S
    NT = 256
    FH = 1792
    NF = FH // 128
    with ExitStack() as ph:
        stg = [S.sb("mbstg%d" % i, [128, 8, 512], F32, ph, side="right") for i in range(2)]
        sel = S.sb("mbsel", [8, 8, 128], F32, ph)
        for e in range(8):
            _ts(S, "dve", sel[:, e, :], g.onesF[0:8, :], g.identF[0:8, e:e + 1], None, ALU.mult, None, [g.onesF, g.identF], [(sel, e)])
        wg = S.sb("mbwg", [128, 8, FH], BF16, ph)
        wu = S.sb("mbwu", [128, 8, FH], BF16, ph)
        wd = S.sb("mbwd", [128, NF, 1024], BF16, ph)
        xa = [S.sb("mbxa%d" % i, [128, 8, NT], F32, ph) for i in range(2)]
        hT = [S.sb("mbhT%d" % i, [128, 8, NT], BF16, ph) for i in range(2)]
        gTt = [S.sb("mbgT%d" % i, [8, NT], F32, ph) for i in range(2)]
        gbs = S.sb("mbgbs", [128, NT], F32, ph)
        sg = [S.sb("mbsg%d" % i, [128, NT], F32, ph) for i in range(3)]
        tg = [S.sb("mbtg%d" % i, [128, NT], F32, ph) for i in range(3)]
        act = [S.sb("mbact%d" % i, [128, NT], BF16, ph) for i in range(4)]
        pgu = [S.ps("mbpgu%d" % i, [128, 512], F32, ph) for i in range(4)]
        yac = [S.ps("mby%d" % i, [128, 2, NT], F32, ph) for i in range(4)]
        lat = [(ti, t0, nt) for ti, (t0, nt) in enumerate(g.tiles) if t0 >= g.TC]
        it = 0
        for e in range(8):
            for fh in range(2):
                def ld(dst, src_ap, K, N):
                    kc = K // 128
                    v = src_ap.rearrange("(kc p) n -> p kc n", p=128)
                    for k0 in range(0, kc, 8):
                        k1 = min(kc, k0 + 8)
                        for n0 in range(0, N, 512):
                            n1 = min(N, n0 + 512)
                            s = stg[g.stg_i % 2]
                            g.stg_i += 1
                            S.dma("sp", s[:, 0:k1 - k0, 0:n1 - n0], v[:, k0:k1, n0:n1], reads=[g.moe_gu, g.moe_dn], writes=[s])
                            _cp(S, ("dve", "pool", "act")[g.stg_i % 3], dst[:, k0:k1, n0:n1], s[:, 0:k1 - k0, 0:n1 - n0], [s], [dst])
                ld(wg, g.moe_gu.t[e, :, fh * FH:(fh + 1) * FH], 1024, FH)
                ld(wu, g.moe_gu.t[e, :, 3584 + fh * FH:3584 + (fh + 1) * FH], 1024, FH)
                ld(wd, g.moe_dn.t[e, fh * FH:(fh + 1) * FH, :], FH, 1024)
                for (ti, t0, nt) in lat:
                    for u0 in range(0, nt, NT):
                        a, h, gt_ = xa[it % 2], hT[it % 2], gTt[it % 2]
                        it += 1
                        S.dma("sp", h[:, :, :], g.h2T_s.t[:, t0 + u0:t0 + u0 + NT].rearrange("(c p) t -> p c t", p=128),
                              reads=[g.h2T_s], writes=[h])
                        S.dma("sp", gt_[:, :], g.gT_s.t[:, t0 + u0:t0 + u0 + NT], reads=[g.gT_s], writes=[gt_])
                        S.dma("sp", a[:, :, :], xT_view(g, t0 + u0, NT), reads=[(g.xT_s, (ti, u0))], writes=[a])
                        _mm(S, pgu[0][:, 0:NT], sel[:, e, :], gt_[:, :], True, True, [(sel, e), gt_], [pgu[0]])
                        _cp(S, "dve", gbs[:, :], pgu[0][:, 0:NT], [pgu[0]], [gbs])
                        def gu(f, h=h):
                            pg = pgu[(f % 2) * 2]
                            pu = pgu[(f % 2) * 2 + 1]
                            for kc in range(8):
                                _mm(S, pg[:, 0:NT], wg[:, kc, f * 128:(f + 1) * 128], h[:, kc, :], kc == 0, kc == 7, [wg, h], [pg])
                            for kc in range(8):
                                _mm(S, pu[:, 0:NT], wu[:, kc, f * 128:(f + 1) * 128], h[:, kc, :], kc == 0, kc == 7, [wu, h], [pu])
                            s_, t_, ac = sg[f % 3], tg[f % 3], act[f % 4]
                            _act(S, s_[:, :], pg[:, 0:NT], AF.Silu, [pg], [s_])
                            _tt(S, "dve", t_[:, :], s_[:, :], pu[:, 0:NT], ALU.mult, [s_, pu], [t_])
                            _tt(S, "pool", ac[:, :], t_[:, :], gbs[:, :], ALU.mult, [t_, gbs], [ac])
                            return ac

                        acs = {0: gu(0), 1: gu(1)}
                        for f in range(NF):
                            if f + 2 < NF:
                                acs[f + 2] = gu(f + 2)
                            ac = acs.pop(f)
                            for dsl in range(8):
                                _mm(S, yac[dsl // 2][:, dsl % 2, :], wd[:, f, dsl * 128:(dsl + 1) * 128], ac[:, :],
                                    f == 0 and dsl % 2 == 0, f == NF - 1 and dsl % 2 == 1, [wd, ac], [yac[dsl // 2]])
                        for dsl in range(8):
                            _stt(S, "dve", a[:, dsl, :], yac[dsl // 2][:, dsl % 2, :], g.modT[:, 1, 40 + dsl, 0:1], a[:, dsl, :],
                                 ALU.mult, ALU.add, [yac[dsl // 2], (a, dsl)], [(a, dsl)])
                        S.dma("pool", xT_view(g, t0 + u0, NT), a[:, :, :], reads=[a], writes=[(g.xT_s, (ti, u0))])
                S.barrier()
        S.emit()


def phase_gdn_prep(g):
    S = g.S
    TT = g.TT
    g.qkvn_s = S.dram("qkvn_s", [12, 128, TT], F32)
    with ExitStack() as ph:
        cw = S.sb("gpcw", [128, 12, 5], F32, ph)
        S.dma("sp", cw[:, :, :], g.conv_col[:, :, :], reads=[g.conv_col], writes=[cw])
        bd = S.sb("gpbd", [128, 128], F32, ph)
        _ms(S, "pool", bd[:, :], 0.0, [bd])
        _ms(S, "pool", bd[0:64, 0:64], 1.0, [bd])
        _ms(S, "pool", bd[64:128, 64:128], 1.0, [bd])
        xh = [S.sb("gpxh%d" % i, [128, 12, 516], F32, ph) for i in range(2)]
        acc = [S.sb("gpacc%d" % i, [128, 512], F32, ph) for i in range(3)]
        y = [S.sb("gpy%d" % i, [128, 512], F32, ph) for i in range(3)]
        sq = [S.sb("gpsq%d" % i, [128, 512], F32, ph) for i in range(2)]
        rn = [S.sb("gprn%d" % i, [128, 512], F32, ph) for i in range(2)]
        pss = [S.ps("gppss%d" % i, [128, 512], F32, ph) for i in range(2)]
        k = 0
        for ti, (t0, nt) in enumerate(g.tiles):
            x = xh[ti % 2]
            lo = 0 if t0 < g.TC else g.TC
            hi = g.TC if t0 < g.TC else TT
            a0 = max(lo, t0 - 2)
            a1 = min(hi, t0 + nt + 2)
            if a0 > t0 - 2:
                _ms(S, "pool", x[:, :, 0:2], 0.0, [x])
            if a1 < t0 + nt + 2:
                _ms(S, "pool", x[:, :, nt + 2:nt + 4], 0.0, [x])
            S.dma("sp", x[:, :, a0 - (t0 - 2):a1 - (t0 - 2)], g.gq_s.t[:, :, a0:a1].rearrange("c p t -> p c t"),
                  reads=[g.gq_s], writes=[x])
            for c in range(12):
                ac, yy = acc[k % 3], y[k % 3]
                s_, r_ = sq[k % 2], rn[k % 2]
                ps = pss[k % 2]
                k += 1
                _ts(S, "dve", ac[:, 0:nt], x[:, c, 0:nt], cw[:, c, 0:1], None, ALU.mult, None, [x, cw], [ac])
                for j in range(1, 5):
                    _stt(S, "dve", ac[:, 0:nt], x[:, c, j:j + nt], cw[:, c, j:j + 1], ac[:, 0:nt], ALU.mult, ALU.add, [x, cw, ac], [ac])
                _act(S, yy[:, 0:nt], ac[:, 0:nt], AF.Silu, [ac], [yy])
                if c < 8:
                    _tt(S, "pool", s_[:, 0:nt], yy[:, 0:nt], yy[:, 0:nt], ALU.mult, [yy], [s_])
                    _mm(S, ps[:, 0:nt], bd[:, :], s_[:, 0:nt], True, True, [bd, s_], [ps])
                    rstd_from_ss(S, "dve", r_[:, 0:nt], ps[:, 0:nt], 1, [ps], [r_])
                    if c < 4:
                        _stt(S, "dve", yy[:, 0:nt], yy[:, 0:nt], 0.125, r_[:, 0:nt], ALU.mult, ALU.mult, [yy, r_], [yy])
                    else:
                        _tt(S, "dve", yy[:, 0:nt], yy[:, 0:nt], r_[:, 0:nt], ALU.mult, [yy, r_], [yy])
                S.dma("pool", g.qkvn_s.t[c, :, t0:t0 + nt], yy[:, 0:nt], reads=[yy], writes=[(g.qkvn_s, (c, ti))])
        S.emit()


def phase_gdn(g):
    S = g.S
    TT = g.TT
    C = 64
    g.o_s = S.dram("gdn_o_s", [2, TT, 512], F32)
    with ExitStack() as ph:
        def sb(name, shape, dt=F32):
            return S.sb("gd_" + name, shape, dt, ph)
        alr = sb("alr", [64, 16])
        dtb = sb("dtb", [64, 16])
        S.dma("sp", alr[:, :], g.alog_rep[0:64, :], reads=[g.alog_rep], writes=[alr])
        S.dma("sp", dtb[:, :], g.dtb_rep[0:64, :], reads=[g.dtb_rep], writes=[dtb])
        nA = sb("nA", [64, 16])
        _act(S, nA[:, :], alr[:, :], AF.Exp, [alr], [nA])
        _ts(S, "dve", nA[:, :], nA[:, :], -1.0, None, ALU.mult, None, [nA], [nA])
        def mask(name, op, sgn=1):
            m = sb(name, [64, 8, 64])
            _ms(S, "pool", m[:, :, :], 1.0, [m])
            S.op("pool", lambda e: e.affine_select(out=m[:, :, :], in_=m[:, :, :], pattern=[[0, 8], [-sgn, 64]],
                                                   compare_op=op, fill=0.0, base=0, channel_multiplier=sgn), reads=[m], writes=[m])
            return m
        mI = [mask("mIf", ALU.is_ge), mask("mIb", ALU.is_ge, -1)]
        mS = [mask("mSf", ALU.is_gt), mask("mSb", ALU.is_gt, -1)]
        I8 = mask("I8", ALU.is_equal)
        Tri = [mI[1], mI[0]]
        St = [sb("S%d" % d, [64, 8, 64]) for d in range(2)]
        for d in range(2):
            _ms(S, "pool", St[d][:, :, :], 0.0, [St[d]])
        nb = 4
        QK = [sb("QK%d" % i, [64, 16, C]) for i in range(nb)]
        Vf = [sb("Vf%d" % i, [64, 8, C]) for i in range(nb)]
        abT = [sb("abT%d" % i, [32, C]) for i in range(nb)]
        names = ["Ktm", "Vtm", "abm", "t16", "gg", "be", "gc", "egc", "gtot", "cd", "kdsc", "bsc", "nbe"]
        TD = [{}, {}]
        big = ["NG", "Mm", "E", "D", "Ds", "P0", "DT", "qkmT", "PT0", "Pa", "PTa", "Pb", "PTb", "TTa", "TTb", "bV", "bK", "U", "WT",
               "vnew", "o2", "od", "Kd", "P0b"]
        for dd in range(2):
            for n in names:
                TD[dd][n] = sb("%s_%d" % (n, dd), [64, 512] if n in ("Ktm", "Vtm") else [64, 32])
            for n in big:
                TD[dd][n] = sb("%s_%d" % (n, dd), [64, 8, 64],
                               F32)
        pb = [S.ps("gd_p%d" % i, [128, 512], F32, ph) for i in range(8)]
        pi = [0]

        def P():
            p = pb[pi[0] % 8]
            pi[0] += 1
            return p

        def v3(p):
            return p[0:64, :].rearrange("p (h e) -> p h e", e=64)

        ev = [0]

        def evac(out_ap, in_ap, R, W):
            ev[0] += 1
            _cp(S, "act" if ev[0] % 2 else "dve", out_ap, in_ap, R, W)

        def chunk(d, c0, it):
            T = TD[d]
            qk, vf, ab = QK[it % nb], Vf[it % nb], abT[it % nb]
            S.dma("sp", qk[:, :, :], g.qkvn_s.t[0:8, :, c0:c0 + C].rearrange("c (hh d) t -> d (c hh) t", d=64),
                  reads=[g.qkvn_s], writes=[qk])
            S.dma("sp", vf[:, :, :], g.qkvn_s.t[8:12, :, c0:c0 + C].rearrange("c (hh d) t -> d (c hh) t", d=64),
                  reads=[g.qkvn_s], writes=[vf])
            S.dma("sp", ab[:, :], g.ab_s.t[:, c0:c0 + C], reads=[g.ab_s], writes=[ab])
            Ktm, Vtm, abm = T["Ktm"], T["Vtm"], T["abm"]
            p = P()
            for h in range(8):
                _tr(S, p[0:64, h * 64:(h + 1) * 64], qk[:, 8 + h, :], g.identF[0:64, 0:64], [qk, g.identF], [p])
            evac(Ktm[:, :], p[0:64, :], [p], [Ktm])
            p = P()
            for h in range(8):
                _tr(S, p[0:64, h * 64:(h + 1) * 64], vf[:, h, :], g.identF[0:64, 0:64], [vf, g.identF], [p])
            evac(Vtm[:, :], p[0:64, :], [p], [Vtm])
            p = P()
            _tr(S, p[0:64, 0:32], ab[:, :], g.identF[0:32, 0:32], [ab, g.identF], [p])
            evac(abm[:, :], p[0:64, 0:32], [p], [abm])
            t16, gg, be, gc, egc, gtot, cd, kdsc, bsc, nbe = (T[n] for n in ("t16", "gg", "be", "gc", "egc", "gtot", "cd", "kdsc", "bsc", "nbe"))
            r8 = slice(d * 8, d * 8 + 8)
            _tt(S, "dve", t16[:, 0:8], abm[:, r8], dtb[:, r8], ALU.add, [abm, dtb], [t16])
            _act(S, t16[:, 0:8], t16[:, 0:8], AF.Exp, [t16], [t16])
            _act(S, t16[:, 0:8], t16[:, 0:8], AF.Ln, [t16], [t16], bias=1.0)
            _tt(S, "dve", gg[:, 0:8], t16[:, 0:8], nA[:, r8], ALU.mult, [t16, nA], [gg])
            _act(S, be[:, 0:8], abm[:, 16 + d * 8:24 + d * 8], AF.Sigmoid, [abm], [be])
            _ts(S, "dve", nbe[:, 0:8], be[:, 0:8], -1.0, None, ALU.mult, None, [be], [nbe])
            p = P()
            _mm(S, p[0:64, 0:8], Tri[d][:, 0, :], gg[:, 0:8], True, True, [Tri[d], gg], [p])
            evac(gc[:, 0:8], p[0:64, 0:8], [p], [gc])
            p = P()
            _mm(S, p[0:64, 0:8], g.onesF[0:64, 0:64], gg[:, 0:8], True, True, [g.onesF, gg], [p])
            evac(gtot[:, 0:8], p[0:64, 0:8], [p], [gtot])
            _act(S, egc[:, 0:8], gc[:, 0:8], AF.Exp, [gc], [egc])
            _act(S, cd[:, 0:8], gtot[:, 0:8], AF.Exp, [gtot], [cd])
            _tt(S, "dve", kdsc[:, 0:8], gtot[:, 0:8], gc[:, 0:8], ALU.subtract, [gtot, gc], [kdsc])
            _act(S, kdsc[:, 0:8], kdsc[:, 0:8], AF.Exp, [kdsc], [kdsc])
            _tt(S, "dve", bsc[:, 0:8], be[:, 0:8], egc[:, 0:8], ALU.mult, [be, egc], [bsc])
            NG, Mm, E, Dm, Ds = T["NG"], T["Mm"], T["E"], T["D"], T["Ds"]
            for h in range(8):
                _ts(S, "dve", NG[:, h, :], g.onesF[0:64, 0:64], gg[:, h:h + 1], -1.0, ALU.mult, ALU.mult, [g.onesF, gg], [(NG, h)])
            p = P()
            for h in range(8):
                _mm(S, v3(p)[:, h, :], NG[:, h, :], Tri[d][:, 0, :], True, True, [(NG, h), Tri[d]], [p])
            for h in range(8):
                _ts(S, "dve", Mm[:, h, :], v3(p)[:, h, :], gc[:, h:h + 1], 0.0, ALU.add, ALU.min, [p, gc], [(Mm, h)])
            _act(S, E[:, :, :], Mm[:, :, :], AF.Exp, [Mm], [E])
            _tt(S, "pool", Dm[:, :, :], E[:, :, :], mI[d][:, :, :], ALU.mult, [E, mI[d]], [Dm])
            _tt(S, "pool", Ds[:, :, :], E[:, :, :], mS[d][:, :, :], ALU.mult, [E, mS[d]], [Ds])
            P0, DT, qkmT, PT0 = T["P0"], T["DT"], T["qkmT"], T["PT0"]
            p = P()
            for h in range(8):
                _mm(S, v3(p)[:, h, :], qk[:, 8 + h, :], qk[:, 8 + h, :], True, True, [qk], [p])
            for h in range(8):
                _stt(S, "dve", P0[:, h, :], v3(p)[:, h, :], nbe[:, h:h + 1], Ds[:, h, :], ALU.mult, ALU.mult, [p, nbe, Ds], [(P0, h)])
            p = P()
            for h in range(8):
                _tr(S, v3(p)[:, h, :], Dm[:, h, :], g.identF[0:64, 0:64], [Dm, g.identF], [p])
            evac(DT[:, :, :], v3(p), [p], [DT])
            p = P()
            for h in range(8):
                _mm(S, v3(p)[:, h, :], qk[:, 8 + h, :], qk[:, h, :], True, True, [qk], [p])
            _tt(S, "dve", qkmT[:, :, :], v3(p), DT[:, :, :], ALU.mult, [p, DT], [qkmT])
            p = P()
            for h in range(8):
                _tr(S, v3(p)[:, h, :], P0[:, h, :], g.identF[0:64, 0:64], [(P0, h), g.identF], [p])
            evac(PT0[:, :, :], v3(p), [p], [PT0])
            TTa, TTb = T["TTa"], T["TTb"]
            _tt(S, "pool", TTa[:, :, :], PT0[:, :, :], I8[:, :, :], ALU.add, [PT0, I8], [TTa])
            Pk, PTk = P0, PT0
            cur, nxt = TTa, TTb
            alt = [(T["Pa"], T["PTa"]), (T["Pb"], T["PTb"])]
            for lv in range(1, 6):
                Pn, PTn = alt[lv % 2]
                p = P()
                for h in range(8):
                    _mm(S, v3(p)[:, h, :], PTk[:, h, :], Pk[:, h, :], True, True, [Pk, PTk], [p])
                evac(Pn[:, :, :], v3(p), [p], [Pn])
                if lv < 5:
                    p = P()
                    for h in range(8):
                        _mm(S, v3(p)[:, h, :], Pk[:, h, :], PTk[:, h, :], True, True, [Pk, PTk], [p])
                    evac(PTn[:, :, :], v3(p), [p], [PTn])
                p = P()
                for h in range(8):
                    _mm(S, v3(p)[:, h, :], Pn[:, h, :], cur[:, h, :], True, True, [Pn, cur], [p])
                _tt(S, "dve", nxt[:, :, :], v3(p), cur[:, :, :], ALU.add, [p, cur], [nxt])
                cur, nxt = nxt, cur
                Pk, PTk = Pn, PTn
            TT_ = cur
            bV, bK, U, WT = T["bV"], T["bK"], T["U"], T["WT"]
            Kv = Ktm[:, :].rearrange("p (h e) -> p h e", e=64)
            Vv = Vtm[:, :].rearrange("p (h e) -> p h e", e=64)
            for h in range(8):
                _ts(S, "dve", bV[:, h, :], Vv[:, h, :], be[:, h:h + 1], None, ALU.mult, None, [Vtm, be], [(bV, h)])
                _ts(S, "pool", bK[:, h, :], Kv[:, h, :], bsc[:, h:h + 1], None, ALU.mult, None, [Ktm, bsc], [(bK, h)])
            p = P()
            for h in range(8):
                _mm(S, v3(p)[:, h, :], TT_[:, h, :], bV[:, h, :], True, True, [TT_, bV], [p])
            evac(U[:, :, :], v3(p), [p], [U])
            p = P()
            for h in range(8):
                _mm(S, v3(p)[:, h, :], bK[:, h, :], TT_[:, h, :], True, True, [TT_, bK], [p])
            evac(WT[:, :, :], v3(p), [p], [WT])
            Sd = St[d]
            vnew, o2, od, Kd = T["vnew"], T["o2"], T["od"], T["Kd"]
            for h in range(8):
                _ts(S, "pool", Kd[:, h, :], Kv[:, h, :], kdsc[:, h:h + 1], None, ALU.mult, None, [Ktm, kdsc], [(Kd, h)])
            pw = P()
            for h in range(8):
                _mm(S, v3(pw)[:, h, :], WT[:, h, :], Sd[:, h, :], True, True, [WT, Sd], [pw])
            pq = P()
            for h in range(8):
                _mm(S, v3(pq)[:, h, :], qk[:, h, :], Sd[:, h, :], True, True, [qk, Sd], [pq])
            _tt(S, "dve", vnew[:, :, :], U[:, :, :], v3(pw), ALU.subtract, [U, pw], [vnew])
            p2 = P()
            for h in range(8):
                _mm(S, v3(p2)[:, h, :], qkmT[:, h, :], vnew[:, h, :], True, True, [qkmT, vnew], [p2])
            evac(o2[:, :, :], v3(p2), [p2], [o2])
            for h in range(8):
                _stt(S, "dve", od[:, h, :], v3(pq)[:, h, :], egc[:, h:h + 1], o2[:, h, :], ALU.mult, ALU.add, [pq, egc, o2], [(od, h)])
            p3 = P()
            for h in range(8):
                _mm(S, v3(p3)[:, h, :], Kd[:, h, :], vnew[:, h, :], True, True, [Kd, vnew], [p3])
            for h in range(8):
                _stt(S, "dve", Sd[:, h, :], Sd[:, h, :], cd[:, h:h + 1], v3(p3)[:, h, :], ALU.mult, ALU.add, [Sd, cd, p3], [Sd])
            odf = od[:, :, :].rearrange("p h e -> p (h e)")
            S.dma("pool", g.o_s.t[d, c0:c0 + C, :], odf, reads=[od], writes=[(g.o_s, (d, c0))])

        nctx = g.TC // C
        nlat = g.TL // C
        it = 0
        orders = [list(range(nctx)) + [nctx + i for i in range(nlat)],
                  list(range(nctx - 1, -1, -1)) + [nctx + i for i in range(nlat - 1, -1, -1)]]
        for step in range(nctx + nlat):
            for d in range(2):
                chunk(d, orders[d][step] * C, it)
                it += 1
        S.barrier()
        S.emit()
    with ExitStack() as ph:
        onr = S.sb("gc_onr", [128, 8, 64], F32, ph)
        S.dma("sp", onr[:, :, :], g.onrm_rep[:, :, :], reads=[g.onrm_rep], writes=[onr])
        o0 = [S.sb("gc_o0%d" % i, [128, 8, 64], F32, ph) for i in range(2)]
        o1 = [S.sb("gc_o1%d" % i, [128, 8, 64], F32, ph) for i in range(2)]
        zt = [S.sb("gc_z%d" % i, [128, 8, 64], F32, ph) for i in range(2)]
        sqo = S.sb("gc_sq", [128, 8, 64], F32, ph)
        ss = S.sb("gc_ss", [128, 16], F32, ph)
        ob = [S.sb("gc_ob%d" % i, [128, 8, 64], BF16, ph) for i in range(2)]
        oT = [S.sb("gc_oT%d" % i, [128, 4, 128], BF16, ph) for i in range(2)]
        ptb = [S.ps("gc_ptb%d" % i, [128, 512], BF16, ph) for i in range(2)]
        for bi in range(TT // 128):
            r0 = bi * 128
            a, b, z, o_b, o_t, pt = o0[bi % 2], o1[bi % 2], zt[bi % 2], ob[bi % 2], oT[bi % 2], ptb[bi % 2]
            S.dma("sp", a[:, :, :], g.o_s.t[0, r0:r0 + 128, :].rearrange("p (h e) -> p h e", e=64), reads=[g.o_s], writes=[a])
            S.dma("sp", b[:, :, :], g.o_s.t[1, r0:r0 + 128, :].rearrange("p (h e) -> p h e", e=64), reads=[g.o_s], writes=[b])
            S.dma("sp", z[:, :, :], g.z_s.t[r0:r0 + 128, :].rearrange("p (h e) -> p h e", e=64), reads=[g.z_s], writes=[z])
            _tt(S, "pool", a[:, :, :], a[:, :, :], b[:, :, :], ALU.add, [a, b], [a])
            _tt(S, "pool", sqo[:, :, :], a[:, :, :], a[:, :, :], ALU.mult, [a], [sqo])
            S.op("dve", lambda e: e.tensor_reduce(out=ss[:, 0:8], in_=sqo[:, :, :], axis=AX.X, op=ALU.add), reads=[sqo], writes=[ss])
            rstd_from_ss(S, "dve", ss[:, 8:16], ss[:, 0:8], 64, [ss], [ss])
            _tt(S, "pool", z[:, :, :], z[:, :, :], onr[:, :, :], ALU.mult, [z, onr], [z])
            for h in range(8):
                _stt(S, "dve", o_b[:, h, :], a[:, h, :], ss[:, 8 + h:9 + h], z[:, h, :], ALU.mult, ALU.mult, [a, ss, z], [o_b])
            obf = o_b[:, :, :].rearrange("p h e -> p (h e)")
            for c in range(4):
                _tr(S, pt[:, c * 128:(c + 1) * 128], obf[:, c * 128:(c + 1) * 128], g.identB[:, :], [o_b, g.identB], [pt])
            _cp(S, "act", o_t[:, :, :], pt[:, :].rearrange("p (c t) -> p c t", t=128), [pt], [o_t])
            S.dma("pool", g.mixT_s.t[512:1024, r0:r0 + 128].rearrange("(c p) t -> p c t", p=128), o_t[:, :, :], reads=[o_t],
                  writes=[(g.mixT_s, ("g", bi))])
        S.emit()


def rope_tab(n, TL, TC):
    nf = n // 4
    inv = 1.0 / (10000.0 ** (np.arange(nf, dtype=np.float32) / nf))
    t = np.arange(TL)
    rows = (t // 64).astype(np.float32)
    cols = (t % 64).astype(np.float32)
    ang_r = rows[None, :] * inv[:, None]
    ang_c = cols[None, :] * inv[:, None]
    cos = np.concatenate([np.cos(ang_r), np.cos(ang_r), np.cos(ang_c), np.cos(ang_c)], 0)
    sin = np.concatenate([np.sin(ang_r), np.sin(ang_r), np.sin(ang_c), np.sin(ang_c)], 0)
    cos = np.concatenate([np.ones((n, TC), np.float32), cos.astype(np.float32)], 1)
    sin = np.concatenate([np.zeros((n, TC), np.float32), sin.astype(np.float32)], 1)
    return np.ascontiguousarray(cos), np.ascontiguousarray(sin)


def col(v, k):
    return np.ascontiguousarray(np.asarray(v, np.float32).reshape(k, 128).T)


def prep_core(inp, b, TL, TC):
    f = lambda a: np.ascontiguousarray(np.asarray(a, np.float32))
    TT = TL + TC
    d = {}
    d["x"] = f(inp["x"][b])
    d["ctx"] = f(inp["ctx"][b])
    d["ccol"] = np.ascontiguousarray(np.stack([col(inp["c"][b], 8), col(inp["c_ctx"], 8)], -1))
    d["mod_w"] = f(inp["mod_w"])
    d["modb"] = np.ascontiguousarray(f(inp["mod_b"]).reshape(2, 48, 128).transpose(2, 0, 1))
    d["normg"] = np.ascontiguousarray(f(inp["norm_g"]).reshape(2, 2, 8, 128).transpose(3, 0, 1, 2))
    d["fnorm"] = col(inp["final_norm"], 8)
    c32, s32 = rope_tab(32, TL, TC)
    sc = np.float32(96 ** -0.5)
    d["cosq96"] = np.ascontiguousarray(np.concatenate([np.full((64, TT), sc, np.float32), c32 * sc], 0))
    d["sinq96"] = np.ascontiguousarray(np.concatenate([np.zeros((64, TT), np.float32), s32 * sc], 0))
    d["cos32"], d["sin32"] = c32, s32
    d["ev_w_in"] = f(inp["ev_w_in"][0])
    d["ev_w_uq"] = f(inp["ev_mla_w_uq"][0])
    d["ev_w_ukv"] = f(inp["ev_mla_w_ukv"][0])
    d["qn_col"] = col(inp["ev_mla_q_norm"][0], 3)
    d["kvn_col"] = col(inp["ev_mla_kv_norm"][0], 2)
    d["ev_w_out"] = f(inp["ev_w_out"][0])
    d["ev_wgu"] = f(inp["ev_ffn_w_gu"][0])
    d["ev_wdn"] = f(inp["ev_ffn_w_down"][0])
    c64, s64 = rope_tab(64, TL, TC)
    c64 = np.ascontiguousarray(np.concatenate([c64, c64], 0)); s64 = np.ascontiguousarray(np.concatenate([s64, s64], 0))
    d["cos64"], d["sin64"] = c64, s64
    d["cosq64"], d["sinq64"] = c64 * np.float32(0.125), s64 * np.float32(0.125)
    d["od_w_in"] = f(inp["od_w_in"][0])
    d["od_w_out"] = f(inp["od_w_out"][0])
    d["router"] = f(inp["od_router_w"][0]); d["moe_gu"] = f(inp["od_moe_w_gu"][0]); d["moe_dn"] = f(inp["od_moe_w_down"][0])
    d["conv_col"] = np.ascontiguousarray(f(inp["ev_gdn_conv"][0]).reshape(5, 12, 128).transpose(2, 1, 0))
    d["alog_rep"] = np.ascontiguousarray(np.broadcast_to(f(inp["ev_gdn_a_log"][0]).reshape(1, 16), (128, 16)))
    d["dtb_rep"] = np.ascontiguousarray(np.broadcast_to(f(inp["ev_gdn_dt_bias"][0]).reshape(1, 16), (128, 16)))
    d["onrm_rep"] = np.ascontiguousarray(np.broadcast_to(f(inp["ev_gdn_out_norm"][0]).reshape(1, 1, 64), (128, 8, 64)))
    perm = np.concatenate([np.arange(16, 32), np.arange(0, 16), np.arange(48, 64), np.arange(32, 48)])
    gq = f(inp["od_gqa_q_norm"][0]); gk = f(inp["od_gqa_k_norm"][0])
    d["gcols"] = np.ascontiguousarray(np.stack([np.tile(gq, 2), np.tile(gq[perm], 2), np.tile(gk, 2), np.tile(gk[perm], 2)], -1))
    d["dlam"] = np.ascontiguousarray(np.broadcast_to(f(inp["od_diff_lambda"][0])[None], (128, 4, 64)))
    d["dncol"] = np.ascontiguousarray(f(inp["od_diff_norm"][0]).reshape(128, 1))
    return d


_NC_CACHE = {}


def kernel(**inputs):
    TL, TC = 8192, 256
    inp = {k: np.asarray(v) for k, v in inputs.items()}
    if "nc" not in _NC_CACHE:
        _NC_CACHE["nc"] = build(TL, TC, stages=("l0", "gdn", "l1"))
    nc = _NC_CACHE["nc"]
    shared = prep_core(inp, 0, TL, TC)
    in_maps = [shared]
    for b in range(1, 8):
        d = dict(shared)
        d["x"] = np.ascontiguousarray(inp["x"][b], dtype=np.float32)
        d["ctx"] = np.ascontiguousarray(inp["ctx"][b], dtype=np.float32)
        d["ccol"] = np.ascontiguousarray(np.stack([col(inp["c"][b], 8), col(inp["c_ctx"], 8)], -1))
        in_maps.append(d)
    res = run_bass_kernel_spmd(nc, in_maps, core_ids=list(range(8)))
    out = np.stack([np.asarray(r["out"], dtype=np.float32) for r in res.results], 0)
    return out
```

```python
import numpy as np
from contextlib import ExitStack
import concourse.bass as bass
import concourse.mybir as mybir
from concourse.bass_utils import run_bass_kernel_spmd

F32 = mybir.dt.float32
BF16 = mybir.dt.bfloat16
AF = mybir.ActivationFunctionType
ALU = mybir.AluOpType
AX = mybir.AxisListType


class Buf:
    ALL = []

    def __init__(self, name, t):
        self.name = name
        self.t = t
        self.st = {}
        Buf.ALL.append(self)

    def __getitem__(self, idx):
        return self.t[idx]


class Sched:
    CE = ("pe", "dve", "act", "pool")

    def __init__(self, nc, stack, ndma=8):
        self.nc = nc
        self.stack = stack
        self.sem = {e: stack.enter_context(nc.semaphore("s_" + e)) for e in self.CE}
        self.cnt = {e: 0 for e in self.CE}
        self.q = {e: [] for e in ("pe", "dve", "act", "pool", "sp")}
        self.seen = {e: {} for e in self.q}
        self.dsem = {}
        self.dcnt = {}
        self.di = {}
        for qn in ("sp", "pool", "act"):
            self.dsem[qn] = [stack.enter_context(nc.semaphore("d_%s%d" % (qn, i))) for i in range(ndma)]
            self.dcnt[qn] = [0] * ndma
            self.di[qn] = 0
        self.out_tokens = []
        self.nbuf = 0

    def sb(self, name, shape, dt, st=None, side=None):
        t = (st or self.stack).enter_context(self.nc.sbuf_tensor(name, list(shape), dt, side=side))
        return Buf(name, t)

    def ps(self, name, shape, dt=F32, st=None):
        t = (st or self.stack).enter_context(self.nc.psum_tensor(name, list(shape), dt))
        b = Buf(name, t)
        b.whole = True
        return b

    def dram(self, name, shape, dt, kind="Internal"):
        t = self.nc.dram_tensor(name, list(shape), dt, kind=kind)
        return Buf(name, t)

    @staticmethod
    def _norm(x):
        if isinstance(x, Buf):
            return x, None
        if getattr(x[0], "whole", False):
            return x[0], None
        return x

    def _deps(self, reads, writes):
        toks = []
        for x in reads:
            b, k = self._norm(x)
            keys = [k] if k is not None else list(b.st.keys())
            if k is not None and None in b.st:
                keys.append(None)
            if k is None and None not in keys:
                keys.append(None)
            for kk in keys:
                s = b.st.get(kk)
                if s and s[0] is not None:
                    toks.append(("raw", s[0]))
        for x in writes:
            b, k = self._norm(x)
            keys = [k] if k is not None else list(b.st.keys())
            if k is not None and None in b.st:
                keys.append(None)
            if k is None and None not in keys:
                keys.append(None)
            for kk in keys:
                s = b.st.get(kk)
                if s:
                    if s[0] is not None:
                        toks.append(("waw", s[0]))
                    for r in s[1].values():
                        toks.append(("war", r))
        return toks

    def _update(self, reads, writes, tok):
        for x in reads:
            b, k = self._norm(x)
            s = b.st.setdefault(k, [None, {}])
            if tok[0] not in s[1] or s[1][tok[0]][2] < tok[2]:
                s[1][tok[0]] = tok
        for x in writes:
            b, k = self._norm(x)
            if k is None:
                b.st = {None: [tok, {}]}
            else:
                b.st[k] = [tok, {}]

    def _emit_waits(self, eng, toks):
        need = {}
        for kind, t in toks:
            semkey, semh, val, src = t
            if src == eng and eng == "pe":
                continue
            if self.seen[eng].get(semkey, 0) >= val:
                continue
            if semkey not in need or need[semkey][1] < val:
                need[semkey] = (semh, val)
        for semkey, (semh, val) in need.items():
            self.seen[eng][semkey] = val
            self.q[eng].append(("wait", semh, val))

    def op(self, eng, fn, reads=(), writes=()):
        toks = self._deps(reads, writes)
        self._emit_waits(eng, toks)
        self.cnt[eng] += 1
        tok = ("c_" + eng, self.sem[eng], self.cnt[eng], eng)
        self.q[eng].append(("op", fn, self.sem[eng], 1))
        self._update(reads, writes, tok)
        return tok

    def dma(self, qn, out, in_, reads=(), writes=(), **kw):
        toks = self._deps(reads, writes)
        i = self.di[qn]
        n = len(self.dsem[qn])
        r = i % n
        self.di[qn] = i + 1
        semh = self.dsem[qn][r]
        semkey = "d_%s%d" % (qn, r)
        if self.dcnt[qn][r] > 0:
            toks.append(("waw", (semkey, semh, self.dcnt[qn][r], "dma")))
        self._emit_waits(qn, toks)
        self.dcnt[qn][r] += 16
        tok = (semkey, semh, self.dcnt[qn][r], "dma")

        def fn(e, out=out, in_=in_, kw=kw):
            return e.dma_start(out=out, in_=in_, **kw)

        self.q[qn].append(("op", fn, semh, 16))
        self._update(reads, writes, tok)
        return tok

    def barrier(self):
        toks = []
        for e in self.CE:
            if self.cnt[e] > 0:
                toks.append(("c_" + e, self.sem[e], self.cnt[e], "x"))
        for qn in self.dsem:
            for r, semh in enumerate(self.dsem[qn]):
                if self.dcnt[qn][r] > 0:
                    toks.append(("d_%s%d" % (qn, r), semh, self.dcnt[qn][r], "dma"))
        for e in self.q:
            self._emit_waits(e, [("raw", t) for t in toks])
        for b in Buf.ALL:
            b.st = {}

    def wait_all(self, eng, toks):
        self._emit_waits(eng, [("raw", t) for t in toks])

    def emit(self):
        nc = self.nc
        emap = {"pe": "tensor", "dve": "vector", "act": "scalar", "pool": "gpsimd", "sp": "sync"}
        with nc.Block() as block:
            for en, bn in emap.items():
                items = self.q[en]

                def body(e, items=items):
                    for it in items:
                        if it[0] == "wait":
                            e.wait_ge(it[1], it[2])
                        else:
                            ins = it[1](e)
                            ins.then_inc(it[2], it[3])

                getattr(block, bn)(body)
        for en in self.q:
            self.q[en] = []


def _mm(S, out, lhsT, rhs, start, stop, R, W):
    return S.op("pe", lambda e: e.matmul(out, lhsT=lhsT, rhs=rhs, start=start, stop=stop), reads=R, writes=W)


def _tr(S, out, in_, ident, R, W):
    return S.op("pe", lambda e: e.transpose(out, in_, ident), reads=R, writes=W)


def _act(S, out, in_, func, R, W, scale=1.0, bias=0.0, eng="act"):
    return S.op(eng, lambda e: e.activation(out=out, in_=in_, func=func, bias=bias, scale=scale), reads=R, writes=W)


def _tt(S, eng, out, in0, in1, op, R, W):
    return S.op(eng, lambda e: e.tensor_tensor(out=out, in0=in0, in1=in1, op=op), reads=R, writes=W)


def _ts(S, eng, out, in0, s1, s2, op0, op1, R, W):
    if s2 is None:
        return S.op(eng, lambda e: e.tensor_scalar(out=out, in0=in0, scalar1=s1, scalar2=None, op0=op0), reads=R, writes=W)
    return S.op(eng, lambda e: e.tensor_scalar(out=out, in0=in0, scalar1=s1, scalar2=s2, op0=op0, op1=op1), reads=R, writes=W)


def _stt(S, eng, out, in0, scalar, in1, op0, op1, R, W):
    return S.op(eng, lambda e: e.scalar_tensor_tensor(out=out, in0=in0, scalar=scalar, in1=in1, op0=op0, op1=op1), reads=R, writes=W)


def _cp(S, eng, out, in_, R, W):
    if eng == "act":
        return S.op(eng, lambda e: e.copy(out=out, in_=in_), reads=R, writes=W)
    return S.op(eng, lambda e: e.tensor_copy(out=out, in_=in_), reads=R, writes=W)


def _ms(S, eng, ap, val, W):
    return S.op(eng, lambda e: e.memset(ap, val), reads=[], writes=W)


D = 1024
EPS = 1e-6


class K:
    pass


def build(TL, TC, stages=("l0", "l1"), dbg=()):
    Buf.ALL.clear()
    nc = bass.Bass("TRN2", target_bir_lowering=False)
    TT = TC + TL
    tiles = [(0, TC)] + [(TC + 512 * i, 512) for i in range(TL // 512)]
    g = K()
    g.nc, g.TL, g.TC, g.TT, g.tiles = nc, TL, TC, TT, tiles
    g.dbgset = set(dbg)
    g.stg_i = 0
    g.has_gdn = "gdn" in stages

    def din(name, shape, dt=F32):
        return Buf(name, nc.dram_tensor(name, list(shape), dt, kind="ExternalInput"))

    g.x_in = din("x", [TL, D])
    g.ctx_in = din("ctx", [TC, D])
    g.ccol = din("ccol", [128, 8, 2])
    g.modw = din("mod_w", [2, D, 6 * D])
    g.modb = din("modb", [128, 2, 48])
    g.normg = din("normg", [128, 2, 2, 8])
    g.fnorm = din("fnorm", [128, 8])
    g.out = Buf("out", nc.dram_tensor("out", [TL, D], F32, kind="ExternalOutput"))
    g.cosq96 = din("cosq96", [96, TT]); g.sinq96 = din("sinq96", [96, TT])
    g.cos32 = din("cos32", [32, TT]); g.sin32 = din("sin32", [32, TT])
    g.ev_w_in = din("ev_w_in", [1024, 2752]); g.ev_w_uq = din("ev_w_uq", [384, 768]); g.ev_w_ukv = din("ev_w_ukv", [256, 1024])
    g.qn_col = din("qn_col", [128, 3]); g.kvn_col = din("kvn_col", [128, 2])
    g.ev_w_out = din("ev_w_out", [1024, 1024]); g.ev_wgu = din("ev_wgu", [1024, 5632]); g.ev_wdn = din("ev_wdn", [2816, 1024])

    g.od_w_in = din("od_w_in", [1024, 2304]); g.od_w_out = din("od_w_out", [1024, 1024])
    g.gcols = din("gcols", [128, 4]); g.dlam = din("dlam", [128, 4, 64]); g.dncol = din("dncol", [128, 1])
    g.cosq64 = din("cosq64", [128, TT]); g.sinq64 = din("sinq64", [128, TT])
    g.cos64 = din("cos64", [128, TT]); g.sin64 = din("sin64", [128, TT])
    g.router = din("router", [1024, 8]); g.moe_gu = din("moe_gu", [8, 1024, 7168]); g.moe_dn = din("moe_dn", [8, 3584, 1024])
    g.conv_col = din("conv_col", [128, 12, 5]); g.alog_rep = din("alog_rep", [128, 16]); g.dtb_rep = din("dtb_rep", [128, 16])
    g.onrm_rep = din("onrm_rep", [128, 8, 64])
    import math
    g.lambda_init = 0.8 - 0.6 * math.exp(-0.3 * 1)

    with ExitStack() as st:
        S = Sched(nc, st)
        g.S = S
        g.xT_s = S.dram("xT_s", [8, 128, TT], F32)
        phase_const(g)
        phase_mod(g)
        phase_x0(g)
        S.barrier()
        if "l0" in stages or "p1" in stages:
            phase_l0_p1(g)
            S.barrier()
        if "l0" in stages or "mla" in stages:
            phase_l0_mla(g)
            S.barrier()
        if "gdn" in stages:
            phase_gdn_prep(g)
            S.barrier()
            phase_gdn(g)
            S.barrier()
        if "l0" in stages or "wout" in stages:
            phase_wout(g, 0, g.ev_w_out)
            S.barrier()
        if "l0" in stages or "ffn" in stages:
            phase_ffn0(g)
            S.barrier()
        if "l1" in stages or "l1a" in stages:
            phase_l1_p1(g)
            S.barrier()
            phase_l1_attn(g)
            S.barrier()
            phase_wout(g, 1, g.od_w_out)
            S.barrier()
        if "l1" in stages or "moe" in stages:
            phase_moe_a(g)
            S.barrier()
            phase_moe_b(g)
            S.barrier()
        phase_final(g)
        S.wait_all("sp", S.out_tokens)
        S.emit()
    return nc


def dbg_out(g, name, buf, ap, shape, dt=F32):
    S = g.S
    o = Buf(name, g.nc.dram_tensor(name, list(shape), dt, kind="ExternalOutput"))
    idx = tuple(slice(None) for _ in shape)
    tok = S.dma("pool", o.t[idx], ap, reads=[buf], writes=[o])
    S.out_tokens.append(tok)


def phase_const(g):
    S = g.S
    g.identF = S.sb("identF", [128, 128], F32)
    g.identB = S.sb("identB", [128, 128], BF16)
    g.onesF = S.sb("onesF", [128, 128], F32)
    g.onesB = S.sb("onesB", [128, 128], BF16)
    _ms(S, "pool", g.onesF[:, :], 1.0, [g.onesF])
    _ms(S, "pool", g.identF[:, :], 1.0, [g.identF])
    S.op("pool", lambda e: e.affine_select(out=g.identF[:, :], in_=g.identF[:, :], pattern=[[-1, 128]],
                                           compare_op=ALU.is_equal, fill=0.0, base=0, channel_multiplier=1),
         reads=[g.identF], writes=[g.identF])
    g.sel65 = S.sb("sel65", [65, 64], F32)
    _ms(S, "pool", g.sel65[:, :], 0.0, [g.sel65])
    _ms(S, "pool", g.sel65[64:65, :], 1.0, [g.sel65])
    _cp(S, "dve", g.identB[:, :], g.identF[:, :], [g.identF], [g.identB])
    _cp(S, "dve", g.onesB[:, :], g.onesF[:, :], [g.onesF], [g.onesB])


def phase_mod(g):
    S = g.S
    nc = g.nc
    g.modT = S.sb("modT", [128, 2, 48, 2], F32)
    g.modA = S.sb("modA", [128, 2, 2, 8, 2], F32)
    g.fn = S.sb("fn", [128, 8], F32)
    with ExitStack() as ph:
        cc = S.sb("cc", [128, 8, 2], F32, ph)
        sc = S.sb("sc", [128, 8, 2], F32, ph)
        mb = S.sb("mb", [128, 2, 48], F32, ph)
        ng = S.sb("ng", [128, 2, 2, 8], F32, ph)
        wb = [S.sb("wb%d" % i, [128, 8, 768], F32, ph) for i in range(2)]
        psm = S.ps("psm", [128, 2, 48, 2], F32, ph)
        S.dma("sp", cc[:, :, :], g.ccol[:, :, :], reads=[g.ccol], writes=[cc])
        S.dma("sp", mb[:, :, :], g.modb[:, :, :], reads=[g.modb], writes=[mb])
        S.dma("sp", ng[:, :, :, :], g.normg[:, :, :, :], reads=[g.normg], writes=[ng])
        S.dma("sp", g.fn[:, :], g.fnorm[:, :], reads=[g.fnorm], writes=[g.fn])
        _act(S, sc[:, :, :], cc[:, :, :], AF.Silu, [cc], [sc])
        i = 0
        for l in range(2):
            wv = g.modw[l].rearrange("(kc p) n -> p kc n", p=128)
            for blk in range(8):
                w = wb[i % 2]
                i += 1
                S.dma("sp", w[:, :, :], wv[:, :, blk * 768:(blk + 1) * 768], reads=[g.modw], writes=[w])
                for fs in range(6):
                    s = blk * 6 + fs
                    for kc in range(8):
                        _mm(S, psm[:, l, s, :], w[:, kc, fs * 128:(fs + 1) * 128], sc[:, kc, :], kc == 0, kc == 7,
                            [w, sc], [(psm, (l, s))])
            for j in range(2):
                _tt(S, "dve", g.modT[:, l, :, j], psm[:, l, :, j], mb[:, l, :], ALU.add, [psm, mb], [(g.modT, (l, j))])
            for u in range(2):
                for j in range(2):
                    _stt(S, "dve", g.modA[:, l, u, :, j], g.modT[:, l, (3 * u + 1) * 8:(3 * u + 2) * 8, j], 1.0,
                         ng[:, l, u, :], ALU.add, ALU.mult, [g.modT, ng], [(g.modA, (l, u, j))])
        if "mod" in g.dbgset:
            dbg_out(g, "dbg_modT", g.modT, g.modT[:, :, :, :], [128, 2, 48, 2])
            dbg_out(g, "dbg_modA", g.modA, g.modA[:, :, :, :, :], [128, 2, 2, 8, 2])
        S.emit()


def tile_src(g, t0, nt):
    if t0 < g.TC:
        src, r0 = g.ctx_in, t0
    else:
        src, r0 = g.x_in, t0 - g.TC
    return src, src[r0:r0 + nt, :].rearrange("(s p) d -> p s d", p=128)


def phase_x0(g):
    S = g.S
    with ExitStack() as ph:
        xin = [S.sb("xin%d" % i, [128, 4, D], F32, ph) for i in range(2)]
        xt = [S.sb("xt%d" % i, [128, 8, 512], F32, ph) for i in range(2)]
        pst = [S.ps("pst%d" % i, [128, 512], F32, ph) for i in range(4)]
        k = 0
        for ti, (t0, nt) in enumerate(g.tiles):
            ns = nt // 128
            a = xin[ti % 2]
            b = xt[ti % 2]
            srcb, sap = tile_src(g, t0, nt)
            S.dma("sp", a[:, 0:ns, :], sap, reads=[srcb], writes=[a])
            for c in range(8):
                p = pst[k % 4]
                k += 1
                for s in range(ns):
                    _tr(S, p[:, s * 128:(s + 1) * 128], a[:, s, c * 128:(c + 1) * 128], g.identF[:, :],
                        [a, g.identF], [p])
                _cp(S, "dve" if c % 2 == 0 else "act", b[:, c, 0:nt], p[:, 0:nt], [p], [(b, c)])
            S.dma("pool", g.xT_s.t[:, :, t0:t0 + nt].rearrange("c p t -> p c t"), b[:, :, 0:nt],
                  reads=[b], writes=[(g.xT_s, ti)])
        S.emit()


def rstd_from_ss(S, eng, out_ap, ps_ap, n, R, W):
    _ts(S, eng, out_ap, ps_ap, 1.0 / n, EPS, ALU.mult, ALU.add, R, W)
    _act(S, out_ap, out_ap, AF.Sqrt, W, W)
    S.op("dve", lambda e: e.reciprocal(out=out_ap, in_=out_ap), reads=W, writes=W)


def phase_final(g):
    S = g.S
    with ExitStack() as ph:
        xt = [S.sb("fxt%d" % i, [128, 8, 512], F32, ph) for i in range(2)]
        sq = S.sb("fsq", [128, 8, 512], F32, ph)
        rs = S.sb("frs", [128, 512], F32, ph)
        yo = [S.sb("fyo%d" % i, [128, 4, D], F32, ph) for i in range(2)]
        pss = S.ps("fpss", [128, 512], F32, ph)
        pst = [S.ps("fpst%d" % i, [128, 512], F32, ph) for i in range(4)]
        k = 0
        for ti, (t0, nt) in enumerate(g.tiles):
            if t0 < g.TC:
                continue
            ns = nt // 128
            a = xt[ti % 2]
            y = yo[ti % 2]
            S.dma("sp", a[:, :, 0:nt], g.xT_s.t[:, :, t0:t0 + nt].rearrange("c p t -> p c t"),
                  reads=[(g.xT_s, ti)], writes=[a])
            for c in range(8):
                _tt(S, "pool" if c % 2 else "dve", sq[:, c, 0:nt], a[:, c, 0:nt], a[:, c, 0:nt], ALU.mult, [a], [(sq, c)])
            for c in range(8):
                _mm(S, pss[:, 0:nt], g.onesF[:, :], sq[:, c, 0:nt], c == 0, c == 7, [g.onesF, (sq, c)], [pss])
            rstd_from_ss(S, "dve", rs[:, 0:nt], pss[:, 0:nt], D, [pss], [rs])
            for c in range(8):
                _stt(S, "dve", sq[:, c, 0:nt], a[:, c, 0:nt], g.fn[:, c:c + 1], rs[:, 0:nt],
                     ALU.mult, ALU.mult, [a, g.fn, rs, (sq, c)], [(sq, c)])
            for s in range(ns):
                for h in range(2):
                    p = pst[k % 4]
                    k += 1
                    for cc in range(4):
                        c = h * 4 + cc
                        _tr(S, p[:, cc * 128:(cc + 1) * 128], sq[:, c, s * 128:(s + 1) * 128], g.identF[:, :],
                            [(sq, c), g.identF], [p])
                    _cp(S, "act" if h else "dve", y[:, s, h * 512:(h + 1) * 512], p[:, :], [p], [(y, (s, h))])
            r0 = t0 - g.TC
            tok = S.dma("pool", g.out.t[r0:r0 + nt, :].rearrange("(s p) d -> p s d", p=128), y[:, 0:ns, :],
                        reads=[y], writes=[(g.out, ti)])
            S.out_tokens.append(tok)
        S.emit()


def load_w(g, ph, name, src_buf, src_ap, K, N, stg):
    S = g.S
    kc = K // 128
    w = S.sb(name, [128, kc, N], BF16, ph)
    v = src_ap.rearrange("(kc p) n -> p kc n", p=128)
    i = 0
    for k0 in range(0, kc, 8):
        k1 = min(kc, k0 + 8)
        for n0 in range(0, N, 512):
            n1 = min(N, n0 + 512)
            s = stg[g.stg_i % len(stg)]
            g.stg_i += 1
            S.dma("sp", s[:, 0:k1 - k0, 0:n1 - n0], v[:, k0:k1, n0:n1], reads=[src_buf], writes=[s])
            eng = ("dve", "pool", "act")[g.stg_i % 3]
            _cp(S, eng, w[:, k0:k1, n0:n1], s[:, 0:k1 - k0, 0:n1 - n0], [s], [(w, (k0, n0))])
    return w


def make_rot(g, rot, w, view, off, n):
    S = g.S
    q = n // 4
    rv, wv = view(rot), view(w)
    _ms(S, "pool", rot[:, :, :], 0.0, [rot])
    for (d0, s0, sign) in ((0, q, -1.0), (q, 0, 1.0), (2 * q, 3 * q, -1.0), (3 * q, 2 * q, 1.0)):
        _ts(S, "dve", rv[:, :, :, off + d0:off + d0 + q], wv[:, :, :, off + s0:off + s0 + q], sign, None, ALU.mult, None,
            [w, rot], [rot])


def norm_mod(g, a, sq, rs, hT, pss, nt, A_ap, B_ap, n=D, nchunk=8, xr=()):
    S = g.S
    for c in range(nchunk):
        _tt(S, "pool" if c % 2 else "dve", sq[:, c, 0:nt], a[:, c, 0:nt], a[:, c, 0:nt], ALU.mult, [(a, c)], [(sq, c)])
    for c in range(nchunk):
        _mm(S, pss[:, 0:nt], g.onesF[:, :], sq[:, c, 0:nt], c == 0, c == nchunk - 1, [g.onesF, (sq, c)], [pss])
    rstd_from_ss(S, "dve", rs[:, 0:nt], pss[:, 0:nt], n, [pss], [rs])
    for c in range(nchunk):
        _stt(S, "dve", sq[:, c, 0:nt], a[:, c, 0:nt], A_ap(c), rs[:, 0:nt], ALU.mult, ALU.mult,
             [(a, c), rs, (sq, c)] + list(xr), [(sq, c)])
        if B_ap is None:
            _cp(S, "act", hT[:, c, 0:nt], sq[:, c, 0:nt], [(sq, c)], [(hT, c)])
        else:
            _act(S, hT[:, c, 0:nt], sq[:, c, 0:nt], AF.Identity, [(sq, c)] + list(xr), [(hT, c)], bias=B_ap(c))


def xT_view(g, t0, nt):
    return g.xT_s.t[:, :, t0:t0 + nt].rearrange("c p t -> p c t")


def phase_l0_p1(g):
    S = g.S
    nc = g.nc
    TT = g.TT
    g.qT_s = S.dram("qT_s", [8, 96, TT], BF16)
    g.kT_s = S.dram("kT_s", [8, 96, TT], BF16)
    g.v_s = S.dram("v_s", [TT, 8, 65], BF16)
    g.mixT_s = S.dram("mixT_s", [1024, TT], BF16)
    g.gq_s = S.dram("gq_s", [12, 128, TT], F32)
    g.ab_s = S.dram("ab_s", [32, TT], F32)
    g.z_s = S.dram("z_s", [TT, 512], F32)
    with ExitStack() as ph:
        g.stg_i = 0
        with ExitStack() as ph2:
            stg = [S.sb("stg%d" % i, [128, 8, 512], F32, ph2, side="right") for i in range(2)]
            w_in = load_w(g, ph, "w_in0", g.ev_w_in, g.ev_w_in.t[:, :], 1024, 2752, stg)
            w_uq = load_w(g, ph, "w_uq", g.ev_w_uq, g.ev_w_uq.t[:, :], 384, 768, stg)
            w_ukv = load_w(g, ph, "w_ukv", g.ev_w_ukv, g.ev_w_ukv.t[:, :], 256, 1024, stg)
            S.barrier()
            S.emit()
        w_uq_r = S.sb("w_uq_r", [128, 3, 768], BF16, ph)
        w_kpe_r = S.sb("w_kpe_r", [128, 8, 32], BF16, ph)
        make_rot(g, w_uq_r, w_uq, lambda b: b[:, :, :].rearrange("p k (h e) -> p k h e", e=96), 64, 32)
        _ms(S, "pool", w_kpe_r[:, :, :], 0.0, [w_kpe_r])
        for (d0, s0, sign) in ((0, 8, -1.0), (8, 0, 1.0), (16, 24, -1.0), (24, 16, 1.0)):
            _ts(S, "dve", w_kpe_r[:, :, d0:d0 + 8], w_in[:, :, 640 + s0:640 + s0 + 8], sign, None, ALU.mult, None,
                [w_in, w_kpe_r], [w_kpe_r])
        qn = S.sb("qn", [128, 3], F32, ph)
        kvn = S.sb("kvn", [128, 2], F32, ph)
        S.dma("sp", qn[:, :], g.qn_col[:, :], reads=[g.qn_col], writes=[qn])
        S.dma("sp", kvn[:, :], g.kvn_col[:, :], reads=[g.kvn_col], writes=[kvn])

        xa = [S.sb("p1xa%d" % i, [128, 8, 512], F32, ph) for i in range(2)]
        sq = S.sb("p1sq", [128, 8, 512], F32, ph)
        rs = S.sb("p1rs", [128, 512], F32, ph)
        hT = S.sb("p1hT", [128, 8, 512], BF16, ph)
        cq = S.sb("p1cq", [128, 3, 512], F32, ph)
        cqn = S.sb("p1cqn", [128, 3, 512], BF16, ph)
        ckv = S.sb("p1ckv", [128, 2, 512], F32, ph)
        ckvn = S.sb("p1ckvn", [128, 2, 512], BF16, ph)
        tq = [S.sb("p1tq%d" % i, [96, 512], F32, ph) for i in range(4)]
        qo = [S.sb("p1qo%d" % i, [96, 512], BF16, ph) for i in range(2)]
        kn = S.sb("p1kn", [64, 8, 512], BF16, ph)
        kp = S.sb("p1kp", [32, 512], BF16, ph)
        va = S.sb("p1va", [128, 4, 8, 65], BF16, ph)
        gqo = [S.sb("p1gq%d" % i, [128, 512], F32, ph) for i in range(3)]
        zo = [S.sb("p1zo%d" % i, [128, 512], F32, ph) for i in range(2)]
        cs = S.sb("p1cs", [96, 512], F32, ph)
        sn = S.sb("p1sn", [96, 512], F32, ph)
        ck = S.sb("p1ck", [32, 512], F32, ph)
        sk = S.sb("p1sk", [32, 512], F32, ph)
        pss = S.ps("p1pss", [128, 512], F32, ph)
        pp = [S.ps("p1pp%d" % i, [128, 512], F32, ph) for i in range(6)]
        _ms(S, "pool", va[:, :, :, :], 1.0, [va])
        pi = [0]

        def nextp():
            p = pp[pi[0] % 6]
            pi[0] += 1
            return p

        ev = [0]

        def evac(out_ap, in_ap, R, W):
            ev[0] += 1
            _cp(S, "act" if ev[0] % 2 else "dve", out_ap, in_ap, R, W)

        for ti, (t0, nt) in enumerate(g.tiles):
            j = 1 if t0 < g.TC else 0
            ns = nt // 128
            a = xa[ti % 2]
            S.dma("sp", a[:, :, 0:nt], xT_view(g, t0, nt), reads=[(g.xT_s, ti)], writes=[a])
            S.dma("sp", cs[:, 0:nt], g.cosq96[:, t0:t0 + nt], reads=[g.cosq96], writes=[cs])
            S.dma("sp", sn[:, 0:nt], g.sinq96[:, t0:t0 + nt], reads=[g.sinq96], writes=[sn])
            S.dma("sp", ck[:, 0:nt], g.cos32[:, t0:t0 + nt], reads=[g.cos32], writes=[ck])
            S.dma("sp", sk[:, 0:nt], g.sin32[:, t0:t0 + nt], reads=[g.sin32], writes=[sk])
            norm_mod(g, a, sq, rs, hT, pss, nt, lambda c: g.modA[:, 0, 0, c, j:j + 1], lambda c: g.modT[:, 0, c, j:j + 1])

            def proj(col0, m, lhs_w=w_in, rhs=hT, nk=8):
                p = nextp()
                for kc in range(nk):
                    _mm(S, p[0:m, 0:nt], lhs_w[:, kc, col0:col0 + m], rhs[:, kc, 0:nt], kc == 0, kc == nk - 1,
                        [lhs_w, rhs], [p])
                return p

            for c in range(3):
                p = proj(c * 128, 128)
                evac(cq[:, c, 0:nt], p[:, 0:nt], [p], [(cq, c)])
            for c in range(2):
                p = proj(384 + c * 128, 128)
                evac(ckv[:, c, 0:nt], p[:, 0:nt], [p], [(ckv, c)])
            norm_mod(g, cq, sq, rs, cqn, pss, nt, lambda c: qn[:, c:c + 1], None, n=384, nchunk=3, xr=[qn])
            norm_mod(g, ckv, sq, rs, ckvn, pss, nt, lambda c: kvn[:, c:c + 1], None, n=256, nchunk=2, xr=[kvn])
            for h in range(8):
                p1 = proj(h * 96, 96, w_uq, cqn, 3)
                p2 = proj(h * 96, 96, w_uq_r, cqn, 3)
                t = tq[h % 2]
                t2 = tq[2 + h % 2]
                o = qo[h % 2]
                _tt(S, "dve", t[:, 0:nt], p1[0:96, 0:nt], cs[:, 0:nt], ALU.mult, [p1, cs], [t])
                _tt(S, "dve", t2[:, 0:nt], p2[0:96, 0:nt], sn[:, 0:nt], ALU.mult, [p2, sn], [t2])
                _tt(S, "pool", o[:, 0:nt], t2[:, 0:nt], t[:, 0:nt], ALU.add, [t2, t], [o])
                S.dma("pool", g.qT_s.t[h, :, t0:t0 + nt], o[:, 0:nt], reads=[o], writes=[(g.qT_s, (h, ti))])
            for h in range(8):
                p = proj(h * 128, 64, w_ukv, ckvn, 2)
                evac(kn[:, h, 0:nt], p[0:64, 0:nt], [p], [(kn, h)])
            S.dma("pool", g.kT_s.t[:, 0:64, t0:t0 + nt].rearrange("h p t -> p h t"), kn[:, :, 0:nt],
                  reads=[kn], writes=[(g.kT_s, ("n", ti))])
            p1 = proj(640, 32)
            p2 = proj(0, 32, w_kpe_r, hT, 8)
            t = tq[0]
            t2 = tq[2]
            _tt(S, "dve", t[0:32, 0:nt], p1[0:32, 0:nt], ck[:, 0:nt], ALU.mult, [p1, ck], [t])
            _tt(S, "dve", t2[0:32, 0:nt], p2[0:32, 0:nt], sk[:, 0:nt], ALU.mult, [p2, sk], [t2])
            _tt(S, "pool", kp[:, 0:nt], t2[0:32, 0:nt], t[0:32, 0:nt], ALU.add, [t2, t], [kp])
            for h in range(8):
                S.dma("pool", g.kT_s.t[h, 64:96, t0:t0 + nt], kp[:, 0:nt], reads=[kp], writes=[(g.kT_s, ("p", h, ti))])
            vw = w_ukv[:, :, :].rearrange("p k (h e) -> p k h e", e=128)
            for s in range(ns):
                p = nextp()
                for kc in range(2):
                    _mm(S, p[:, :].rearrange("p (h e) -> p h e", e=64), ckvn[:, kc, s * 128:(s + 1) * 128],
                        vw[:, kc, :, 64:128], kc == 0, kc == 1, [ckvn, w_ukv], [p])
                evac(va[:, s, :, 0:64], p[:, :].rearrange("p (h e) -> p h e", e=64), [p], [(va, s)])
            S.dma("pool", g.v_s.t[t0:t0 + nt, :, :].rearrange("(s p) h e -> p s h e", p=128), va[:, 0:ns, :, :],
                  reads=[va], writes=[(g.v_s, ti)])
            for c in range(12):
                p = proj(672 + c * 128, 128)
                o = gqo[c % 3]
                evac(o[:, 0:nt], p[:, 0:nt], [p], [o])
                S.dma("pool", g.gq_s.t[c, :, t0:t0 + nt], o[:, 0:nt], reads=[o], writes=[(g.gq_s, (c, ti))])
            p = proj(2720, 32)
            o = gqo[0]
            evac(o[0:32, 0:nt], p[0:32, 0:nt], [p], [o])
            S.dma("pool", g.ab_s.t[:, t0:t0 + nt], o[0:32, 0:nt], reads=[o], writes=[(g.ab_s, ti)])
            for s in range(ns):
                p = nextp()
                for kc in range(8):
                    _mm(S, p[:, :], hT[:, kc, s * 128:(s + 1) * 128], w_in[:, kc, 2208:2720], kc == 0, kc == 7,
                        [hT, w_in], [p])
                o = zo[s % 2]
                _act(S, o[:, :], p[:, :], AF.Silu, [p], [o])
                S.dma("pool", g.z_s.t[t0 + s * 128:t0 + (s + 1) * 128, :], o[:, :], reads=[o], writes=[(g.z_s, (ti, s))])
        S.emit()


def attn_core(g, KT, V, QT, d, dva, kbs, acc, psc, pT, nq, cnt):
    S = g.S
    nkb = len(kbs)
    LA = len(psc) - 1
    slots = []

    def score(i):
        kb = kbs[i]
        sc = psc[cnt[0] % len(psc)]
        p = pT[cnt[0] % len(pT)]
        cnt[0] += 1
        _mm(S, sc[:, 0:nq], KT[0:d, kb * 128:(kb + 1) * 128], QT[0:d, 0:nq], True, True, [KT, QT], [sc])
        _act(S, p[:, 0:nq], sc[:, 0:nq], AF.Exp, [sc], [p])
        slots.append(p)

    for i in range(min(LA, nkb)):
        score(i)
    for i, kb in enumerate(kbs):
        if i + LA < nkb:
            score(i + LA)
        p = slots[i]
        for qs in range(nq // 128):
            _mm(S, acc[:, qs, 0:dva], p[:, qs * 128:(qs + 1) * 128], V[:, kb, 0:dva], i == 0 and qs == 0,
                i == nkb - 1 and qs == nq // 128 - 1, [p, V], [(acc, qs)])


def attn_core3(g, KT, V, QT, d, dv, kbs, accO, Pacc, psc, pT, nq, cnt):
    S = g.S
    nkb = len(kbs)
    LA = len(psc) - 1
    slots = []

    def score(i):
        kb = kbs[i]
        sc = psc[cnt[0] % len(psc)]
        p = pT[cnt[0] % len(pT)]
        cnt[0] += 1
        _mm(S, sc[:, 0:nq], KT[0:d, kb * 128:(kb + 1) * 128], QT[0:d, 0:nq], True, True, [KT, QT], [sc])
        _act(S, p[:, 0:nq], sc[:, 0:nq], AF.Exp, [sc], [p])
        slots.append(p)

    for i in range(min(LA, nkb)):
        score(i)
    seen = {"dve": False, "pool": False}
    for i, kb in enumerate(kbs):
        if i + LA < nkb:
            score(i + LA)
        p = slots[i]
        _mm(S, accO[0:dv, 0:nq], V[:, kb, 0:dv], p[:, 0:nq], i == 0, i == nkb - 1, [p, V], [accO])
        if Pacc is not None:
            eng = "pool" if i % 3 == 2 else "dve"
            pa = Pacc[1] if eng == "pool" else Pacc[0]
            if not seen[eng]:
                seen[eng] = True
                _cp(S, eng, pa[:, 0:nq], p[:, 0:nq], [p], [pa])
            else:
                _tt(S, eng, pa[:, 0:nq], pa[:, 0:nq], p[:, 0:nq], ALU.add, [pa, p], [pa])


def attn_simple_post(g, accO, psm, oS, rec, o, nq, dst_ap, dst_dep):
    S = g.S
    _cp(S, "dve", oS[0:65, 0:nq], accO[0:65, 0:nq], [accO], [oS])
    _mm(S, psm[0:64, 0:nq], g.sel65[0:65, :], oS[0:65, 0:nq], True, True, [g.sel65, oS], [psm])
    S.op("dve", lambda e: e.reciprocal(out=rec[0:64, 0:nq], in_=psm[0:64, 0:nq]), reads=[psm], writes=[rec])
    _tt(S, "dve", o[0:64, 0:nq], oS[0:64, 0:nq], rec[0:64, 0:nq], ALU.mult, [oS, rec], [o])
    S.dma("pool", dst_ap, o[0:64, 0:nq], reads=[o], writes=[dst_dep])


def phase_l0_mla(g):
    S = g.S
    TT = g.TT
    NKB = TT // 128
    with ExitStack() as ph:
        KT = [S.sb("mKT%d" % i, [96, TT], BF16, ph) for i in range(2)]
        V = [S.sb("mV%d" % i, [128, NKB, 65], BF16, ph) for i in range(2)]
        QT = [S.sb("mQT%d" % i, [96, 512], BF16, ph) for i in range(2)]
        pT = [S.sb("mpT%d" % i, [128, 512], BF16, ph) for i in range(4)]
        oS = [S.sb("moS%d" % i, [65, 512], F32, ph) for i in range(2)]
        rec = S.sb("mrec", [64, 512], F32, ph)
        o = [S.sb("mo%d" % i, [64, 512], BF16, ph) for i in range(2)]
        psc = [S.ps("mpsc%d" % i, [128, 512], F32, ph) for i in range(4)]
        acc = [S.ps("macc%d" % i, [128, 512], F32, ph) for i in range(2)]
        psm = S.ps("mpsm", [128, 512], F32, ph)
        cnt = [0]
        qi = 0
        if not g.has_gdn:
            zt = S.sb("mzt", [128, 4, 512], BF16, ph)
            _ms(S, "pool", zt[:, :, :], 0.0, [zt])
            for ti, (t0, nt) in enumerate(g.tiles):
                S.dma("pool", g.mixT_s.t[512:1024, t0:t0 + nt].rearrange("(c p) t -> p c t", p=128), zt[:, :, 0:nt],
                      reads=[zt], writes=[(g.mixT_s, ("z", ti))])
        for h in range(8):
            kt, v = KT[h % 2], V[h % 2]
            S.dma("sp", kt[:, :], g.kT_s.t[h, :, :], reads=[g.kT_s], writes=[kt])
            S.dma("sp", v[:, :, :], g.v_s.t[:, h, :].rearrange("(n p) e -> p n e", p=128), reads=[g.v_s], writes=[v])
            for ti, (t0, nt) in enumerate(g.tiles):
                q = QT[qi % 2]
                ac = acc[qi % 2]
                os_ = oS[qi % 2]
                oo = o[qi % 2]
                qi += 1
                S.dma("sp", q[:, 0:nt], g.qT_s.t[h, :, t0:t0 + nt], reads=[g.qT_s], writes=[q])
                kbs = list(range(g.TC // 128)) if t0 < g.TC else list(range(NKB))
                attn_core3(g, kt, v, q, 96, 65, kbs, ac, None, psc, pT, nt, cnt)
                attn_simple_post(g, ac, psm, os_, rec, oo, nt, g.mixT_s.t[h * 64:(h + 1) * 64, t0:t0 + nt], (g.mixT_s, (h, ti)))
        S.emit()


def phase_wout(g, layer, w_src):
    S = g.S
    with ExitStack() as ph:
        with ExitStack() as ph2:
            stg = [S.sb("wo%d_stg%d" % (layer, i), [128, 8, 512], F32, ph2, side="right") for i in range(2)]
            w = load_w(g, ph, "w_out%d" % layer, w_src, w_src.t[:, :], 1024, 1024, stg)
            S.barrier()
            S.emit()
        xa = [S.sb("wo%dxa%d" % (layer, i), [128, 8, 512], F32, ph) for i in range(2)]
        m = [S.sb("wo%dm%d" % (layer, i), [128, 8, 512], BF16, ph) for i in range(2)]
        pp = [S.ps("wo%dpp%d" % (layer, i), [128, 512], F32, ph) for i in range(4)]
        k = 0
        for ti, (t0, nt) in enumerate(g.tiles):
            j = 1 if t0 < g.TC else 0
            if j == 1 and layer == 1:
                continue
            a, mm_ = xa[ti % 2], m[ti % 2]
            S.dma("sp", a[:, :, 0:nt], xT_view(g, t0, nt), reads=[(g.xT_s, ti)], writes=[a])
            S.dma("sp", mm_[:, :, 0:nt], g.mixT_s.t[:, t0:t0 + nt].rearrange("(c p) t -> p c t", p=128),
                  reads=[g.mixT_s], writes=[mm_])
            for dsl in range(8):
                p = pp[k % 4]
                k += 1
                for kc in range(8):
                    _mm(S, p[:, 0:nt], w[:, kc, dsl * 128:(dsl + 1) * 128], mm_[:, kc, 0:nt], kc == 0, kc == 7, [w, mm_], [p])
                _stt(S, "dve", a[:, dsl, 0:nt], p[:, 0:nt], g.modT[:, layer, 16 + dsl, j:j + 1], a[:, dsl, 0:nt],
                     ALU.mult, ALU.add, [p, (a, dsl)], [(a, dsl)])
            S.dma("pool", xT_view(g, t0, nt), a[:, :, 0:nt], reads=[a], writes=[(g.xT_s, ti)])
        S.emit()


def phase_ffn0(g):
    S = g.S
    NT = 256
    with ExitStack() as ph:
        with ExitStack() as ph2:
            stg = [S.sb("ff_stg%d" % i, [128, 8, 512], F32, ph2, side="right") for i in range(2)]
            wgu = load_w(g, ph, "ff_wgu", g.ev_wgu, g.ev_wgu.t[:, :], 1024, 5632, stg)
            wdn = load_w(g, ph, "ff_wdn", g.ev_wdn, g.ev_wdn.t[:, :], 2816, 1024, stg)
            S.barrier()
            S.emit()
        xa = [S.sb("ffxa%d" % i, [128, 8, NT], F32, ph) for i in range(2)]
        sq = S.sb("ffsq", [128, 8, NT], F32, ph)
        rs = S.sb("ffrs", [128, NT], F32, ph)
        hT = S.sb("ffhT", [128, 8, NT], BF16, ph)
        sg = [S.sb("ffsg%d" % i, [128, NT], F32, ph) for i in range(3)]
        act = [S.sb("ffact%d" % i, [128, NT], BF16, ph) for i in range(4)]
        pgu = [S.ps("ffpgu%d" % i, [128, 512], F32, ph) for i in range(4)]
        pss = pgu[0]
        yac = [S.ps("ffy%d" % i, [128, 2, NT], F32, ph) for i in range(4)]
        ti2 = 0
        for ti, (t0, nt) in enumerate(g.tiles):
            j = 1 if t0 < g.TC else 0
            for u0 in range(0, nt, NT):
                a = xa[ti2 % 2]
                ti2 += 1
                S.dma("sp", a[:, :, :], xT_view(g, t0 + u0, NT), reads=[(g.xT_s, ti)], writes=[a])
                norm_mod(g, a, sq, rs, hT, pss, NT, lambda c: g.modA[:, 0, 1, c, j:j + 1], lambda c: g.modT[:, 0, 24 + c, j:j + 1])
                def gu(f):
                    pg = pgu[(f % 2) * 2]
                    pu = pgu[(f % 2) * 2 + 1]
                    for kc in range(8):
                        _mm(S, pg[:, 0:NT], wgu[:, kc, f * 128:(f + 1) * 128], hT[:, kc, :], kc == 0, kc == 7, [wgu, hT], [pg])
                    for kc in range(8):
                        _mm(S, pu[:, 0:NT], wgu[:, kc, 2816 + f * 128:2816 + (f + 1) * 128], hT[:, kc, :], kc == 0, kc == 7,
                            [wgu, hT], [pu])
                    s_ = sg[f % 3]
                    ac = act[f % 4]
                    _act(S, s_[:, :], pg[:, 0:NT], AF.Silu, [pg], [s_])
                    _tt(S, "dve", ac[:, :], s_[:, :], pu[:, 0:NT], ALU.mult, [s_, pu], [ac])
                    return ac

                acs = {0: gu(0), 1: gu(1)}
                for f in range(22):
                    if f + 2 < 22:
                        acs[f + 2] = gu(f + 2)
                    ac = acs.pop(f)
                    for dsl in range(8):
                        _mm(S, yac[dsl // 2][:, dsl % 2, :], wdn[:, f, dsl * 128:(dsl + 1) * 128], ac[:, :],
                            f == 0 and dsl % 2 == 0, f == 21 and dsl % 2 == 1, [wdn, ac], [(yac[dsl // 2], dsl % 2)])
                for dsl in range(8):
                    _stt(S, "dve", a[:, dsl, :], yac[dsl // 2][:, dsl % 2, :], g.modT[:, 0, 40 + dsl, j:j + 1], a[:, dsl, :],
                         ALU.mult, ALU.add, [(yac[dsl // 2], dsl % 2), (a, dsl)], [(a, dsl)])
                S.dma("pool", xT_view(g, t0 + u0, NT), a[:, :, :], reads=[a], writes=[(g.xT_s, ti)])
        S.emit()


def phase_l1_p1(g):
    S = g.S
    TT = g.TT
    g.dqT_s = S.dram("dqT_s", [512, TT], BF16)
    g.dkT_s = S.dram("dkT_s", [512, TT], BF16)
    g.gqT_s = S.dram("gqT_s", [512, TT], BF16)
    g.gkT_s = S.dram("gkT_s", [128, TT], BF16)
    g.dv_s = S.dram("dv_s", [TT, 4, 129], BF16)
    g.gv_s = S.dram("gv_s", [TT, 2, 65], BF16)
    with ExitStack() as ph:
        with ExitStack() as ph2:
            stg = [S.sb("l1stg%d" % i, [128, 8, 512], F32, ph2, side="right") for i in range(2)]
            w = load_w(g, ph, "w_in1", g.od_w_in, g.od_w_in.t[:, :], 1024, 2304, stg)
            S.barrier()
            S.emit()
        wr = S.sb("w_in1r", [128, 8, 2304], BF16, ph)
        view = lambda b: b[:, :, :].rearrange("p k (h e) -> p k h e", e=64)
        make_rot(g, wr, w, view, 0, 64)
        gc = S.sb("l1gc", [128, 4], F32, ph)
        S.dma("sp", gc[:, :], g.gcols[:, :], reads=[g.gcols], writes=[gc])
        bd = S.sb("l1bd", [128, 128], F32, ph)
        _ms(S, "pool", bd[:, :], 0.0, [bd])
        _ms(S, "pool", bd[0:64, 0:64], 1.0, [bd])
        _ms(S, "pool", bd[64:128, 64:128], 1.0, [bd])
        xa = [S.sb("l1xa%d" % i, [128, 8, 512], F32, ph) for i in range(2)]
        sq = S.sb("l1sq", [128, 8, 512], F32, ph)
        rs = S.sb("l1rs", [128, 512], F32, ph)
        hT = S.sb("l1hT", [128, 8, 512], BF16, ph)
        cq = S.sb("l1cq", [128, 512], F32, ph)
        sq_ = S.sb("l1sq_", [128, 512], F32, ph)
        ck = S.sb("l1ck", [128, 512], F32, ph)
        sk = S.sb("l1sk", [128, 512], F32, ph)
        t1 = [S.sb("l1t1%d" % i, [128, 512], F32, ph) for i in range(2)]
        t2 = [S.sb("l1t2%d" % i, [128, 512], F32, ph) for i in range(2)]
        t3 = S.sb("l1t3", [128, 512], F32, ph)
        ob = [S.sb("l1ob%d" % i, [128, 512], BF16, ph) for i in range(3)]
        dva = S.sb("l1dva", [128, 4, 4, 129], BF16, ph)
        gva = S.sb("l1gva", [128, 4, 2, 65], BF16, ph)
        _ms(S, "pool", dva[:, :, :, :], 1.0, [dva])
        _ms(S, "pool", gva[:, :, :, :], 1.0, [gva])
        pss = S.ps("l1pss", [128, 512], F32, ph)
        pp = [S.ps("l1pp%d" % i, [128, 512], F32, ph) for i in range(6)]
        pi = [0]
        oi = [0]

        def nextp():
            p = pp[pi[0] % 6]
            pi[0] += 1
            return p

        for ti, (t0, nt) in enumerate(g.tiles):
            j = 1 if t0 < g.TC else 0
            ns = nt // 128
            a = xa[ti % 2]
            S.dma("sp", a[:, :, 0:nt], xT_view(g, t0, nt), reads=[(g.xT_s, ti)], writes=[a])
            S.dma("sp", cq[:, 0:nt], g.cosq64[:, t0:t0 + nt], reads=[g.cosq64], writes=[cq])
            S.dma("sp", sq_[:, 0:nt], g.sinq64[:, t0:t0 + nt], reads=[g.sinq64], writes=[sq_])
            S.dma("sp", ck[:, 0:nt], g.cos64[:, t0:t0 + nt], reads=[g.cos64], writes=[ck])
            S.dma("sp", sk[:, 0:nt], g.sin64[:, t0:t0 + nt], reads=[g.sin64], writes=[sk])
            norm_mod(g, a, sq, rs, hT, pss, nt, lambda c: g.modA[:, 1, 0, c, j:j + 1], lambda c: g.modT[:, 1, c, j:j + 1])

            def proj(wt, col0):
                p = nextp()
                for kc in range(8):
                    _mm(S, p[:, 0:nt], wt[:, kc, col0:col0 + 128], hT[:, kc, 0:nt], kc == 0, kc == 7, [wt, hT], [p])
                return p

            def roped(col0, cos_t, sin_t, dst, row0, gcol=None, grcol=None):
                p1 = proj(w, col0)
                p2 = proj(wr, col0)
                k = oi[0]
                oi[0] += 1
                a1, a2, o = t1[k % 2], t2[k % 2], ob[k % 3]
                if gcol is None:
                    _tt(S, "dve", a1[:, 0:nt], p1[:, 0:nt], cos_t[:, 0:nt], ALU.mult, [p1, cos_t], [a1])
                    _tt(S, "dve", a2[:, 0:nt], p2[:, 0:nt], sin_t[:, 0:nt], ALU.mult, [p2, sin_t], [a2])
                    _tt(S, "pool", o[:, 0:nt], a1[:, 0:nt], a2[:, 0:nt], ALU.add, [a1, a2], [o])
                else:
                    _act(S, t3[:, 0:nt], p1[:, 0:nt], AF.Square, [p1], [t3])
                    _mm(S, pss[:, 0:nt], bd[:, :], t3[:, 0:nt], True, True, [bd, t3], [pss])
                    rstd_from_ss(S, "dve", t3[:, 0:nt], pss[:, 0:nt], 64, [pss], [t3])
                    _stt(S, "dve", a1[:, 0:nt], p1[:, 0:nt], gcol, cos_t[:, 0:nt], ALU.mult, ALU.mult, [p1, cos_t, gc], [a1])
                    _stt(S, "dve", a2[:, 0:nt], p2[:, 0:nt], grcol, sin_t[:, 0:nt], ALU.mult, ALU.mult, [p2, sin_t, gc], [a2])
                    _tt(S, "pool", a1[:, 0:nt], a1[:, 0:nt], a2[:, 0:nt], ALU.add, [a1, a2], [a1])
                    _tt(S, "dve", o[:, 0:nt], a1[:, 0:nt], t3[:, 0:nt], ALU.mult, [a1, t3], [o])
                S.dma("pool", dst.t[row0:row0 + 128, t0:t0 + nt], o[:, 0:nt], reads=[o], writes=[(dst, (row0, ti))])

            for m in range(4):
                if j == 0:
                    roped(m * 128, cq, sq_, g.dqT_s, m * 128)
                roped(512 + m * 128, ck, sk, g.dkT_s, m * 128)
            if j == 0:
                for m in range(4):
                    roped(1536 + m * 128, cq, sq_, g.gqT_s, m * 128, gc[:, 0:1], gc[:, 1:2])
            roped(2048, ck, sk, g.gkT_s, 0, gc[:, 2:3], gc[:, 3:4])
            for s in range(ns):
                p = nextp()
                for kc in range(8):
                    _mm(S, p[:, :], hT[:, kc, s * 128:(s + 1) * 128], w[:, kc, 1024:1536], kc == 0, kc == 7, [hT, w], [p])
                _cp(S, "act", dva[:, s, :, 0:128], p[:, :].rearrange("p (h e) -> p h e", e=128), [p], [(dva, s)])
                p = nextp()
                for kc in range(8):
                    _mm(S, p[:, 0:128], hT[:, kc, s * 128:(s + 1) * 128], w[:, kc, 2176:2304], kc == 0, kc == 7, [hT, w], [p])
                _cp(S, "dve", gva[:, s, :, 0:64], p[:, 0:128].rearrange("p (h e) -> p h e", e=64), [p], [(gva, s)])
            S.dma("pool", g.dv_s.t[t0:t0 + nt, :, :].rearrange("(s p) h e -> p s h e", p=128), dva[:, 0:ns, :, :],
                  reads=[dva], writes=[(g.dv_s, ti)])
            S.dma("pool", g.gv_s.t[t0:t0 + nt, :, :].rearrange("(s p) h e -> p s h e", p=128), gva[:, 0:ns, :, :],
                  reads=[gva], writes=[(g.gv_s, ti)])
        S.emit()


def attn_core2(g, KT, V, QT, d, dva, kbs, accf, psc, pT, nq, cnt):
    S = g.S
    nkb = len(kbs)
    LA = len(psc) - 1
    slots = []

    def score(i):
        kb = kbs[i]
        sc = psc[cnt[0] % len(psc)]
        p = pT[cnt[0] % len(pT)]
        cnt[0] += 1
        _mm(S, sc[:, 0:nq], KT[0:d, kb * 128:(kb + 1) * 128], QT[0:d, 0:nq], True, True, [KT, QT], [sc])
        _act(S, p[:, 0:nq], sc[:, 0:nq], AF.Exp, [sc], [p])
        slots.append(p)

    for i in range(min(LA, nkb)):
        score(i)
    for i, kb in enumerate(kbs):
        if i + LA < nkb:
            score(i + LA)
        p = slots[i]
        for qs in range(nq // 128):
            b, ap, first, last = accf(qs)
            _mm(S, ap, p[:, qs * 128:(qs + 1) * 128], V[:, kb, 0:dva], i == 0 and first, i == nkb - 1 and last, [p, V], [b])


def phase_l1_attn(g):
    S = g.S
    TT = g.TT
    NKB = TT // 128
    li = g.lambda_init
    with ExitStack() as ph:
        KT = [S.sb("aKT%d" % i, [128, TT], BF16, ph) for i in range(2)]
        V = S.sb("aV", [128, NKB, 129], BF16, ph)
        QT = [S.sb("aQT%d" % i, [128, 512], BF16, ph) for i in range(2)]
        for b_ in KT + QT:
            _ms(S, "pool", b_[64:128, :], 0.0, [(b_, "pad")])
        pT = [S.sb("apT%d" % i, [128, 512], BF16, ph) for i in range(4)]
        Pacc = [(S.sb("aPaccD%d" % i, [128, 512], F32, ph), S.sb("aPaccP%d" % i, [128, 512], F32, ph)) for i in range(4)]
        oS = [S.sb("aoS%d" % i, [65, 512], F32, ph) for i in range(2)]
        rec = [S.sb("arec%d" % i, [128, 512], F32, ph) for i in range(2)]
        t1 = S.sb("at1", [128, 512], F32, ph)
        t2 = S.sb("at2", [128, 512], F32, ph)
        od = S.sb("aod", [128, 512], F32, ph)
        o = [S.sb("ao%d" % i, [128, 512], BF16, ph) for i in range(2)]
        lam = S.sb("alam", [128, 4], F32, ph)
        dl = S.sb("adl", [128, 4, 64], F32, ph)
        dn = S.sb("adn", [128, 1], F32, ph)
        tmp = S.sb("atmp", [128, 64], F32, ph)
        S.dma("sp", dl[:, :, :], g.dlam[:, :, :], reads=[g.dlam], writes=[dl])
        S.dma("sp", dn[:, :], g.dncol[:, :], reads=[g.dncol], writes=[dn])
        _ts(S, "dve", dn[:, :], dn[:, :], 1.0 - li, None, ALU.mult, None, [dn], [dn])
        for kk in range(2):
            _tt(S, "dve", tmp[:, 0:64], dl[:, 2 * kk, :], dl[:, 2 * kk + 1, :], ALU.mult, [dl], [tmp])
            S.op("dve", lambda e, kk=kk: e.tensor_reduce(out=lam[:, kk:kk + 1], in_=tmp[:, 0:64], axis=AX.X, op=ALU.add), reads=[tmp], writes=[lam])
        _act(S, lam[:, 0:2], lam[:, 0:2], AF.Exp, [lam], [lam])
        _tt(S, "dve", lam[:, 2:3], lam[:, 0:1], lam[:, 1:2], ALU.subtract, [lam], [lam])
        _ts(S, "dve", lam[:, 3:4], lam[:, 2:3], li, None, ALU.add, None, [lam], [lam])
        cnt = [0]
        qi = 0
        lat_tiles = [(ti, t0, nt) for ti, (t0, nt) in enumerate(g.tiles) if t0 >= g.TC]
        phd = ExitStack()
        psc = [S.ps("apsc%d" % i, [128, 512], F32, phd) for i in range(4)]
        acc = [S.ps("aacc%d" % i, [128, 512], F32, phd) for i in range(4)]
        psm = psc[3]
        for h in range(4):
            S.dma("sp", V[:, :, :], g.dv_s.t[:, h, :].rearrange("(n p) e -> p n e", p=128), reads=[g.dv_s], writes=[V])
            for m in range(2):
                S.dma("sp", KT[m][0:64, :], g.dkT_s.t[(2 * h + m) * 64:(2 * h + m + 1) * 64, :], reads=[g.dkT_s], writes=[(KT[m], "d")])
            for (ti, t0, nt) in lat_tiles:
                par = qi % 2
                qi += 1
                for m in range(2):
                    q = QT[m]
                    S.dma("sp", q[0:64, 0:nt], g.dqT_s.t[(2 * h + m) * 64:(2 * h + m + 1) * 64, t0:t0 + nt], reads=[g.dqT_s], writes=[(q, "d")])
                    attn_core3(g, KT[m], V, q, 128, 128, list(range(NKB)), acc[2 * par + m], Pacc[2 * par + m], psc, pT, nt, cnt)
                a1, a2 = acc[2 * par], acc[2 * par + 1]
                for m in range(2):
                    pd, pp_ = Pacc[2 * par + m]
                    _mm(S, psm[:, 0:nt], g.onesF[:, :], pd[:, 0:nt], True, False, [g.onesF, pd], [psm])
                    _mm(S, psm[:, 0:nt], g.onesF[:, :], pp_[:, 0:nt], False, True, [g.onesF, pp_], [psm])
                    S.op("dve", lambda e, m=m, nt=nt: e.reciprocal(out=rec[m][:, 0:nt], in_=psm[:, 0:nt]), reads=[psm], writes=[rec[m]])
                _tt(S, "dve", t1[:, 0:nt], a1[:, 0:nt], rec[0][:, 0:nt], ALU.mult, [a1, rec[0]], [t1])
                _stt(S, "dve", t2[:, 0:nt], a2[:, 0:nt], lam[:, 3:4], rec[1][:, 0:nt], ALU.mult, ALU.mult, [a2, lam, rec[1]], [t2])
                _tt(S, "pool", od[:, 0:nt], t1[:, 0:nt], t2[:, 0:nt], ALU.subtract, [t1, t2], [od])
                _tt(S, "pool", t1[:, 0:nt], od[:, 0:nt], od[:, 0:nt], ALU.mult, [od], [t1])
                _mm(S, psm[:, 0:nt], g.onesF[:, :], t1[:, 0:nt], True, True, [g.onesF, t1], [psm])
                rstd_from_ss(S, "dve", t2[:, 0:nt], psm[:, 0:nt], 128, [psm], [t2])
                oo = o[par]
                _stt(S, "dve", oo[:, 0:nt], od[:, 0:nt], dn[:, 0:1], t2[:, 0:nt], ALU.mult, ALU.mult, [od, dn, t2], [oo])
                S.dma("pool", g.mixT_s.t[h * 128:(h + 1) * 128, t0:t0 + nt], oo[:, 0:nt], reads=[oo], writes=[(g.mixT_s, (h, ti))])
        S.barrier()
        S.emit()
        phd.close()
        psc = [S.ps("bpsc%d" % i, [128, 512], F32, ph) for i in range(4)]
        acc = [S.ps("bacc%d" % i, [128, 512], F32, ph) for i in range(2)]
        psm = S.ps("bpsm", [128, 512], F32, ph)
        for kvh in range(2):
            S.dma("sp", V[:, :, 0:65], g.gv_s.t[:, kvh, :].rearrange("(n p) e -> p n e", p=128), reads=[g.gv_s], writes=[V])
            S.dma("sp", KT[0][0:64, :], g.gkT_s.t[kvh * 64:(kvh + 1) * 64, :], reads=[g.gkT_s], writes=[(KT[0], "d")])
            for grp in range(4):
                hd = kvh * 4 + grp
                for (ti, t0, nt) in lat_tiles:
                    par = qi % 2
                    qi += 1
                    q = QT[par]
                    S.dma("sp", q[0:64, 0:nt], g.gqT_s.t[hd * 64:(hd + 1) * 64, t0:t0 + nt], reads=[g.gqT_s], writes=[(q, "d")])
                    attn_core3(g, KT[0], V, q, 128, 65, list(range(NKB)), acc[par], None, psc, pT, nt, cnt)
                    attn_simple_post(g, acc[par], psm, oS[par], rec[0], o[par], nt,
                                     g.mixT_s.t[512 + hd * 64:512 + (hd + 1) * 64, t0:t0 + nt], (g.mixT_s, (8 + hd, ti)))
        S.emit()


def phase_moe_a(g):
    S = g.S
    TT = g.TT
    g.h2T_s = S.dram("h2T_s", [1024, TT], BF16)
    g.gT_s = S.dram("gT_s", [8, TT], F32)
    with ExitStack() as ph:
        rw = S.sb("mrw", [128, 8, 8], F32, ph)
        S.dma("sp", rw[:, :, :], g.router.t[:, :].rearrange("(kc p) e -> p kc e", p=128), reads=[g.router], writes=[rw])
        xa = [S.sb("maxa%d" % i, [128, 8, 512], F32, ph) for i in range(2)]
        sq = S.sb("masq", [128, 8, 512], F32, ph)
        rs = S.sb("mars", [128, 512], F32, ph)
        hT = [S.sb("mahT%d" % i, [128, 8, 512], BF16, ph) for i in range(2)]
        hF = S.sb("mahF", [128, 8, 512], F32, ph)
        lg = S.sb("malg", [128, 8], F32, ph)
        m8 = S.sb("mam8", [128, 8], F32, ph)
        sm = S.sb("masm", [128, 4], F32, ph)
        mask = S.sb("mamask", [128, 8], F32, ph)
        ex = S.sb("maex", [128, 8], F32, ph)
        gt = S.sb("magt", [128, 8], F32, ph)
        gT = [S.sb("magT%d" % i, [8, 512], F32, ph) for i in range(2)]
        pss = S.ps("mapss", [128, 512], F32, ph)
        pl = [S.ps("mapl%d" % i, [128, 512], F32, ph) for i in range(2)]
        pt = [S.ps("mapt%d" % i, [128, 512], F32, ph) for i in range(2)]
        k = 0
        for ti, (t0, nt) in enumerate(g.tiles):
            if t0 < g.TC:
                continue
            ns = nt // 128
            a = xa[ti % 2]
            h = hT[ti % 2]
            gTt = gT[ti % 2]
            S.dma("sp", a[:, :, 0:nt], xT_view(g, t0, nt), reads=[(g.xT_s, ti)], writes=[a])
            norm_mod(g, a, sq, rs, h, pss, nt, lambda c: g.modA[:, 1, 1, c, 0:1], lambda c: g.modT[:, 1, 24 + c, 0:1])
            S.dma("pool", g.h2T_s.t[:, t0:t0 + nt].rearrange("(c p) t -> p c t", p=128), h[:, :, 0:nt], reads=[h],
                  writes=[(g.h2T_s, ti)])
            for c in range(8):
                _ts(S, "dve", hF[:, c, 0:nt], sq[:, c, 0:nt], g.modT[:, 1, 24 + c, 0:1], None, ALU.add, None, [(sq, c)], [(hF, c)])
            for s in range(ns):
                p = pl[k % 2]
                p2 = pt[k % 2]
                k += 1
                for kc in range(8):
                    _mm(S, p[:, 0:8], hF[:, kc, s * 128:(s + 1) * 128], rw[:, kc, :], kc == 0, kc == 7, [hF, rw], [p])
                _cp(S, "dve", lg[:, :], p[:, 0:8], [p], [lg])
                S.op("dve", lambda e: e.max(out=m8[:, :], in_=lg[:, :]), reads=[lg], writes=[m8])
                _ts(S, "dve", sm[:, 0:1], m8[:, 0:1], -1.0, None, ALU.mult, None, [m8], [sm])
                _ts(S, "dve", mask[:, :], lg[:, :], m8[:, 1:2], None, ALU.is_ge, None, [lg, m8], [mask])
                _act(S, ex[:, :], lg[:, :], AF.Exp, [lg, sm], [ex], bias=sm[:, 0:1])
                _act(S, sm[:, 1:2], m8[:, 1:2], AF.Exp, [m8, sm], [sm], bias=sm[:, 0:1])
                _ts(S, "dve", sm[:, 2:3], sm[:, 1:2], 1.0, None, ALU.add, None, [sm], [sm])
                S.op("dve", lambda e: e.reciprocal(out=sm[:, 3:4], in_=sm[:, 2:3]), reads=[sm], writes=[sm])
                _stt(S, "dve", gt[:, :], ex[:, :], sm[:, 3:4], mask[:, :], ALU.mult, ALU.mult, [ex, sm, mask], [gt])
                _tr(S, p2[0:8, 0:128], gt[:, :], g.identF[:, :], [gt, g.identF], [p2])
                _cp(S, "dve", gTt[:, s * 128:(s + 1) * 128], p2[0:8, 0:128], [p2], [gTt])
            S.dma("pool", g.gT_s.t[:, t0:t0 + nt], gTt[:, 0:nt], reads=[gTt], writes=[(g.gT_s, ti)])
        S.emit()


def phase_moe_b(g):
    S = g.S
    NT = 256
    FH = 896
    NF = FH // 128
    NP = 3584 // FH
    with ExitStack() as ph:
        stg = [S.sb("mbstg%d" % i, [128, 8, 512], F32, ph, side="right") for i in range(2)]
        sel = S.sb("mbsel", [8, 8, 128], F32, ph)
        for e in range(8):
            _ts(S, "dve", sel[:, e, :], g.onesF[0:8, :], g.identF[0:8, e:e + 1], None, ALU.mult, None, [g.onesF, g.identF], [(sel, e)])
        wg = [S.sb("mbwg%d" % i, [128, 8, FH], BF16, ph) for i in range(2)]
        wu = [S.sb("mbwu%d" % i, [128, 8, FH], BF16, ph) for i in range(2)]
        wd = [S.sb("mbwd%d" % i, [128, NF, 1024], BF16, ph) for i in range(2)]
        xa = [S.sb("mbxa%d" % i, [128, 8, NT], F32, ph) for i in range(2)]
        hT = [S.sb("mbhT%d" % i, [128, 8, NT], BF16, ph) for i in range(2)]
        gTt = [S.sb("mbgT%d" % i, [8, NT], F32, ph) for i in range(2)]
        gbs = S.sb("mbgbs", [128, NT], F32, ph)
        sg = [S.sb("mbsg%d" % i, [128, NT], F32, ph) for i in range(3)]
        tg = [S.sb("mbtg%d" % i, [128, NT], F32, ph) for i in range(3)]
        act = [S.sb("mbact%d" % i, [128, NT], BF16, ph) for i in range(4)]
        pgu = [S.ps("mbpgu%d" % i, [128, 512], F32, ph) for i in range(4)]
        yac = [S.ps("mby%d" % i, [128, 2, NT], F32, ph) for i in range(4)]
        lat = [(ti, t0, nt) for ti, (t0, nt) in enumerate(g.tiles) if t0 >= g.TC]
        it = 0

        def ld(dst, src_ap, K, N):
            kc = K // 128
            v = src_ap.rearrange("(kc p) n -> p kc n", p=128)
            for k0 in range(0, kc, 8):
                k1 = min(kc, k0 + 8)
                for n0 in range(0, N, 512):
                    n1 = min(N, n0 + 512)

                    def one(k0=k0, k1=k1, n0=n0, n1=n1):
                        s = stg[g.stg_i % 2]
                        g.stg_i += 1
                        S.dma("sp", s[:, 0:k1 - k0, 0:n1 - n0], v[:, k0:k1, n0:n1], reads=[g.moe_gu, g.moe_dn], writes=[s])
                        _cp(S, ("dve", "pool")[g.stg_i % 2], dst[:, k0:k1, n0:n1], s[:, 0:k1 - k0, 0:n1 - n0], [s], [dst])
                    yield one

        def load_pass(pi_):
            e, fh = pi_ // NP, pi_ % NP
            par = pi_ % 2
            yield from ld(wg[par], g.moe_gu.t[e, :, fh * FH:(fh + 1) * FH], 1024, FH)
            yield from ld(wu[par], g.moe_gu.t[e, :, 3584 + fh * FH:3584 + (fh + 1) * FH], 1024, FH)
            yield from ld(wd[par], g.moe_dn.t[e, fh * FH:(fh + 1) * FH, :], FH, 1024)

        npass = 8 * NP
        for one in load_pass(0):
            one()
        gbs2 = [gbs, S.sb("mbgbs1", [128, NT], F32, ph)]
        tiles256 = [(ti, t0 + u0) for (ti, t0, nt) in lat for u0 in range(0, nt, NT)]
        ctxs = []
        for pi_ in range(npass):
            for j, (ti, tt0) in enumerate(tiles256):
                ctxs.append(dict(pi=pi_, e=pi_ // NP, par=pi_ % 2, ti=ti, t0=tt0, j=j, started=False, acs={}))
        steps = [(c, f) for c in ctxs for f in range(NF)]
        pend = {}

        def prologue(c):
            n = c["n"]
            a, h, gt_, gb = xa[n % 2], hT[n % 2], gTt[n % 2], gbs2[n % 2]
            c.update(a=a, h=h, gb=gb)
            t0_ = c["t0"]
            S.dma("sp", h[:, :, :], g.h2T_s.t[:, t0_:t0_ + NT].rearrange("(c p) t -> p c t", p=128), reads=[g.h2T_s], writes=[h])
            S.dma("sp", gt_[:, :], g.gT_s.t[:, t0_:t0_ + NT], reads=[g.gT_s], writes=[gt_])
            S.dma("sp", a[:, :, :], xT_view(g, t0_, NT), reads=[(g.xT_s, t0_)], writes=[a])
            _mm(S, pgu[0][:, 0:NT], sel[:, c["e"], :], gt_[:, :], True, True, [(sel, c["e"]), gt_], [pgu[0]])
            _cp(S, "dve", gb[:, :], pgu[0][:, 0:NT], [pgu[0]], [gb])
            if c["j"] == 0 and c["pi"] + 1 < npass:
                pend[c["pi"]] = list(load_pass(c["pi"] + 1))

        gcount = [0]

        def gu(c, f):
            if not c["started"]:
                c["started"] = True
                if c["j"] == 0:
                    pl0 = pend.get(c["pi"] - 1)
                    while pl0:
                        pl0.pop(0)()
                prologue(c)
            k_ = gcount[0]
            gcount[0] += 1
            wg_, wu_ = wg[c["par"]], wu[c["par"]]
            h = c["h"]
            pg = pgu[(k_ % 2) * 2]
            pu = pgu[(k_ % 2) * 2 + 1]
            for kc in range(8):
                _mm(S, pg[:, 0:NT], wg_[:, kc, f * 128:(f + 1) * 128], h[:, kc, :], kc == 0, kc == 7, [wg_, h], [pg])
            for kc in range(8):
                _mm(S, pu[:, 0:NT], wu_[:, kc, f * 128:(f + 1) * 128], h[:, kc, :], kc == 0, kc == 7, [wu_, h], [pu])
            s_, t_, ac = sg[k_ % 3], tg[k_ % 3], act[k_ % 4]
            _act(S, s_[:, :], pg[:, 0:NT], AF.Silu, [pg], [s_])
            _tt(S, "dve", t_[:, :], s_[:, :], pu[:, 0:NT], ALU.mult, [s_, pu], [t_])
            _tt(S, "pool", ac[:, :], t_[:, :], c["gb"][:, :], ALU.mult, [t_, c["gb"]], [ac])
            c["acs"][f] = ac

        for n, c in enumerate(ctxs):
            c["n"] = n
        issued = 0
        for idx, (c, f) in enumerate(steps):
            while issued < len(steps) and issued <= idx + 2:
                cc, ff = steps[issued]
                gu(cc, ff)
                issued += 1
            ac = c["acs"].pop(f)
            wd_ = wd[c["par"]]
            for dsl in range(8):
                _mm(S, yac[dsl // 2][:, dsl % 2, :], wd_[:, f, dsl * 128:(dsl + 1) * 128], ac[:, :],
                    f == 0 and dsl % 2 == 0, f == NF - 1 and dsl % 2 == 1, [wd_, ac], [yac[dsl // 2]])
            if f == NF - 1:
                a = c["a"]
                for dsl in range(8):
                    _stt(S, "dve", a[:, dsl, :], yac[dsl // 2][:, dsl % 2, :], g.modT[:, 1, 40 + dsl, 0:1], a[:, dsl, :],
                         ALU.mult, ALU.add, [yac[dsl // 2], (a, dsl)], [(a, dsl)])
                S.dma("pool", xT_view(g, c["t0"], NT), a[:, :, :], reads=[a], writes=[(g.xT_s, c["t0"])])
                pl = pend.get(c["pi"])
                if pl:
                    pl.pop(0)()
                if c["j"] == len(tiles256) - 1:
                    while pl:
                        pl.pop(0)()
        S.emit()


def phase_gdn_prep(g):
    S = g.S
    TT = g.TT
    g.qkvn_s = S.dram("qkvn_s", [12, 128, TT], F32)
    with ExitStack() as ph:
        cw = S.sb("gpcw", [128, 12, 5], F32, ph)
        S.dma("sp", cw[:, :, :], g.conv_col[:, :, :], reads=[g.conv_col], writes=[cw])
        bd = S.sb("gpbd", [128, 128], F32, ph)
        _ms(S, "pool", bd[:, :], 0.0, [bd])
        _ms(S, "pool", bd[0:64, 0:64], 1.0, [bd])
        _ms(S, "pool", bd[64:128, 64:128], 1.0, [bd])
        xh = [S.sb("gpxh%d" % i, [128, 12, 516], F32, ph) for i in range(2)]
        acc = [S.sb("gpacc%d" % i, [128, 512], F32, ph) for i in range(3)]
        y = [S.sb("gpy%d" % i, [128, 512], F32, ph) for i in range(3)]
        sq = [S.sb("gpsq%d" % i, [128, 512], F32, ph) for i in range(2)]
        rn = [S.sb("gprn%d" % i, [128, 512], F32, ph) for i in range(2)]
        pss = [S.ps("gppss%d" % i, [128, 512], F32, ph) for i in range(2)]
        k = 0
        for ti, (t0, nt) in enumerate(g.tiles):
            x = xh[ti % 2]
            lo = 0 if t0 < g.TC else g.TC
            hi = g.TC if t0 < g.TC else TT
            a0 = max(lo, t0 - 2)
            a1 = min(hi, t0 + nt + 2)
            if a0 > t0 - 2:
                _ms(S, "pool", x[:, :, 0:2], 0.0, [x])
            if a1 < t0 + nt + 2:
                _ms(S, "pool", x[:, :, nt + 2:nt + 4], 0.0, [x])
            S.dma("sp", x[:, :, a0 - (t0 - 2):a1 - (t0 - 2)], g.gq_s.t[:, :, a0:a1].rearrange("c p t -> p c t"),
                  reads=[g.gq_s], writes=[x])
            for c in range(12):
                ac, yy = acc[k % 3], y[k % 3]
                s_, r_ = sq[k % 2], rn[k % 2]
                ps = pss[k % 2]
                k += 1
                _ts(S, "dve", ac[:, 0:nt], x[:, c, 0:nt], cw[:, c, 0:1], None, ALU.mult, None, [x, cw], [ac])
                for j in range(1, 5):
                    _stt(S, "dve", ac[:, 0:nt], x[:, c, j:j + nt], cw[:, c, j:j + 1], ac[:, 0:nt], ALU.mult, ALU.add, [x, cw, ac], [ac])
                _act(S, yy[:, 0:nt], ac[:, 0:nt], AF.Silu, [ac], [yy])
                if c < 8:
                    _tt(S, "pool", s_[:, 0:nt], yy[:, 0:nt], yy[:, 0:nt], ALU.mult, [yy], [s_])
                    _mm(S, ps[:, 0:nt], bd[:, :], s_[:, 0:nt], True, True, [bd, s_], [ps])
                    rstd_from_ss(S, "dve", r_[:, 0:nt], ps[:, 0:nt], 1, [ps], [r_])
                    if c < 4:
                        _stt(S, "dve", yy[:, 0:nt], yy[:, 0:nt], 0.125, r_[:, 0:nt], ALU.mult, ALU.mult, [yy, r_], [yy])
                    else:
                        _tt(S, "dve", yy[:, 0:nt], yy[:, 0:nt], r_[:, 0:nt], ALU.mult, [yy, r_], [yy])
                S.dma("pool", g.qkvn_s.t[c, :, t0:t0 + nt], yy[:, 0:nt], reads=[yy], writes=[(g.qkvn_s, (c, ti))])
        S.emit()


def phase_gdn(g):
    S = g.S
    TT = g.TT
    C = 64
    g.o_s = S.dram("gdn_o_s", [2, TT, 512], F32)
    with ExitStack() as ph:
        def sb(name, shape, dt=F32):
            return S.sb("gd_" + name, shape, dt, ph)
        alr = sb("alr", [64, 16])
        dtb = sb("dtb", [64, 16])
        S.dma("sp", alr[:, :], g.alog_rep[0:64, :], reads=[g.alog_rep], writes=[alr])
        S.dma("sp", dtb[:, :], g.dtb_rep[0:64, :], reads=[g.dtb_rep], writes=[dtb])
        nA = sb("nA", [64, 16])
        _act(S, nA[:, :], alr[:, :], AF.Exp, [alr], [nA])
        _ts(S, "dve", nA[:, :], nA[:, :], -1.0, None, ALU.mult, None, [nA], [nA])
        def mask(name, op, sgn=1):
            m = sb(name, [64, 8, 64])
            _ms(S, "pool", m[:, :, :], 1.0, [m])
            S.op("pool", lambda e: e.affine_select(out=m[:, :, :], in_=m[:, :, :], pattern=[[0, 8], [-sgn, 64]],
                                                   compare_op=op, fill=0.0, base=0, channel_multiplier=sgn), reads=[m], writes=[m])
            return m
        mI = [mask("mIf", ALU.is_ge), mask("mIb", ALU.is_ge, -1)]
        mS = [mask("mSf", ALU.is_gt), mask("mSb", ALU.is_gt, -1)]
        I8 = mask("I8", ALU.is_equal)
        Tri = [mI[1], mI[0]]
        St = [sb("S%d" % d, [64, 8, 64]) for d in range(2)]
        for d in range(2):
            _ms(S, "pool", St[d][:, :, :], 0.0, [St[d]])
        nb = 4
        QK = [sb("QK%d" % i, [64, 16, C]) for i in range(nb)]
        Vf = [sb("Vf%d" % i, [64, 8, C]) for i in range(nb)]
        abT = [sb("abT%d" % i, [32, C]) for i in range(nb)]
        names = ["Ktm", "Vtm", "abm", "t16", "gg", "be", "gc", "egc", "gtot", "cd", "kdsc", "bsc", "nbe"]
        TD = [{}, {}]
        big = ["NG", "Mm", "E", "D", "Ds", "P0", "DT", "qkmT", "PT0", "Pa", "PTa", "Pb", "PTb", "TTa", "TTb", "bV", "bK", "U", "WT",
               "vnew", "o2", "od", "Kd", "P0b"]
        for dd in range(2):
            for n in names:
                TD[dd][n] = sb("%s_%d" % (n, dd), [64, 512] if n in ("Ktm", "Vtm") else [64, 32])
            for n in big:
                TD[dd][n] = sb("%s_%d" % (n, dd), [64, 8, 64],
                               F32)
        pb = [S.ps("gd_p%d" % i, [128, 512], F32, ph) for i in range(8)]
        pi = [0]

        def P():
            p = pb[pi[0] % 8]
            pi[0] += 1
            return p

        def v3(p):
            return p[0:64, :].rearrange("p (h e) -> p h e", e=64)

        ev = [0]

        def evac(out_ap, in_ap, R, W):
            ev[0] += 1
            _cp(S, "act" if ev[0] % 2 else "dve", out_ap, in_ap, R, W)

        def chunk(d, c0, it):
            T = TD[d]
            qk, vf, ab = QK[it % nb], Vf[it % nb], abT[it % nb]
            S.dma("sp", qk[:, :, :], g.qkvn_s.t[0:8, :, c0:c0 + C].rearrange("c (hh d) t -> d (c hh) t", d=64),
                  reads=[g.qkvn_s], writes=[qk])
            S.dma("sp", vf[:, :, :], g.qkvn_s.t[8:12, :, c0:c0 + C].rearrange("c (hh d) t -> d (c hh) t", d=64),
                  reads=[g.qkvn_s], writes=[vf])
            S.dma("sp", ab[:, :], g.ab_s.t[:, c0:c0 + C], reads=[g.ab_s], writes=[ab])
            Ktm, Vtm, abm = T["Ktm"], T["Vtm"], T["abm"]
            p = P()
            for h in range(8):
                _tr(S, p[0:64, h * 64:(h + 1) * 64], qk[:, 8 + h, :], g.identF[0:64, 0:64], [qk, g.identF], [p])
            evac(Ktm[:, :], p[0:64, :], [p], [Ktm])
            p = P()
            for h in range(8):
                _tr(S, p[0:64, h * 64:(h + 1) * 64], vf[:, h, :], g.identF[0:64, 0:64], [vf, g.identF], [p])
            evac(Vtm[:, :], p[0:64, :], [p], [Vtm])
            p = P()
            _tr(S, p[0:64, 0:32], ab[:, :], g.identF[0:32, 0:32], [ab, g.identF], [p])
            evac(abm[:, :], p[0:64, 0:32], [p], [abm])
            t16, gg, be, gc, egc, gtot, cd, kdsc, bsc, nbe = (T[n] for n in ("t16", "gg", "be", "gc", "egc", "gtot", "cd", "kdsc", "bsc", "nbe"))
            r8 = slice(d * 8, d * 8 + 8)
            _tt(S, "dve", t16[:, 0:8], abm[:, r8], dtb[:, r8], ALU.add, [abm, dtb], [t16])
            _act(S, t16[:, 0:8], t16[:, 0:8], AF.Exp, [t16], [t16])
            _act(S, t16[:, 0:8], t16[:, 0:8], AF.Ln, [t16], [t16], bias=1.0)
            _tt(S, "dve", gg[:, 0:8], t16[:, 0:8], nA[:, r8], ALU.mult, [t16, nA], [gg])
            _act(S, be[:, 0:8], abm[:, 16 + d * 8:24 + d * 8], AF.Sigmoid, [abm], [be])
            _ts(S, "dve", nbe[:, 0:8], be[:, 0:8], -1.0, None, ALU.mult, None, [be], [nbe])
            p = P()
            _mm(S, p[0:64, 0:8], Tri[d][:, 0, :], gg[:, 0:8], True, True, [Tri[d], gg], [p])
            evac(gc[:, 0:8], p[0:64, 0:8], [p], [gc])
            p = P()
            _mm(S, p[0:64, 0:8], g.onesF[0:64, 0:64], gg[:, 0:8], True, True, [g.onesF, gg], [p])
            evac(gtot[:, 0:8], p[0:64, 0:8], [p], [gtot])
            _act(S, egc[:, 0:8], gc[:, 0:8], AF.Exp, [gc], [egc])
            _act(S, cd[:, 0:8], gtot[:, 0:8], AF.Exp, [gtot], [cd])
            _tt(S, "dve", kdsc[:, 0:8], gtot[:, 0:8], gc[:, 0:8], ALU.subtract, [gtot, gc], [kdsc])
            _act(S, kdsc[:, 0:8], kdsc[:, 0:8], AF.Exp, [kdsc], [kdsc])
            _tt(S, "dve", bsc[:, 0:8], be[:, 0:8], egc[:, 0:8], ALU.mult, [be, egc], [bsc])
            NG, Mm, E, Dm, Ds = T["NG"], T["Mm"], T["E"], T["D"], T["Ds"]
            for h in range(8):
                _ts(S, "dve", NG[:, h, :], g.onesF[0:64, 0:64], gg[:, h:h + 1], -1.0, ALU.mult, ALU.mult, [g.onesF, gg], [(NG, h)])
            p = P()
            for h in range(8):
                _mm(S, v3(p)[:, h, :], NG[:, h, :], Tri[d][:, 0, :], True, True, [(NG, h), Tri[d]], [p])
            for h in range(8):
                _ts(S, "dve", Mm[:, h, :], v3(p)[:, h, :], gc[:, h:h + 1], 0.0, ALU.add, ALU.min, [p, gc], [(Mm, h)])
            _act(S, E[:, :, :], Mm[:, :, :], AF.Exp, [Mm], [E])
            _tt(S, "pool", Dm[:, :, :], E[:, :, :], mI[d][:, :, :], ALU.mult, [E, mI[d]], [Dm])
            _tt(S, "pool", Ds[:, :, :], E[:, :, :], mS[d][:, :, :], ALU.mult, [E, mS[d]], [Ds])
            P0, DT, qkmT, PT0 = T["P0"], T["DT"], T["qkmT"], T["PT0"]
            p = P()
            for h in range(8):
                _mm(S, v3(p)[:, h, :], qk[:, 8 + h, :], qk[:, 8 + h, :], True, True, [qk], [p])
            for h in range(8):
                _stt(S, "dve", P0[:, h, :], v3(p)[:, h, :], nbe[:, h:h + 1], Ds[:, h, :], ALU.mult, ALU.mult, [p, nbe, Ds], [(P0, h)])
            p = P()
            for h in range(8):
                _tr(S, v3(p)[:, h, :], Dm[:, h, :], g.identF[0:64, 0:64], [Dm, g.identF], [p])
            evac(DT[:, :, :], v3(p), [p], [DT])
            p = P()
            for h in range(8):
                _mm(S, v3(p)[:, h, :], qk[:, 8 + h, :], qk[:, h, :], True, True, [qk], [p])
            _tt(S, "dve", qkmT[:, :, :], v3(p), DT[:, :, :], ALU.mult, [p, DT], [qkmT])
            p = P()
            for h in range(8):
                _tr(S, v3(p)[:, h, :], P0[:, h, :], g.identF[0:64, 0:64], [(P0, h), g.identF], [p])
            evac(PT0[:, :, :], v3(p), [p], [PT0])
            TTa, TTb = T["TTa"], T["TTb"]
            _tt(S, "pool", TTa[:, :, :], PT0[:, :, :], I8[:, :, :], ALU.add, [PT0, I8], [TTa])
            Pk, PTk = P0, PT0
            cur, nxt = TTa, TTb
            alt = [(T["Pa"], T["PTa"]), (T["Pb"], T["PTb"])]
            for lv in range(1, 6):
                Pn, PTn = alt[lv % 2]
                p = P()
                for h in range(8):
                    _mm(S, v3(p)[:, h, :], PTk[:, h, :], Pk[:, h, :], True, True, [Pk, PTk], [p])
                evac(Pn[:, :, :], v3(p), [p], [Pn])
                if lv < 5:
                    p = P()
                    for h in range(8):
                        _mm(S, v3(p)[:, h, :], Pk[:, h, :], PTk[:, h, :], True, True, [Pk, PTk], [p])
                    evac(PTn[:, :, :], v3(p), [p], [PTn])
                p = P()
                for h in range(8):
                    _mm(S, v3(p)[:, h, :], Pn[:, h, :], cur[:, h, :], True, True, [Pn, cur], [p])
                _tt(S, "dve", nxt[:, :, :], v3(p), cur[:, :, :], ALU.add, [p, cur], [nxt])
                cur, nxt = nxt, cur
                Pk, PTk = Pn, PTn
            TT_ = cur
            bV, bK, U, WT = T["bV"], T["bK"], T["U"], T["WT"]
            Kv = Ktm[:, :].rearrange("p (h e) -> p h e", e=64)
            Vv = Vtm[:, :].rearrange("p (h e) -> p h e", e=64)
            for h in range(8):
                _ts(S, "dve", bV[:, h, :], Vv[:, h, :], be[:, h:h + 1], None, ALU.mult, None, [Vtm, be], [(bV, h)])
                _ts(S, "pool", bK[:, h, :], Kv[:, h, :], bsc[:, h:h + 1], None, ALU.mult, None, [Ktm, bsc], [(bK, h)])
            p = P()
            for h in range(8):
                _mm(S, v3(p)[:, h, :], TT_[:, h, :], bV[:, h, :], True, True, [TT_, bV], [p])
            evac(U[:, :, :], v3(p), [p], [U])
            p = P()
            for h in range(8):
                _mm(S, v3(p)[:, h, :], bK[:, h, :], TT_[:, h, :], True, True, [TT_, bK], [p])
            evac(WT[:, :, :], v3(p), [p], [WT])
            Sd = St[d]
            vnew, o2, od, Kd = T["vnew"], T["o2"], T["od"], T["Kd"]
            for h in range(8):
                _ts(S, "pool", Kd[:, h, :], Kv[:, h, :], kdsc[:, h:h + 1], None, ALU.mult, None, [Ktm, kdsc], [(Kd, h)])
            pw = P()
            for h in range(8):
                _mm(S, v3(pw)[:, h, :], WT[:, h, :], Sd[:, h, :], True, True, [WT, Sd], [pw])
            pq = P()
            for h in range(8):
                _mm(S, v3(pq)[:, h, :], qk[:, h, :], Sd[:, h, :], True, True, [qk, Sd], [pq])
            _tt(S, "dve", vnew[:, :, :], U[:, :, :], v3(pw), ALU.subtract, [U, pw], [vnew])
            p2 = P()
            for h in range(8):
                _mm(S, v3(p2)[:, h, :], qkmT[:, h, :], vnew[:, h, :], True, True, [qkmT, vnew], [p2])
            evac(o2[:, :, :], v3(p2), [p2], [o2])
            for h in range(8):
                _stt(S, "dve", od[:, h, :], v3(pq)[:, h, :], egc[:, h:h + 1], o2[:, h, :], ALU.mult, ALU.add, [pq, egc, o2], [(od, h)])
            p3 = P()
            for h in range(8):
                _mm(S, v3(p3)[:, h, :], Kd[:, h, :], vnew[:, h, :], True, True, [Kd, vnew], [p3])
            for h in range(8):
                _stt(S, "dve", Sd[:, h, :], Sd[:, h, :], cd[:, h:h + 1], v3(p3)[:, h, :], ALU.mult, ALU.add, [Sd, cd, p3], [Sd])
            odf = od[:, :, :].rearrange("p h e -> p (h e)")
            S.dma("pool", g.o_s.t[d, c0:c0 + C, :], odf, reads=[od], writes=[(g.o_s, (d, c0))])

        nctx = g.TC // C
        nlat = g.TL // C
        it = 0
        orders = [list(range(nctx)) + [nctx + i for i in range(nlat)],
                  list(range(nctx - 1, -1, -1)) + [nctx + i for i in range(nlat - 1, -1, -1)]]
        for step in range(nctx + nlat):
            for d in range(2):
                chunk(d, orders[d][step] * C, it)
                it += 1
        S.barrier()
        S.emit()
    with ExitStack() as ph:
        onr = S.sb("gc_onr", [128, 8, 64], F32, ph)
        S.dma("sp", onr[:, :, :], g.onrm_rep[:, :, :], reads=[g.onrm_rep], writes=[onr])
        o0 = [S.sb("gc_o0%d" % i, [128, 8, 64], F32, ph) for i in range(2)]
        o1 = [S.sb("gc_o1%d" % i, [128, 8, 64], F32, ph) for i in range(2)]
        zt = [S.sb("gc_z%d" % i, [128, 8, 64], F32, ph) for i in range(2)]
        sqo = S.sb("gc_sq", [128, 8, 64], F32, ph)
        ss = S.sb("gc_ss", [128, 16], F32, ph)
        ob = [S.sb("gc_ob%d" % i, [128, 8, 64], BF16, ph) for i in range(2)]
        oT = [S.sb("gc_oT%d" % i, [128, 4, 128], BF16, ph) for i in range(2)]
        ptb = [S.ps("gc_ptb%d" % i, [128, 512], BF16, ph) for i in range(2)]
        for bi in range(TT // 128):
            r0 = bi * 128
            a, b, z, o_b, o_t, pt = o0[bi % 2], o1[bi % 2], zt[bi % 2], ob[bi % 2], oT[bi % 2], ptb[bi % 2]
            S.dma("sp", a[:, :, :], g.o_s.t[0, r0:r0 + 128, :].rearrange("p (h e) -> p h e", e=64), reads=[g.o_s], writes=[a])
            S.dma("sp", b[:, :, :], g.o_s.t[1, r0:r0 + 128, :].rearrange("p (h e) -> p h e", e=64), reads=[g.o_s], writes=[b])
            S.dma("sp", z[:, :, :], g.z_s.t[r0:r0 + 128, :].rearrange("p (h e) -> p h e", e=64), reads=[g.z_s], writes=[z])
            _tt(S, "pool", a[:, :, :], a[:, :, :], b[:, :, :], ALU.add, [a, b], [a])
            _tt(S, "pool", sqo[:, :, :], a[:, :, :], a[:, :, :], ALU.mult, [a], [sqo])
            S.op("dve", lambda e: e.tensor_reduce(out=ss[:, 0:8], in_=sqo[:, :, :], axis=AX.X, op=ALU.add), reads=[sqo], writes=[ss])
            rstd_from_ss(S, "dve", ss[:, 8:16], ss[:, 0:8], 64, [ss], [ss])
            _tt(S, "pool", z[:, :, :], z[:, :, :], onr[:, :, :], ALU.mult, [z, onr], [z])
            for h in range(8):
                _stt(S, "dve", o_b[:, h, :], a[:, h, :], ss[:, 8 + h:9 + h], z[:, h, :], ALU.mult, ALU.mult, [a, ss, z], [o_b])
            obf = o_b[:, :, :].rearrange("p h e -> p (h e)")
            for c in range(4):
                _tr(S, pt[:, c * 128:(c + 1) * 128], obf[:, c * 128:(c + 1) * 128], g.identB[:, :], [o_b, g.identB], [pt])
            _cp(S, "act", o_t[:, :, :], pt[:, :].rearrange("p (c t) -> p c t", t=128), [pt], [o_t])
            S.dma("pool", g.mixT_s.t[512:1024, r0:r0 + 128].rearrange("(c p) t -> p c t", p=128), o_t[:, :, :], reads=[o_t],
                  writes=[(g.mixT_s, ("g", bi))])
        S.emit()


def rope_tab(n, TL, TC):
    nf = n // 4
    inv = 1.0 / (10000.0 ** (np.arange(nf, dtype=np.float32) / nf))
    t = np.arange(TL)
    rows = (t // 64).astype(np.float32)
    cols = (t % 64).astype(np.float32)
    ang_r = rows[None, :] * inv[:, None]
    ang_c = cols[None, :] * inv[:, None]
    cos = np.concatenate([np.cos(ang_r), np.cos(ang_r), np.cos(ang_c), np.cos(ang_c)], 0)
    sin = np.concatenate([np.sin(ang_r), np.sin(ang_r), np.sin(ang_c), np.sin(ang_c)], 0)
    cos = np.concatenate([np.ones((n, TC), np.float32), cos.astype(np.float32)], 1)
    sin = np.concatenate([np.zeros((n, TC), np.float32), sin.astype(np.float32)], 1)
    return np.ascontiguousarray(cos), np.ascontiguousarray(sin)


def col(v, k):
    return np.ascontiguousarray(np.asarray(v, np.float32).reshape(k, 128).T)


def prep_core(inp, b, TL, TC):
    f = lambda a: np.ascontiguousarray(np.asarray(a, np.float32))
    TT = TL + TC
    d = {}
    d["x"] = f(inp["x"][b])
    d["ctx"] = f(inp["ctx"][b])
    d["ccol"] = np.ascontiguousarray(np.stack([col(inp["c"][b], 8), col(inp["c_ctx"], 8)], -1))
    d["mod_w"] = f(inp["mod_w"])
    d["modb"] = np.ascontiguousarray(f(inp["mod_b"]).reshape(2, 48, 128).transpose(2, 0, 1))
    d["normg"] = np.ascontiguousarray(f(inp["norm_g"]).reshape(2, 2, 8, 128).transpose(3, 0, 1, 2))
    d["fnorm"] = col(inp["final_norm"], 8)
    c32, s32 = rope_tab(32, TL, TC)
    sc = np.float32(96 ** -0.5)
    d["cosq96"] = np.ascontiguousarray(np.concatenate([np.full((64, TT), sc, np.float32), c32 * sc], 0))
    d["sinq96"] = np.ascontiguousarray(np.concatenate([np.zeros((64, TT), np.float32), s32 * sc], 0))
    d["cos32"], d["sin32"] = c32, s32
    d["ev_w_in"] = f(inp["ev_w_in"][0])
    d["ev_w_uq"] = f(inp["ev_mla_w_uq"][0])
    d["ev_w_ukv"] = f(inp["ev_mla_w_ukv"][0])
    d["qn_col"] = col(inp["ev_mla_q_norm"][0], 3)
    d["kvn_col"] = col(inp["ev_mla_kv_norm"][0], 2)
    d["ev_w_out"] = f(inp["ev_w_out"][0])
    d["ev_wgu"] = f(inp["ev_ffn_w_gu"][0])
    d["ev_wdn"] = f(inp["ev_ffn_w_down"][0])
    c64, s64 = rope_tab(64, TL, TC)
    c64 = np.ascontiguousarray(np.concatenate([c64, c64], 0)); s64 = np.ascontiguousarray(np.concatenate([s64, s64], 0))
    d["cos64"], d["sin64"] = c64, s64
    d["cosq64"], d["sinq64"] = c64 * np.float32(0.125), s64 * np.float32(0.125)
    d["od_w_in"] = f(inp["od_w_in"][0])
    d["od_w_out"] = f(inp["od_w_out"][0])
    d["router"] = f(inp["od_router_w"][0]); d["moe_gu"] = f(inp["od_moe_w_gu"][0]); d["moe_dn"] = f(inp["od_moe_w_down"][0])
    d["conv_col"] = np.ascontiguousarray(f(inp["ev_gdn_conv"][0]).reshape(5, 12, 128).transpose(2, 1, 0))
    d["alog_rep"] = np.ascontiguousarray(np.broadcast_to(f(inp["ev_gdn_a_log"][0]).reshape(1, 16), (128, 16)))
    d["dtb_rep"] = np.ascontiguousarray(np.broadcast_to(f(inp["ev_gdn_dt_bias"][0]).reshape(1, 16), (128, 16)))
    d["onrm_rep"] = np.ascontiguousarray(np.broadcast_to(f(inp["ev_gdn_out_norm"][0]).reshape(1, 1, 64), (128, 8, 64)))
    perm = np.concatenate([np.arange(16, 32), np.arange(0, 16), np.arange(48, 64), np.arange(32, 48)])
    gq = f(inp["od_gqa_q_norm"][0]); gk = f(inp["od_gqa_k_norm"][0])
    d["gcols"] = np.ascontiguousarray(np.stack([np.tile(gq, 2), np.tile(gq[perm], 2), np.tile(gk, 2), np.tile(gk[perm], 2)], -1))
    d["dlam"] = np.ascontiguousarray(np.broadcast_to(f(inp["od_diff_lambda"][0])[None], (128, 4, 64)))
    d["dncol"] = np.ascontiguousarray(f(inp["od_diff_norm"][0]).reshape(128, 1))
    return d


_NC_CACHE = {}


def kernel(**inputs):
    TL, TC = 8192, 256
    inp = {k: np.asarray(v) for k, v in inputs.items()}
    if "nc" not in _NC_CACHE:
        _NC_CACHE["nc"] = build(TL, TC, stages=("l0", "gdn", "l1"))
    nc = _NC_CACHE["nc"]
    shared = prep_core(inp, 0, TL, TC)
    in_maps = [shared]
    for b in range(1, 8):
        d = dict(shared)
        d["x"] = np.ascontiguousarray(inp["x"][b], dtype=np.float32)
        d["ctx"] = np.ascontiguousarray(inp["ctx"][b], dtype=np.float32)
        d["ccol"] = np.ascontiguousarray(np.stack([col(inp["c"][b], 8), col(inp["c_ctx"], 8)], -1))
        in_maps.append(d)
    res = run_bass_kernel_spmd(nc, in_maps, core_ids=list(range(8)))
    out = np.stack([np.asarray(r["out"], dtype=np.float32) for r in res.results], 0)
    return out
```

```python
import numpy as np
from contextlib import ExitStack
import concourse.bass as bass
import concourse.mybir as mybir
from concourse.bass_utils import run_bass_kernel_spmd

F32 = mybir.dt.float32
BF16 = mybir.dt.bfloat16
AF = mybir.ActivationFunctionType
ALU = mybir.AluOpType
AX = mybir.AxisListType


class Buf:
    ALL = []

    def __init__(self, name, t):
        self.name = name
        self.t = t
        self.st = {}
        Buf.ALL.append(self)

    def __getitem__(self, idx):
        return self.t[idx]


class Sched:
    CE = ("pe", "dve", "act", "pool")

    def __init__(self, nc, stack, ndma=8):
        self.nc = nc
        self.stack = stack
        self.sem = {e: stack.enter_context(nc.semaphore("s_" + e)) for e in self.CE}
        self.cnt = {e: 0 for e in self.CE}
        self.q = {e: [] for e in ("pe", "dve", "act", "pool", "sp")}
        self.seen = {e: {} for e in self.q}
        self.dsem = {}
        self.dcnt = {}
        self.di = {}
        for qn in ("sp", "pool", "act"):
            self.dsem[qn] = [stack.enter_context(nc.semaphore("d_%s%d" % (qn, i))) for i in range(ndma)]
            self.dcnt[qn] = [0] * ndma
            self.di[qn] = 0
        self.out_tokens = []
        self.nbuf = 0

    def sb(self, name, shape, dt, st=None, side=None):
        t = (st or self.stack).enter_context(self.nc.sbuf_tensor(name, list(shape), dt, side=side))
        return Buf(name, t)

    def ps(self, name, shape, dt=F32, st=None):
        t = (st or self.stack).enter_context(self.nc.psum_tensor(name, list(shape), dt))
        b = Buf(name, t)
        b.whole = True
        return b

    def dram(self, name, shape, dt, kind="Internal"):
        t = self.nc.dram_tensor(name, list(shape), dt, kind=kind)
        return Buf(name, t)

    @staticmethod
    def _norm(x):
        if isinstance(x, Buf):
            return x, None
        if getattr(x[0], "whole", False):
            return x[0], None
        return x

    def _deps(self, reads, writes):
        toks = []
        for x in reads:
            b, k = self._norm(x)
            keys = [k] if k is not None else list(b.st.keys())
            if k is not None and None in b.st:
                keys.append(None)
            if k is None and None not in keys:
                keys.append(None)
            for kk in keys:
                s = b.st.get(kk)
                if s and s[0] is not None:
                    toks.append(("raw", s[0]))
        for x in writes:
            b, k = self._norm(x)
            keys = [k] if k is not None else list(b.st.keys())
            if k is not None and None in b.st:
                keys.append(None)
            if k is None and None not in keys:
                keys.append(None)
            for kk in keys:
                s = b.st.get(kk)
                if s:
                    if s[0] is not None:
                        toks.append(("waw", s[0]))
                    for r in s[1].values():
                        toks.append(("war", r))
        return toks

    def _update(self, reads, writes, tok):
        for x in reads:
            b, k = self._norm(x)
            s = b.st.setdefault(k, [None, {}])
            if tok[0] not in s[1] or s[1][tok[0]][2] < tok[2]:
                s[1][tok[0]] = tok
        for x in writes:
            b, k = self._norm(x)
            if k is None:
                b.st = {None: [tok, {}]}
            else:
                b.st[k] = [tok, {}]

    def _emit_waits(self, eng, toks):
        need = {}
        for kind, t in toks:
            semkey, semh, val, src = t
            if src == eng and eng == "pe":
                continue
            if self.seen[eng].get(semkey, 0) >= val:
                continue
            if semkey not in need or need[semkey][1] < val:
                need[semkey] = (semh, val)
        for semkey, (semh, val) in need.items():
            self.seen[eng][semkey] = val
            self.q[eng].append(("wait", semh, val))

    def op(self, eng, fn, reads=(), writes=()):
        toks = self._deps(reads, writes)
        self._emit_waits(eng, toks)
        self.cnt[eng] += 1
        tok = ("c_" + eng, self.sem[eng], self.cnt[eng], eng)
        self.q[eng].append(("op", fn, self.sem[eng], 1))
        self._update(reads, writes, tok)
        return tok

    def dma(self, qn, out, in_, reads=(), writes=(), **kw):
        toks = self._deps(reads, writes)
        i = self.di[qn]
        n = len(self.dsem[qn])
        r = i % n
        self.di[qn] = i + 1
        semh = self.dsem[qn][r]
        semkey = "d_%s%d" % (qn, r)
        if self.dcnt[qn][r] > 0:
            toks.append(("waw", (semkey, semh, self.dcnt[qn][r], "dma")))
        self._emit_waits(qn, toks)
        self.dcnt[qn][r] += 16
        tok = (semkey, semh, self.dcnt[qn][r], "dma")

        def fn(e, out=out, in_=in_, kw=kw):
            return e.dma_start(out=out, in_=in_, **kw)

        self.q[qn].append(("op", fn, semh, 16))
        self._update(reads, writes, tok)
        return tok

    def barrier(self):
        toks = []
        for e in self.CE:
            if self.cnt[e] > 0:
                toks.append(("c_" + e, self.sem[e], self.cnt[e], "x"))
        for qn in self.dsem:
            for r, semh in enumerate(self.dsem[qn]):
                if self.dcnt[qn][r] > 0:
                    toks.append(("d_%s%d" % (qn, r), semh, self.dcnt[qn][r], "dma"))
        for e in self.q:
            self._emit_waits(e, [("raw", t) for t in toks])
        for b in Buf.ALL:
            b.st = {}

    def wait_all(self, eng, toks):
        self._emit_waits(eng, [("raw", t) for t in toks])

    def emit(self):
        nc = self.nc
        emap = {"pe": "tensor", "dve": "vector", "act": "scalar", "pool": "gpsimd", "sp": "sync"}
        with nc.Block() as block:
            for en, bn in emap.items():
                items = self.q[en]

                def body(e, items=items):
                    for it in items:
                        if it[0] == "wait":
                            e.wait_ge(it[1], it[2])
                        else:
                            ins = it[1](e)
                            ins.then_inc(it[2], it[3])

                getattr(block, bn)(body)
        for en in self.q:
            self.q[en] = []


def _mm(S, out, lhsT, rhs, start, stop, R, W):
    return S.op("pe", lambda e: e.matmul(out, lhsT=lhsT, rhs=rhs, start=start, stop=stop), reads=R, writes=W)


def _tr(S, out, in_, ident, R, W):
    return S.op("pe", lambda e: e.transpose(out, in_, ident), reads=R, writes=W)


def _act(S, out, in_, func, R, W, scale=1.0, bias=0.0, eng="act"):
    return S.op(eng, lambda e: e.activation(out=out, in_=in_, func=func, bias=bias, scale=scale), reads=R, writes=W)


def _tt(S, eng, out, in0, in1, op, R, W):
    return S.op(eng, lambda e: e.tensor_tensor(out=out, in0=in0, in1=in1, op=op), reads=R, writes=W)


def _ts(S, eng, out, in0, s1, s2, op0, op1, R, W):
    if s2 is None:
        return S.op(eng, lambda e: e.tensor_scalar(out=out, in0=in0, scalar1=s1, scalar2=None, op0=op0), reads=R, writes=W)
    return S.op(eng, lambda e: e.tensor_scalar(out=out, in0=in0, scalar1=s1, scalar2=s2, op0=op0, op1=op1), reads=R, writes=W)


def _stt(S, eng, out, in0, scalar, in1, op0, op1, R, W):
    return S.op(eng, lambda e: e.scalar_tensor_tensor(out=out, in0=in0, scalar=scalar, in1=in1, op0=op0, op1=op1), reads=R, writes=W)


def _cp(S, eng, out, in_, R, W):
    if eng == "act":
        return S.op(eng, lambda e: e.copy(out=out, in_=in_), reads=R, writes=W)
    return S.op(eng, lambda e: e.tensor_copy(out=out, in_=in_), reads=R, writes=W)


def _ms(S, eng, ap, val, W):
    return S.op(eng, lambda e: e.memset(ap, val), reads=[], writes=W)


D = 1024
EPS = 1e-6


class K:
    pass


def build(TL, TC, stages=("l0", "l1"), dbg=()):
    Buf.ALL.clear()
    nc = bass.Bass("TRN2", target_bir_lowering=False)
    TT = TC + TL
    tiles = [(0, TC)] + [(TC + 512 * i, 512) for i in range(TL // 512)]
    g = K()
    g.nc, g.TL, g.TC, g.TT, g.tiles = nc, TL, TC, TT, tiles
    g.dbgset = set(dbg)
    g.stg_i = 0
    g.has_gdn = "gdn" in stages

    def din(name, shape, dt=F32):
        return Buf(name, nc.dram_tensor(name, list(shape), dt, kind="ExternalInput"))

    g.x_in = din("x", [TL, D])
    g.ctx_in = din("ctx", [TC, D])
    g.ccol = din("ccol", [128, 8, 2])
    g.modw = din("mod_w", [2, D, 6 * D])
    g.modb = din("modb", [128, 2, 48])
    g.normg = din("normg", [128, 2, 2, 8])
    g.fnorm = din("fnorm", [128, 8])
    g.out = Buf("out", nc.dram_tensor("out", [TL, D], F32, kind="ExternalOutput"))
    g.cosq96 = din("cosq96", [96, TT]); g.sinq96 = din("sinq96", [96, TT])
    g.cos32 = din("cos32", [32, TT]); g.sin32 = din("sin32", [32, TT])
    g.ev_w_in = din("ev_w_in", [1024, 2752]); g.ev_w_uq = din("ev_w_uq", [384, 768]); g.ev_w_ukv = din("ev_w_ukv", [256, 1024])
    g.qn_col = din("qn_col", [128, 3]); g.kvn_col = din("kvn_col", [128, 2])
    g.ev_w_out = din("ev_w_out", [1024, 1024]); g.ev_wgu = din("ev_wgu", [1024, 5632]); g.ev_wdn = din("ev_wdn", [2816, 1024])

    g.od_w_in = din("od_w_in", [1024, 2304]); g.od_w_out = din("od_w_out", [1024, 1024])
    g.gcols = din("gcols", [128, 4]); g.dlam = din("dlam", [128, 4, 64]); g.dncol = din("dncol", [128, 1])
    g.cosq64 = din("cosq64", [128, TT]); g.sinq64 = din("sinq64", [128, TT])
    g.cos64 = din("cos64", [128, TT]); g.sin64 = din("sin64", [128, TT])
    g.router = din("router", [1024, 8]); g.moe_gu = din("moe_gu", [8, 1024, 7168]); g.moe_dn = din("moe_dn", [8, 3584, 1024])
    g.conv_col = din("conv_col", [128, 12, 5]); g.alog_rep = din("alog_rep", [128, 16]); g.dtb_rep = din("dtb_rep", [128, 16])
    g.onrm_rep = din("onrm_rep", [128, 8, 64])
    import math
    g.lambda_init = 0.8 - 0.6 * math.exp(-0.3 * 1)

    with ExitStack() as st:
        S = Sched(nc, st)
        g.S = S
        g.xT_s = S.dram("xT_s", [8, 128, TT], F32)
        phase_const(g)
        phase_mod(g)
        phase_x0(g)
        S.barrier()
        if "l0" in stages or "p1" in stages:
            phase_l0_p1(g)
            S.barrier()
        if "l0" in stages or "mla" in stages:
            phase_l0_mla(g)
            S.barrier()
        if "gdn" in stages:
            phase_gdn_prep(g)
            S.barrier()
            phase_gdn(g)
            S.barrier()
        if "l0" in stages or "wout" in stages:
            phase_wout(g, 0, g.ev_w_out)
            S.barrier()
        if "l0" in stages or "ffn" in stages:
            phase_ffn0(g)
            S.barrier()
        if "l1" in stages or "l1a" in stages:
            phase_l1_p1(g)
            S.barrier()
            phase_l1_attn(g)
            S.barrier()
            phase_wout(g, 1, g.od_w_out)
            S.barrier()
        if "l1" in stages or "moe" in stages:
            phase_moe_a(g)
            S.barrier()
            phase_moe_b(g)
            S.barrier()
        phase_final(g)
        S.wait_all("sp", S.out_tokens)
        S.emit()
    return nc


def dbg_out(g, name, buf, ap, shape, dt=F32):
    S = g.S
    o = Buf(name, g.nc.dram_tensor(name, list(shape), dt, kind="ExternalOutput"))
    idx = tuple(slice(None) for _ in shape)
    tok = S.dma("pool", o.t[idx], ap, reads=[buf], writes=[o])
    S.out_tokens.append(tok)


def phase_const(g):
    S = g.S
    g.identF = S.sb("identF", [128, 128], F32)
    g.identB = S.sb("identB", [128, 128], BF16)
    g.onesF = S.sb("onesF", [128, 128], F32)
    g.onesB = S.sb("onesB", [128, 128], BF16)
    _ms(S, "pool", g.onesF[:, :], 1.0, [g.onesF])
    _ms(S, "pool", g.identF[:, :], 1.0, [g.identF])
    S.op("pool", lambda e: e.affine_select(out=g.identF[:, :], in_=g.identF[:, :], pattern=[[-1, 128]],
                                           compare_op=ALU.is_equal, fill=0.0, base=0, channel_multiplier=1),
         reads=[g.identF], writes=[g.identF])
    g.sel65 = S.sb("sel65", [65, 64], F32)
    _ms(S, "pool", g.sel65[:, :], 0.0, [g.sel65])
    _ms(S, "pool", g.sel65[64:65, :], 1.0, [g.sel65])
    _cp(S, "dve", g.identB[:, :], g.identF[:, :], [g.identF], [g.identB])
    _cp(S, "dve", g.onesB[:, :], g.onesF[:, :], [g.onesF], [g.onesB])


def phase_mod(g):
    S = g.S
    nc = g.nc
    g.modT = S.sb("modT", [128, 2, 48, 2], F32)
    g.modA = S.sb("modA", [128, 2, 2, 8, 2], F32)
    g.fn = S.sb("fn", [128, 8], F32)
    with ExitStack() as ph:
        cc = S.sb("cc", [128, 8, 2], F32, ph)
        sc = S.sb("sc", [128, 8, 2], F32, ph)
        mb = S.sb("mb", [128, 2, 48], F32, ph)
        ng = S.sb("ng", [128, 2, 2, 8], F32, ph)
        wb = [S.sb("wb%d" % i, [128, 8, 768], F32, ph) for i in range(2)]
        psm = S.ps("psm", [128, 2, 48, 2], F32, ph)
        S.dma("sp", cc[:, :, :], g.ccol[:, :, :], reads=[g.ccol], writes=[cc])
        S.dma("sp", mb[:, :, :], g.modb[:, :, :], reads=[g.modb], writes=[mb])
        S.dma("sp", ng[:, :, :, :], g.normg[:, :, :, :], reads=[g.normg], writes=[ng])
        S.dma("sp", g.fn[:, :], g.fnorm[:, :], reads=[g.fnorm], writes=[g.fn])
        _act(S, sc[:, :, :], cc[:, :, :], AF.Silu, [cc], [sc])
        i = 0
        for l in range(2):
            wv = g.modw[l].rearrange("(kc p) n -> p kc n", p=128)
            for blk in range(8):
                w = wb[i % 2]
                i += 1
                S.dma("sp", w[:, :, :], wv[:, :, blk * 768:(blk + 1) * 768], reads=[g.modw], writes=[w])
                for fs in range(6):
                    s = blk * 6 + fs
                    for kc in range(8):
                        _mm(S, psm[:, l, s, :], w[:, kc, fs * 128:(fs + 1) * 128], sc[:, kc, :], kc == 0, kc == 7,
                            [w, sc], [(psm, (l, s))])
            for j in range(2):
                _tt(S, "dve", g.modT[:, l, :, j], psm[:, l, :, j], mb[:, l, :], ALU.add, [psm, mb], [(g.modT, (l, j))])
            for u in range(2):
                for j in range(2):
                    _stt(S, "dve", g.modA[:, l, u, :, j], g.modT[:, l, (3 * u + 1) * 8:(3 * u + 2) * 8, j], 1.0,
                         ng[:, l, u, :], ALU.add, ALU.mult, [g.modT, ng], [(g.modA, (l, u, j))])
        if "mod" in g.dbgset:
            dbg_out(g, "dbg_modT", g.modT, g.modT[:, :, :, :], [128, 2, 48, 2])
            dbg_out(g, "dbg_modA", g.modA, g.modA[:, :, :, :, :], [128, 2, 2, 8, 2])
        S.emit()


def tile_src(g, t0, nt):
    if t0 < g.TC:
        src, r0 = g.ctx_in, t0
    else:
        src, r0 = g.x_in, t0 - g.TC
    return src, src[r0:r0 + nt, :].rearrange("(s p) d -> p s d", p=128)


def phase_x0(g):
    S = g.S
    with ExitStack() as ph:
        xin = [S.sb("xin%d" % i, [128, 4, D], F32, ph) for i in range(2)]
        xt = [S.sb("xt%d" % i, [128, 8, 512], F32, ph) for i in range(2)]
        pst = [S.ps("pst%d" % i, [128, 512], F32, ph) for i in range(4)]
        k = 0
        for ti, (t0, nt) in enumerate(g.tiles):
            ns = nt // 128
            a = xin[ti % 2]
            b = xt[ti % 2]
            srcb, sap = tile_src(g, t0, nt)
            S.dma("sp", a[:, 0:ns, :], sap, reads=[srcb], writes=[a])
            for c in range(8):
                p = pst[k % 4]
                k += 1
                for s in range(ns):
                    _tr(S, p[:, s * 128:(s + 1) * 128], a[:, s, c * 128:(c + 1) * 128], g.identF[:, :],
                        [a, g.identF], [p])
                _cp(S, "dve" if c % 2 == 0 else "act", b[:, c, 0:nt], p[:, 0:nt], [p], [(b, c)])
            S.dma("pool", g.xT_s.t[:, :, t0:t0 + nt].rearrange("c p t -> p c t"), b[:, :, 0:nt],
                  reads=[b], writes=[(g.xT_s, ti)])
        S.emit()


def rstd_from_ss(S, eng, out_ap, ps_ap, n, R, W):
    _ts(S, eng, out_ap, ps_ap, 1.0 / n, EPS, ALU.mult, ALU.add, R, W)
    _act(S, out_ap, out_ap, AF.Sqrt, W, W)
    S.op("dve", lambda e: e.reciprocal(out=out_ap, in_=out_ap), reads=W, writes=W)


def phase_final(g):
    S = g.S
    with ExitStack() as ph:
        xt = [S.sb("fxt%d" % i, [128, 8, 512], F32, ph) for i in range(2)]
        sq = S.sb("fsq", [128, 8, 512], F32, ph)
        rs = S.sb("frs", [128, 512], F32, ph)
        yo = [S.sb("fyo%d" % i, [128, 4, D], F32, ph) for i in range(2)]
        pss = S.ps("fpss", [128, 512], F32, ph)
        pst = [S.ps("fpst%d" % i, [128, 512], F32, ph) for i in range(4)]
        k = 0
        for ti, (t0, nt) in enumerate(g.tiles):
            if t0 < g.TC:
                continue
            ns = nt // 128
            a = xt[ti % 2]
            y = yo[ti % 2]
            S.dma("sp", a[:, :, 0:nt], g.xT_s.t[:, :, t0:t0 + nt].rearrange("c p t -> p c t"),
                  reads=[(g.xT_s, ti)], writes=[a])
            for c in range(8):
                _tt(S, "pool" if c % 2 else "dve", sq[:, c, 0:nt], a[:, c, 0:nt], a[:, c, 0:nt], ALU.mult, [a], [(sq, c)])
            for c in range(8):
                _mm(S, pss[:, 0:nt], g.onesF[:, :], sq[:, c, 0:nt], c == 0, c == 7, [g.onesF, (sq, c)], [pss])
            rstd_from_ss(S, "dve", rs[:, 0:nt], pss[:, 0:nt], D, [pss], [rs])
            for c in range(8):
                _stt(S, "dve", sq[:, c, 0:nt], a[:, c, 0:nt], g.fn[:, c:c + 1], rs[:, 0:nt],
                     ALU.mult, ALU.mult, [a, g.fn, rs, (sq, c)], [(sq, c)])
            for s in range(ns):
                for h in range(2):
                    p = pst[k % 4]
                    k += 1
                    for cc in range(4):
                        c = h * 4 + cc
                        _tr(S, p[:, cc * 128:(cc + 1) * 128], sq[:, c, s * 128:(s + 1) * 128], g.identF[:, :],
                            [(sq, c), g.identF], [p])
                    _cp(S, "act" if h else "dve", y[:, s, h * 512:(h + 1) * 512], p[:, :], [p], [(y, (s, h))])
            r0 = t0 - g.TC
            tok = S.dma("pool", g.out.t[r0:r0 + nt, :].rearrange("(s p) d -> p s d", p=128), y[:, 0:ns, :],
                        reads=[y], writes=[(g.out, ti)])
            S.out_tokens.append(tok)
        S.emit()


def load_w(g, ph, name, src_buf, src_ap, K, N, stg):
    S = g.S
    kc = K // 128
    w = S.sb(name, [128, kc, N], BF16, ph)
    v = src_ap.rearrange("(kc p) n -> p kc n", p=128)
    i = 0
    for k0 in range(0, kc, 8):
        k1 = min(kc, k0 + 8)
        for n0 in range(0, N, 512):
            n1 = min(N, n0 + 512)
            s = stg[g.stg_i % len(stg)]
            g.stg_i += 1
            S.dma("sp", s[:, 0:k1 - k0, 0:n1 - n0], v[:, k0:k1, n0:n1], reads=[src_buf], writes=[s])
            eng = ("dve", "pool", "act")[g.stg_i % 3]
            _cp(S, eng, w[:, k0:k1, n0:n1], s[:, 0:k1 - k0, 0:n1 - n0], [s], [(w, (k0, n0))])
    return w


def make_rot(g, rot, w, view, off, n):
    S = g.S
    q = n // 4
    rv, wv = view(rot), view(w)
    _ms(S, "pool", rot[:, :, :], 0.0, [rot])
    for (d0, s0, sign) in ((0, q, -1.0), (q, 0, 1.0), (2 * q, 3 * q, -1.0), (3 * q, 2 * q, 1.0)):
        _ts(S, "dve", rv[:, :, :, off + d0:off + d0 + q], wv[:, :, :, off + s0:off + s0 + q], sign, None, ALU.mult, None,
            [w, rot], [rot])


def norm_mod(g, a, sq, rs, hT, pss, nt, A_ap, B_ap, n=D, nchunk=8, xr=()):
    S = g.S
    for c in range(nchunk):
        _tt(S, "pool" if c % 2 else "dve", sq[:, c, 0:nt], a[:, c, 0:nt], a[:, c, 0:nt], ALU.mult, [(a, c)], [(sq, c)])
    for c in range(nchunk):
        _mm(S, pss[:, 0:nt], g.onesF[:, :], sq[:, c, 0:nt], c == 0, c == nchunk - 1, [g.onesF, (sq, c)], [pss])
    rstd_from_ss(S, "dve", rs[:, 0:nt], pss[:, 0:nt], n, [pss], [rs])
    for c in range(nchunk):
        _stt(S, "dve", sq[:, c, 0:nt], a[:, c, 0:nt], A_ap(c), rs[:, 0:nt], ALU.mult, ALU.mult,
             [(a, c), rs, (sq, c)] + list(xr), [(sq, c)])
        if B_ap is None:
            _cp(S, "act", hT[:, c, 0:nt], sq[:, c, 0:nt], [(sq, c)], [(hT, c)])
        else:
            _act(S, hT[:, c, 0:nt], sq[:, c, 0:nt], AF.Identity, [(sq, c)] + list(xr), [(hT, c)], bias=B_ap(c))


def xT_view(g, t0, nt):
    return g.xT_s.t[:, :, t0:t0 + nt].rearrange("c p t -> p c t")


def phase_l0_p1(g):
    S = g.S
    nc = g.nc
    TT = g.TT
    g.qT_s = S.dram("qT_s", [8, 96, TT], BF16)
    g.kT_s = S.dram("kT_s", [8, 96, TT], BF16)
    g.v_s = S.dram("v_s", [TT, 8, 65], BF16)
    g.mixT_s = S.dram("mixT_s", [1024, TT], BF16)
    g.gq_s = S.dram("gq_s", [12, 128, TT], F32)
    g.ab_s = S.dram("ab_s", [32, TT], F32)
    g.z_s = S.dram("z_s", [TT, 512], F32)
    with ExitStack() as ph:
        g.stg_i = 0
        with ExitStack() as ph2:
            stg = [S.sb("stg%d" % i, [128, 8, 512], F32, ph2, side="right") for i in range(2)]
            w_in = load_w(g, ph, "w_in0", g.ev_w_in, g.ev_w_in.t[:, :], 1024, 2752, stg)
            w_uq = load_w(g, ph, "w_uq", g.ev_w_uq, g.ev_w_uq.t[:, :], 384, 768, stg)
            w_ukv = load_w(g, ph, "w_ukv", g.ev_w_ukv, g.ev_w_ukv.t[:, :], 256, 1024, stg)
            S.barrier()
            S.emit()
        w_uq_r = S.sb("w_uq_r", [128, 3, 768], BF16, ph)
        w_kpe_r = S.sb("w_kpe_r", [128, 8, 32], BF16, ph)
        make_rot(g, w_uq_r, w_uq, lambda b: b[:, :, :].rearrange("p k (h e) -> p k h e", e=96), 64, 32)
        _ms(S, "pool", w_kpe_r[:, :, :], 0.0, [w_kpe_r])
        for (d0, s0, sign) in ((0, 8, -1.0), (8, 0, 1.0), (16, 24, -1.0), (24, 16, 1.0)):
            _ts(S, "dve", w_kpe_r[:, :, d0:d0 + 8], w_in[:, :, 640 + s0:640 + s0 + 8], sign, None, ALU.mult, None,
                [w_in, w_kpe_r], [w_kpe_r])
        qn = S.sb("qn", [128, 3], F32, ph)
        kvn = S.sb("kvn", [128, 2], F32, ph)
        S.dma("sp", qn[:, :], g.qn_col[:, :], reads=[g.qn_col], writes=[qn])
        S.dma("sp", kvn[:, :], g.kvn_col[:, :], reads=[g.kvn_col], writes=[kvn])

        xa = [S.sb("p1xa%d" % i, [128, 8, 512], F32, ph) for i in range(2)]
        sq = S.sb("p1sq", [128, 8, 512], F32, ph)
        rs = S.sb("p1rs", [128, 512], F32, ph)
        hT = S.sb("p1hT", [128, 8, 512], BF16, ph)
        cq = S.sb("p1cq", [128, 3, 512], F32, ph)
        cqn = S.sb("p1cqn", [128, 3, 512], BF16, ph)
        ckv = S.sb("p1ckv", [128, 2, 512], F32, ph)
        ckvn = S.sb("p1ckvn", [128, 2, 512], BF16, ph)
        tq = [S.sb("p1tq%d" % i, [96, 512], F32, ph) for i in range(4)]
        qo = [S.sb("p1qo%d" % i, [96, 512], BF16, ph) for i in range(2)]
        kn = S.sb("p1kn", [64, 8, 512], BF16, ph)
        kp = S.sb("p1kp", [32, 512], BF16, ph)
        va = S.sb("p1va", [128, 4, 8, 65], BF16, ph)
        gqo = [S.sb("p1gq%d" % i, [128, 512], F32, ph) for i in range(3)]
        zo = [S.sb("p1zo%d" % i, [128, 512], F32, ph) for i in range(2)]
        cs = S.sb("p1cs", [96, 512], F32, ph)
        sn = S.sb("p1sn", [96, 512], F32, ph)
        ck = S.sb("p1ck", [32, 512], F32, ph)
        sk = S.sb("p1sk", [32, 512], F32, ph)
        pss = S.ps("p1pss", [128, 512], F32, ph)
        pp = [S.ps("p1pp%d" % i, [128, 512], F32, ph) for i in range(6)]
        _ms(S, "pool", va[:, :, :, :], 1.0, [va])
        pi = [0]

        def nextp():
            p = pp[pi[0] % 6]
            pi[0] += 1
            return p

        ev = [0]

        def evac(out_ap, in_ap, R, W):
            ev[0] += 1
            _cp(S, "act" if ev[0] % 2 else "dve", out_ap, in_ap, R, W)

        for ti, (t0, nt) in enumerate(g.tiles):
            j = 1 if t0 < g.TC else 0
            ns = nt // 128
            a = xa[ti % 2]
            S.dma("sp", a[:, :, 0:nt], xT_view(g, t0, nt), reads=[(g.xT_s, ti)], writes=[a])
            S.dma("sp", cs[:, 0:nt], g.cosq96[:, t0:t0 + nt], reads=[g.cosq96], writes=[cs])
            S.dma("sp", sn[:, 0:nt], g.sinq96[:, t0:t0 + nt], reads=[g.sinq96], writes=[sn])
            S.dma("sp", ck[:, 0:nt], g.cos32[:, t0:t0 + nt], reads=[g.cos32], writes=[ck])
            S.dma("sp", sk[:, 0:nt], g.sin32[:, t0:t0 + nt], reads=[g.sin32], writes=[sk])
            norm_mod(g, a, sq, rs, hT, pss, nt, lambda c: g.modA[:, 0, 0, c, j:j + 1], lambda c: g.modT[:, 0, c, j:j + 1])

            def proj(col0, m, lhs_w=w_in, rhs=hT, nk=8):
                p = nextp()
                for kc in range(nk):
                    _mm(S, p[0:m, 0:nt], lhs_w[:, kc, col0:col0 + m], rhs[:, kc, 0:nt], kc == 0, kc == nk - 1,
                        [lhs_w, rhs], [p])
                return p

            for c in range(3):
                p = proj(c * 128, 128)
                evac(cq[:, c, 0:nt], p[:, 0:nt], [p], [(cq, c)])
            for c in range(2):
                p = proj(384 + c * 128, 128)
                evac(ckv[:, c, 0:nt], p[:, 0:nt], [p], [(ckv, c)])
            norm_mod(g, cq, sq, rs, cqn, pss, nt, lambda c: qn[:, c:c + 1], None, n=384, nchunk=3, xr=[qn])
            norm_mod(g, ckv, sq, rs, ckvn, pss, nt, lambda c: kvn[:, c:c + 1], None, n=256, nchunk=2, xr=[kvn])
            for h in range(8):
                p1 = proj(h * 96, 96, w_uq, cqn, 3)
                p2 = proj(h * 96, 96, w_uq_r, cqn, 3)
                t = tq[h % 2]
                t2 = tq[2 + h % 2]
                o = qo[h % 2]
                _tt(S, "dve", t[:, 0:nt], p1[0:96, 0:nt], cs[:, 0:nt], ALU.mult, [p1, cs], [t])
                _tt(S, "dve", t2[:, 0:nt], p2[0:96, 0:nt], sn[:, 0:nt], ALU.mult, [p2, sn], [t2])
                _tt(S, "pool", o[:, 0:nt], t2[:, 0:nt], t[:, 0:nt], ALU.add, [t2, t], [o])
                S.dma("pool", g.qT_s.t[h, :, t0:t0 + nt], o[:, 0:nt], reads=[o], writes=[(g.qT_s, (h, ti))])
            for h in range(8):
                p = proj(h * 128, 64, w_ukv, ckvn, 2)
                evac(kn[:, h, 0:nt], p[0:64, 0:nt], [p], [(kn, h)])
            S.dma("pool", g.kT_s.t[:, 0:64, t0:t0 + nt].rearrange("h p t -> p h t"), kn[:, :, 0:nt],
                  reads=[kn], writes=[(g.kT_s, ("n", ti))])
            p1 = proj(640, 32)
            p2 = proj(0, 32, w_kpe_r, hT, 8)
            t = tq[0]
            t2 = tq[2]
            _tt(S, "dve", t[0:32, 0:nt], p1[0:32, 0:nt], ck[:, 0:nt], ALU.mult, [p1, ck], [t])
            _tt(S, "dve", t2[0:32, 0:nt], p2[0:32, 0:nt], sk[:, 0:nt], ALU.mult, [p2, sk], [t2])
            _tt(S, "pool", kp[:, 0:nt], t2[0:32, 0:nt], t[0:32, 0:nt], ALU.add, [t2, t], [kp])
            for h in range(8):
                S.dma("pool", g.kT_s.t[h, 64:96, t0:t0 + nt], kp[:, 0:nt], reads=[kp], writes=[(g.kT_s, ("p", h, ti))])
            vw = w_ukv[:, :, :].rearrange("p k (h e) -> p k h e", e=128)
            for s in range(ns):
                p = nextp()
                for kc in range(2):
                    _mm(S, p[:, :].rearrange("p (h e) -> p h e", e=64), ckvn[:, kc, s * 128:(s + 1) * 128],
                        vw[:, kc, :, 64:128], kc == 0, kc == 1, [ckvn, w_ukv], [p])
                evac(va[:, s, :, 0:64], p[:, :].rearrange("p (h e) -> p h e", e=64), [p], [(va, s)])
            S.dma("pool", g.v_s.t[t0:t0 + nt, :, :].rearrange("(s p) h e -> p s h e", p=128), va[:, 0:ns, :, :],
                  reads=[va], writes=[(g.v_s, ti)])
            for c in range(12):
                p = proj(672 + c * 128, 128)
                o = gqo[c % 3]
                evac(o[:, 0:nt], p[:, 0:nt], [p], [o])
                S.dma("pool", g.gq_s.t[c, :, t0:t0 + nt], o[:, 0:nt], reads=[o], writes=[(g.gq_s, (c, ti))])
            p = proj(2720, 32)
            o = gqo[0]
            evac(o[0:32, 0:nt], p[0:32, 0:nt], [p], [o])
            S.dma("pool", g.ab_s.t[:, t0:t0 + nt], o[0:32, 0:nt], reads=[o], writes=[(g.ab_s, ti)])
            for s in range(ns):
                p = nextp()
                for kc in range(8):
                    _mm(S, p[:, :], hT[:, kc, s * 128:(s + 1) * 128], w_in[:, kc, 2208:2720], kc == 0, kc == 7,
                        [hT, w_in], [p])
                o = zo[s % 2]
                _act(S, o[:, :], p[:, :], AF.Silu, [p], [o])
                S.dma("pool", g.z_s.t[t0 + s * 128:t0 + (s + 1) * 128, :], o[:, :], reads=[o], writes=[(g.z_s, (ti, s))])
        S.emit()


def attn_core(g, KT, V, QT, d, dva, kbs, acc, psc, pT, nq, cnt):
    S = g.S
    nkb = len(kbs)
    LA = len(psc) - 1
    slots = []

    def score(i):
        kb = kbs[i]
        sc = psc[cnt[0] % len(psc)]
        p = pT[cnt[0] % len(pT)]
        cnt[0] += 1
        _mm(S, sc[:, 0:nq], KT[0:d, kb * 128:(kb + 1) * 128], QT[0:d, 0:nq], True, True, [KT, QT], [sc])
        _act(S, p[:, 0:nq], sc[:, 0:nq], AF.Exp, [sc], [p])
        slots.append(p)

    for i in range(min(LA, nkb)):
        score(i)
    for i, kb in enumerate(kbs):
        if i + LA < nkb:
            score(i + LA)
        p = slots[i]
        for qs in range(nq // 128):
            _mm(S, acc[:, qs, 0:dva], p[:, qs * 128:(qs + 1) * 128], V[:, kb, 0:dva], i == 0 and qs == 0,
                i == nkb - 1 and qs == nq // 128 - 1, [p, V], [(acc, qs)])


def attn_core3(g, KT, V, QT, d, dv, kbs, accO, Pacc, psc, pT, nq, cnt):
    S = g.S
    nkb = len(kbs)
    LA = len(psc) - 1
    slots = []

    def score(i):
        kb = kbs[i]
        sc = psc[cnt[0] % len(psc)]
        p = pT[cnt[0] % len(pT)]
        cnt[0] += 1
        _mm(S, sc[:, 0:nq], KT[0:d, kb * 128:(kb + 1) * 128], QT[0:d, 0:nq], True, True, [KT, QT], [sc])
        _act(S, p[:, 0:nq], sc[:, 0:nq], AF.Exp, [sc], [p])
        slots.append(p)

    for i in range(min(LA, nkb)):
        score(i)
    seen = {"dve": False, "pool": False}
    for i, kb in enumerate(kbs):
        if i + LA < nkb:
            score(i + LA)
        p = slots[i]
        _mm(S, accO[0:dv, 0:nq], V[:, kb, 0:dv], p[:, 0:nq], i == 0, i == nkb - 1, [p, V], [accO])
        if Pacc is not None:
            eng = "pool" if i % 5 in (1, 3) else "dve"
            pa = Pacc[1] if eng == "pool" else Pacc[0]
            if not seen[eng]:
                seen[eng] = True
                _cp(S, eng, pa[:, 0:nq], p[:, 0:nq], [p], [pa])
            else:
                _tt(S, eng, pa[:, 0:nq], pa[:, 0:nq], p[:, 0:nq], ALU.add, [pa, p], [pa])


def attn_simple_post(g, accO, psm, oS, rec, o, nq, dst_ap, dst_dep):
    S = g.S
    _cp(S, "dve", oS[0:65, 0:nq], accO[0:65, 0:nq], [accO], [oS])
    _mm(S, psm[0:64, 0:nq], g.sel65[0:65, :], oS[0:65, 0:nq], True, True, [g.sel65, oS], [psm])
    S.op("dve", lambda e: e.reciprocal(out=rec[0:64, 0:nq], in_=psm[0:64, 0:nq]), reads=[psm], writes=[rec])
    _tt(S, "dve", o[0:64, 0:nq], oS[0:64, 0:nq], rec[0:64, 0:nq], ALU.mult, [oS, rec], [o])
    S.dma("pool", dst_ap, o[0:64, 0:nq], reads=[o], writes=[dst_dep])


def phase_l0_mla(g):
    S = g.S
    TT = g.TT
    NKB = TT // 128
    with ExitStack() as ph:
        KT = [S.sb("mKT%d" % i, [96, TT], BF16, ph) for i in range(2)]
        V = [S.sb("mV%d" % i, [128, NKB, 65], BF16, ph) for i in range(2)]
        QT = [S.sb("mQT%d" % i, [96, 512], BF16, ph) for i in range(2)]
        pT = [S.sb("mpT%d" % i, [128, 512], BF16, ph) for i in range(4)]
        oS = [S.sb("moS%d" % i, [65, 512], F32, ph) for i in range(2)]
        rec = S.sb("mrec", [64, 512], F32, ph)
        o = [S.sb("mo%d" % i, [64, 512], BF16, ph) for i in range(2)]
        psc = [S.ps("mpsc%d" % i, [128, 512], F32, ph) for i in range(4)]
        acc = [S.ps("macc%d" % i, [128, 512], F32, ph) for i in range(2)]
        psm = S.ps("mpsm", [128, 512], F32, ph)
        cnt = [0]
        qi = 0
        if not g.has_gdn:
            zt = S.sb("mzt", [128, 4, 512], BF16, ph)
            _ms(S, "pool", zt[:, :, :], 0.0, [zt])
            for ti, (t0, nt) in enumerate(g.tiles):
                S.dma("pool", g.mixT_s.t[512:1024, t0:t0 + nt].rearrange("(c p) t -> p c t", p=128), zt[:, :, 0:nt],
                      reads=[zt], writes=[(g.mixT_s, ("z", ti))])
        for h in range(8):
            kt, v = KT[h % 2], V[h % 2]
            S.dma("sp", kt[:, :], g.kT_s.t[h, :, :], reads=[g.kT_s], writes=[kt])
            S.dma("sp", v[:, :, :], g.v_s.t[:, h, :].rearrange("(n p) e -> p n e", p=128), reads=[g.v_s], writes=[v])
            for ti, (t0, nt) in enumerate(g.tiles):
                q = QT[qi % 2]
                ac = acc[qi % 2]
                os_ = oS[qi % 2]
                oo = o[qi % 2]
                qi += 1
                S.dma("sp", q[:, 0:nt], g.qT_s.t[h, :, t0:t0 + nt], reads=[g.qT_s], writes=[q])
                kbs = list(range(g.TC // 128)) if t0 < g.TC else list(range(NKB))
                attn_core3(g, kt, v, q, 96, 65, kbs, ac, None, psc, pT, nt, cnt)
                attn_simple_post(g, ac, psm, os_, rec, oo, nt, g.mixT_s.t[h * 64:(h + 1) * 64, t0:t0 + nt], (g.mixT_s, (h, ti)))
        S.emit()


def phase_wout(g, layer, w_src):
    S = g.S
    with ExitStack() as ph:
        with ExitStack() as ph2:
            stg = [S.sb("wo%d_stg%d" % (layer, i), [128, 8, 512], F32, ph2, side="right") for i in range(2)]
            w = load_w(g, ph, "w_out%d" % layer, w_src, w_src.t[:, :], 1024, 1024, stg)
            S.barrier()
            S.emit()
        xa = [S.sb("wo%dxa%d" % (layer, i), [128, 8, 512], F32, ph) for i in range(2)]
        m = [S.sb("wo%dm%d" % (layer, i), [128, 8, 512], BF16, ph) for i in range(2)]
        pp = [S.ps("wo%dpp%d" % (layer, i), [128, 512], F32, ph) for i in range(4)]
        k = 0
        for ti, (t0, nt) in enumerate(g.tiles):
            j = 1 if t0 < g.TC else 0
            if j == 1 and layer == 1:
                continue
            a, mm_ = xa[ti % 2], m[ti % 2]
            S.dma("sp", a[:, :, 0:nt], xT_view(g, t0, nt), reads=[(g.xT_s, ti)], writes=[a])
            S.dma("sp", mm_[:, :, 0:nt], g.mixT_s.t[:, t0:t0 + nt].rearrange("(c p) t -> p c t", p=128),
                  reads=[g.mixT_s], writes=[mm_])
            for dsl in range(8):
                p = pp[k % 4]
                k += 1
                for kc in range(8):
                    _mm(S, p[:, 0:nt], w[:, kc, dsl * 128:(dsl + 1) * 128], mm_[:, kc, 0:nt], kc == 0, kc == 7, [w, mm_], [p])
                _stt(S, "dve", a[:, dsl, 0:nt], p[:, 0:nt], g.modT[:, layer, 16 + dsl, j:j + 1], a[:, dsl, 0:nt],
                     ALU.mult, ALU.add, [p, (a, dsl)], [(a, dsl)])
            S.dma("pool", xT_view(g, t0, nt), a[:, :, 0:nt], reads=[a], writes=[(g.xT_s, ti)])
        S.emit()


def phase_ffn0(g):
    S = g.S
    NT = 256
    with ExitStack() as ph:
        with ExitStack() as ph2:
            stg = [S.sb("ff_stg%d" % i, [128, 8, 512], F32, ph2, side="right") for i in range(2)]
            wgu = load_w(g, ph, "ff_wgu", g.ev_wgu, g.ev_wgu.t[:, :], 1024, 5632, stg)
            wdn = load_w(g, ph, "ff_wdn", g.ev_wdn, g.ev_wdn.t[:, :], 2816, 1024, stg)
            S.barrier()
            S.emit()
        xa = [S.sb("ffxa%d" % i, [128, 8, NT], F32, ph) for i in range(2)]
        sq = S.sb("ffsq", [128, 8, NT], F32, ph)
        rs = S.sb("ffrs", [128, NT], F32, ph)
        hT = S.sb("ffhT", [128, 8, NT], BF16, ph)
        sg = [S.sb("ffsg%d" % i, [128, NT], F32, ph) for i in range(3)]
        act = [S.sb("ffact%d" % i, [128, NT], BF16, ph) for i in range(4)]
        pgu = [S.ps("ffpgu%d" % i, [128, 512], F32, ph) for i in range(4)]
        pss = pgu[0]
        yac = [S.ps("ffy%d" % i, [128, 2, NT], F32, ph) for i in range(4)]
        ti2 = 0
        for ti, (t0, nt) in enumerate(g.tiles):
            j = 1 if t0 < g.TC else 0
            for u0 in range(0, nt, NT):
                a = xa[ti2 % 2]
                ti2 += 1
                S.dma("sp", a[:, :, :], xT_view(g, t0 + u0, NT), reads=[(g.xT_s, ti)], writes=[a])
                norm_mod(g, a, sq, rs, hT, pss, NT, lambda c: g.modA[:, 0, 1, c, j:j + 1], lambda c: g.modT[:, 0, 24 + c, j:j + 1])
                def gu(f):
                    pg = pgu[(f % 2) * 2]
                    pu = pgu[(f % 2) * 2 + 1]
                    for kc in range(8):
                        _mm(S, pg[:, 0:NT], wgu[:, kc, f * 128:(f + 1) * 128], hT[:, kc, :], kc == 0, kc == 7, [wgu, hT], [pg])
                    for kc in range(8):
                        _mm(S, pu[:, 0:NT], wgu[:, kc, 2816 + f * 128:2816 + (f + 1) * 128], hT[:, kc, :], kc == 0, kc == 7,
                            [wgu, hT], [pu])
                    s_ = sg[f % 3]
                    ac = act[f % 4]
                    _act(S, s_[:, :], pg[:, 0:NT], AF.Silu, [pg], [s_])
                    _tt(S, "dve", ac[:, :], s_[:, :], pu[:, 0:NT], ALU.mult, [s_, pu], [ac])
                    return ac

                acs = {0: gu(0), 1: gu(1)}
                for f in range(22):
                    if f + 2 < 22:
                        acs[f + 2] = gu(f + 2)
                    ac = acs.pop(f)
                    for dsl in range(8):
                        _mm(S, yac[dsl // 2][:, dsl % 2, :], wdn[:, f, dsl * 128:(dsl + 1) * 128], ac[:, :],
                            f == 0 and dsl % 2 == 0, f == 21 and dsl % 2 == 1, [wdn, ac], [(yac[dsl // 2], dsl % 2)])
                for dsl in range(8):
                    _stt(S, "dve", a[:, dsl, :], yac[dsl // 2][:, dsl % 2, :], g.modT[:, 0, 40 + dsl, j:j + 1], a[:, dsl, :],
                         ALU.mult, ALU.add, [(yac[dsl // 2], dsl % 2), (a, dsl)], [(a, dsl)])
                S.dma("pool", xT_view(g, t0 + u0, NT), a[:, :, :], reads=[a], writes=[(g.xT_s, ti)])
        S.emit()


def phase_l1_p1(g):
    S = g.S
    TT = g.TT
    g.dqT_s = S.dram("dqT_s", [512, TT], BF16)
    g.dkT_s = S.dram("dkT_s", [512, TT], BF16)
    g.gqT_s = S.dram("gqT_s", [512, TT], BF16)
    g.gkT_s = S.dram("gkT_s", [128, TT], BF16)
    g.dv_s = S.dram("dv_s", [TT, 4, 129], BF16)
    g.gv_s = S.dram("gv_s", [TT, 2, 65], BF16)
    with ExitStack() as ph:
        with ExitStack() as ph2:
            stg = [S.sb("l1stg%d" % i, [128, 8, 512], F32, ph2, side="right") for i in range(2)]
            w = load_w(g, ph, "w_in1", g.od_w_in, g.od_w_in.t[:, :], 1024, 2304, stg)
            S.barrier()
            S.emit()
        wr = S.sb("w_in1r", [128, 8, 2304], BF16, ph)
        view = lambda b: b[:, :, :].rearrange("p k (h e) -> p k h e", e=64)
        make_rot(g, wr, w, view, 0, 64)
        gc = S.sb("l1gc", [128, 4], F32, ph)
        S.dma("sp", gc[:, :], g.gcols[:, :], reads=[g.gcols], writes=[gc])
        bd = S.sb("l1bd", [128, 128], F32, ph)
        _ms(S, "pool", bd[:, :], 0.0, [bd])
        _ms(S, "pool", bd[0:64, 0:64], 1.0, [bd])
        _ms(S, "pool", bd[64:128, 64:128], 1.0, [bd])
        xa = [S.sb("l1xa%d" % i, [128, 8, 512], F32, ph) for i in range(2)]
        sq = S.sb("l1sq", [128, 8, 512], F32, ph)
        rs = S.sb("l1rs", [128, 512], F32, ph)
        hT = S.sb("l1hT", [128, 8, 512], BF16, ph)
        cq = S.sb("l1cq", [128, 512], F32, ph)
        sq_ = S.sb("l1sq_", [128, 512], F32, ph)
        ck = S.sb("l1ck", [128, 512], F32, ph)
        sk = S.sb("l1sk", [128, 512], F32, ph)
        t1 = [S.sb("l1t1%d" % i, [128, 512], F32, ph) for i in range(2)]
        t2 = [S.sb("l1t2%d" % i, [128, 512], F32, ph) for i in range(2)]
        t3 = S.sb("l1t3", [128, 512], F32, ph)
        ob = [S.sb("l1ob%d" % i, [128, 512], BF16, ph) for i in range(3)]
        dva = S.sb("l1dva", [128, 4, 4, 129], BF16, ph)
        gva = S.sb("l1gva", [128, 4, 2, 65], BF16, ph)
        _ms(S, "pool", dva[:, :, :, :], 1.0, [dva])
        _ms(S, "pool", gva[:, :, :, :], 1.0, [gva])
        pss = S.ps("l1pss", [128, 512], F32, ph)
        pp = [S.ps("l1pp%d" % i, [128, 512], F32, ph) for i in range(6)]
        pi = [0]
        oi = [0]

        def nextp():
            p = pp[pi[0] % 6]
            pi[0] += 1
            return p

        for ti, (t0, nt) in enumerate(g.tiles):
            j = 1 if t0 < g.TC else 0
            ns = nt // 128
            a = xa[ti % 2]
            S.dma("sp", a[:, :, 0:nt], xT_view(g, t0, nt), reads=[(g.xT_s, ti)], writes=[a])
            S.dma("sp", cq[:, 0:nt], g.cosq64[:, t0:t0 + nt], reads=[g.cosq64], writes=[cq])
            S.dma("sp", sq_[:, 0:nt], g.sinq64[:, t0:t0 + nt], reads=[g.sinq64], writes=[sq_])
            S.dma("sp", ck[:, 0:nt], g.cos64[:, t0:t0 + nt], reads=[g.cos64], writes=[ck])
            S.dma("sp", sk[:, 0:nt], g.sin64[:, t0:t0 + nt], reads=[g.sin64], writes=[sk])
            norm_mod(g, a, sq, rs, hT, pss, nt, lambda c: g.modA[:, 1, 0, c, j:j + 1], lambda c: g.modT[:, 1, c, j:j + 1])

            def proj(wt, col0):
                p = nextp()
                for kc in range(8):
                    _mm(S, p[:, 0:nt], wt[:, kc, col0:col0 + 128], hT[:, kc, 0:nt], kc == 0, kc == 7, [wt, hT], [p])
                return p

            def roped(col0, cos_t, sin_t, dst, row0, gcol=None, grcol=None):
                p1 = proj(w, col0)
                p2 = proj(wr, col0)
                k = oi[0]
                oi[0] += 1
                a1, a2, o = t1[k % 2], t2[k % 2], ob[k % 3]
                if gcol is None:
                    _tt(S, "dve", a1[:, 0:nt], p1[:, 0:nt], cos_t[:, 0:nt], ALU.mult, [p1, cos_t], [a1])
                    _tt(S, "dve", a2[:, 0:nt], p2[:, 0:nt], sin_t[:, 0:nt], ALU.mult, [p2, sin_t], [a2])
                    _tt(S, "pool", o[:, 0:nt], a1[:, 0:nt], a2[:, 0:nt], ALU.add, [a1, a2], [o])
                else:
                    _act(S, t3[:, 0:nt], p1[:, 0:nt], AF.Square, [p1], [t3])
                    _mm(S, pss[:, 0:nt], bd[:, :], t3[:, 0:nt], True, True, [bd, t3], [pss])
                    rstd_from_ss(S, "dve", t3[:, 0:nt], pss[:, 0:nt], 64, [pss], [t3])
                    _stt(S, "dve", a1[:, 0:nt], p1[:, 0:nt], gcol, cos_t[:, 0:nt], ALU.mult, ALU.mult, [p1, cos_t, gc], [a1])
                    _stt(S, "dve", a2[:, 0:nt], p2[:, 0:nt], grcol, sin_t[:, 0:nt], ALU.mult, ALU.mult, [p2, sin_t, gc], [a2])
                    _tt(S, "pool", a1[:, 0:nt], a1[:, 0:nt], a2[:, 0:nt], ALU.add, [a1, a2], [a1])
                    _tt(S, "dve", o[:, 0:nt], a1[:, 0:nt], t3[:, 0:nt], ALU.mult, [a1, t3], [o])
                S.dma("pool", dst.t[row0:row0 + 128, t0:t0 + nt], o[:, 0:nt], reads=[o], writes=[(dst, (row0, ti))])

            for m in range(4):
                if j == 0:
                    roped(m * 128, cq, sq_, g.dqT_s, m * 128)
                roped(512 + m * 128, ck, sk, g.dkT_s, m * 128)
            if j == 0:
                for m in range(4):
                    roped(1536 + m * 128, cq, sq_, g.gqT_s, m * 128, gc[:, 0:1], gc[:, 1:2])
            roped(2048, ck, sk, g.gkT_s, 0, gc[:, 2:3], gc[:, 3:4])
            for s in range(ns):
                p = nextp()
                for kc in range(8):
                    _mm(S, p[:, :], hT[:, kc, s * 128:(s + 1) * 128], w[:, kc, 1024:1536], kc == 0, kc == 7, [hT, w], [p])
                _cp(S, "act", dva[:, s, :, 0:128], p[:, :].rearrange("p (h e) -> p h e", e=128), [p], [(dva, s)])
                p = nextp()
                for kc in range(8):
                    _mm(S, p[:, 0:128], hT[:, kc, s * 128:(s + 1) * 128], w[:, kc, 2176:2304], kc == 0, kc == 7, [hT, w], [p])
                _cp(S, "dve", gva[:, s, :, 0:64], p[:, 0:128].rearrange("p (h e) -> p h e", e=64), [p], [(gva, s)])
            S.dma("pool", g.dv_s.t[t0:t0 + nt, :, :].rearrange("(s p) h e -> p s h e", p=128), dva[:, 0:ns, :, :],
                  reads=[dva], writes=[(g.dv_s, ti)])
            S.dma("pool", g.gv_s.t[t0:t0 + nt, :, :].rearrange("(s p) h e -> p s h e", p=128), gva[:, 0:ns, :, :],
                  reads=[gva], writes=[(g.gv_s, ti)])
        S.emit()


def attn_core2(g, KT, V, QT, d, dva, kbs, accf, psc, pT, nq, cnt):
    S = g.S
    nkb = len(kbs)
    LA = len(psc) - 1
    slots = []

    def score(i):
        kb = kbs[i]
        sc = psc[cnt[0] % len(psc)]
        p = pT[cnt[0] % len(pT)]
        cnt[0] += 1
        _mm(S, sc[:, 0:nq], KT[0:d, kb * 128:(kb + 1) * 128], QT[0:d, 0:nq], True, True, [KT, QT], [sc])
        _act(S, p[:, 0:nq], sc[:, 0:nq], AF.Exp, [sc], [p])
        slots.append(p)

    for i in range(min(LA, nkb)):
        score(i)
    for i, kb in enumerate(kbs):
        if i + LA < nkb:
            score(i + LA)
        p = slots[i]
        for qs in range(nq // 128):
            b, ap, first, last = accf(qs)
            _mm(S, ap, p[:, qs * 128:(qs + 1) * 128], V[:, kb, 0:dva], i == 0 and first, i == nkb - 1 and last, [p, V], [b])


def phase_l1_attn(g):
    S = g.S
    TT = g.TT
    NKB = TT // 128
    li = g.lambda_init
    with ExitStack() as ph:
        KT = [S.sb("aKT%d" % i, [128, TT], BF16, ph) for i in range(2)]
        V = S.sb("aV", [128, NKB, 129], BF16, ph)
        QT = [S.sb("aQT%d" % i, [128, 512], BF16, ph) for i in range(2)]
        for b_ in KT + QT:
            _ms(S, "pool", b_[64:128, :], 0.0, [(b_, "pad")])
        pT = [S.sb("apT%d" % i, [128, 512], BF16, ph) for i in range(8)]
        Pacc = [(S.sb("aPaccD%d" % i, [128, 512], F32, ph), S.sb("aPaccP%d" % i, [128, 512], F32, ph)) for i in range(4)]
        oS = [S.sb("aoS%d" % i, [65, 512], F32, ph) for i in range(2)]
        rec = [S.sb("arec%d" % i, [128, 512], F32, ph) for i in range(2)]
        t1 = S.sb("at1", [128, 512], F32, ph)
        t2 = S.sb("at2", [128, 512], F32, ph)
        od = S.sb("aod", [128, 512], F32, ph)
        o = [S.sb("ao%d" % i, [128, 512], BF16, ph) for i in range(2)]
        lam = S.sb("alam", [128, 4], F32, ph)
        dl = S.sb("adl", [128, 4, 64], F32, ph)
        dn = S.sb("adn", [128, 1], F32, ph)
        tmp = S.sb("atmp", [128, 64], F32, ph)
        S.dma("sp", dl[:, :, :], g.dlam[:, :, :], reads=[g.dlam], writes=[dl])
        S.dma("sp", dn[:, :], g.dncol[:, :], reads=[g.dncol], writes=[dn])
        _ts(S, "dve", dn[:, :], dn[:, :], 1.0 - li, None, ALU.mult, None, [dn], [dn])
        for kk in range(2):
            _tt(S, "dve", tmp[:, 0:64], dl[:, 2 * kk, :], dl[:, 2 * kk + 1, :], ALU.mult, [dl], [tmp])
            S.op("dve", lambda e, kk=kk: e.tensor_reduce(out=lam[:, kk:kk + 1], in_=tmp[:, 0:64], axis=AX.X, op=ALU.add), reads=[tmp], writes=[lam])
        _act(S, lam[:, 0:2], lam[:, 0:2], AF.Exp, [lam], [lam])
        _tt(S, "dve", lam[:, 2:3], lam[:, 0:1], lam[:, 1:2], ALU.subtract, [lam], [lam])
        _ts(S, "dve", lam[:, 3:4], lam[:, 2:3], li, None, ALU.add, None, [lam], [lam])
        cnt = [0]
        qi = 0
        lat_tiles = [(ti, t0, nt) for ti, (t0, nt) in enumerate(g.tiles) if t0 >= g.TC]
        phd = ExitStack()
        psc = [S.ps("apsc%d" % i, [128, 512], F32, phd) for i in range(4)]
        acc = [S.ps("aacc%d" % i, [128, 512], F32, phd) for i in range(4)]
        psm = psc[3]
        for h in range(4):
            S.dma("sp", V[:, :, :], g.dv_s.t[:, h, :].rearrange("(n p) e -> p n e", p=128), reads=[g.dv_s], writes=[V])
            for m in range(2):
                S.dma("sp", KT[m][0:64, :], g.dkT_s.t[(2 * h + m) * 64:(2 * h + m + 1) * 64, :], reads=[g.dkT_s], writes=[(KT[m], "d")])
            for (ti, t0, nt) in lat_tiles:
                par = qi % 2
                qi += 1
                for m in range(2):
                    q = QT[m]
                    S.dma("sp", q[0:64, 0:nt], g.dqT_s.t[(2 * h + m) * 64:(2 * h + m + 1) * 64, t0:t0 + nt], reads=[g.dqT_s], writes=[(q, "d")])
                    attn_core3(g, KT[m], V, q, 128, 128, list(range(NKB)), acc[2 * par + m], Pacc[2 * par + m], psc, pT, nt, cnt)
                a1, a2 = acc[2 * par], acc[2 * par + 1]
                for m in range(2):
                    pd, pp_ = Pacc[2 * par + m]
                    _mm(S, psm[:, 0:nt], g.onesF[:, :], pd[:, 0:nt], True, False, [g.onesF, pd], [psm])
                    _mm(S, psm[:, 0:nt], g.onesF[:, :], pp_[:, 0:nt], False, True, [g.onesF, pp_], [psm])
                    S.op("dve", lambda e, m=m, nt=nt: e.reciprocal(out=rec[m][:, 0:nt], in_=psm[:, 0:nt]), reads=[psm], writes=[rec[m]])
                _tt(S, "dve", t1[:, 0:nt], a1[:, 0:nt], rec[0][:, 0:nt], ALU.mult, [a1, rec[0]], [t1])
                _stt(S, "dve", t2[:, 0:nt], a2[:, 0:nt], lam[:, 3:4], rec[1][:, 0:nt], ALU.mult, ALU.mult, [a2, lam, rec[1]], [t2])
                _tt(S, "pool", od[:, 0:nt], t1[:, 0:nt], t2[:, 0:nt], ALU.subtract, [t1, t2], [od])
                _tt(S, "pool", t1[:, 0:nt], od[:, 0:nt], od[:, 0:nt], ALU.mult, [od], [t1])
                _mm(S, psm[:, 0:nt], g.onesF[:, :], t1[:, 0:nt], True, True, [g.onesF, t1], [psm])
                rstd_from_ss(S, "dve", t2[:, 0:nt], psm[:, 0:nt], 128, [psm], [t2])
                oo = o[par]
                _stt(S, "dve", oo[:, 0:nt], od[:, 0:nt], dn[:, 0:1], t2[:, 0:nt], ALU.mult, ALU.mult, [od, dn, t2], [oo])
                S.dma("pool", g.mixT_s.t[h * 128:(h + 1) * 128, t0:t0 + nt], oo[:, 0:nt], reads=[oo], writes=[(g.mixT_s, (h, ti))])
        S.barrier()
        S.emit()
        phd.close()
        psc = [S.ps("bpsc%d" % i, [128, 512], F32, ph) for i in range(4)]
        acc = [S.ps("bacc%d" % i, [128, 512], F32, ph) for i in range(2)]
        psm = S.ps("bpsm", [128, 512], F32, ph)
        for kvh in range(2):
            S.dma("sp", V[:, :, 0:65], g.gv_s.t[:, kvh, :].rearrange("(n p) e -> p n e", p=128), reads=[g.gv_s], writes=[V])
            S.dma("sp", KT[0][0:64, :], g.gkT_s.t[kvh * 64:(kvh + 1) * 64, :], reads=[g.gkT_s], writes=[(KT[0], "d")])
            for grp in range(4):
                hd = kvh * 4 + grp
                for (ti, t0, nt) in lat_tiles:
                    par = qi % 2
                    qi += 1
                    q = QT[par]
                    S.dma("sp", q[0:64, 0:nt], g.gqT_s.t[hd * 64:(hd + 1) * 64, t0:t0 + nt], reads=[g.gqT_s], writes=[(q, "d")])
                    attn_core3(g, KT[0], V, q, 128, 65, list(range(NKB)), acc[par], None, psc, pT, nt, cnt)
                    attn_simple_post(g, acc[par], psm, oS[par], rec[0], o[par], nt,
                                     g.mixT_s.t[512 + hd * 64:512 + (hd + 1) * 64, t0:t0 + nt], (g.mixT_s, (8 + hd, ti)))
        S.emit()


def phase_moe_a(g):
    S = g.S
    TT = g.TT
    g.h2T_s = S.dram("h2T_s", [1024, TT], BF16)
    g.gT_s = S.dram("gT_s", [8, TT], F32)
    with ExitStack() as ph:
        rw = S.sb("mrw", [128, 8, 8], F32, ph)
        S.dma("sp", rw[:, :, :], g.router.t[:, :].rearrange("(kc p) e -> p kc e", p=128), reads=[g.router], writes=[rw])
        xa = [S.sb("maxa%d" % i, [128, 8, 512], F32, ph) for i in range(2)]
        sq = S.sb("masq", [128, 8, 512], F32, ph)
        rs = S.sb("mars", [128, 512], F32, ph)
        hT = [S.sb("mahT%d" % i, [128, 8, 512], BF16, ph) for i in range(2)]
        hF = S.sb("mahF", [128, 8, 512], F32, ph)
        lg = S.sb("malg", [128, 8], F32, ph)
        m8 = S.sb("mam8", [128, 8], F32, ph)
        sm = S.sb("masm", [128, 4], F32, ph)
        mask = S.sb("mamask", [128, 8], F32, ph)
        ex = S.sb("maex", [128, 8], F32, ph)
        gt = S.sb("magt", [128, 8], F32, ph)
        gT = [S.sb("magT%d" % i, [8, 512], F32, ph) for i in range(2)]
        pss = S.ps("mapss", [128, 512], F32, ph)
        pl = [S.ps("mapl%d" % i, [128, 512], F32, ph) for i in range(2)]
        pt = [S.ps("mapt%d" % i, [128, 512], F32, ph) for i in range(2)]
        k = 0
        for ti, (t0, nt) in enumerate(g.tiles):
            if t0 < g.TC:
                continue
            ns = nt // 128
            a = xa[ti % 2]
            h = hT[ti % 2]
            gTt = gT[ti % 2]
            S.dma("sp", a[:, :, 0:nt], xT_view(g, t0, nt), reads=[(g.xT_s, ti)], writes=[a])
            norm_mod(g, a, sq, rs, h, pss, nt, lambda c: g.modA[:, 1, 1, c, 0:1], lambda c: g.modT[:, 1, 24 + c, 0:1])
            S.dma("pool", g.h2T_s.t[:, t0:t0 + nt].rearrange("(c p) t -> p c t", p=128), h[:, :, 0:nt], reads=[h],
                  writes=[(g.h2T_s, ti)])
            for c in range(8):
                _ts(S, "dve", hF[:, c, 0:nt], sq[:, c, 0:nt], g.modT[:, 1, 24 + c, 0:1], None, ALU.add, None, [(sq, c)], [(hF, c)])
            for s in range(ns):
                p = pl[k % 2]
                p2 = pt[k % 2]
                k += 1
                for kc in range(8):
                    _mm(S, p[:, 0:8], hF[:, kc, s * 128:(s + 1) * 128], rw[:, kc, :], kc == 0, kc == 7, [hF, rw], [p])
                _cp(S, "dve", lg[:, :], p[:, 0:8], [p], [lg])
                S.op("dve", lambda e: e.max(out=m8[:, :], in_=lg[:, :]), reads=[lg], writes=[m8])
                _ts(S, "dve", sm[:, 0:1], m8[:, 0:1], -1.0, None, ALU.mult, None, [m8], [sm])
                _ts(S, "dve", mask[:, :], lg[:, :], m8[:, 1:2], None, ALU.is_ge, None, [lg, m8], [mask])
                _act(S, ex[:, :], lg[:, :], AF.Exp, [lg, sm], [ex], bias=sm[:, 0:1])
                _act(S, sm[:, 1:2], m8[:, 1:2], AF.Exp, [m8, sm], [sm], bias=sm[:, 0:1])
                _ts(S, "dve", sm[:, 2:3], sm[:, 1:2], 1.0, None, ALU.add, None, [sm], [sm])
                S.op("dve", lambda e: e.reciprocal(out=sm[:, 3:4], in_=sm[:, 2:3]), reads=[sm], writes=[sm])
                _stt(S, "dve", gt[:, :], ex[:, :], sm[:, 3:4], mask[:, :], ALU.mult, ALU.mult, [ex, sm, mask], [gt])
                _tr(S, p2[0:8, 0:128], gt[:, :], g.identF[:, :], [gt, g.identF], [p2])
                _cp(S, "dve", gTt[:, s * 128:(s + 1) * 128], p2[0:8, 0:128], [p2], [gTt])
            S.dma("pool", g.gT_s.t[:, t0:t0 + nt], gTt[:, 0:nt], reads=[gTt], writes=[(g.gT_s, ti)])
        S.emit()


def phase_moe_b(g):
    S = g.S
    NT = 256
    FH = 896
    NF = FH // 128
    NP = 3584 // FH
    with ExitStack() as ph:
        stg = [S.sb("mbstg%d" % i, [128, 8, 512], F32, ph, side="right") for i in range(2)]
        sel = S.sb("mbsel", [8, 8, 128], F32, ph)
        for e in range(8):
            _ts(S, "dve", sel[:, e, :], g.onesF[0:8, :], g.identF[0:8, e:e + 1], None, ALU.mult, None, [g.onesF, g.identF], [(sel, e)])
        wg = [S.sb("mbwg%d" % i, [128, 8, FH], BF16, ph) for i in range(2)]
        wu = [S.sb("mbwu%d" % i, [128, 8, FH], BF16, ph) for i in range(2)]
        wd = [S.sb("mbwd%d" % i, [128, NF, 1024], BF16, ph) for i in range(2)]
        xa = [S.sb("mbxa%d" % i, [128, 8, NT], F32, ph) for i in range(2)]
        hT = [S.sb("mbhT%d" % i, [128, 8, NT], BF16, ph) for i in range(2)]
        gTt = [S.sb("mbgT%d" % i, [8, NT], F32, ph) for i in range(2)]
        gbs = S.sb("mbgbs", [128, NT], F32, ph)
        sg = [S.sb("mbsg%d" % i, [128, NT], F32, ph) for i in range(3)]
        tg = [S.sb("mbtg%d" % i, [128, NT], F32, ph) for i in range(3)]
        act = [S.sb("mbact%d" % i, [128, NT], BF16, ph) for i in range(4)]
        pgu = [S.ps("mbpgu%d" % i, [128, 512], F32, ph) for i in range(4)]
        yac = [S.ps("mby%d" % i, [128, 2, NT], F32, ph) for i in range(4)]
        lat = [(ti, t0, nt) for ti, (t0, nt) in enumerate(g.tiles) if t0 >= g.TC]
        it = 0

        def ld(dst, src_ap, K, N):
            kc = K // 128
            v = src_ap.rearrange("(kc p) n -> p kc n", p=128)
            for k0 in range(0, kc, 8):
                k1 = min(kc, k0 + 8)
                for n0 in range(0, N, 512):
                    n1 = min(N, n0 + 512)

                    def one(k0=k0, k1=k1, n0=n0, n1=n1):
                        s = stg[g.stg_i % 2]
                        g.stg_i += 1
                        S.dma("sp", s[:, 0:k1 - k0, 0:n1 - n0], v[:, k0:k1, n0:n1], reads=[g.moe_gu, g.moe_dn], writes=[s])
                        _cp(S, ("dve", "pool")[g.stg_i % 2], dst[:, k0:k1, n0:n1], s[:, 0:k1 - k0, 0:n1 - n0], [s], [dst])
                    yield one

        def load_pass(pi_):
            e, fh = pi_ // NP, pi_ % NP
            par = pi_ % 2
            yield from ld(wg[par], g.moe_gu.t[e, :, fh * FH:(fh + 1) * FH], 1024, FH)
            yield from ld(wu[par], g.moe_gu.t[e, :, 3584 + fh * FH:3584 + (fh + 1) * FH], 1024, FH)
            yield from ld(wd[par], g.moe_dn.t[e, fh * FH:(fh + 1) * FH, :], FH, 1024)

        npass = 8 * NP
        for one in load_pass(0):
            one()
        gbs2 = [gbs, S.sb("mbgbs1", [128, NT], F32, ph)]
        tiles256 = [(ti, t0 + u0) for (ti, t0, nt) in lat for u0 in range(0, nt, NT)]
        ctxs = []
        for pi_ in range(npass):
            for j, (ti, tt0) in enumerate(tiles256):
                ctxs.append(dict(pi=pi_, e=pi_ // NP, par=pi_ % 2, ti=ti, t0=tt0, j=j, started=False, acs={}))
        steps = [(c, f) for c in ctxs for f in range(NF)]
        pend = {}

        def prologue(c):
            n = c["n"]
            a, h, gt_, gb = xa[n % 2], hT[n % 2], gTt[n % 2], gbs2[n % 2]
            c.update(a=a, h=h, gb=gb)
            t0_ = c["t0"]
            S.dma("sp", h[:, :, :], g.h2T_s.t[:, t0_:t0_ + NT].rearrange("(c p) t -> p c t", p=128), reads=[g.h2T_s], writes=[h])
            S.dma("sp", gt_[:, :], g.gT_s.t[:, t0_:t0_ + NT], reads=[g.gT_s], writes=[gt_])
            S.dma("sp", a[:, :, :], xT_view(g, t0_, NT), reads=[(g.xT_s, t0_)], writes=[a])
            _mm(S, pgu[0][:, 0:NT], sel[:, c["e"], :], gt_[:, :], True, True, [(sel, c["e"]), gt_], [pgu[0]])
            _cp(S, "dve", gb[:, :], pgu[0][:, 0:NT], [pgu[0]], [gb])
            if c["j"] == 0 and c["pi"] + 1 < npass:
                pend[c["pi"]] = list(load_pass(c["pi"] + 1))

        gcount = [0]

        def gu(c, f):
            if not c["started"]:
                c["started"] = True
                if c["j"] == 0:
                    pl0 = pend.get(c["pi"] - 1)
                    while pl0:
                        pl0.pop(0)()
                prologue(c)
            k_ = gcount[0]
            gcount[0] += 1
            wg_, wu_ = wg[c["par"]], wu[c["par"]]
            h = c["h"]
            pg = pgu[(k_ % 2) * 2]
            pu = pgu[(k_ % 2) * 2 + 1]
            for kc in range(8):
                _mm(S, pg[:, 0:NT], wg_[:, kc, f * 128:(f + 1) * 128], h[:, kc, :], kc == 0, kc == 7, [wg_, h], [pg])
            for kc in range(8):
                _mm(S, pu[:, 0:NT], wu_[:, kc, f * 128:(f + 1) * 128], h[:, kc, :], kc == 0, kc == 7, [wu_, h], [pu])
            s_, t_, ac = sg[k_ % 3], tg[k_ % 3], act[k_ % 4]
            _act(S, s_[:, :], pg[:, 0:NT], AF.Silu, [pg], [s_])
            _tt(S, "dve", t_[:, :], s_[:, :], pu[:, 0:NT], ALU.mult, [s_, pu], [t_])
            _tt(S, "pool", ac[:, :], t_[:, :], c["gb"][:, :], ALU.mult, [t_, c["gb"]], [ac])
            c["acs"][f] = ac

        for n, c in enumerate(ctxs):
            c["n"] = n
        issued = 0
        for idx, (c, f) in enumerate(steps):
            while issued < len(steps) and issued <= idx + 2:
                cc, ff = steps[issued]
                gu(cc, ff)
                issued += 1
            ac = c["acs"].pop(f)
            wd_ = wd[c["par"]]
            for dsl in range(8):
                _mm(S, yac[dsl // 2][:, dsl % 2, :], wd_[:, f, dsl * 128:(dsl + 1) * 128], ac[:, :],
                    f == 0 and dsl % 2 == 0, f == NF - 1 and dsl % 2 == 1, [wd_, ac], [yac[dsl // 2]])
            if f == NF - 1:
                a = c["a"]
                for dsl in range(8):
                    _stt(S, "dve", a[:, dsl, :], yac[dsl // 2][:, dsl % 2, :], g.modT[:, 1, 40 + dsl, 0:1], a[:, dsl, :],
                         ALU.mult, ALU.add, [yac[dsl // 2], (a, dsl)], [(a, dsl)])
                S.dma("pool", xT_view(g, c["t0"], NT), a[:, :, :], reads=[a], writes=[(g.xT_s, c["t0"])])
                pl = pend.get(c["pi"])
                if pl:
                    pl.pop(0)()
                if c["j"] == len(tiles256) - 1:
                    while pl:
                        pl.pop(0)()
        S.emit()


def phase_gdn_prep(g):
    S = g.S
    TT = g.TT
    g.qkvn_s = S.dram("qkvn_s", [12, 128, TT], F32)
    with ExitStack() as ph:
        cw = S.sb("gpcw", [128, 12, 5], F32, ph)
        S.dma("sp", cw[:, :, :], g.conv_col[:, :, :], reads=[g.conv_col], writes=[cw])
        bd = S.sb("gpbd", [128, 128], F32, ph)
        _ms(S, "pool", bd[:, :], 0.0, [bd])
        _ms(S, "pool", bd[0:64, 0:64], 1.0, [bd])
        _ms(S, "pool", bd[64:128, 64:128], 1.0, [bd])
        xh = [S.sb("gpxh%d" % i, [128, 12, 516], F32, ph) for i in range(2)]
        acc = [S.sb("gpacc%d" % i, [128, 512], F32, ph) for i in range(3)]
        y = [S.sb("gpy%d" % i, [128, 512], F32, ph) for i in range(3)]
        sq = [S.sb("gpsq%d" % i, [128, 512], F32, ph) for i in range(2)]
        rn = [S.sb("gprn%d" % i, [128, 512], F32, ph) for i in range(2)]
        pss = [S.ps("gppss%d" % i, [128, 512], F32, ph) for i in range(2)]
        k = 0
        for ti, (t0, nt) in enumerate(g.tiles):
            x = xh[ti % 2]
            lo = 0 if t0 < g.TC else g.TC
            hi = g.TC if t0 < g.TC else TT
            a0 = max(lo, t0 - 2)
            a1 = min(hi, t0 + nt + 2)
            if a0 > t0 - 2:
                _ms(S, "pool", x[:, :, 0:2], 0.0, [x])
            if a1 < t0 + nt + 2:
                _ms(S, "pool", x[:, :, nt + 2:nt + 4], 0.0, [x])
            S.dma("sp", x[:, :, a0 - (t0 - 2):a1 - (t0 - 2)], g.gq_s.t[:, :, a0:a1].rearrange("c p t -> p c t"),
                  reads=[g.gq_s], writes=[x])
            for c in range(12):
                ac, yy = acc[k % 3], y[k % 3]
                s_, r_ = sq[k % 2], rn[k % 2]
                ps = pss[k % 2]
                k += 1
                _ts(S, "dve", ac[:, 0:nt], x[:, c, 0:nt], cw[:, c, 0:1], None, ALU.mult, None, [x, cw], [ac])
                for j in range(1, 5):
                    _stt(S, "dve", ac[:, 0:nt], x[:, c, j:j + nt], cw[:, c, j:j + 1], ac[:, 0:nt], ALU.mult, ALU.add, [x, cw, ac], [ac])
                _act(S, yy[:, 0:nt], ac[:, 0:nt], AF.Silu, [ac], [yy])
                if c < 8:
                    _tt(S, "pool", s_[:, 0:nt], yy[:, 0:nt], yy[:, 0:nt], ALU.mult, [yy], [s_])
                    _mm(S, ps[:, 0:nt], bd[:, :], s_[:, 0:nt], True, True, [bd, s_], [ps])
                    rstd_from_ss(S, "dve", r_[:, 0:nt], ps[:, 0:nt], 1, [ps], [r_])
                    if c < 4:
                        _stt(S, "dve", yy[:, 0:nt], yy[:, 0:nt], 0.125, r_[:, 0:nt], ALU.mult, ALU.mult, [yy, r_], [yy])
                    else:
                        _tt(S, "dve", yy[:, 0:nt], yy[:, 0:nt], r_[:, 0:nt], ALU.mult, [yy, r_], [yy])
                S.dma("pool", g.qkvn_s.t[c, :, t0:t0 + nt], yy[:, 0:nt], reads=[yy], writes=[(g.qkvn_s, (c, ti))])
        S.emit()


def phase_gdn(g):
    S = g.S
    TT = g.TT
    C = 64
    g.o_s = S.dram("gdn_o_s", [2, TT, 512], F32)
    with ExitStack() as ph:
        def sb(name, shape, dt=F32):
            return S.sb("gd_" + name, shape, dt, ph)
        alr = sb("alr", [64, 16])
        dtb = sb("dtb", [64, 16])
        S.dma("sp", alr[:, :], g.alog_rep[0:64, :], reads=[g.alog_rep], writes=[alr])
        S.dma("sp", dtb[:, :], g.dtb_rep[0:64, :], reads=[g.dtb_rep], writes=[dtb])
        nA = sb("nA", [64, 16])
        _act(S, nA[:, :], alr[:, :], AF.Exp, [alr], [nA])
        _ts(S, "dve", nA[:, :], nA[:, :], -1.0, None, ALU.mult, None, [nA], [nA])
        def mask(name, op, sgn=1):
            m = sb(name, [64, 8, 64])
            _ms(S, "pool", m[:, :, :], 1.0, [m])
            S.op("pool", lambda e: e.affine_select(out=m[:, :, :], in_=m[:, :, :], pattern=[[0, 8], [-sgn, 64]],
                                                   compare_op=op, fill=0.0, base=0, channel_multiplier=sgn), reads=[m], writes=[m])
            return m
        mI = [mask("mIf", ALU.is_ge), mask("mIb", ALU.is_ge, -1)]
        mS = [mask("mSf", ALU.is_gt), mask("mSb", ALU.is_gt, -1)]
        I8 = mask("I8", ALU.is_equal)
        Tri = [mI[1], mI[0]]
        St = [sb("S%d" % d, [64, 8, 64]) for d in range(2)]
        for d in range(2):
            _ms(S, "pool", St[d][:, :, :], 0.0, [St[d]])
        nb = 4
        QK = [sb("QK%d" % i, [64, 16, C]) for i in range(nb)]
        Vf = [sb("Vf%d" % i, [64, 8, C]) for i in range(nb)]
        abT = [sb("abT%d" % i, [32, C]) for i in range(nb)]
        names = ["Ktm", "Vtm", "abm", "t16", "gg", "be", "gc", "egc", "gtot", "cd", "kdsc", "bsc", "nbe"]
        TD = [{}, {}]
        big = ["NG", "Mm", "E", "D", "Ds", "P0", "DT", "qkmT", "PT0", "Pa", "PTa", "Pb", "PTb", "TTa", "TTb", "bV", "bK", "U", "WT",
               "vnew", "o2", "od", "Kd", "P0b"]
        for dd in range(2):
            for n in names:
                TD[dd][n] = sb("%s_%d" % (n, dd), [64, 512] if n in ("Ktm", "Vtm") else [64, 32])
            for n in big:
                TD[dd][n] = sb("%s_%d" % (n, dd), [64, 8, 64],
                               F32)
        pb = [S.ps("gd_p%d" % i, [128, 512], F32, ph) for i in range(8)]
        pi = [0]

        def P():
            p = pb[pi[0] % 8]
            pi[0] += 1
            return p

        def v3(p):
            return p[0:64, :].rearrange("p (h e) -> p h e", e=64)

        ev = [0]

        def evac(out_ap, in_ap, R, W):
            ev[0] += 1
            _cp(S, "act" if ev[0] % 2 else "dve", out_ap, in_ap, R, W)

        def chunk(d, c0, it):
            T = TD[d]
            qk, vf, ab = QK[it % nb], Vf[it % nb], abT[it % nb]
            S.dma("sp", qk[:, :, :], g.qkvn_s.t[0:8, :, c0:c0 + C].rearrange("c (hh d) t -> d (c hh) t", d=64),
                  reads=[g.qkvn_s], writes=[qk])
            S.dma("sp", vf[:, :, :], g.qkvn_s.t[8:12, :, c0:c0 + C].rearrange("c (hh d) t -> d (c hh) t", d=64),
                  reads=[g.qkvn_s], writes=[vf])
            S.dma("sp", ab[:, :], g.ab_s.t[:, c0:c0 + C], reads=[g.ab_s], writes=[ab])
            Ktm, Vtm, abm = T["Ktm"], T["Vtm"], T["abm"]
            p = P()
            for h in range(8):
                _tr(S, p[0:64, h * 64:(h + 1) * 64], qk[:, 8 + h, :], g.identF[0:64, 0:64], [qk, g.identF], [p])
            evac(Ktm[:, :], p[0:64, :], [p], [Ktm])
            p = P()
            for h in range(8):
                _tr(S, p[0:64, h * 64:(h + 1) * 64], vf[:, h, :], g.identF[0:64, 0:64], [vf, g.identF], [p])
            evac(Vtm[:, :], p[0:64, :], [p], [Vtm])
            p = P()
            _tr(S, p[0:64, 0:32], ab[:, :], g.identF[0:32, 0:32], [ab, g.identF], [p])
            evac(abm[:, :], p[0:64, 0:32], [p], [abm])
            t16, gg, be, gc, egc, gtot, cd, kdsc, bsc, nbe = (T[n] for n in ("t16", "gg", "be", "gc", "egc", "gtot", "cd", "kdsc", "bsc", "nbe"))
            r8 = slice(d * 8, d * 8 + 8)
            _tt(S, "dve", t16[:, 0:8], abm[:, r8], dtb[:, r8], ALU.add, [abm, dtb], [t16])
            _act(S, t16[:, 0:8], t16[:, 0:8], AF.Exp, [t16], [t16])
            _act(S, t16[:, 0:8], t16[:, 0:8], AF.Ln, [t16], [t16], bias=1.0)
            _tt(S, "dve", gg[:, 0:8], t16[:, 0:8], nA[:, r8], ALU.mult, [t16, nA], [gg])
            _act(S, be[:, 0:8], abm[:, 16 + d * 8:24 + d * 8], AF.Sigmoid, [abm], [be])
            _ts(S, "dve", nbe[:, 0:8], be[:, 0:8], -1.0, None, ALU.mult, None, [be], [nbe])
            p = P()
            _mm(S, p[0:64, 0:8], Tri[d][:, 0, :], gg[:, 0:8], True, True, [Tri[d], gg], [p])
            evac(gc[:, 0:8], p[0:64, 0:8], [p], [gc])
            p = P()
            _mm(S, p[0:64, 0:8], g.onesF[0:64, 0:64], gg[:, 0:8], True, True, [g.onesF, gg], [p])
            evac(gtot[:, 0:8], p[0:64, 0:8], [p], [gtot])
            _act(S, egc[:, 0:8], gc[:, 0:8], AF.Exp, [gc], [egc])
            _act(S, cd[:, 0:8], gtot[:, 0:8], AF.Exp, [gtot], [cd])
            _tt(S, "dve", kdsc[:, 0:8], gtot[:, 0:8], gc[:, 0:8], ALU.subtract, [gtot, gc], [kdsc])
            _act(S, kdsc[:, 0:8], kdsc[:, 0:8], AF.Exp, [kdsc], [kdsc])
            _tt(S, "dve", bsc[:, 0:8], be[:, 0:8], egc[:, 0:8], ALU.mult, [be, egc], [bsc])
            NG, Mm, E, Dm, Ds = T["NG"], T["Mm"], T["E"], T["D"], T["Ds"]
            for h in range(8):
                _ts(S, "dve", NG[:, h, :], g.onesF[0:64, 0:64], gg[:, h:h + 1], -1.0, ALU.mult, ALU.mult, [g.onesF, gg], [(NG, h)])
            p = P()
            for h in range(8):
                _mm(S, v3(p)[:, h, :], NG[:, h, :], Tri[d][:, 0, :], True, True, [(NG, h), Tri[d]], [p])
            for h in range(8):
                _ts(S, "dve", Mm[:, h, :], v3(p)[:, h, :], gc[:, h:h + 1], 0.0, ALU.add, ALU.min, [p, gc], [(Mm, h)])
            _act(S, E[:, :, :], Mm[:, :, :], AF.Exp, [Mm], [E])
            _tt(S, "pool", Dm[:, :, :], E[:, :, :], mI[d][:, :, :], ALU.mult, [E, mI[d]], [Dm])
            _tt(S, "pool", Ds[:, :, :], E[:, :, :], mS[d][:, :, :], ALU.mult, [E, mS[d]], [Ds])
            P0, DT, qkmT, PT0 = T["P0"], T["DT"], T["qkmT"], T["PT0"]
            p = P()
            for h in range(8):
                _mm(S, v3(p)[:, h, :], qk[:, 8 + h, :], qk[:, 8 + h, :], True, True, [qk], [p])
            for h in range(8):
                _stt(S, "dve", P0[:, h, :], v3(p)[:, h, :], nbe[:, h:h + 1], Ds[:, h, :], ALU.mult, ALU.mult, [p, nbe, Ds], [(P0, h)])
            p = P()
            for h in range(8):
                _tr(S, v3(p)[:, h, :], Dm[:, h, :], g.identF[0:64, 0:64], [Dm, g.identF], [p])
            evac(DT[:, :, :], v3(p), [p], [DT])
            p = P()
            for h in range(8):
                _mm(S, v3(p)[:, h, :], qk[:, 8 + h, :], qk[:, h, :], True, True, [qk], [p])
            _tt(S, "dve", qkmT[:, :, :], v3(p), DT[:, :, :], ALU.mult, [p, DT], [qkmT])
            p = P()
            for h in range(8):
                _tr(S, v3(p)[:, h, :], P0[:, h, :], g.identF[0:64, 0:64], [(P0, h), g.identF], [p])
            evac(PT0[:, :, :], v3(p), [p], [PT0])
            TTa, TTb = T["TTa"], T["TTb"]
            _tt(S, "pool", TTa[:, :, :], PT0[:, :, :], I8[:, :, :], ALU.add, [PT0, I8], [TTa])
            Pk, PTk = P0, PT0
            cur, nxt = TTa, TTb
            alt = [(T["Pa"], T["PTa"]), (T["Pb"], T["PTb"])]
            for lv in range(1, 6):
                Pn, PTn = alt[lv % 2]
                p = P()
                for h in range(8):
                    _mm(S, v3(p)[:, h, :], PTk[:, h, :], Pk[:, h, :], True, True, [Pk, PTk], [p])
                evac(Pn[:, :, :], v3(p), [p], [Pn])
                if lv < 5:
                    p = P()
                    for h in range(8):
                        _mm(S, v3(p)[:, h, :], Pk[:, h, :], PTk[:, h, :], True, True, [Pk, PTk], [p])
                    evac(PTn[:, :, :], v3(p), [p], [PTn])
                p = P()
                for h in range(8):
                    _mm(S, v3(p)[:, h, :], Pn[:, h, :], cur[:, h, :], True, True, [Pn, cur], [p])
                _tt(S, "dve", nxt[:, :, :], v3(p), cur[:, :, :], ALU.add, [p, cur], [nxt])
                cur, nxt = nxt, cur
                Pk, PTk = Pn, PTn
            TT_ = cur
            bV, bK, U, WT = T["bV"], T["bK"], T["U"], T["WT"]
            Kv = Ktm[:, :].rearrange("p (h e) -> p h e", e=64)
            Vv = Vtm[:, :].rearrange("p (h e) -> p h e", e=64)
            for h in range(8):
                _ts(S, "dve", bV[:, h, :], Vv[:, h, :], be[:, h:h + 1], None, ALU.mult, None, [Vtm, be], [(bV, h)])
                _ts(S, "pool", bK[:, h, :], Kv[:, h, :], bsc[:, h:h + 1], None, ALU.mult, None, [Ktm, bsc], [(bK, h)])
            p = P()
            for h in range(8):
                _mm(S, v3(p)[:, h, :], TT_[:, h, :], bV[:, h, :], True, True, [TT_, bV], [p])
            evac(U[:, :, :], v3(p), [p], [U])
            p = P()
            for h in range(8):
                _mm(S, v3(p)[:, h, :], bK[:, h, :], TT_[:, h, :], True, True, [TT_, bK], [p])
            evac(WT[:, :, :], v3(p), [p], [WT])
            Sd = St[d]
            vnew, o2, od, Kd = T["vnew"], T["o2"], T["od"], T["Kd"]
            for h in range(8):
                _ts(S, "pool", Kd[:, h, :], Kv[:, h, :], kdsc[:, h:h + 1], None, ALU.mult, None, [Ktm, kdsc], [(Kd, h)])
            pw = P()
            for h in range(8):
                _mm(S, v3(pw)[:, h, :], WT[:, h, :], Sd[:, h, :], True, True, [WT, Sd], [pw])
            pq = P()
            for h in range(8):
                _mm(S, v3(pq)[:, h, :], qk[:, h, :], Sd[:, h, :], True, True, [qk, Sd], [pq])
            _tt(S, "dve", vnew[:, :, :], U[:, :, :], v3(pw), ALU.subtract, [U, pw], [vnew])
            p2 = P()
            for h in range(8):
                _mm(S, v3(p2)[:, h, :], qkmT[:, h, :], vnew[:, h, :], True, True, [qkmT, vnew], [p2])
            evac(o2[:, :, :], v3(p2), [p2], [o2])
            for h in range(8):
                _stt(S, "dve", od[:, h, :], v3(pq)[:, h, :], egc[:, h:h + 1], o2[:, h, :], ALU.mult, ALU.add, [pq, egc, o2], [(od, h)])
            p3 = P()
            for h in range(8):
                _mm(S, v3(p3)[:, h, :], Kd[:, h, :], vnew[:, h, :], True, True, [Kd, vnew], [p3])
            for h in range(8):
                _stt(S, "dve", Sd[:, h, :], Sd[:, h, :], cd[:, h:h + 1], v3(p3)[:, h, :], ALU.mult, ALU.add, [Sd, cd, p3], [Sd])
            odf = od[:, :, :].rearrange("p h e -> p (h e)")
            S.dma("pool", g.o_s.t[d, c0:c0 + C, :], odf, reads=[od], writes=[(g.o_s, (d, c0))])

        nctx = g.TC // C
        nlat = g.TL // C
        it = 0
        orders = [list(range(nctx)) + [nctx + i for i in range(nlat)],
                  list(range(nctx - 1, -1, -1)) + [nctx + i for i in range(nlat - 1, -1, -1)]]
        for step in range(nctx + nlat):
            for d in range(2):
                chunk(d, orders[d][step] * C, it)
                it += 1
        S.barrier()
        S.emit()
    with ExitStack() as ph:
        onr = S.sb("gc_onr", [128, 8, 64], F32, ph)
        S.dma("sp", onr[:, :, :], g.onrm_rep[:, :, :], reads=[g.onrm_rep], writes=[onr])
        o0 = [S.sb("gc_o0%d" % i, [128, 8, 64], F32, ph) for i in range(2)]
        o1 = [S.sb("gc_o1%d" % i, [128, 8, 64], F32, ph) for i in range(2)]
        zt = [S.sb("gc_z%d" % i, [128, 8, 64], F32, ph) for i in range(2)]
        sqo = S.sb("gc_sq", [128, 8, 64], F32, ph)
        ss = S.sb("gc_ss", [128, 16], F32, ph)
        ob = [S.sb("gc_ob%d" % i, [128, 8, 64], BF16, ph) for i in range(2)]
        oT = [S.sb("gc_oT%d" % i, [128, 4, 128], BF16, ph) for i in range(2)]
        ptb = [S.ps("gc_ptb%d" % i, [128, 512], BF16, ph) for i in range(2)]
        for bi in range(TT // 128):
            r0 = bi * 128
            a, b, z, o_b, o_t, pt = o0[bi % 2], o1[bi % 2], zt[bi % 2], ob[bi % 2], oT[bi % 2], ptb[bi % 2]
            S.dma("sp", a[:, :, :], g.o_s.t[0, r0:r0 + 128, :].rearrange("p (h e) -> p h e", e=64), reads=[g.o_s], writes=[a])
            S.dma("sp", b[:, :, :], g.o_s.t[1, r0:r0 + 128, :].rearrange("p (h e) -> p h e", e=64), reads=[g.o_s], writes=[b])
            S.dma("sp", z[:, :, :], g.z_s.t[r0:r0 + 128, :].rearrange("p (h e) -> p h e", e=64), reads=[g.z_s], writes=[z])
            _tt(S, "pool", a[:, :, :], a[:, :, :], b[:, :, :], ALU.add, [a, b], [a])
            _tt(S, "pool", sqo[:, :, :], a[:, :, :], a[:, :, :], ALU.mult, [a], [sqo])
            S.op("dve", lambda e: e.tensor_reduce(out=ss[:, 0:8], in_=sqo[:, :, :], axis=AX.X, op=ALU.add), reads=[sqo], writes=[ss])
            rstd_from_ss(S, "dve", ss[:, 8:16], ss[:, 0:8], 64, [ss], [ss])
            _tt(S, "pool", z[:, :, :], z[:, :, :], onr[:, :, :], ALU.mult, [z, onr], [z])
            for h in range(8):
                _stt(S, "dve", o_b[:, h, :], a[:, h, :], ss[:, 8 + h:9 + h], z[:, h, :], ALU.mult, ALU.mult, [a, ss, z], [o_b])
            obf = o_b[:, :, :].rearrange("p h e -> p (h e)")
            for c in range(4):
                _tr(S, pt[:, c * 128:(c + 1) * 128], obf[:, c * 128:(c + 1) * 128], g.identB[:, :], [o_b, g.identB], [pt])
            _cp(S, "act", o_t[:, :, :], pt[:, :].rearrange("p (c t) -> p c t", t=128), [pt], [o_t])
            S.dma("pool", g.mixT_s.t[512:1024, r0:r0 + 128].rearrange("(c p) t -> p c t", p=128), o_t[:, :, :], reads=[o_t],
                  writes=[(g.mixT_s, ("g", bi))])
        S.emit()


def rope_tab(n, TL, TC):
    nf = n // 4
    inv = 1.0 / (10000.0 ** (np.arange(nf, dtype=np.float32) / nf))
    t = np.arange(TL)
    rows = (t // 64).astype(np.float32)
    cols = (t % 64).astype(np.float32)
    ang_r = rows[None, :] * inv[:, None]
    ang_c = cols[None, :] * inv[:, None]
    cos = np.concatenate([np.cos(ang_r), np.cos(ang_r), np.cos(ang_c), np.cos(ang_c)], 0)
    sin = np.concatenate([np.sin(ang_r), np.sin(ang_r), np.sin(ang_c), np.sin(ang_c)], 0)
    cos = np.concatenate([np.ones((n, TC), np.float32), cos.astype(np.float32)], 1)
    sin = np.concatenate([np.zeros((n, TC), np.float32), sin.astype(np.float32)], 1)
    return np.ascontiguousarray(cos), np.ascontiguousarray(sin)


def col(v, k):
    return np.ascontiguousarray(np.asarray(v, np.float32).reshape(k, 128).T)


def prep_core(inp, b, TL, TC):
    f = lambda a: np.ascontiguousarray(np.asarray(a, np.float32))
    TT = TL + TC
    d = {}
    d["x"] = f(inp["x"][b])
    d["ctx"] = f(inp["ctx"][b])
    d["ccol"] = np.ascontiguousarray(np.stack([col(inp["c"][b], 8), col(inp["c_ctx"], 8)], -1))
    d["mod_w"] = f(inp["mod_w"])
    d["modb"] = np.ascontiguousarray(f(inp["mod_b"]).reshape(2, 48, 128).transpose(2, 0, 1))
    d["normg"] = np.ascontiguousarray(f(inp["norm_g"]).reshape(2, 2, 8, 128).transpose(3, 0, 1, 2))
    d["fnorm"] = col(inp["final_norm"], 8)
    c32, s32 = rope_tab(32, TL, TC)
    sc = np.float32(96 ** -0.5)
    d["cosq96"] = np.ascontiguousarray(np.concatenate([np.full((64, TT), sc, np.float32), c32 * sc], 0))
    d["sinq96"] = np.ascontiguousarray(np.concatenate([np.zeros((64, TT), np.float32), s32 * sc], 0))
    d["cos32"], d["sin32"] = c32, s32
    d["ev_w_in"] = f(inp["ev_w_in"][0])
    d["ev_w_uq"] = f(inp["ev_mla_w_uq"][0])
    d["ev_w_ukv"] = f(inp["ev_mla_w_ukv"][0])
    d["qn_col"] = col(inp["ev_mla_q_norm"][0], 3)
    d["kvn_col"] = col(inp["ev_mla_kv_norm"][0], 2)
    d["ev_w_out"] = f(inp["ev_w_out"][0])
    d["ev_wgu"] = f(inp["ev_ffn_w_gu"][0])
    d["ev_wdn"] = f(inp["ev_ffn_w_down"][0])
    c64, s64 = rope_tab(64, TL, TC)
    c64 = np.ascontiguousarray(np.concatenate([c64, c64], 0)); s64 = np.ascontiguousarray(np.concatenate([s64, s64], 0))
    d["cos64"], d["sin64"] = c64, s64
    d["cosq64"], d["sinq64"] = c64 * np.float32(0.125), s64 * np.float32(0.125)
    d["od_w_in"] = f(inp["od_w_in"][0])
    d["od_w_out"] = f(inp["od_w_out"][0])
    d["router"] = f(inp["od_router_w"][0]); d["moe_gu"] = f(inp["od_moe_w_gu"][0]); d["moe_dn"] = f(inp["od_moe_w_down"][0])
    d["conv_col"] = np.ascontiguousarray(f(inp["ev_gdn_conv"][0]).reshape(5, 12, 128).transpose(2, 1, 0))
    d["alog_rep"] = np.ascontiguousarray(np.broadcast_to(f(inp["ev_gdn_a_log"][0]).reshape(1, 16), (128, 16)))
    d["dtb_rep"] = np.ascontiguousarray(np.broadcast_to(f(inp["ev_gdn_dt_bias"][0]).reshape(1, 16), (128, 16)))
    d["onrm_rep"] = np.ascontiguousarray(np.broadcast_to(f(inp["ev_gdn_out_norm"][0]).reshape(1, 1, 64), (128, 8, 64)))
    perm = np.concatenate([np.arange(16, 32), np.arange(0, 16), np.arange(48, 64), np.arange(32, 48)])
    gq = f(inp["od_gqa_q_norm"][0]); gk = f(inp["od_gqa_k_norm"][0])
    d["gcols"] = np.ascontiguousarray(np.stack([np.tile(gq, 2), np.tile(gq[perm], 2), np.tile(gk, 2), np.tile(gk[perm], 2)], -1))
    d["dlam"] = np.ascontiguousarray(np.broadcast_to(f(inp["od_diff_lambda"][0])[None], (128, 4, 64)))
    d["dncol"] = np.ascontiguousarray(f(inp["od_diff_norm"][0]).reshape(128, 1))
    return d


_NC_CACHE = {}


def kernel(**inputs):
    TL, TC = 8192, 256
    inp = {k: np.asarray(v) for k, v in inputs.items()}
    if "nc" not in _NC_CACHE:
        _NC_CACHE["nc"] = build(TL, TC, stages=("l0", "gdn", "l1"))
    nc = _NC_CACHE["nc"]
    shared = prep_core(inp, 0, TL, TC)
    in_maps = [shared]
    for b in range(1, 8):
        d = dict(shared)
        d["x"] = np.ascontiguousarray(inp["x"][b], dtype=np.float32)
        d["ctx"] = np.ascontiguousarray(inp["ctx"][b], dtype=np.float32)
        d["ccol"] = np.ascontiguousarray(np.stack([col(inp["c"][b], 8), col(inp["c_ctx"], 8)], -1))
        in_maps.append(d)
    res = run_bass_kernel_spmd(nc, in_maps, core_ids=list(range(8)))
    out = np.stack([np.asarray(r["out"], dtype=np.float32) for r in res.results], 0)
    return out
```
